# Optimizing a Trainium2 kernel written in Bass

```python
import jax, jax.numpy as jnp
from jax import lax
import numpy as np

D_MODEL = 1024
BATCH = 2
SEQ = 8192
DEPTH = 1
DEC_BATCH = 128
DEC_SEQ = 8
PAST_LEN = 8192
PAGE_SIZE = 128

D_MIX = D_MODEL
HEAD_DIM = 64
N_RET_HEADS = D_MIX // (2 * HEAD_DIM)
N_ATT_HEADS = D_MIX // (2 * HEAD_DIM)
D_RET = N_RET_HEADS * HEAD_DIM
D_ATT = N_ATT_HEADS * HEAD_DIM
D_IN = 4 * D_RET + 3 * D_ATT
RET_CHUNK = 128
DIL_PATTERNS = ((128, 1), (512, 4), (2048, 16))
MAX_WINDOW = 2048
N_EXPERTS = 32
TOP_K = 4
D_FF = D_MODEL
SWIGLU_LIMIT = 7.0
SWIGLU_ALPHA = 1.702
MOE_BLOCK = 128
EPS = 1e-6
NEG_INF = -1e30

kernel_name = 'hybrid_retention_dilated_alibi_moe_step'


def rms_norm(x, g):
    xf = x.astype(jnp.float32)
    y = xf * lax.rsqrt(jnp.mean(xf * xf, axis=-1, keepdims=True) + EPS)
    return (y * g.astype(jnp.float32)).astype(x.dtype)


def ret_log_decay():
    return jnp.log(1.0 - 2.0 ** (-5.0 - jnp.arange(N_RET_HEADS, dtype=jnp.float32)))


def alibi_slopes():
    return 2.0 ** (-8.0 * (jnp.arange(N_ATT_HEADS, dtype=jnp.float32) + 1.0) / N_ATT_HEADS)


def ada_mod(c, w_ada, b_ada):
    m = (jax.nn.silu(c) @ w_ada + b_ada)[:, None, :]
    return jnp.split(m, 6, axis=-1)


def hybrid_proj(h, w_in):
    B, S, _ = h.shape
    z = h @ w_in
    cuts = [D_RET, 2 * D_RET, 3 * D_RET, 4 * D_RET, 4 * D_RET + D_ATT, 4 * D_RET + 2 * D_ATT]
    qr, kr, vr, gr, qa, ka, va = jnp.split(z, cuts, axis=-1)
    hd = lambda t: t.reshape(B, S, -1, HEAD_DIM)
    return hd(qr), hd(kr), hd(vr), gr, hd(qa), hd(ka), hd(va)


def retention_inputs(qr, kr, vr):
    f = jnp.float32
    return qr.astype(f), kr.astype(f) * HEAD_DIM ** -0.5, vr.astype(f)


def retention_chunk(q, k, v, state):
    L = q.shape[1]
    lg = ret_log_decay()
    pos = jnp.arange(L, dtype=jnp.float32)
    diff = pos[:, None] - pos[None, :]
    causal = diff >= 0
    dmask = jnp.where(causal[None], jnp.exp(jnp.where(causal, diff, 0.0)[None] * lg[:, None, None]), 0.0)
    scores = jnp.einsum('bihe,bjhe->bhij', q, k) * dmask[None]
    inner = jnp.einsum('bhij,bjhe->bihe', scores, v)
    cross = jnp.einsum('bihe,bhef->bihf', q, state) * jnp.exp((pos[:, None] + 1.0) * lg[None, :])[None, :, :, None]
    kdec = k * jnp.exp((L - 1.0 - pos)[:, None] * lg[None, :])[None, :, :, None]
    new_state = jnp.exp(L * lg)[None, :, None, None] * state + jnp.einsum('bjhe,bjhf->bhef', kdec, v)
    return inner + cross, new_state


def retention_prompt(q, k, v):
    B, S, H, E = q.shape
    nc = S // RET_CHUNK
    to_chunks = lambda t: t.reshape(B, nc, RET_CHUNK, H, E).swapaxes(0, 1)

    def step(state, qkv):
        o, state = retention_chunk(qkv[0], qkv[1], qkv[2], state)
        return state, o

    state, o = lax.scan(step, jnp.zeros((B, H, E, E), jnp.float32), (to_chunks(q), to_chunks(k), to_chunks(v)))
    return o.swapaxes(0, 1).reshape(B, S, H, E), state


def dilated_prompt_group(q, k, v, window, dilation):
    B, S, H, E = q.shape
    ns = window // dilation
    L = S // dilation
    nb = -(-L // ns)
    Lp = nb * ns

    def prep(t):
        t = t.reshape(B, L, dilation, H, E)
        t = jnp.pad(t, ((0, 0), (0, Lp - L), (0, 0), (0, 0), (0, 0)))
        return t.reshape(B, nb, ns, dilation, H, E)

    def with_prev(t):
        prev = jnp.pad(t[:, :-1], ((0, 0), (1, 0), (0, 0), (0, 0), (0, 0), (0, 0)))
        return jnp.concatenate([prev, t], axis=2)

    qb = prep(q)
    kk = with_prev(prep(k))
    vv = with_prev(prep(v))
    s = jnp.einsum('bnqrhe,bnkrhe->bnrhqk', qb, kk, preferred_element_type=jnp.float32) * HEAD_DIM ** -0.5
    qi = jnp.arange(ns)[:, None]
    kj = jnp.arange(2 * ns)[None, :]
    step = qi + ns - kj
    in_win = (step >= 0) & (step <= ns)
    has_key = (jnp.arange(nb) > 0)[:, None, None] | (kj >= ns)[None]
    mask = in_win[None] & has_key
    bias = -alibi_slopes()[:, None, None] * (step * dilation).astype(jnp.float32)[None]
    s = jnp.where(mask[None, :, None, None], s + bias[None, None, None], NEG_INF)
    m = s.max(axis=-1)
    p = jnp.exp(s - m[..., None])
    l = p.sum(axis=-1)
    o = jnp.einsum('bnrhqk,bnkrhe->bnqrhe', p, vv)
    o = o.reshape(B, Lp, dilation, H, E)[:, :L].reshape(B, S, H, E)
    back = lambda t: t.transpose(0, 1, 4, 2, 3).reshape(B, Lp, dilation, H)[:, :L].reshape(B, S, H)
    return o, back(m), back(l)


def dilated_sample_group(q, ctx_k, ctx_v, window, dilation):
    B, T, H, E = q.shape
    buf = ctx_k.shape[1] - T
    ns = window // dilation
    steps = jnp.arange(ns + 1)
    idx = (buf + jnp.arange(T))[:, None] - steps[None, :] * dilation
    valid = idx >= 0
    idx = jnp.maximum(idx, 0)
    kg = ctx_k[:, idx]
    vg = ctx_v[:, idx]
    s = jnp.einsum('bthe,btjhe->bhtj', q, kg, preferred_element_type=jnp.float32) * HEAD_DIM ** -0.5
    bias = -alibi_slopes()[:, None, None] * (steps * dilation).astype(jnp.float32)[None, None, :]
    s = jnp.where(valid[None, None], s + bias[None], NEG_INF)
    m = s.max(axis=-1)
    p = jnp.exp(s - m[..., None])
    l = p.sum(axis=-1)
    o = jnp.einsum('bhtj,btjhe->bthe', p, vg)
    return o, m.transpose(0, 2, 1), l.transpose(0, 2, 1)


def combine_groups(parts):
    mx = jnp.stack([m for _, m, _ in parts]).max(axis=0)
    num = sum(o * jnp.exp(m - mx)[..., None] for o, m, _ in parts)
    den = sum(l * jnp.exp(m - mx) for _, m, l in parts)
    return num / den[..., None]


def mixer_out(ret_o, gr, att_o, g_ret, w_out, dtype):
    B, S = ret_o.shape[:2]
    mu = jnp.mean(ret_o, axis=-1, keepdims=True)
    var = jnp.mean(jnp.square(ret_o - mu), axis=-1, keepdims=True)
    ret_n = ((ret_o - mu) * lax.rsqrt(var + EPS)).reshape(B, S, D_RET) * g_ret.astype(jnp.float32)
    ret_y = (jax.nn.silu(gr.astype(jnp.float32)) * ret_n).astype(dtype)
    att_y = att_o.reshape(B, S, D_ATT).astype(dtype)
    return jnp.concatenate([ret_y, att_y], axis=-1) @ w_out


def moe(h, w_router, b_router, w_gu, b_gu, w_down, b_down):
    N, D = h.shape
    logits = jnp.dot(h, w_router, preferred_element_type=jnp.float32) + b_router.astype(jnp.float32)
    top_val, top_idx = lax.top_k(logits, TOP_K)
    top_w = jax.nn.softmax(top_val, axis=-1)
    n_assign = N * TOP_K
    flat_e = top_idx.reshape(-1)
    flat_tok = jnp.repeat(jnp.arange(N, dtype=jnp.int32), TOP_K)
    flat_w = top_w.reshape(-1)
    order = jnp.argsort(flat_e)
    se, stok, sw = flat_e[order], flat_tok[order], flat_w[order]
    counts = jnp.bincount(flat_e, length=N_EXPERTS)
    padded = (counts + MOE_BLOCK - 1) // MOE_BLOCK * MOE_BLOCK
    pad_end = jnp.cumsum(padded)
    pad_start = pad_end - padded
    start = jnp.cumsum(counts) - counts
    dest = pad_start[se] + jnp.arange(n_assign) - start[se]
    n_blocks = n_assign // MOE_BLOCK + N_EXPERTS
    n_rows = n_blocks * MOE_BLOCK
    row_tok = jnp.zeros((n_rows,), jnp.int32).at[dest].set(stok)
    row_w = jnp.zeros((n_rows,), jnp.float32).at[dest].set(sw)
    block_e = jnp.minimum(jnp.searchsorted(pad_end, jnp.arange(n_blocks) * MOE_BLOCK, side='right'), N_EXPERTS - 1)

    def expert_block(args):
        toks, e = args
        gu = h[toks] @ w_gu[e] + b_gu[e]
        gate = jnp.minimum(gu[:, 0::2], SWIGLU_LIMIT)
        up = jnp.clip(gu[:, 1::2], -SWIGLU_LIMIT, SWIGLU_LIMIT)
        act = gate * jax.nn.sigmoid(SWIGLU_ALPHA * gate) * (up + 1.0)
        return act @ w_down[e] + b_down[e]

    out = lax.map(expert_block, (row_tok.reshape(n_blocks, MOE_BLOCK), block_e)).reshape(n_rows, D)
    y = jnp.zeros((N, D), jnp.float32).at[row_tok].add(out.astype(jnp.float32) * row_w[:, None])
    return y.astype(h.dtype)


def prompt_mixer(h, w_in):
    qr, kr, vr, gr, qa, ka, va = hybrid_proj(h, w_in)
    q, k, v = retention_inputs(qr, kr, vr)
    ret_o, ret_state = retention_prompt(q, k, v)
    att_o = combine_groups([dilated_prompt_group(qa, ka, va, w, d) for (w, d) in DIL_PATTERNS])
    keep = min(MAX_WINDOW, h.shape[1])
    return ret_o, gr, att_o, ret_state, ka[:, -keep:], va[:, -keep:]


def sample_mixer(h, w_in, state_ret, cache_k, cache_v):
    qr, kr, vr, gr, qa, ka, va = hybrid_proj(h, w_in)
    q, k, v = retention_inputs(qr, kr, vr)
    ret_o, ret_state = retention_chunk(q, k, v, state_ret.astype(jnp.float32))
    ctx_k = jnp.concatenate([cache_k.astype(ka.dtype), ka], axis=1)
    ctx_v = jnp.concatenate([cache_v.astype(va.dtype), va], axis=1)
    att_o = combine_groups([dilated_sample_group(qa, ctx_k, ctx_v, w, d) for (w, d) in DIL_PATTERNS])
    return ret_o, gr, att_o, ret_state, ka, va


def decoder_layer(x, c, mixer, w_ada, b_ada, g_pre_mix, g_post_mix, g_pre_ffn, g_post_ffn,
                  g_ret, w_out, w_router, b_router, w_gu, b_gu, w_down, b_down):
    sh_a, sc_a, gt_a, sh_f, sc_f, gt_f = ada_mod(c, w_ada, b_ada)
    h = rms_norm(x, g_pre_mix) * (1.0 + sc_a) + sh_a
    ret_o, gr, att_o, ret_state, win_k, win_v = mixer(h)
    mix = mixer_out(ret_o, gr, att_o, g_ret, w_out, x.dtype)
    x = x + gt_a * rms_norm(mix, g_post_mix)
    h = rms_norm(x, g_pre_ffn) * (1.0 + sc_f) + sh_f
    B, S, D = h.shape
    f = moe(h.reshape(B * S, D), w_router, b_router, w_gu, b_gu, w_down, b_down).reshape(B, S, D)
    x = x + gt_f * rms_norm(f, g_post_ffn)
    return x, ret_state, win_k, win_v


def setup_inputs(seed: int = 0) -> dict:
    key = jax.random.key(seed)
    ks = jax.random.split(key, 24)
    f32 = jnp.float32
    nrm = lambda k, shape, s: jax.random.normal(k, shape, f32) * s
    buf = min(MAX_WINDOW, PAST_LEN)
    D = D_MODEL
    return {
        'x_prompt': nrm(ks[0], (BATCH, SEQ, D), 1.0),
        'x_sample': nrm(ks[1], (DEC_BATCH, DEC_SEQ, D), 1.0),
        'state_ret': nrm(ks[2], (DEPTH, DEC_BATCH, N_RET_HEADS, HEAD_DIM, HEAD_DIM), 0.1),
        'cache_win_k': nrm(ks[3], (DEPTH, DEC_BATCH, buf, N_ATT_HEADS, HEAD_DIM), 1.0),
        'cache_win_v': nrm(ks[4], (DEPTH, DEC_BATCH, buf, N_ATT_HEADS, HEAD_DIM), 1.0),
        'c_prompt': nrm(ks[5], (BATCH, D), 1.0),
        'c_sample': nrm(ks[6], (DEC_BATCH, D), 1.0),
        'w_ada': nrm(ks[7], (DEPTH, D, 6 * D), 0.5 * D ** -0.5),
        'b_ada': nrm(ks[8], (DEPTH, 6 * D), 0.02),
        'g_pre_mix': 1.0 + nrm(ks[9], (DEPTH, D), 0.05),
        'g_post_mix': 1.0 + nrm(ks[10], (DEPTH, D), 0.05),
        'g_pre_ffn': 1.0 + nrm(ks[11], (DEPTH, D), 0.05),
        'g_post_ffn': 1.0 + nrm(ks[12], (DEPTH, D), 0.05),
        'w_in': nrm(ks[13], (DEPTH, D, D_IN), D ** -0.5),
        'g_ret': 1.0 + nrm(ks[14], (DEPTH, D_RET), 0.05),
        'w_out': nrm(ks[15], (DEPTH, D_MIX, D), D_MIX ** -0.5),
        'w_router': nrm(ks[16], (DEPTH, D, N_EXPERTS), D ** -0.5),
        'b_router': nrm(ks[17], (DEPTH, N_EXPERTS), 0.01),
        'w_gate_up': nrm(ks[18], (DEPTH, N_EXPERTS, D, 2 * D_FF), D ** -0.5),
        'b_gate_up': nrm(ks[19], (DEPTH, N_EXPERTS, 2 * D_FF), 0.02),
        'w_down': nrm(ks[20], (DEPTH, N_EXPERTS, D_FF, D), D_FF ** -0.5),
        'b_down': nrm(ks[21], (DEPTH, N_EXPERTS, D), 0.02),
    }


def reference(x_prompt, x_sample, state_ret, cache_win_k, cache_win_v, c_prompt, c_sample,
              w_ada, b_ada, g_pre_mix, g_post_mix, g_pre_ffn, g_post_ffn, w_in, g_ret, w_out,
              w_router, b_router, w_gate_up, b_gate_up, w_down, b_down):
    yp, ys = x_prompt, x_sample
    rp, rs, kp, vp, kс_dummy = [], [], [], [], None
    kp_list, vp_list, ks_list, vs_list = [], [], [], []
    for layer in range(DEPTH):
        shared = (w_ada[layer], b_ada[layer], g_pre_mix[layer], g_post_mix[layer], g_pre_ffn[layer],
                  g_post_ffn[layer], g_ret[layer], w_out[layer], w_router[layer], b_router[layer],
                  w_gate_up[layer], b_gate_up[layer], w_down[layer], b_down[layer])
        yp, r_p, k_p, v_p = decoder_layer(yp, c_prompt, lambda h: prompt_mixer(h, w_in[layer]), *shared)
        ys, r_s, k_s, v_s = decoder_layer(
            ys, c_sample,
            lambda h: sample_mixer(h, w_in[layer], state_ret[layer], cache_win_k[layer], cache_win_v[layer]),
            *shared)
        rp.append(r_p)
        rs.append(r_s)
        kp_list.append(k_p)
        vp_list.append(v_p)
        ks_list.append(k_s)
        vs_list.append(v_s)
    return (yp, ys, jnp.stack(rp), jnp.stack(rs), jnp.stack(kp_list), jnp.stack(vp_list),
            jnp.stack(ks_list), jnp.stack(vs_list))
```

```python
import math
from contextlib import ExitStack

import numpy as np
import concourse.bass as bass
import concourse.mybir as mybir
from concourse.bass_utils import run_bass_kernel_spmd

F32 = mybir.dt.float32
BF16 = mybir.dt.bfloat16
AF = mybir.ActivationFunctionType
ALU = mybir.AluOpType
AX = mybir.AxisListType

D = 1024
NH = 8
E = 64
DIN = 3584
NEXP = 32
EPS = 1e-6
NCORE = 8
OWN_T = 16
PRE_T = 48
SPAN_T = 32
NSEQ = 16
TSEQ = 8
NT = OWN_T + 1
TOEP_W = 384 + 128 * 16 + 512

ENGINES = ("tensor", "vector", "scalar", "gpsimd", "sync")
DMA_K = 6


class Sched:
    def __init__(self, nc, es):
        self.nc = nc
        self.q = {e: [] for e in ENGINES}
        self.cnt = {}
        self.seen = {}
        self.lastw = {}
        self.readers = {}
        self.sems = {}
        self.final = {}
        self.es = es
        self.n_ops = 0

    def _sem(self, name):
        if name not in self.sems:
            self.sems[name] = self.es.enter_context(self.nc.semaphore(name))
        return self.sems[name]

    def _new_token(self, eng, is_dma):
        if is_dma:
            st = "d_" + eng
            i = self.cnt.get(st, 0)
            self.cnt[st] = i + 1
            return ("%s%d" % (st, i % DMA_K), 16 * (i // DMA_K + 1)), i
        st = "c_" + eng
        i = self.cnt.get(st, 0) + 1
        self.cnt[st] = i
        return (st, i), i

    def _emit(self, eng, fn, reads, writes, is_dma):
        deps = {}

        def add(tok):
            if tok is None:
                return
            s, v = tok
            if deps.get(s, 0) < v:
                deps[s] = v

        for k in reads:
            add(self.lastw.get(k))
        for k in writes:
            add(self.lastw.get(k))
            for tok in self.readers.get(k, {}).values():
                add(tok)
        tok, idx = self._new_token(eng, is_dma)
        if is_dma and idx >= DMA_K:
            add((tok[0], tok[1] - 16))
        waits = []
        for s, v in deps.items():
            if eng == "tensor" and s == "c_tensor":
                continue
            if self.seen.get((eng, s), 0) >= v:
                continue
            self.seen[(eng, s)] = v
            waits.append((s, v))
        self.q[eng].append((waits, fn, tok, 16 if is_dma else 1))
        for k in reads:
            self.readers.setdefault(k, {})[tok[0]] = tok
        for k in writes:
            self.lastw[k] = tok
            self.readers[k] = {}
        if self.final.get(tok[0], 0) < tok[1]:
            self.final[tok[0]] = tok[1]
        self.n_ops += 1
        return tok

    def op(self, eng, fn, reads=(), writes=()):
        return self._emit(eng, fn, reads, writes, False)

    def dma(self, eng, out, in_, reads=(), writes=(), **kw):
        return self._emit(eng, lambda e: e.dma_start(out=out, in_=in_, **kw), reads, writes, True)

    def flush(self):
        nc = self.nc
        final = dict(self.final)
        for e in ENGINES:
            for waits, fn, tok, inc in self.q[e]:
                self._sem(tok[0])
        with nc.Block() as block:
            def run(engname):
                def body(e):
                    for waits, fn, tok, inc in self.q[engname]:
                        for s, v in waits:
                            e.wait_ge(self.sems[s], v)
                        ins = fn(e)
                        ins.then_inc(self.sems[tok[0]], inc)
                    for s, v in final.items():
                        if self.seen.get((engname, s), 0) < v:
                            e.wait_ge(self.sems[s], v)
                            self.seen[(engname, s)] = v
                return body
            block.tensor(run("tensor"))
            block.vector(run("vector"))
            block.scalar(run("scalar"))
            block.gpsimd(run("gpsimd"))
            block.sync(run("sync"))
        self.q = {e: [] for e in ENGINES}
        self.lastw = {}
        self.readers = {}


def _gammas():
    return 1.0 - 2.0 ** (-5.0 - np.arange(NH, dtype=np.float64))


def _ret_tables(L):
    g = _gammas()
    lg = np.log(g)
    j = np.arange(128)[:, None]
    i = np.arange(128)[None, :]
    dm = np.zeros((128, NH, 128), np.float64)
    ok = (i >= j) & (i < L) & (j < L)
    for h in range(NH):
        dm[:, h, :] = np.where(ok, np.exp((i - j) * lg[h]) / 8.0, 0.0)
    kd = np.zeros((128, NH, E), np.float64)
    for h in range(NH):
        col = np.where(np.arange(128) < L, np.exp((L - 1.0 - np.arange(128)) * lg[h]) / 8.0, 0.0)
        kd[:, h, :] = col[:, None]
    qd = np.zeros((64, NH, 128), np.float64)
    gd = np.zeros((64, NH, E), np.float64)
    for h in range(NH):
        qd[:, h, :] = np.exp((np.arange(128) + 1.0) * lg[h])[None, :]
        gd[:, h, :] = np.exp(L * lg[h])
    return (dm.astype(np.float32), kd.reshape(128, NH * E).astype(np.float32),
            qd.astype(np.float32), gd.astype(np.float32))


def _alibi_logw(dist):
    dist = np.asarray(dist, np.int64)
    cnt = ((dist >= 0) & (dist <= 128)).astype(np.float64)
    cnt += ((dist >= 0) & (dist <= 512) & (dist % 4 == 0))
    cnt += ((dist >= 0) & (dist <= 2048) & (dist % 16 == 0))
    return cnt


def _bias_fn(dist, h):
    slope = 2.0 ** (-8.0 * (h + 1.0) / NH)
    cnt = _alibi_logw(dist)
    with np.errstate(divide="ignore"):
        out = np.where(cnt > 0, -slope * np.maximum(dist, 0) + np.log(np.maximum(cnt, 1e-30)), -1e30)
    return out


def _toeplitz_prompt():
    p = np.arange(128)[:, None]
    c = np.arange(TOEP_W)[None, :]
    out = np.zeros((NH, 128, TOEP_W), np.float32)
    for h in range(NH):
        out[h] = _bias_fn(c - 384 - p, h).astype(np.float32)
    return out


def _sample_bias():
    out = np.zeros((128, 17, NH, TSEQ), np.float32)
    j = np.arange(128)[:, None]
    t = np.arange(TSEQ)[None, :]
    for a in range(16):
        for h in range(NH):
            out[:, a, h, :] = _bias_fn(2048 + t - (128 * a + j), h)
    for h in range(NH):
        b = _bias_fn(t - j, h)
        b = np.where(j < TSEQ, b, -1e30)
        out[:, 16, h, :] = b
    return out


def build_nc(cfg):
    nc = bass.Bass("TRN2", target_bir_lowering=False)
    dbg = cfg.get("debug", False)
    n_pre = cfg.get("n_pre", PRE_T)
    n_exp = cfg.get("n_exp", NEXP)
    stages = cfg.get("stages", "0ABCDE")

    def din(name, shape, dt=F32):
        return nc.dram_tensor(name, list(shape), dt, kind="ExternalInput").ap()

    def dout(name, shape, dt=F32):
        return nc.dram_tensor(name, list(shape), dt, kind="ExternalOutput").ap()

    def dscr(name, shape, dt):
        return nc.dram_tensor(name, list(shape), dt).ap()

    xp = din("xp", [(PRE_T + OWN_T) * 128, D])
    pvalid = din("pvalid", [128, PRE_T])
    xs_pad = din("xs_pad", [NSEQ * 128, D])
    xs = din("xs", [128, D])
    cvec = din("cvec", [17, D])
    state_in = din("state_in", [NSEQ, NH, E, E])
    bigb = "B" in stages
    ck = din("ck", [NSEQ if bigb else 1, 2048, NH * E])
    cv = din("cv", [NSEQ if bigb else 1, 2048, NH * E])
    w_ada = din("w_ada", [D, 6 * D])
    b_ada = din("b_ada", [1, 6 * D])
    g1T = din("g1T", [128, 8])
    g3T = din("g3T", [128, 8])
    g2b = din("g2b", [128, D])
    g4b = din("g4b", [128, D])
    gretb = din("gretb", [128, 512])
    w_in = din("w_in", [D, DIN])
    w_out = din("w_out", [D, D])
    w_router = din("w_router", [D, NEXP])
    b_router = din("b_router", [1, NEXP])
    big = "D" in stages
    w_gu = din("w_gu", [NEXP if big else 1, D, 2 * D])
    b_gu = din("b_gu", [NEXP, 2 * D])
    w_down = din("w_down", [NEXP if big else 1, D, D])
    b_down = din("b_down", [NEXP, D])
    c_ident = din("c_ident", [128, 128])
    c_sel = din("c_sel", [2, 17, 128])
    c_dm = din("c_dm", [2, 128, NH, 128])
    c_kd = din("c_kd", [2, 128, 512])
    c_qd = din("c_qd", [2, 64, NH, 128])
    c_gd = din("c_gd", [2, 64, NH, E])
    c_toep = din("c_toep", [NH, 128, TOEP_W])
    c_sbias = din("c_sbias", [128, 17, NH, TSEQ])

    yp = dout("yp", [OWN_T * 128, D])
    ys = dout("ys", [128, D])
    rp = dout("rp", [NH, E, E])
    rs = dout("rs", [NSEQ, NH, E, E])
    wk = dout("wk", [OWN_T * 128, 512])
    wv = dout("wv", [OWN_T * 128, 512])
    sk = dout("sk", [128, 512])
    sv = dout("sv", [128, 512])

    kT_scr = dscr("kT_scr", [NH, 64, SPAN_T * 128], BF16)
    qT_scr = dscr("qT_scr", [NH, 64, OWN_T * 128], BF16)
    v_scr = dscr("v_scr", [SPAN_T, 128, NH * 65], BF16)
    qTs_scr = dscr("qTs_scr", [NSEQ, 64, NH, 128], BF16)
    kTs_scr = dscr("kTs_scr", [NSEQ, 64, NH, 128], BF16)
    vs_scr = dscr("vs_scr", [NSEQ, 128, NH * 65], BF16)
    y_scr = dscr("y_scr", [NT, 128, D], BF16)
    x1_scr = dscr("x1_scr", [NT, 128, D], F32)
    gtab_scr = dscr("gtab_scr", [4, 128, D], F32)

    dbg_outs = {}

    def dbg_out(name, shape, dt=F32):
        if dbg:
            dbg_outs[name] = dout("dbg_" + name, shape, dt)
            return dbg_outs[name]
        return None

    with ExitStack() as es_all:
        S = Sched(nc, es_all)

        def sb(es, name, shape, dt=F32):
            return es.enter_context(nc.sbuf_tensor(name, list(shape), dt))

        def ps(es, name, shape, dt=F32):
            return es.enter_context(nc.psum_tensor(name, list(shape), dt))

        ident_f = sb(es_all, "ident_f", [128, 128])
        ident_b = sb(es_all, "ident_b", [128, 128], BF16)
        A1 = sb(es_all, "A1", [128, 8, 17])
        B1 = sb(es_all, "B1", [128, 8, 17])
        A2 = sb(es_all, "A2", [128, 8, 17])
        B2 = sb(es_all, "B2", [128, 8, 17])
        S.dma("sync", ident_f[:], c_ident[:, :], writes=["ident_f"])
        S.op("vector", lambda e: e.tensor_copy(ident_b[:], ident_f[:]), reads=["ident_f"], writes=["ident_b"])

        if "0" in stages:
            with ExitStack() as es:
                c17 = sb(es, "c17", [17, D])
                sc17 = sb(es, "sc17", [17, D])
                scT = sb(es, "scT", [128, 8, 17])
                brow = sb(es, "brow", [1, 6 * D])
                ones1 = sb(es, "ones1", [1, 32])
                g1 = sb(es, "g1", [128, 8])
                g3 = sb(es, "g3", [128, 8])
                gpost = [sb(es, "gpost0", [128, D]), sb(es, "gpost1", [128, D])]
                sel = sb(es, "sel", [17, 2, 128])
                blk = [sb(es, "wablk0", [128, 8, D]), sb(es, "wablk1", [128, 8, D])]
                gt_tok = sb(es, "gt_tok", [17, D])
                gtab = sb(es, "gtab", [128, D])
                ps_tr = ps(es, "ps_tr0", [128, 8, 17])
                ps_m = [ps(es, "ps_m0", [128, 8, 17]), ps(es, "ps_m1", [128, 8, 17])]
                ps_g = ps(es, "ps_g", [17, D])
                ps_t = ps(es, "ps_t", [128, D])

                S.dma("sync", c17[:], cvec[:, :], writes=["c17"])
                S.dma("sync", brow[:], b_ada[:, :], writes=["brow"])
                S.dma("sync", g1[:], g1T[:, :], writes=["g1"])
                S.dma("sync", g3[:], g3T[:, :], writes=["g3"])
                S.dma("sync", gpost[0][:], g2b[:, :], writes=["gpost0"])
                S.dma("sync", gpost[1][:], g4b[:, :], writes=["gpost1"])
                S.dma("sync", sel[:], c_sel.rearrange("a s p -> s a p"), writes=["sel"])
                S.op("gpsimd", lambda e: e.memset(ones1[:], 1.0), writes=["ones1"])
                S.op("scalar", lambda e: e.activation(sc17[:], c17[:], AF.Silu), reads=["c17"], writes=["sc17"])
                for k in range(8):
                    S.op("tensor", lambda e, k=k: e.transpose(ps_tr[:, k, :], sc17[0:17, k * 128:(k + 1) * 128],
                                                              ident_f[0:17, 0:17]),
                         reads=["sc17", "ident_f"], writes=["ps_tr0"])
                S.op("vector", lambda e: e.tensor_copy(scT[:], ps_tr[:]), reads=["ps_tr0"], writes=["scT"])

                wada_v = w_ada.rearrange("(k p) c -> p k c", p=128)
                for j in range(6):
                    bk = blk[j % 2]
                    bkn = "wablk%d" % (j % 2)
                    S.dma("sync", bk[:, 0:4, :], wada_v[:, 0:4, j * D:(j + 1) * D], writes=[bkn])
                    S.dma("scalar", bk[:, 4:8, :], wada_v[:, 4:8, j * D:(j + 1) * D], writes=[bkn + "b"])
                    if j in (0, 1, 3, 4):
                        pm = ps_m[j % 2]
                        pmn = "ps_m%d" % (j % 2)
                        for cc in range(8):
                            for k in range(8):
                                S.op("tensor", lambda e, cc=cc, k=k, pm=pm, bk=bk: e.matmul(
                                    pm[:, cc, :], bk[:, k, cc * 128:(cc + 1) * 128], scT[:, k, :],
                                    start=(k == 0), stop=False),
                                    reads=[bkn, bkn + "b", "scT"], writes=[pmn])
                            S.op("tensor", lambda e, cc=cc, pm=pm, j=j: e.matmul(
                                pm[:, cc, :], brow[0:1, j * D + cc * 128:j * D + (cc + 1) * 128], ones1[0:1, 0:17],
                                start=False, stop=True),
                                reads=["brow", "ones1"], writes=[pmn])
                        if j == 0:
                            S.op("vector", lambda e, pm=pm: e.tensor_copy(B1[:], pm[:]), reads=[pmn], writes=["B1"])
                        elif j == 3:
                            S.op("vector", lambda e, pm=pm: e.tensor_copy(B2[:], pm[:]), reads=[pmn], writes=["B2"])
                        else:
                            dst, gg, gn = (A1, g1, "g1") if j == 1 else (A2, g3, "g3")
                            dn = "A1" if j == 1 else "A2"
                            for cc in range(8):
                                S.op("vector", lambda e, cc=cc, pm=pm, dst=dst, gg=gg: e.tensor_scalar(
                                    dst[:, cc, :], pm[:, cc, :], 1.0, gg[:, cc:cc + 1], ALU.add, ALU.mult),
                                    reads=[pmn, gn], writes=[dn])
                    else:
                        gi = 0 if j == 2 else 1
                        for half in range(2):
                            for k in range(8):
                                S.op("tensor", lambda e, half=half, k=k, bk=bk: e.matmul(
                                    ps_g[0:17, half * 512:(half + 1) * 512], scT[:, k, :],
                                    bk[:, k, half * 512:(half + 1) * 512], start=(k == 0), stop=False),
                                    reads=[bkn, bkn + "b", "scT"], writes=["ps_g"])
                            S.op("tensor", lambda e, half=half, j=j: e.matmul(
                                ps_g[0:17, half * 512:(half + 1) * 512], ones1[0:1, 0:17],
                                brow[0:1, j * D + half * 512:j * D + (half + 1) * 512], start=False, stop=True),
                                reads=["brow", "ones1"], writes=["ps_g"])
                        S.op("vector", lambda e: e.tensor_copy(gt_tok[:], ps_g[:]), reads=["ps_g"], writes=["gt_tok"])
                        for which in range(2):
                            for half in range(2):
                                S.op("tensor", lambda e, which=which, half=half: e.matmul(
                                    ps_t[:, half * 512:(half + 1) * 512], sel[0:17, which, :],
                                    gt_tok[0:17, half * 512:(half + 1) * 512], start=True, stop=True),
                                    reads=["sel", "gt_tok"], writes=["ps_t"])
                            S.op("vector", lambda e, gi=gi: e.tensor_tensor(gtab[:], ps_t[:], gpost[gi][:], ALU.mult),
                                 reads=["ps_t", "gpost%d" % gi], writes=["gtab"])
                            S.dma("sync", gtab_scr[gi * 2 + which], gtab[:], reads=["gtab"], writes=["gtab_scr"])
                if dbg:
                    for nm, t in (("A1", A1), ("B1", B1), ("A2", A2), ("B2", B2)):
                        o = dbg_out(nm, [128, 8, 17])
                        S.dma("sync", o, t[:], reads=[nm], writes=["dbg_" + nm])
                    o = dbg_out("gtab", [4, 128, D])
                    with ExitStack() as es2:
                        pass
                S.flush()
                if dbg:
                    pass


        if "A" in stages:
            with ExitStack() as es:
                w_in_bf = sb(es, "w_in_bf", [128, 8, DIN], BF16)
                tabs = []
                for kind in range(2):
                    tabs.append(dict(
                        dm=sb(es, "dm%d" % kind, [128, NH, 128]), kd=sb(es, "kd%d" % kind, [128, 512]),
                        qd=sb(es, "qd%d" % kind, [64, NH, 128]), gd=sb(es, "gd%d" % kind, [64, NH, E])))
                gret = sb(es, "gret", [128, 512])
                pval = sb(es, "pval", [128, PRE_T])
                ones_c = sb(es, "ones_c", [128, 1])
                xt = [sb(es, "xt0", [128, D]), sb(es, "xt1", [128, D])]
                junk = sb(es, "junk", [128, D], BF16)
                ssum = sb(es, "ssum", [128, 1])
                rstd = sb(es, "rstd", [128, 1])
                xn = sb(es, "xn", [128, D], BF16)
                tmpm = sb(es, "tmpm", [128, 8, 128])
                hT = sb(es, "hT", [128, 8, 512], BF16)
                qrT = sb(es, "qrT", [64, NH, 512], BF16)
                krT = sb(es, "krT", [64, NH, 512], BF16)
                qaT = sb(es, "qaT", [64, NH, 512], BF16)
                kaT = sb(es, "kaT", [64, NH, 512], BF16)
                kdec = sb(es, "kdec", [128, 512], BF16)
                vr = sb(es, "vr", [128, 512], BF16)
                sg = sb(es, "sg", [128, 512])
                kaf = sb(es, "kaf", [128, 512])
                vaf = sb(es, "vaf", [128, 512])
                vext = sb(es, "vext", [128, NH, 65], BF16)
                PT = sb(es, "PT", [128, NH, 128], BF16)
                qdec = sb(es, "qdec", [64, NH, 128], BF16)
                Sp = sb(es, "Sp", [64, NH, E])
                Ss = sb(es, "Ss", [64, NH, E])
                Sbf = sb(es, "Sbf", [64, NH, E], BF16)
                osb = sb(es, "osb", [128, 512])
                cen = sb(es, "cen", [128, 512])
                sq = sb(es, "sq", [128, 512])
                st8 = sb(es, "st8", [128, 8])
                st8b = sb(es, "st8b", [128, 8])
                rety = sb(es, "rety", [128, 512], BF16)
                ps_tr = ps(es, "ps_trA", [128, 8, 128], BF16)
                ps_f = [ps(es, "ps_f0", [128, 512])]
                ps_k = [ps(es, "ps_k0", [128, 512]), ps(es, "ps_k1", [128, 512])]
                ps_A = ps(es, "ps_A", [128, NH, 128])
                ps_o = ps(es, "ps_o", [128, 512])

                w_in_v = w_in.rearrange("(k p) c -> p k c", p=128)
                for cb in range(7):
                    S.dma("gpsimd", w_in_bf[:, :, cb * 512:(cb + 1) * 512], w_in_v[:, :, cb * 512:(cb + 1) * 512],
                          writes=["w_in_bf"])
                for kind in range(2):
                    S.dma("sync", tabs[kind]["dm"][:], c_dm[kind], writes=["tabs"])
                    S.dma("sync", tabs[kind]["kd"][:], c_kd[kind], writes=["tabs"])
                    S.dma("sync", tabs[kind]["qd"][:], c_qd[kind], writes=["tabs"])
                    S.dma("sync", tabs[kind]["gd"][:], c_gd[kind], writes=["tabs"])
                S.dma("sync", gret[:], gretb[:, :], writes=["gret"])
                S.dma("sync", pval[:], pvalid[:, :], writes=["pval"])
                S.op("gpsimd", lambda e: e.memset(ones_c[:], 1.0), writes=["ones_c"])
                S.op("gpsimd", lambda e: e.memset(Sp[:], 0.0), writes=["Sp"])
                S.op("gpsimd", lambda e: e.memset(Sbf[:], 0.0), writes=["Sbf"])

                fcnt = [0]
                kcnt = [0]
                tcnt = [0]

                def norm_tile(x_src, col, t_in_grp):
                    i = tcnt[0] % 2
                    tcnt[0] += 1
                    xb, xbn = xt[i], "xt%d" % i
                    S.dma("sync", xb[:], x_src, writes=[xbn])
                    S.op("gpsimd", lambda e: e.memset(ssum[:], 0.0), writes=["ssum"])
                    S.op("scalar", lambda e: e.activation(junk[:], xb[:], AF.Square, accum_out=ssum[:]),
                         reads=[xbn], writes=["junk", "ssum"])
                    S.op("vector", lambda e: e.tensor_scalar(rstd[:], ssum[:], 1.0 / D, EPS, ALU.mult, ALU.add),
                         reads=["ssum"], writes=["rstd"])
                    S.op("scalar", lambda e: e.activation(rstd[:], rstd[:], AF.Sqrt), reads=["rstd"], writes=["rstd"])
                    S.op("vector", lambda e: e.reciprocal(rstd[:], rstd[:]), reads=["rstd"], writes=["rstd"])
                    S.op("vector", lambda e: e.tensor_scalar(xn[:], xb[:], rstd[:, 0:1], None, ALU.mult),
                         reads=[xbn, "rstd"], writes=["xn"])
                    for k in range(8):
                        S.op("tensor", lambda e, k=k: e.transpose(ps_tr[:, k, :], xn[:, k * 128:(k + 1) * 128], ident_b[:]),
                             reads=["xn", "ident_b"], writes=["ps_trA"])
                    S.op("vector", lambda e: e.tensor_tensor(
                        tmpm[:], ps_tr[:], A1[:, :, col:col + 1].to_broadcast([128, 8, 128]), ALU.mult),
                        reads=["ps_trA", "A1"], writes=["tmpm"])
                    c0 = t_in_grp * 128
                    S.op("vector", lambda e: e.tensor_tensor(
                        hT[:, :, c0:c0 + 128], tmpm[:], B1[:, :, col:col + 1].to_broadcast([128, 8, 128]), ALU.add),
                        reads=["tmpm", "B1"], writes=["hT"])

                def feat_proj(dst, dname, col_off, ntok):
                    for p in range(4):
                        pf, pfn = ps_f[0], "ps_f0"
                        for k in range(8):
                            S.op("tensor", lambda e, k=k, p=p, pf=pf: e.matmul(
                                pf[:, 0:ntok], w_in_bf[:, k, col_off + 128 * p:col_off + 128 * (p + 1)], hT[:, k, 0:ntok],
                                start=(k == 0), stop=(k == 7)),
                                reads=["w_in_bf", "hT"], writes=[pfn])
                        eng = "scalar" if (p % 2 == 0) else "vector"
                        if eng == "scalar":
                            S.op("scalar", lambda e, p=p, pf=pf: e.copy(dst[:, p, 0:ntok], pf[:, 0:ntok]),
                                 reads=[pfn], writes=[dname])
                        else:
                            S.op("vector", lambda e, p=p, pf=pf: e.tensor_copy(dst[:, p, 0:ntok], pf[:, 0:ntok]),
                                 reads=[pfn], writes=[dname])

                def feat_proj_h(dst, dname, col_off, ntok):
                    for h in range(NH):
                        pf, pfn = ps_f[0], "ps_f0"
                        for k in range(8):
                            S.op("tensor", lambda e, k=k, h=h, pf=pf: e.matmul(
                                pf[0:64, 0:ntok], w_in_bf[:, k, col_off + 64 * h:col_off + 64 * (h + 1)], hT[:, k, 0:ntok],
                                start=(k == 0), stop=(k == 7)),
                                reads=["w_in_bf", "hT"], writes=[pfn])
                        if h % 2 == 0:
                            S.op("scalar", lambda e, h=h, pf=pf: e.copy(dst[:, h, 0:ntok], pf[0:64, 0:ntok]),
                                 reads=[pfn], writes=[dname])
                        else:
                            S.op("vector", lambda e, h=h, pf=pf: e.tensor_copy(dst[:, h, 0:ntok], pf[0:64, 0:ntok]),
                                 reads=[pfn], writes=[dname])

                def tok_proj(col_off, t_in_grp):
                    i = kcnt[0] % 2
                    kcnt[0] += 1
                    pk, pkn = ps_k[i], "ps_k%d" % i
                    c0 = t_in_grp * 128
                    for k in range(8):
                        S.op("tensor", lambda e, k=k, pk=pk: e.matmul(
                            pk[:], hT[:, k, c0:c0 + 128], w_in_bf[:, k, col_off:col_off + 512],
                            start=(k == 0), stop=(k == 7)),
                            reads=["w_in_bf", "hT"], writes=[pkn])
                    return pk, pkn

                def do_group(tiles):
                    ntok = 128 * len(tiles)
                    mode = tiles[0]["mode"]
                    for ti, t in enumerate(tiles):
                        norm_tile(t["x"], t["col"], ti)
                    cut = cfg.get("cut", 99)
                    if cut <= 1:
                        return
                    if mode in ("own", "smp"):
                        feat_proj_h(qrT, "qrT", 0, ntok)
                        feat_proj_h(krT, "krT", 512, ntok)
                        feat_proj_h(qaT, "qaT", 2048, ntok)
                    if mode in ("win", "own", "smp"):
                        feat_proj_h(kaT, "kaT", 2560, ntok)
                    if mode in ("win", "own"):
                        sp0 = tiles[0]["span"] * 128
                        S.dma("sync", kT_scr[:, :, sp0:sp0 + ntok].rearrange("p q t -> q p t"), kaT[:, :, 0:ntok],
                              reads=["kaT"], writes=["kT_scr"])
                    if mode == "own":
                        o0 = tiles[0]["own"] * 128
                        S.dma("sync", qT_scr[:, :, o0:o0 + ntok].rearrange("p q t -> q p t"), qaT[:, :, 0:ntok],
                              reads=["qaT"], writes=["qT_scr"])
                    if mode == "smp":
                        s = tiles[0]["seq"]
                        S.dma("sync", qTs_scr[s], qaT[:, :, 0:128], reads=["qaT"], writes=["qTs_scr"])
                        S.dma("sync", kTs_scr[s], kaT[:, :, 0:128], reads=["kaT"], writes=["kTs_scr"])
                    if cut <= 2:
                        return
                    for ti, t in enumerate(tiles):
                        do_tile(ti, t, mode, cut)

                def do_tile(ti, t, mode, cut):
                    for _once in (0,):
                        c0 = ti * 128
                        tb = tabs[t["kind"]]
                        St, Sn = t["state"]
                        vcol = t["valid"]
                        pk, pkn = tok_proj(512, ti)
                        S.op("vector", lambda e, pk=pk, tb=tb: e.tensor_tensor(kdec[:], pk[:], tb["kd"][:], ALU.mult),
                             reads=[pkn, "tabs"], writes=["kdec"])
                        pk, pkn = tok_proj(1024, ti)
                        S.op("scalar", lambda e, pk=pk: e.copy(vr[:], pk[:]), reads=[pkn], writes=["vr"])
                        if mode in ("own", "smp"):
                            if not cfg.get("skip_sg"):
                                pk, pkn = tok_proj(1536, ti)
                                S.op("scalar", lambda e, pk=pk: e.activation(sg[:], pk[:], AF.Copy if cfg.get("nosilu") else AF.Silu), reads=[pkn], writes=["sg"])
                            if not cfg.get("skip_kaf"):
                                pk, pkn = tok_proj(2560, ti)
                                S.op("scalar", lambda e, pk=pk: e.copy(kaf[:], pk[:]), reads=[pkn], writes=["kaf"])
                            if mode == "own":
                                r0 = t["own"] * 128
                                if not cfg.get("nowk"):
                                    S.dma("sync", wk[r0:r0 + 128, :], kaf[:], reads=["kaf"], writes=["wk"])
                            else:
                                s = t["seq"]
                                S.dma("sync", sk[s * TSEQ:(s + 1) * TSEQ, :], kaf[0:TSEQ, :], reads=["kaf"], writes=["sk"])
                        if mode in ("win", "own", "smp"):
                            pk, pkn = tok_proj(3072, ti)
                            if mode != "win" and not cfg.get("skip_vaf"):
                                S.op("scalar", lambda e, pk=pk: e.copy(vaf[:], pk[:]), reads=[pkn], writes=["vaf"])
                                if mode == "own":
                                    r0 = t["own"] * 128
                                    if not cfg.get("nowk"):
                                        S.dma("sync", wv[r0:r0 + 128, :], vaf[:], reads=["vaf"], writes=["wv"])
                                else:
                                    s = t["seq"]
                                    S.dma("sync", sv[s * TSEQ:(s + 1) * TSEQ, :], vaf[0:TSEQ, :], reads=["vaf"], writes=["sv"])
                            vsrc = vcol if vcol is not None else ones_c[:, 0:1]
                            vin, vinn = (pk, pkn) if mode == "win" else (vaf, "vaf")
                            S.op("vector", lambda e, vin=vin, vsrc=vsrc: e.tensor_scalar(
                                vext[:, :, 0:64], vin[:].rearrange("p (h e) -> p h e", e=64), vsrc, None, ALU.mult),
                                reads=[vinn, "pval", "ones_c"], writes=["vext"])
                            S.op("vector", lambda e, vsrc=vsrc: e.tensor_copy(
                                vext[:, :, 64:65], vsrc.unsqueeze(1).to_broadcast([128, NH, 1])),
                                reads=["pval", "ones_c"], writes=["vext"])
                            if mode == "smp":
                                S.dma("sync", vs_scr[t["seq"]], vext[:].rearrange("p h e -> p (h e)"), reads=["vext"], writes=["vs_scr"])
                            else:
                                S.dma("sync", v_scr[t["span"]], vext[:].rearrange("p h e -> p (h e)"), reads=["vext"], writes=["v_scr"])
                        if cut <= 3:
                            continue
                        if mode in ("own", "smp"):
                            for h in range(NH):
                                p_, hf = h // 2, h % 2
                                pr = slice(0, 64) if cfg.get('pr0') else slice(64 * hf, 64 * hf + 64)
                                S.op("tensor", lambda e, h=h: e.matmul(
                                    ps_A[:, h, :], krT[:, h, c0:c0 + 128], qrT[:, h, c0:c0 + 128], start=True, stop=True),
                                    reads=["krT", "qrT"], writes=["ps_A"])
                            S.op("vector", lambda e, tb=tb: e.tensor_tensor(PT[:, 0:4, :], ps_A[:, 0:4, :], tb["dm"][:, 0:4, :], ALU.mult),
                                 reads=["ps_A", "tabs"], writes=["PT"])
                            S.op("vector", lambda e, tb=tb: e.tensor_tensor(PT[:, 4:8, :], ps_A[:, 4:8, :], tb["dm"][:, 4:8, :], ALU.mult),
                                 reads=["ps_A", "tabs"], writes=["PT"])
                            S.op("vector" if cfg.get("qdv") else "gpsimd", lambda e, tb=tb: e.tensor_tensor(qdec[:], qrT[:, :, c0:c0 + 128], tb["qd"][:], ALU.mult),
                                 reads=["qrT", "tabs"], writes=["qdec"])
                            for h in range(NH):
                                p_, hf = h // 2, h % 2
                                pr = slice(0, 64) if cfg.get('pr0') else slice(64 * hf, 64 * hf + 64)
                                S.op("tensor", lambda e, h=h: e.matmul(
                                    ps_o[:, 64 * h:64 * h + 64], PT[:, h, :], vr[:, 64 * h:64 * h + 64], start=True, stop=False),
                                    reads=["PT", "vr"], writes=["ps_o"])
                                S.op("tensor", lambda e, h=h: e.matmul(
                                    ps_o[:, 64 * h:64 * h + 64], qdec[:, h, :], Sbf[:, h, :], start=False, stop=True),
                                    reads=["qdec", "Sbf"], writes=["ps_o"])
                        if cut <= 4:
                            continue
                        for h in range(NH):
                            S.op("tensor", lambda e, h=h: e.matmul(
                                ps_S[:, h, :], kdec[:, 64 * h:64 * h + 64], vr[:, 64 * h:64 * h + 64],
                                start=True, stop=True),
                                reads=["kdec", "vr"], writes=["ps_S"])
                        S.op("vector", lambda e, St=St, tb=tb: e.tensor_tensor(St[:], St[:], tb["gd"][:], ALU.mult),
                             reads=[Sn, "tabs"], writes=[Sn])
                        vsrc = vcol if vcol is not None else ones_c[:, 0:1]
                        S.op("vector", lambda e, St=St, vsrc=vsrc: e.scalar_tensor_tensor(
                            St[:], ps_S[:], vsrc[0:64, :], St[:], ALU.mult, ALU.add),
                            reads=["ps_S", Sn, "pval", "ones_c"], writes=[Sn])
                        S.op("vector", lambda e, St=St: e.tensor_copy(Sbf[:], St[:]), reads=[Sn], writes=["Sbf"])
                        if cut <= 5:
                            continue
                        if mode in ("own", "smp"):
                            o3 = lambda t_: t_[:].rearrange("p (h e) -> p h e", e=64)
                            S.op("scalar", lambda e: e.copy(osb[:], ps_o[:]), reads=["ps_o"], writes=["osb"])
                            S.op("vector", lambda e: e.tensor_reduce(st8[:], o3(osb), AX.X, ALU.add), reads=["osb"], writes=["st8"])
                            S.op("vector", lambda e: e.tensor_scalar(st8[:], st8[:], 1.0 / E, None, ALU.mult), reads=["st8"], writes=["st8"])
                            S.op("vector", lambda e: e.tensor_tensor(o3(cen), o3(osb), st8[:].unsqueeze(2).to_broadcast([128, NH, E]), ALU.subtract),
                                 reads=["osb", "st8"], writes=["cen"])
                            S.op("gpsimd", lambda e: e.tensor_tensor(sq[:], cen[:], cen[:], ALU.mult), reads=["cen"], writes=["sq"])
                            S.op("vector", lambda e: e.tensor_reduce(st8b[:], o3(sq), AX.X, ALU.add), reads=["sq"], writes=["st8b"])
                            S.op("vector", lambda e: e.tensor_scalar(st8b[:], st8b[:], 1.0 / E, EPS, ALU.mult, ALU.add), reads=["st8b"], writes=["st8b"])
                            S.op("scalar", lambda e: e.activation(st8b[:], st8b[:], AF.Sqrt), reads=["st8b"], writes=["st8b"])
                            S.op("vector", lambda e: e.reciprocal(st8b[:], st8b[:]), reads=["st8b"], writes=["st8b"])
                            S.op("vector", lambda e: e.tensor_tensor(o3(cen), o3(cen), st8b[:].unsqueeze(2).to_broadcast([128, NH, E]), ALU.mult),
                                 reads=["cen", "st8b"], writes=["cen"])
                            S.op("gpsimd", lambda e: e.tensor_tensor(sq[:], sg[:], gret[:], ALU.mult), reads=["sg", "gret"], writes=["sq"])
                            S.op("vector", lambda e: e.tensor_tensor(rety[:], cen[:], sq[:], ALU.mult), reads=["cen", "sq"], writes=["rety"])
                            if mode == "own":
                                S.dma("sync", y_scr[t["own"], :, 0:512], rety[:], reads=["rety"], writes=["y_scr"])
                            else:
                                s = t["seq"]
                                S.dma("sync", y_scr[OWN_T, s * TSEQ:(s + 1) * TSEQ, 0:512], rety[0:TSEQ, :], reads=["rety"], writes=["y_scr"])

                ps_S = ps(es, "ps_S", [64, NH, E])

                pre_skip = PRE_T - n_pre
                for g in range(pre_skip // 4, PRE_T // 4):
                    tl = []
                    for ti in range(4):
                        t = 4 * g + ti
                        md = "win" if t >= PRE_T - 16 else "pre"
                        tl.append(dict(x=xp[t * 128:(t + 1) * 128, :], col=0, kind=0, mode=md, valid=pval[:, t:t + 1],
                                       state=(Sp, "Sp"), span=t - (PRE_T - 16)))
                    do_group(tl)
                for g in range(cfg.get('n_own', OWN_T) // 4):
                    tl = []
                    for ti in range(4):
                        t = 4 * g + ti
                        tl.append(dict(x=xp[(PRE_T + t) * 128:(PRE_T + t + 1) * 128, :], col=0, kind=0, mode="own", valid=None,
                                       state=(Sp, "Sp"), span=16 + t, own=t))
                    do_group(tl)
                S.dma("sync", rp.rearrange("h e f -> e h f"), Sp[:], reads=["Sp"], writes=["rp"])
                n_smp = cfg.get("n_smp", NSEQ)
                for s in range(n_smp):
                    S.dma("sync", Ss[:], state_in[s].rearrange("h e f -> e h f"), writes=["Ss"])
                    S.op("vector", lambda e: e.tensor_copy(Sbf[:], Ss[:]), reads=["Ss"], writes=["Sbf"])
                    do_group([dict(x=xs_pad[s * 128:(s + 1) * 128, :], col=1 + s, kind=1, mode="smp", valid=None,
                                   state=(Ss, "Ss"), seq=s)])
                    S.dma("sync", rs[s].rearrange("h e f -> e h f"), Ss[:], reads=["Ss"], writes=["rs"])
                S.flush()
                if dbg and "B" not in stages:
                    o = dbg_out("y_scr", [NT, 128, D], BF16)
                    S.dma("sync", o[0:OWN_T, :, 0:512], y_scr[0:OWN_T, :, 0:512], reads=["y_scr"], writes=["dbg_y"])
                    S.dma("sync", o[OWN_T, 0:TSEQ * n_smp, 0:512], y_scr[OWN_T, 0:TSEQ * n_smp, 0:512], reads=["y_scr"], writes=["dbg_y"])
                    S.flush()


        if "B" in stages:
            with ExitStack() as es:
                kTh = [sb(es, "kTh0", [64, SPAN_T * 128], BF16), sb(es, "kTh1", [64, SPAN_T * 128], BF16)]
                qTh = [sb(es, "qTh0", [64, OWN_T * 128], BF16), sb(es, "qTh1", [64, OWN_T * 128], BF16)]
                v_all = sb(es, "v_all", [128, SPAN_T, NH * 65], BF16)
                Th = [sb(es, "Th0", [128, TOEP_W]), sb(es, "Th1", [128, TOEP_W])]
                s_sb = [sb(es, "s_sb0", [128, 512]), sb(es, "s_sb1", [128, 512])]
                PTb = [sb(es, "PTb0", [128, 512], BF16), sb(es, "PTb1", [128, 512], BF16)]
                rec = sb(es, "rec", [128, 4, 1])
                attb = sb(es, "attb", [128, 4, 64], BF16)
                ps_s = [ps(es, "ps_s0", [128, 512]), ps(es, "ps_s1", [128, 512])]
                ps_acc = [ps(es, "ps_acc0", [128, 4, 128]), ps(es, "ps_acc1", [128, 4, 128])]
                for a in range(SPAN_T):
                    S.dma("sync" if a % 2 == 0 else "scalar", v_all[:, a, :], v_scr[a], reads=["v_scr"], writes=["v_all"])
                it = 0
                n_heads_b = cfg.get("n_heads_b", NH)
                for h in range(n_heads_b):
                    hb = h % 2
                    S.dma("sync", kTh[hb][:], kT_scr[h], reads=["kT_scr"], writes=["kTh%d" % hb])
                    S.dma("scalar", qTh[hb][:], qT_scr[h], reads=["qT_scr"], writes=["qTh%d" % hb])
                    S.dma("sync", Th[hb][:], c_toep[h], writes=["Th%d" % hb])
                    for G in range(OWN_T // 4):
                        acc = ps_acc[G % 2]
                        accn = "ps_acc%d" % (G % 2)
                        for a in range(4 * G, 4 * G + 20):
                            i = it % 2
                            it += 1
                            cs = 384 + 128 * (16 + 4 * G - a)
                            S.op("tensor", lambda e, i=i, hb=hb, a=a, G=G: e.matmul(
                                ps_s[i][:], kTh[hb][:, a * 128:(a + 1) * 128], qTh[hb][:, G * 512:(G + 1) * 512],
                                start=True, stop=True),
                                reads=["kTh%d" % hb, "qTh%d" % hb], writes=["ps_s%d" % i])
                            S.op("vector", lambda e, i=i, hb=hb, cs=cs: e.scalar_tensor_tensor(
                                s_sb[i][:], ps_s[i][:], 0.125, Th[hb][:, cs:cs + 512], ALU.mult, ALU.add),
                                reads=["ps_s%d" % i, "Th%d" % hb], writes=["s_sb%d" % i])
                            S.op("scalar", lambda e, i=i: e.activation(PTb[i][:], s_sb[i][:], AF.Exp),
                                 reads=["s_sb%d" % i], writes=["PTb%d" % i])
                            for qi in range(4):
                                first, last = 4 * G + qi, 16 + 4 * G + qi
                                if a < first or a > last:
                                    continue
                                S.op("tensor", lambda e, i=i, qi=qi, a=a, h=h, acc=acc, first=first, last=last: e.matmul(
                                    acc[:, qi, 0:65], PTb[i][:, qi * 128:(qi + 1) * 128], v_all[:, a, h * 65:(h + 1) * 65],
                                    start=(a == first), stop=(a == last)),
                                    reads=["PTb%d" % i, "v_all"], writes=[accn])
                        S.op("vector", lambda e, acc=acc: e.reciprocal(rec[:], acc[:, :, 64:65]), reads=[accn], writes=["rec"])
                        S.op("vector", lambda e, acc=acc: e.tensor_tensor(
                            attb[:], acc[:, :, 0:64], rec[:].to_broadcast([128, 4, 64]), ALU.mult),
                            reads=[accn, "rec"], writes=["attb"])
                        S.dma("sync", y_scr[4 * G:4 * G + 4, :, 512 + 64 * h:512 + 64 * h + 64].rearrange("t p e -> p t e"),
                              attb[:], reads=["attb"], writes=["y_scr"])
                S.flush()
                if dbg and "C" not in stages:
                    o = dbg_out("y_att", [OWN_T, 128, 512], BF16)
                    S.dma("sync", o[:, :, 0:64 * n_heads_b], y_scr[0:OWN_T, :, 512:512 + 64 * n_heads_b], reads=["y_scr"], writes=["dbg_y"])
                    S.flush()


        if "B" in stages:
            with ExitStack() as es:
                sbias = sb(es, "sbias", [128, 17, NH * TSEQ])
                qTs = sb(es, "qTs", [64, NH, 128], BF16)
                kTs = sb(es, "kTs", [64, NH, 128], BF16)
                vs = sb(es, "vs", [128, NH * 65], BF16)
                ckb = [sb(es, "ckb0", [128, 4, 512]), sb(es, "ckb1", [128, 4, 512])]
                cvb = [sb(es, "cvb0", [128, 4, 512]), sb(es, "cvb1", [128, 4, 512])]
                kcT = [sb(es, "kcT0", [64, NH, 128], BF16), sb(es, "kcT1", [64, NH, 128], BF16)]
                vx = [sb(es, "vx0", [128, 4, NH, 65], BF16), sb(es, "vx1", [128, 4, NH, 65], BF16)]
                s4 = sb(es, "s4", [128, 4, NH * TSEQ])
                P4 = [sb(es, "P40", [128, 4, NH * TSEQ], BF16), sb(es, "P41", [128, 4, NH * TSEQ], BF16)]
                recs = sb(es, "recs", [TSEQ, NH, 1])
                atts = sb(es, "atts", [TSEQ, NH, 64], BF16)
                ps_kT = [ps(es, "ps_kT0", [64, 4, 128]), ps(es, "ps_kT1", [64, 4, 128])]
                ps_s4 = [ps(es, "ps_s40", [128, 4, NH * TSEQ]), ps(es, "ps_s41", [128, 4, NH * TSEQ])]
                ps_as = ps(es, "ps_as", [TSEQ, NH, 128])
                S.dma("sync", sbias[:], c_sbias.rearrange("p a h t -> p a (h t)"), writes=["sbias"])
                for i in range(2):
                    S.op("gpsimd", lambda e, i=i: e.memset(vx[i][:], 1.0), writes=["vx%d" % i])
                n_smp_b = cfg.get("n_smp", NSEQ)
                ci = 0
                kti = 0
                for s in range(n_smp_b):
                    S.dma("sync", qTs[:], qTs_scr[s], reads=["qTs_scr"], writes=["qTs"])
                    S.dma("sync", kTs[:], kTs_scr[s], reads=["kTs_scr"], writes=["kTs"])
                    S.dma("sync", vs[:], vs_scr[s], reads=["vs_scr"], writes=["vs"])
                    for ch in range(5):
                        b = ci % 2
                        ci += 1
                        ntile = 4 if ch < 4 else 1
                        if ch < 4:
                            S.dma("sync", ckb[b][:], ck[s, 512 * ch:512 * (ch + 1), :].rearrange("(a p) c -> p a c", p=128),
                                  writes=["ckb%d" % b])
                            S.dma("scalar", cvb[b][:], cv[s, 512 * ch:512 * (ch + 1), :].rearrange("(a p) c -> p a c", p=128),
                                  writes=["cvb%d" % b])
                            S.op("scalar", lambda e, b=b: e.copy(vx[b][:, :, :, 0:64], cvb[b][:].rearrange("p a (h e) -> p a h e", e=64)),
                                 reads=["cvb%d" % b], writes=["vx%d" % b])
                        pss = ps_s4[b]
                        pssn = "ps_s4%d" % b
                        for j in range(ntile):
                            if ch < 4:
                                kb = kti % 2
                                kti += 1
                                for hh in range(2):
                                    pk_ = ps_kT[hh]
                                    for h4 in range(4):
                                        h = 4 * hh + h4
                                        S.op("tensor", lambda e, b=b, j=j, h=h, h4=h4, pk_=pk_: e.transpose(
                                            pk_[:, h4, :], ckb[b][:, j, 64 * h:64 * h + 64], ident_f[:]),
                                            reads=["ckb%d" % b, "ident_f"], writes=["ps_kT%d" % hh])
                                    if hh == 0:
                                        S.op("vector", lambda e, kb=kb, pk_=pk_: e.tensor_copy(kcT[kb][:, 0:4, :], pk_[:]),
                                             reads=["ps_kT0"], writes=["kcT%d" % kb])
                                    else:
                                        S.op("scalar", lambda e, kb=kb, pk_=pk_: e.copy(kcT[kb][:, 4:8, :], pk_[:]),
                                             reads=["ps_kT1"], writes=["kcT%d" % kb])
                                ksrc, ksn = kcT[kb], "kcT%d" % kb
                            else:
                                ksrc, ksn = kTs, "kTs"
                            for h in range(NH):
                                S.op("tensor", lambda e, j=j, h=h, ksrc=ksrc, pss=pss: e.matmul(
                                    pss[:, j, h * TSEQ:(h + 1) * TSEQ], ksrc[:, h, :], qTs[:, h, 0:TSEQ], start=True, stop=True),
                                    reads=[ksn, "qTs"], writes=[pssn])
                        a0 = 4 * ch
                        S.op("vector", lambda e, pss=pss, a0=a0, ntile=ntile: e.scalar_tensor_tensor(
                            s4[:, 0:ntile, :], pss[:, 0:ntile, :], 0.125, sbias[:, a0:a0 + ntile, :], ALU.mult, ALU.add),
                            reads=[pssn, "sbias"], writes=["s4"])
                        S.op("scalar", lambda e, b=b, ntile=ntile: e.activation(P4[b][:, 0:ntile, :], s4[:, 0:ntile, :], AF.Exp),
                             reads=["s4"], writes=["P4%d" % b])
                        for j in range(ntile):
                            for h in range(NH):
                                if ch < 4:
                                    rhs = vx[b][:, j, h, :]
                                    rn = "vx%d" % b
                                else:
                                    rhs = vs[:, h * 65:(h + 1) * 65]
                                    rn = "vs"
                                S.op("tensor", lambda e, b=b, j=j, h=h, rhs=rhs, first=(ch == 0 and j == 0), last=(ch == 4): e.matmul(
                                    ps_as[:, h, 0:65], P4[b][:, j, h * TSEQ:(h + 1) * TSEQ], rhs, start=first, stop=last),
                                    reads=["P4%d" % b, rn], writes=["ps_as"])
                    S.op("vector", lambda e: e.reciprocal(recs[:], ps_as[:, :, 64:65]), reads=["ps_as"], writes=["recs"])
                    S.op("vector", lambda e: e.tensor_tensor(atts[:], ps_as[:, :, 0:64], recs[:].to_broadcast([TSEQ, NH, 64]), ALU.mult),
                         reads=["ps_as", "recs"], writes=["atts"])
                    S.dma("sync", y_scr[OWN_T, s * TSEQ:(s + 1) * TSEQ, 512:1024], atts[:].rearrange("p h e -> p (h e)"),
                          reads=["atts"], writes=["y_scr"])
                S.flush()
                if dbg and "C" not in stages:
                    o = dbg_out("y_atts", [128, 512], BF16)
                    S.dma("sync", o[0:TSEQ * n_smp_b, :], y_scr[OWN_T, 0:TSEQ * n_smp_b, 512:1024], reads=["y_scr"], writes=["dbg_y"])
                    S.flush()


        if "C" in stages:
            h2T_all = sb(es_all, "h2T_all", [128, 8, NT * 128], BF16)
            G_all = sb(es_all, "G_all", [128, NT, NEXP])
            y_acc = sb(es_all, "y_acc", [128, NT, D])
            with ExitStack() as es:
                w_out_bf = sb(es, "w_out_bf", [128, 8, D], BF16)
                GA = [sb(es, "GA0", [128, D]), sb(es, "GA1", [128, D])]
                wr_f = sb(es, "wr_f", [128, 8, NEXP])
                br = sb(es, "br", [1, NEXP])
                ones_r = sb(es, "ones_r", [1, 128])
                bd_sb = sb(es, "bd_sb", [NEXP, D])
                ybf = [sb(es, "ybf0", [128, D], BF16), sb(es, "ybf1", [128, D], BF16)]
                xc = [sb(es, "xc0", [128, D]), sb(es, "xc1", [128, D])]
                yT = sb(es, "yT", [128, 8, 128], BF16)
                junkc = sb(es, "junkc", [128, D], BF16)
                ssc = sb(es, "ssc", [128, 1])
                ssc2 = sb(es, "ssc2", [128, 2])
                rsc = sb(es, "rsc", [128, 1])
                t1 = sb(es, "t1", [128, D])
                x1 = sb(es, "x1", [128, D])
                xn2 = sb(es, "xn2", [128, D])
                tmp2 = sb(es, "tmp2", [128, 8, 128])
                h2f = sb(es, "h2f", [128, 8, 128])
                lg = sb(es, "lg", [128, NEXP])
                mx8 = sb(es, "mx8", [128, 8])
                nmx = sb(es, "nmx", [128, 1])
                msk = sb(es, "msk", [128, NEXP])
                ex = sb(es, "ex", [128, NEXP])
                s3 = sb(es, "s3", [128, 1])
                GT = sb(es, "GT", [NEXP, 128])
                ps_trc = ps(es, "ps_trc", [128, 8, 128], BF16)
                ps_mix = ps(es, "ps_mix", [128, D])
                ps_tr2 = ps(es, "ps_tr2", [128, 8, 128])
                ps_lg = ps(es, "ps_lg", [128, NEXP])
                ps_gt = ps(es, "ps_gt", [NEXP, 128])

                S.dma("gpsimd", w_out_bf[:, :, 0:512], w_out.rearrange("(k p) c -> p k c", p=128)[:, :, 0:512], writes=["w_out_bf"])
                S.dma("gpsimd", w_out_bf[:, :, 512:D], w_out.rearrange("(k p) c -> p k c", p=128)[:, :, 512:D], writes=["w_out_bf"])
                S.dma("sync", GA[0][:], gtab_scr[0], writes=["GA0"])
                S.dma("sync", GA[1][:], gtab_scr[1], writes=["GA1"])
                S.dma("sync", wr_f[:], w_router.rearrange("(k p) n -> p k n", p=128), writes=["wr_f"])
                S.dma("sync", br[:], b_router[:, :], writes=["br"])
                S.dma("sync", bd_sb[:], b_down[:, :], writes=["bd_sb"])
                S.op("gpsimd", lambda e: e.memset(ones_r[:], 1.0), writes=["ones_r"])
                n_tc = cfg.get("n_tc", NT)
                for tt in list(range(n_tc - 1)) + [NT - 1]:
                    smp = (tt == NT - 1)
                    i = tt % 2
                    yb, ybn = ybf[i], "ybf%d" % i
                    xb, xbn = xc[i], "xc%d" % i
                    ga, gan = (GA[1], "GA1") if smp else (GA[0], "GA0")
                    S.dma("sync", yb[:], y_scr[tt], reads=["y_scr"], writes=[ybn])
                    if smp:
                        S.dma("scalar", xb[:], xs[:, :], writes=[xbn])
                    else:
                        S.dma("scalar", xb[:], xp[(PRE_T + tt) * 128:(PRE_T + tt + 1) * 128, :], writes=[xbn])
                    for k in range(8):
                        S.op("tensor", lambda e, k=k, yb=yb: e.transpose(ps_trc[:, k, :], yb[:, k * 128:(k + 1) * 128], ident_b[:]),
                             reads=[ybn, "ident_b"], writes=["ps_trc"])
                    S.op("scalar", lambda e: e.copy(yT[:], ps_trc[:]), reads=["ps_trc"], writes=["yT"])
                    for half in range(2):
                        for k in range(8):
                            S.op("tensor", lambda e, k=k, half=half: e.matmul(
                                ps_mix[:, half * 512:(half + 1) * 512], yT[:, k, :], w_out_bf[:, k, half * 512:(half + 1) * 512],
                                start=(k == 0), stop=(k == 7)),
                                reads=["yT", "w_out_bf"], writes=["ps_mix"])
                    for half in range(2):
                        S.op("scalar", lambda e, half=half: e.activation(
                            junkc[:, half * 512:(half + 1) * 512], ps_mix[:, half * 512:(half + 1) * 512], AF.Square,
                            accum_out=ssc2[:, half:half + 1]),
                            reads=["ps_mix"], writes=["junkc", "ssc2"])
                    S.op("vector", lambda e: e.tensor_tensor(ssc[:], ssc2[:, 0:1], ssc2[:, 1:2], ALU.add), reads=["ssc2"], writes=["ssc"])
                    S.op("vector", lambda e: e.tensor_scalar(rsc[:], ssc[:], 1.0 / D, EPS, ALU.mult, ALU.add), reads=["ssc"], writes=["rsc"])
                    S.op("scalar", lambda e: e.activation(rsc[:], rsc[:], AF.Sqrt), reads=["rsc"], writes=["rsc"])
                    S.op("vector", lambda e: e.reciprocal(rsc[:], rsc[:]), reads=["rsc"], writes=["rsc"])
                    for half in range(2):
                        hs = slice(half * 512, (half + 1) * 512)
                        S.op("vector", lambda e, hs=hs, ga=ga: e.scalar_tensor_tensor(
                            t1[:, hs], ps_mix[:, hs], rsc[:, 0:1], ga[:, hs], ALU.mult, ALU.mult),
                            reads=["ps_mix", "rsc", gan], writes=["t1"])
                    S.op("gpsimd", lambda e, xb=xb: e.tensor_tensor(x1[:], t1[:], xb[:], ALU.add), reads=["t1", xbn], writes=["x1"])
                    S.dma("sync", x1_scr[tt], x1[:], reads=["x1"], writes=["x1_scr"])
                    S.op("gpsimd", lambda e: e.memset(ssc[:], 0.0), reads=["rsc"], writes=["ssc"])
                    S.op("scalar", lambda e: e.activation(junkc[:], x1[:], AF.Square, accum_out=ssc[:]),
                         reads=["x1", "ssc"], writes=["junkc", "ssc"])
                    S.op("vector", lambda e: e.tensor_scalar(rsc[:], ssc[:], 1.0 / D, EPS, ALU.mult, ALU.add), reads=["ssc"], writes=["rsc"])
                    S.op("scalar", lambda e: e.activation(rsc[:], rsc[:], AF.Sqrt), reads=["rsc"], writes=["rsc"])
                    S.op("vector", lambda e: e.reciprocal(rsc[:], rsc[:]), reads=["rsc"], writes=["rsc"])
                    S.op("vector", lambda e: e.tensor_scalar(xn2[:], x1[:], rsc[:, 0:1], None, ALU.mult), reads=["x1", "rsc"], writes=["xn2"])
                    for k in range(8):
                        S.op("tensor", lambda e, k=k: e.transpose(ps_tr2[:, k, :], xn2[:, k * 128:(k + 1) * 128], ident_f[:]),
                             reads=["xn2", "ident_f"], writes=["ps_tr2"])
                    for hh in range(2):
                        ks = slice(4 * hh, 4 * hh + 4)
                        if smp:
                            a_b = A2[:, ks, 1:17].unsqueeze(3).to_broadcast([128, 4, NSEQ, TSEQ])
                            b_b = B2[:, ks, 1:17].unsqueeze(3).to_broadcast([128, 4, NSEQ, TSEQ])
                            v4 = lambda t_, ks=ks: t_[:, ks, :].rearrange("p k (s t) -> p k s t", t=TSEQ)
                        else:
                            a_b = A2[:, ks, 0:1].to_broadcast([128, 4, 128])
                            b_b = B2[:, ks, 0:1].to_broadcast([128, 4, 128])
                            v4 = lambda t_, ks=ks: t_[:, ks, :]
                        S.op("vector", lambda e, v4=v4, a_b=a_b: e.tensor_tensor(v4(tmp2), v4(ps_tr2), a_b, ALU.mult),
                             reads=["ps_tr2", "A2"], writes=["tmp2"])
                        S.op("vector", lambda e, v4=v4, b_b=b_b: e.tensor_tensor(v4(h2f), v4(tmp2), b_b, ALU.add),
                             reads=["tmp2", "B2"], writes=["h2f"])
                    S.op("gpsimd", lambda e, tt=tt: e.tensor_copy(h2T_all[:, :, tt * 128:(tt + 1) * 128], h2f[:]),
                         reads=["h2f"], writes=["h2T_all"])
                    for k in range(8):
                        S.op("tensor", lambda e, k=k: e.matmul(ps_lg[:], h2f[:, k, :], wr_f[:, k, :], start=(k == 0), stop=False),
                             reads=["h2f", "wr_f"], writes=["ps_lg"])
                    S.op("tensor", lambda e: e.matmul(ps_lg[:], ones_r[0:1, :], br[0:1, :], start=False, stop=True),
                         reads=["ones_r", "br"], writes=["ps_lg"])
                    S.op("vector", lambda e: e.tensor_copy(lg[:], ps_lg[:]), reads=["ps_lg"], writes=["lg"])
                    S.op("vector", lambda e: e.max(mx8[:], lg[:]), reads=["lg"], writes=["mx8"])
                    S.op("vector", lambda e: e.tensor_scalar(msk[:], lg[:], mx8[:, 3:4], None, ALU.is_ge), reads=["lg", "mx8"], writes=["msk"])
                    S.op("vector", lambda e: e.tensor_scalar(nmx[:], mx8[:, 0:1], -1.0, None, ALU.mult), reads=["mx8"], writes=["nmx"])
                    S.op("scalar", lambda e: e.activation(ex[:], lg[:], AF.Exp, bias=nmx[:, 0:1]), reads=["lg", "nmx"], writes=["ex"])
                    S.op("vector", lambda e: e.tensor_tensor(ex[:], ex[:], msk[:], ALU.mult), reads=["ex", "msk"], writes=["ex"])
                    S.op("vector", lambda e: e.tensor_reduce(s3[:], ex[:], AX.X, ALU.add), reads=["ex"], writes=["s3"])
                    S.op("vector", lambda e: e.reciprocal(s3[:], s3[:]), reads=["s3"], writes=["s3"])
                    S.op("vector", lambda e, tt=tt: e.tensor_scalar(G_all[:, tt, :], ex[:], s3[:, 0:1], None, ALU.mult),
                         reads=["ex", "s3"], writes=["G_all"])
                    S.op("tensor", lambda e, tt=tt: e.transpose(ps_gt[:], G_all[:, tt, :], ident_f[:]),
                         reads=["G_all", "ident_f"], writes=["ps_gt"])
                    S.op("vector", lambda e: e.tensor_copy(GT[:], ps_gt[:]), reads=["ps_gt"], writes=["GT"])
                    for half in range(2):
                        S.op("tensor", lambda e, half=half: e.matmul(
                            ps_mix[:, half * 512:(half + 1) * 512], GT[:, :], bd_sb[:, half * 512:(half + 1) * 512], start=True, stop=True),
                            reads=["GT", "bd_sb"], writes=["ps_mix"])
                    for half in range(2):
                        hs = slice(half * 512, (half + 1) * 512)
                        S.op("scalar", lambda e, hs=hs, tt=tt: e.copy(y_acc[:, tt, hs], ps_mix[:, hs]),
                             reads=["ps_mix"], writes=["y_acc"])
                if dbg and "D" not in stages:
                    o = dbg_out("G_all", [128, NT, NEXP])
                    S.dma("sync", o, G_all[:], reads=["G_all"], writes=["dbg_G"])
                    o = dbg_out("h2T", [128, 8, NT * 128], BF16)
                    S.dma("sync", o, h2T_all[:], reads=["h2T_all"], writes=["dbg_h"])
                    o = dbg_out("y_acc", [128, NT, D])
                    S.dma("sync", o, y_acc[:], reads=["y_acc"], writes=["dbg_ya"])
                S.flush()
                if dbg and "D" not in stages:
                    o = dbg_out("x1", [NT, 128, D])
                    for tt in list(range(n_tc - 1)) + [NT - 1]:
                        S.dma("sync", o[tt], x1_scr[tt], reads=["x1_scr"], writes=["dbg_x1"])
                    S.flush()


        if "D" in stages:
            with ExitStack() as es:
                actT = sb(es, "actT", [128, 8, NT * 128], BF16)
                Ub = [sb(es, "Ub0", [128, 8, 1024], BF16), sb(es, "Ub1", [128, 8, 1024], BF16)]
                Wd = sb(es, "Wd", [128, 8, D], BF16)
                bgu_raw = sb(es, "bgu_raw", [NEXP, 2 * D])
                bgu = sb(es, "bgu", [128, 8, 2, NEXP])
                gc = sb(es, "gc", [128, 512])
                sig = sb(es, "sig", [128, 512])
                uu = sb(es, "uu", [128, 512])
                ps_g = [ps(es, "ps_g0", [128, 512]), ps(es, "ps_g1", [128, 512])]
                ps_u = [ps(es, "ps_u0", [128, 512]), ps(es, "ps_u1", [128, 512])]
                ps_d = [ps(es, "ps_d0", [128, D]), ps(es, "ps_d1", [128, D])]

                S.dma("sync", bgu_raw[:], b_gu[:, :], writes=["bgu_raw"])
                braw = bgu_raw[:].rearrange("e (f j two) -> e f two j", f=8, j=128, two=2)
                for f in range(8):
                    for two in range(2):
                        S.op("tensor", lambda e, f=f, two=two: e.transpose(
                            ps_g[0][:, (2 * f + two) * NEXP:(2 * f + two + 1) * NEXP], braw[:, f, two, :], ident_f[0:NEXP, 0:NEXP]),
                            reads=["bgu_raw", "ident_f"], writes=["ps_g0"])
                S.op("vector", lambda e: e.tensor_copy(bgu[:].rearrange("p f two e -> p (f two e)"), ps_g[0][:, 0:16 * NEXP]),
                     reads=["ps_g0"], writes=["bgu"])

                wgu_v = w_gu.rearrange("e (k p) c -> e p k c", p=128)
                wd_v = w_down.rearrange("e (f p) c -> e p f c", p=128)
                groups = [(0, 512), (512, 512), (1024, 512), (1536, 512), (2048, 128)]

                def load_unit(u):
                    e_, fh = u // 2, u % 2
                    b = u % 2
                    for kh in range(2):
                        S.dma("gpsimd", Ub[b][:, 4 * kh:4 * kh + 4, :], wgu_v[e_, :, 4 * kh:4 * kh + 4, 1024 * fh:1024 * (fh + 1)],
                              writes=["Ub%d" % b])

                load_unit(0)
                cnt = 0
                dcnt = 0
                for e_ in range(n_exp):
                    for fh in range(2):
                        u = 2 * e_ + fh
                        b = u % 2
                        if u + 1 < 2 * n_exp:
                            load_unit(u + 1)
                        U5 = Ub[b][:].rearrange("p k (f j two) -> p k f two j", f=4, j=128, two=2)
                        for f4 in range(4):
                            f = 4 * fh + f4
                            for (t0, n) in groups:
                                i = cnt % 2
                                cnt += 1
                                pg, pu = ps_g[i], ps_u[i]
                                for k in range(8):
                                    S.op("tensor", lambda e, k=k, f4=f4, pg=pg, t0=t0, n=n, U5=U5: e.matmul(
                                        pg[:, 0:n], U5[:, k, f4, 0, :], h2T_all[:, k, t0:t0 + n], start=(k == 0), stop=(k == 7)),
                                        reads=["Ub%d" % b, "h2T_all"], writes=["ps_g%d" % i])
                                for k in range(8):
                                    S.op("tensor", lambda e, k=k, f4=f4, pu=pu, t0=t0, n=n, U5=U5: e.matmul(
                                        pu[:, 0:n], U5[:, k, f4, 1, :], h2T_all[:, k, t0:t0 + n], start=(k == 0), stop=(k == 7)),
                                        reads=["Ub%d" % b, "h2T_all"], writes=["ps_u%d" % i])
                                S.op("vector", lambda e, pg=pg, n=n, f=f, e_=e_: e.tensor_scalar(
                                    gc[:, 0:n], pg[:, 0:n], bgu[:, f, 0, e_:e_ + 1], 7.0, ALU.add, ALU.min),
                                    reads=["ps_g%d" % i, "bgu"], writes=["gc"])
                                S.op("scalar", lambda e, n=n: e.activation(sig[:, 0:n], gc[:, 0:n], AF.Sigmoid, scale=1.702),
                                     reads=["gc"], writes=["sig"])
                                S.op("vector", lambda e, pu=pu, n=n, f=f, e_=e_: e.tensor_scalar(
                                    uu[:, 0:n], pu[:, 0:n], bgu[:, f, 1, e_:e_ + 1], 7.0, ALU.add, ALU.min),
                                    reads=["ps_u%d" % i, "bgu"], writes=["uu"])
                                S.op("gpsimd", lambda e, n=n: e.tensor_scalar(uu[:, 0:n], uu[:, 0:n], -7.0, 1.0, ALU.max, ALU.add),
                                     reads=["uu"], writes=["uu"])
                                S.op("gpsimd", lambda e, n=n: e.tensor_tensor(sig[:, 0:n], sig[:, 0:n], gc[:, 0:n], ALU.mult),
                                     reads=["sig", "gc"], writes=["sig"])
                                S.op("vector", lambda e, n=n, f=f, t0=t0: e.tensor_tensor(
                                    actT[:, f, t0:t0 + n], sig[:, 0:n], uu[:, 0:n], ALU.mult),
                                    reads=["sig", "uu"], writes=["actT"])
                    for fhalf in range(2):
                        S.dma("gpsimd", Wd[:, 4 * fhalf:4 * fhalf + 4, :], wd_v[e_, :, 4 * fhalf:4 * fhalf + 4, :], writes=["Wd"])
                    for tt in range(NT):
                        i = dcnt % 2
                        dcnt += 1
                        pd = ps_d[i]
                        for half in range(2):
                            for f in range(8):
                                S.op("tensor", lambda e, f=f, half=half, tt=tt, pd=pd: e.matmul(
                                    pd[:, half * 512:(half + 1) * 512], actT[:, f, tt * 128:(tt + 1) * 128],
                                    Wd[:, f, half * 512:(half + 1) * 512], start=(f == 0), stop=(f == 7)),
                                    reads=["actT", "Wd"], writes=["ps_d%d" % i])
                        for half in range(2):
                            hs = slice(half * 512, (half + 1) * 512)
                            S.op("vector", lambda e, hs=hs, tt=tt, pd=pd, e_=e_: e.scalar_tensor_tensor(
                                y_acc[:, tt, hs], pd[:, hs], G_all[:, tt, e_:e_ + 1], y_acc[:, tt, hs], ALU.mult, ALU.add),
                                reads=["ps_d%d" % i, "G_all", "y_acc"], writes=["y_acc"])
                if dbg and "E" not in stages:
                    o = dbg_out("y_acc2", [128, NT, D])
                    S.dma("sync", o, y_acc[:], reads=["y_acc"], writes=["dbg_ya2"])
                S.flush()

        if "E" in stages:
            with ExitStack() as es:
                GF = [sb(es, "GF0", [128, D]), sb(es, "GF1", [128, D])]
                x1b = [sb(es, "x1b0", [128, D]), sb(es, "x1b1", [128, D])]
                junke = sb(es, "junke", [128, D], BF16)
                sse = sb(es, "sse", [128, 1])
                rse = sb(es, "rse", [128, 1])
                te = [sb(es, "te0", [128, D]), sb(es, "te1", [128, D])]
                S.dma("sync", GF[0][:], gtab_scr[2], writes=["GF0"])
                S.dma("sync", GF[1][:], gtab_scr[3], writes=["GF1"])
                for tt in range(NT):
                    smp = (tt == NT - 1)
                    i = tt % 2
                    gf, gfn = (GF[1], "GF1") if smp else (GF[0], "GF0")
                    S.dma("sync", x1b[i][:], x1_scr[tt], writes=["x1b%d" % i])
                    S.op("scalar", lambda e, tt=tt: e.activation(junke[:], y_acc[:, tt, :], AF.Square, accum_out=sse[:]),
                         reads=["y_acc"], writes=["junke", "sse"])
                    S.op("vector", lambda e: e.tensor_scalar(rse[:], sse[:], 1.0 / D, EPS, ALU.mult, ALU.add), reads=["sse"], writes=["rse"])
                    S.op("scalar", lambda e: e.activation(rse[:], rse[:], AF.Sqrt), reads=["rse"], writes=["rse"])
                    S.op("vector", lambda e: e.reciprocal(rse[:], rse[:]), reads=["rse"], writes=["rse"])
                    S.op("vector", lambda e, tt=tt, i=i, gf=gf: e.scalar_tensor_tensor(
                        te[i][:], y_acc[:, tt, :], rse[:, 0:1], gf[:], ALU.mult, ALU.mult),
                        reads=["y_acc", "rse", gfn], writes=["te%d" % i])
                    S.op("gpsimd", lambda e, i=i: e.tensor_tensor(te[i][:], te[i][:], x1b[i][:], ALU.add),
                         reads=["te%d" % i, "x1b%d" % i], writes=["te%d" % i])
                    if smp:
                        S.dma("sync", ys[:, :], te[i][:], reads=["te%d" % i], writes=["ys"])
                    else:
                        S.dma("sync", yp[tt * 128:(tt + 1) * 128, :], te[i][:], reads=["te%d" % i], writes=["yp"])
                S.flush()

        if "E" not in stages:
            S.dma("sync", yp[:, :], xp[PRE_T * 128:(PRE_T + OWN_T) * 128, :], writes=["yp"])
            S.dma("sync", ys[:, :], xs[:, :], writes=["ys"])
            S.flush()
    return nc, dbg_outs


_CONST = {}


def _consts():
    if _CONST:
        return _CONST
    sel = np.zeros((2, 17, 128), np.float32)
    sel[0, 0, :] = 1.0
    for p in range(128):
        sel[1, 1 + p // TSEQ, p] = 1.0
    tp = _ret_tables(128)
    ts = _ret_tables(TSEQ)
    _CONST.update(
        c_ident=np.eye(128, dtype=np.float32),
        c_sel=sel,
        c_dm=np.stack([tp[0], ts[0]]),
        c_kd=np.stack([tp[1], ts[1]]),
        c_qd=np.stack([tp[2], ts[2]]),
        c_gd=np.stack([tp[3], ts[3]]),
        c_toep=_toeplitz_prompt(),
        c_sbias=_sample_bias(),
    )
    return _CONST


def prep_core_inputs(inp, c):
    b, qtr = c // 4, c % 4
    T0 = 2048 * qtr
    f = np.float32
    xpad = np.zeros(((PRE_T + OWN_T) * 128, D), f)
    lo = T0 - PRE_T * 128
    src_lo = max(lo, 0)
    xpad[src_lo - lo:] = inp["x_prompt"][b, src_lo:T0 + 2048]
    pvalid = np.zeros((128, PRE_T), f)
    for t in range(PRE_T):
        if lo + 128 * t >= 0:
            pvalid[:, t] = 1.0
    sl = slice(NSEQ * c, NSEQ * (c + 1))
    xs = inp["x_sample"][sl]
    xs_pad = np.zeros((NSEQ, 128, D), f)
    xs_pad[:, :TSEQ] = xs
    cvec = np.concatenate([inp["c_prompt"][b:b + 1], inp["c_sample"][sl]], axis=0)
    tr8 = lambda g: np.ascontiguousarray(g.reshape(8, 128).T)
    bc = lambda g, n: np.ascontiguousarray(np.broadcast_to(g.reshape(1, -1), (128, n)))
    m = dict(
        xp=xpad, pvalid=pvalid, xs_pad=xs_pad.reshape(NSEQ * 128, D), xs=np.ascontiguousarray(xs.reshape(128, D)),
        cvec=np.ascontiguousarray(cvec),
        state_in=np.ascontiguousarray(inp["state_ret"][0, sl]),
        ck=np.ascontiguousarray(inp["cache_win_k"][0, sl].reshape(NSEQ, 2048, 512)),
        cv=np.ascontiguousarray(inp["cache_win_v"][0, sl].reshape(NSEQ, 2048, 512)),
        w_ada=inp["w_ada"][0], b_ada=inp["b_ada"],
        g1T=tr8(inp["g_pre_mix"][0]), g3T=tr8(inp["g_pre_ffn"][0]),
        g2b=bc(inp["g_post_mix"][0], D), g4b=bc(inp["g_post_ffn"][0], D), gretb=bc(inp["g_ret"][0], 512),
        w_in=inp["w_in"][0], w_out=inp["w_out"][0], w_router=inp["w_router"][0], b_router=inp["b_router"],
        w_gu=inp["w_gate_up"][0], b_gu=inp["b_gate_up"][0], w_down=inp["w_down"][0], b_down=inp["b_down"][0],
    )
    m.update(_consts())
    return {k: np.ascontiguousarray(v, dtype=np.float32) for k, v in m.items()}


STAGES = "0ABCDE"


def kernel(**inputs):
    inp = {k: np.asarray(v) for k, v in inputs.items()}
    cfg = dict(stages=STAGES)
    nc, _ = build_nc(cfg)
    in_maps = []
    for c in range(NCORE):
        m = prep_core_inputs(inp, c)
        if "D" not in STAGES:
            m["w_gu"] = m["w_gu"][:1]
            m["w_down"] = m["w_down"][:1]
        if "B" not in STAGES:
            m["ck"] = m["ck"][:1]
            m["cv"] = m["cv"][:1]
        in_maps.append(m)
    res = run_bass_kernel_spmd(nc, in_maps, core_ids=list(range(NCORE)))
    r = res.results
    f = np.float32
    y_prompt = np.zeros((2, 8192, D), f)
    y_sample = np.zeros((128, TSEQ, D), f)
    ret_p = np.zeros((1, 2, NH, E, E), f)
    ret_s = np.zeros((1, 128, NH, E, E), f)
    wk_p = np.zeros((1, 2, 2048, NH, E), f)
    wv_p = np.zeros((1, 2, 2048, NH, E), f)
    k_s = np.zeros((1, 128, TSEQ, NH, E), f)
    v_s = np.zeros((1, 128, TSEQ, NH, E), f)
    for c in range(NCORE):
        b, qtr = c // 4, c % 4
        y_prompt[b, 2048 * qtr:2048 * (qtr + 1)] = r[c]["yp"]
        y_sample[NSEQ * c:NSEQ * (c + 1)] = r[c]["ys"].reshape(NSEQ, TSEQ, D)
        ret_s[0, NSEQ * c:NSEQ * (c + 1)] = r[c]["rs"]
        k_s[0, NSEQ * c:NSEQ * (c + 1)] = r[c]["sk"].reshape(NSEQ, TSEQ, NH, E)
        v_s[0, NSEQ * c:NSEQ * (c + 1)] = r[c]["sv"].reshape(NSEQ, TSEQ, NH, E)
        if qtr == 3:
            ret_p[0, b] = r[c]["rp"]
            wk_p[0, b] = r[c]["wk"].reshape(2048, NH, E)
            wv_p[0, b] = r[c]["wv"].reshape(2048, NH, E)
    return (y_prompt, y_sample, ret_p, ret_s, wk_p, wv_p, k_s, v_s)
```

```python
import math
from contextlib import ExitStack

import numpy as np
import concourse.bass as bass
import concourse.mybir as mybir
from concourse.bass_utils import run_bass_kernel_spmd

F32 = mybir.dt.float32
BF16 = mybir.dt.bfloat16
AF = mybir.ActivationFunctionType
ALU = mybir.AluOpType
AX = mybir.AxisListType

D = 1024
NH = 8
E = 64
DIN = 3584
NEXP = 32
EPS = 1e-6
NCORE = 8
OWN_T = 16
PRE_T = 48
SPAN_T = 32
NSEQ = 16
TSEQ = 8
NT = OWN_T + 1
TOEP_W = 384 + 128 * 16 + 512

ENGINES = ("tensor", "vector", "scalar", "gpsimd", "sync")
DMA_K = 6


class Sched:
    def __init__(self, nc, es):
        self.nc = nc
        self.q = {e: [] for e in ENGINES}
        self.cnt = {}
        self.seen = {}
        self.lastw = {}
        self.readers = {}
        self.sems = {}
        self.final = {}
        self.es = es
        self.n_ops = 0

    def _sem(self, name):
        if name not in self.sems:
            self.sems[name] = self.es.enter_context(self.nc.semaphore(name))
        return self.sems[name]

    def _new_token(self, eng, is_dma):
        if is_dma:
            st = "d_" + eng
            i = self.cnt.get(st, 0)
            self.cnt[st] = i + 1
            return ("%s%d" % (st, i % DMA_K), 16 * (i // DMA_K + 1)), i
        st = "c_" + eng
        i = self.cnt.get(st, 0) + 1
        self.cnt[st] = i
        return (st, i), i

    def _emit(self, eng, fn, reads, writes, is_dma):
        deps = {}

        def add(tok):
            if tok is None:
                return
            s, v = tok
            if deps.get(s, 0) < v:
                deps[s] = v

        for k in reads:
            add(self.lastw.get(k))
        for k in writes:
            add(self.lastw.get(k))
            for tok in self.readers.get(k, {}).values():
                add(tok)
        tok, idx = self._new_token(eng, is_dma)
        if is_dma and idx >= DMA_K:
            add((tok[0], tok[1] - 16))
        waits = []
        for s, v in deps.items():
            if eng == "tensor" and s == "c_tensor":
                continue
            if self.seen.get((eng, s), 0) >= v:
                continue
            self.seen[(eng, s)] = v
            waits.append((s, v))
        self.q[eng].append((waits, fn, tok, 16 if is_dma else 1))
        for k in reads:
            self.readers.setdefault(k, {})[tok[0]] = tok
        for k in writes:
            self.lastw[k] = tok
            self.readers[k] = {}
        if self.final.get(tok[0], 0) < tok[1]:
            self.final[tok[0]] = tok[1]
        self.n_ops += 1
        return tok

    def op(self, eng, fn, reads=(), writes=()):
        return self._emit(eng, fn, reads, writes, False)

    def dma(self, eng, out, in_, reads=(), writes=(), **kw):
        return self._emit(eng, lambda e: e.dma_start(out=out, in_=in_, **kw), reads, writes, True)

    def flush(self):
        nc = self.nc
        final = dict(self.final)
        for e in ENGINES:
            for waits, fn, tok, inc in self.q[e]:
                self._sem(tok[0])
        with nc.Block() as block:
            def run(engname):
                def body(e):
                    for waits, fn, tok, inc in self.q[engname]:
                        for s, v in waits:
                            e.wait_ge(self.sems[s], v)
                        ins = fn(e)
                        ins.then_inc(self.sems[tok[0]], inc)
                    for s, v in final.items():
                        if self.seen.get((engname, s), 0) < v:
                            e.wait_ge(self.sems[s], v)
                            self.seen[(engname, s)] = v
                return body
            block.tensor(run("tensor"))
            block.vector(run("vector"))
            block.scalar(run("scalar"))
            block.gpsimd(run("gpsimd"))
            block.sync(run("sync"))
        self.q = {e: [] for e in ENGINES}
        self.lastw = {}
        self.readers = {}


def _gammas():
    return 1.0 - 2.0 ** (-5.0 - np.arange(NH, dtype=np.float64))


def _ret_tables(L):
    g = _gammas()
    lg = np.log(g)
    j = np.arange(128)[:, None]
    i = np.arange(128)[None, :]
    dm = np.zeros((128, NH, 128), np.float64)
    ok = (i >= j) & (i < L) & (j < L)
    for h in range(NH):
        dm[:, h, :] = np.where(ok, np.exp((i - j) * lg[h]) / 8.0, 0.0)
    kd = np.zeros((128, NH, E), np.float64)
    for h in range(NH):
        col = np.where(np.arange(128) < L, np.exp((L - 1.0 - np.arange(128)) * lg[h]) / 8.0, 0.0)
        kd[:, h, :] = col[:, None]
    qd = np.zeros((64, NH, 128), np.float64)
    gd = np.zeros((64, NH, E), np.float64)
    for h in range(NH):
        qd[:, h, :] = np.exp((np.arange(128) + 1.0) * lg[h])[None, :]
        gd[:, h, :] = np.exp(L * lg[h])
    return (dm.astype(np.float32), kd.reshape(128, NH * E).astype(np.float32),
            qd.astype(np.float32), gd.astype(np.float32))


def _alibi_logw(dist):
    dist = np.asarray(dist, np.int64)
    cnt = ((dist >= 0) & (dist <= 128)).astype(np.float64)
    cnt += ((dist >= 0) & (dist <= 512) & (dist % 4 == 0))
    cnt += ((dist >= 0) & (dist <= 2048) & (dist % 16 == 0))
    return cnt


def _bias_fn(dist, h):
    slope = 2.0 ** (-8.0 * (h + 1.0) / NH)
    cnt = _alibi_logw(dist)
    with np.errstate(divide="ignore"):
        out = np.where(cnt > 0, -slope * np.maximum(dist, 0) + np.log(np.maximum(cnt, 1e-30)), -1e30)
    return out


def _toeplitz_prompt():
    p = np.arange(128)[:, None]
    c = np.arange(TOEP_W)[None, :]
    out = np.zeros((NH, 128, TOEP_W), np.float32)
    for h in range(NH):
        out[h] = _bias_fn(c - 384 - p, h).astype(np.float32)
    return out


def _sample_bias():
    out = np.zeros((128, 17, NH, TSEQ), np.float32)
    j = np.arange(128)[:, None]
    t = np.arange(TSEQ)[None, :]
    for a in range(16):
        for h in range(NH):
            out[:, a, h, :] = _bias_fn(2048 + t - (128 * a + j), h)
    for h in range(NH):
        b = _bias_fn(t - j, h)
        b = np.where(j < TSEQ, b, -1e30)
        out[:, 16, h, :] = b
    return out


def build_nc(cfg):
    nc = bass.Bass("TRN2", target_bir_lowering=False)
    dbg = cfg.get("debug", False)
    n_pre = cfg.get("n_pre", PRE_T)
    n_exp = cfg.get("n_exp", NEXP)
    stages = cfg.get("stages", "0ABCDE")

    def din(name, shape, dt=F32):
        return nc.dram_tensor(name, list(shape), dt, kind="ExternalInput").ap()

    def dout(name, shape, dt=F32):
        return nc.dram_tensor(name, list(shape), dt, kind="ExternalOutput").ap()

    def dscr(name, shape, dt):
        return nc.dram_tensor(name, list(shape), dt).ap()

    xp = din("xp", [(PRE_T + OWN_T) * 128, D])
    pvalid = din("pvalid", [128, PRE_T])
    xs_pad = din("xs_pad", [NSEQ * 128, D])
    xs = din("xs", [128, D])
    cvec = din("cvec", [17, D])
    state_in = din("state_in", [NSEQ, NH, E, E])
    bigb = "B" in stages
    ck = din("ck", [NSEQ if bigb else 1, 2048, NH * E])
    cv = din("cv", [NSEQ if bigb else 1, 2048, NH * E])
    w_ada = din("w_ada", [D, 6 * D])
    b_ada = din("b_ada", [1, 6 * D])
    g1T = din("g1T", [128, 8])
    g3T = din("g3T", [128, 8])
    g2b = din("g2b", [128, D])
    g4b = din("g4b", [128, D])
    gretb = din("gretb", [128, 512])
    w_in = din("w_in", [D, DIN])
    w_out = din("w_out", [D, D])
    w_router = din("w_router", [D, NEXP])
    b_router = din("b_router", [1, NEXP])
    big = "D" in stages
    w_gu = din("w_gu", [NEXP if big else 1, D, 2 * D])
    b_gu = din("b_gu", [NEXP, 2 * D])
    w_down = din("w_down", [NEXP if big else 1, D, D])
    b_down = din("b_down", [NEXP, D])
    c_ident = din("c_ident", [128, 128])
    c_sel = din("c_sel", [2, 17, 128])
    c_dm = din("c_dm", [2, 128, NH, 128])
    c_kd = din("c_kd", [2, 128, 512])
    c_qd = din("c_qd", [2, 64, NH, 128])
    c_gd = din("c_gd", [2, 64, NH, E])
    c_toep = din("c_toep", [NH, 128, TOEP_W])
    c_sbias = din("c_sbias", [128, 17, NH, TSEQ])

    yp = dout("yp", [OWN_T * 128, D])
    ys = dout("ys", [128, D])
    rp = dout("rp", [NH, E, E])
    rs = dout("rs", [NSEQ, NH, E, E])
    wk = dout("wk", [OWN_T * 128, 512])
    wv = dout("wv", [OWN_T * 128, 512])
    sk = dout("sk", [128, 512])
    sv = dout("sv", [128, 512])

    kT_scr = dscr("kT_scr", [NH, 64, SPAN_T * 128], BF16)
    qT_scr = dscr("qT_scr", [NH, 64, OWN_T * 128], BF16)
    v_scr = dscr("v_scr", [SPAN_T, 128, NH * 65], BF16)
    qTs_scr = dscr("qTs_scr", [NSEQ, 64, NH, 128], BF16)
    kTs_scr = dscr("kTs_scr", [NSEQ, 64, NH, 128], BF16)
    vs_scr = dscr("vs_scr", [NSEQ, 128, NH * 65], BF16)
    y_scr = dscr("y_scr", [NT, 128, D], BF16)
    x1_scr = dscr("x1_scr", [NT, 128, D], F32)
    gtab_scr = dscr("gtab_scr", [4, 128, D], F32)

    dbg_outs = {}

    def dbg_out(name, shape, dt=F32):
        if dbg:
            dbg_outs[name] = dout("dbg_" + name, shape, dt)
            return dbg_outs[name]
        return None

    with ExitStack() as es_all:
        S = Sched(nc, es_all)

        def sb(es, name, shape, dt=F32):
            return es.enter_context(nc.sbuf_tensor(name, list(shape), dt))

        def ps(es, name, shape, dt=F32):
            return es.enter_context(nc.psum_tensor(name, list(shape), dt))

        ident_f = sb(es_all, "ident_f", [128, 128])
        ident_b = sb(es_all, "ident_b", [128, 128], BF16)
        A1 = sb(es_all, "A1", [128, 8, 17])
        B1 = sb(es_all, "B1", [128, 8, 17])
        A2 = sb(es_all, "A2", [128, 8, 17])
        B2 = sb(es_all, "B2", [128, 8, 17])
        S.dma("sync", ident_f[:], c_ident[:, :], writes=["ident_f"])
        S.op("vector", lambda e: e.tensor_copy(ident_b[:], ident_f[:]), reads=["ident_f"], writes=["ident_b"])

        if "0" in stages:
            with ExitStack() as es:
                c17 = sb(es, "c17", [17, D])
                sc17 = sb(es, "sc17", [17, D])
                scT = sb(es, "scT", [128, 8, 17])
                brow = sb(es, "brow", [1, 6 * D])
                ones1 = sb(es, "ones1", [1, 32])
                g1 = sb(es, "g1", [128, 8])
                g3 = sb(es, "g3", [128, 8])
                gpost = [sb(es, "gpost0", [128, D]), sb(es, "gpost1", [128, D])]
                sel = sb(es, "sel", [17, 2, 128])
                blk = [sb(es, "wablk0", [128, 8, D]), sb(es, "wablk1", [128, 8, D])]
                gt_tok = sb(es, "gt_tok", [17, D])
                gtab = sb(es, "gtab", [128, D])
                ps_tr = ps(es, "ps_tr0", [128, 8, 17])
                ps_m = [ps(es, "ps_m0", [128, 8, 17]), ps(es, "ps_m1", [128, 8, 17])]
                ps_g = ps(es, "ps_g", [17, D])
                ps_t = ps(es, "ps_t", [128, D])

                S.dma("sync", c17[:], cvec[:, :], writes=["c17"])
                S.dma("sync", brow[:], b_ada[:, :], writes=["brow"])
                S.dma("sync", g1[:], g1T[:, :], writes=["g1"])
                S.dma("sync", g3[:], g3T[:, :], writes=["g3"])
                S.dma("sync", gpost[0][:], g2b[:, :], writes=["gpost0"])
                S.dma("sync", gpost[1][:], g4b[:, :], writes=["gpost1"])
                S.dma("sync", sel[:], c_sel.rearrange("a s p -> s a p"), writes=["sel"])
                S.op("gpsimd", lambda e: e.memset(ones1[:], 1.0), writes=["ones1"])
                S.op("scalar", lambda e: e.activation(sc17[:], c17[:], AF.Silu), reads=["c17"], writes=["sc17"])
                for k in range(8):
                    S.op("tensor", lambda e, k=k: e.transpose(ps_tr[:, k, :], sc17[0:17, k * 128:(k + 1) * 128],
                                                              ident_f[0:17, 0:17]),
                         reads=["sc17", "ident_f"], writes=["ps_tr0"])
                S.op("vector", lambda e: e.tensor_copy(scT[:], ps_tr[:]), reads=["ps_tr0"], writes=["scT"])

                wada_v = w_ada.rearrange("(k p) c -> p k c", p=128)
                for j in range(6):
                    bk = blk[j % 2]
                    bkn = "wablk%d" % (j % 2)
                    S.dma("sync", bk[:, 0:4, :], wada_v[:, 0:4, j * D:(j + 1) * D], writes=[bkn])
                    S.dma("scalar", bk[:, 4:8, :], wada_v[:, 4:8, j * D:(j + 1) * D], writes=[bkn + "b"])
                    if j in (0, 1, 3, 4):
                        pm = ps_m[j % 2]
                        pmn = "ps_m%d" % (j % 2)
                        for cc in range(8):
                            for k in range(8):
                                S.op("tensor", lambda e, cc=cc, k=k, pm=pm, bk=bk: e.matmul(
                                    pm[:, cc, :], bk[:, k, cc * 128:(cc + 1) * 128], scT[:, k, :],
                                    start=(k == 0), stop=False),
                                    reads=[bkn, bkn + "b", "scT"], writes=[pmn])
                            S.op("tensor", lambda e, cc=cc, pm=pm, j=j: e.matmul(
                                pm[:, cc, :], brow[0:1, j * D + cc * 128:j * D + (cc + 1) * 128], ones1[0:1, 0:17],
                                start=False, stop=True),
                                reads=["brow", "ones1"], writes=[pmn])
                        if j == 0:
                            S.op("vector", lambda e, pm=pm: e.tensor_copy(B1[:], pm[:]), reads=[pmn], writes=["B1"])
                        elif j == 3:
                            S.op("vector", lambda e, pm=pm: e.tensor_copy(B2[:], pm[:]), reads=[pmn], writes=["B2"])
                        else:
                            dst, gg, gn = (A1, g1, "g1") if j == 1 else (A2, g3, "g3")
                            dn = "A1" if j == 1 else "A2"
                            for cc in range(8):
                                S.op("vector", lambda e, cc=cc, pm=pm, dst=dst, gg=gg: e.tensor_scalar(
                                    dst[:, cc, :], pm[:, cc, :], 1.0, gg[:, cc:cc + 1], ALU.add, ALU.mult),
                                    reads=[pmn, gn], writes=[dn])
                    else:
                        gi = 0 if j == 2 else 1
                        for half in range(2):
                            for k in range(8):
                                S.op("tensor", lambda e, half=half, k=k, bk=bk: e.matmul(
                                    ps_g[0:17, half * 512:(half + 1) * 512], scT[:, k, :],
                                    bk[:, k, half * 512:(half + 1) * 512], start=(k == 0), stop=False),
                                    reads=[bkn, bkn + "b", "scT"], writes=["ps_g"])
                            S.op("tensor", lambda e, half=half, j=j: e.matmul(
                                ps_g[0:17, half * 512:(half + 1) * 512], ones1[0:1, 0:17],
                                brow[0:1, j * D + half * 512:j * D + (half + 1) * 512], start=False, stop=True),
                                reads=["brow", "ones1"], writes=["ps_g"])
                        S.op("vector", lambda e: e.tensor_copy(gt_tok[:], ps_g[:]), reads=["ps_g"], writes=["gt_tok"])
                        for which in range(2):
                            for half in range(2):
                                S.op("tensor", lambda e, which=which, half=half: e.matmul(
                                    ps_t[:, half * 512:(half + 1) * 512], sel[0:17, which, :],
                                    gt_tok[0:17, half * 512:(half + 1) * 512], start=True, stop=True),
                                    reads=["sel", "gt_tok"], writes=["ps_t"])
                            S.op("vector", lambda e, gi=gi: e.tensor_tensor(gtab[:], ps_t[:], gpost[gi][:], ALU.mult),
                                 reads=["ps_t", "gpost%d" % gi], writes=["gtab"])
                            S.dma("sync", gtab_scr[gi * 2 + which], gtab[:], reads=["gtab"], writes=["gtab_scr"])
                if dbg:
                    for nm, t in (("A1", A1), ("B1", B1), ("A2", A2), ("B2", B2)):
                        o = dbg_out(nm, [128, 8, 17])
                        S.dma("sync", o, t[:], reads=[nm], writes=["dbg_" + nm])
                    o = dbg_out("gtab", [4, 128, D])
                    with ExitStack() as es2:
                        pass
                S.flush()
                if dbg:
                    pass


        if "A" in stages:
            with ExitStack() as es:
                w_in_bf = sb(es, "w_in_bf", [128, 8, DIN], BF16)
                tabs = []
                for kind in range(2):
                    tabs.append(dict(
                        dm=sb(es, "dm%d" % kind, [128, NH, 128]), kd=sb(es, "kd%d" % kind, [128, 512]),
                        qd=sb(es, "qd%d" % kind, [64, NH, 128]), gd=sb(es, "gd%d" % kind, [64, NH, E])))
                gret = sb(es, "gret", [128, 512])
                pval = sb(es, "pval", [128, PRE_T])
                ones_c = sb(es, "ones_c", [128, 1])
                xt = [sb(es, "xt0", [128, D]), sb(es, "xt1", [128, D])]
                junk = sb(es, "junk", [128, D], BF16)
                ssum = sb(es, "ssum", [128, 1])
                rstd = sb(es, "rstd", [128, 1])
                xn = sb(es, "xn", [128, D], BF16)
                tmpm = sb(es, "tmpm", [128, 8, 128])
                hT = sb(es, "hT", [128, 8, 512], BF16)
                qrT = sb(es, "qrT", [64, NH, 512], BF16)
                krT = sb(es, "krT", [64, NH, 512], BF16)
                qaT = sb(es, "qaT", [64, NH, 512], BF16)
                kaT = sb(es, "kaT", [64, NH, 512], BF16)
                kdec = sb(es, "kdec", [128, 512], BF16)
                vr = sb(es, "vr", [128, 512], BF16)
                sg = sb(es, "sg", [128, 512])
                kaf = sb(es, "kaf", [128, 512])
                vaf = sb(es, "vaf", [128, 512])
                vext = sb(es, "vext", [128, NH, 65], BF16)
                PT = sb(es, "PT", [128, NH, 128], BF16)
                qdec = sb(es, "qdec", [64, NH, 128], BF16)
                Sp = sb(es, "Sp", [64, NH, E])
                Ss = sb(es, "Ss", [64, NH, E])
                Sbf = sb(es, "Sbf", [64, NH, E], BF16)
                osb = sb(es, "osb", [128, 512])
                cen = sb(es, "cen", [128, 512])
                sq = sb(es, "sq", [128, 512])
                st8 = sb(es, "st8", [128, 8])
                st8b = sb(es, "st8b", [128, 8])
                rety = sb(es, "rety", [128, 512], BF16)
                ps_tr = ps(es, "ps_trA", [128, 8, 128], BF16)
                ps_f = [ps(es, "ps_f0", [128, 512])]
                ps_k = [ps(es, "ps_k0", [128, 512]), ps(es, "ps_k1", [128, 512])]
                ps_A = ps(es, "ps_A", [128, NH, 128])
                ps_o = ps(es, "ps_o", [128, 512])

                w_in_v = w_in.rearrange("(k p) c -> p k c", p=128)
                for cb in range(7):
                    S.dma("gpsimd", w_in_bf[:, :, cb * 512:(cb + 1) * 512], w_in_v[:, :, cb * 512:(cb + 1) * 512],
                          writes=["w_in_bf"])
                for kind in range(2):
                    S.dma("sync", tabs[kind]["dm"][:], c_dm[kind], writes=["tabs"])
                    S.dma("sync", tabs[kind]["kd"][:], c_kd[kind], writes=["tabs"])
                    S.dma("sync", tabs[kind]["qd"][:], c_qd[kind], writes=["tabs"])
                    S.dma("sync", tabs[kind]["gd"][:], c_gd[kind], writes=["tabs"])
                S.dma("sync", gret[:], gretb[:, :], writes=["gret"])
                S.dma("sync", pval[:], pvalid[:, :], writes=["pval"])
                S.op("gpsimd", lambda e: e.memset(ones_c[:], 1.0), writes=["ones_c"])
                S.op("gpsimd", lambda e: e.memset(Sp[:], 0.0), writes=["Sp"])
                S.op("gpsimd", lambda e: e.memset(Sbf[:], 0.0), writes=["Sbf"])

                fcnt = [0]
                kcnt = [0]
                tcnt = [0]

                def norm_tile(x_src, col, t_in_grp):
                    i = tcnt[0] % 2
                    tcnt[0] += 1
                    xb, xbn = xt[i], "xt%d" % i
                    S.dma("sync", xb[:], x_src, writes=[xbn])
                    S.op("gpsimd", lambda e: e.memset(ssum[:], 0.0), writes=["ssum"])
                    S.op("scalar", lambda e: e.activation(junk[:], xb[:], AF.Square, accum_out=ssum[:]),
                         reads=[xbn], writes=["junk", "ssum"])
                    S.op("vector", lambda e: e.tensor_scalar(rstd[:], ssum[:], 1.0 / D, EPS, ALU.mult, ALU.add),
                         reads=["ssum"], writes=["rstd"])
                    S.op("scalar", lambda e: e.activation(rstd[:], rstd[:], AF.Sqrt), reads=["rstd"], writes=["rstd"])
                    S.op("vector", lambda e: e.reciprocal(rstd[:], rstd[:]), reads=["rstd"], writes=["rstd"])
                    S.op("vector", lambda e: e.tensor_scalar(xn[:], xb[:], rstd[:, 0:1], None, ALU.mult),
                         reads=[xbn, "rstd"], writes=["xn"])
                    for k in range(8):
                        S.op("tensor", lambda e, k=k: e.transpose(ps_tr[:, k, :], xn[:, k * 128:(k + 1) * 128], ident_b[:]),
                             reads=["xn", "ident_b"], writes=["ps_trA"])
                    S.op("vector", lambda e: e.tensor_tensor(
                        tmpm[:], ps_tr[:], A1[:, :, col:col + 1].to_broadcast([128, 8, 128]), ALU.mult),
                        reads=["ps_trA", "A1"], writes=["tmpm"])
                    c0 = t_in_grp * 128
                    S.op("vector", lambda e: e.tensor_tensor(
                        hT[:, :, c0:c0 + 128], tmpm[:], B1[:, :, col:col + 1].to_broadcast([128, 8, 128]), ALU.add),
                        reads=["tmpm", "B1"], writes=["hT"])

                def feat_proj(dst, dname, col_off, ntok):
                    for p in range(4):
                        pf, pfn = ps_f[0], "ps_f0"
                        for k in range(8):
                            S.op("tensor", lambda e, k=k, p=p, pf=pf: e.matmul(
                                pf[:, 0:ntok], w_in_bf[:, k, col_off + 128 * p:col_off + 128 * (p + 1)], hT[:, k, 0:ntok],
                                start=(k == 0), stop=(k == 7)),
                                reads=["w_in_bf", "hT"], writes=[pfn])
                        eng = "scalar" if (p % 2 == 0) else "vector"
                        if eng == "scalar":
                            S.op("scalar", lambda e, p=p, pf=pf: e.copy(dst[:, p, 0:ntok], pf[:, 0:ntok]),
                                 reads=[pfn], writes=[dname])
                        else:
                            S.op("vector", lambda e, p=p, pf=pf: e.tensor_copy(dst[:, p, 0:ntok], pf[:, 0:ntok]),
                                 reads=[pfn], writes=[dname])

                def feat_proj_h(dst, dname, col_off, ntok):
                    for h in range(NH):
                        pf, pfn = ps_f[0], "ps_f0"
                        for k in range(8):
                            S.op("tensor", lambda e, k=k, h=h, pf=pf: e.matmul(
                                pf[0:64, 0:ntok], w_in_bf[:, k, col_off + 64 * h:col_off + 64 * (h + 1)], hT[:, k, 0:ntok],
                                start=(k == 0), stop=(k == 7)),
                                reads=["w_in_bf", "hT"], writes=[pfn])
                        if h % 2 == 0:
                            S.op("scalar", lambda e, h=h, pf=pf: e.copy(dst[:, h, 0:ntok], pf[0:64, 0:ntok]),
                                 reads=[pfn], writes=[dname])
                        else:
                            S.op("vector", lambda e, h=h, pf=pf: e.tensor_copy(dst[:, h, 0:ntok], pf[0:64, 0:ntok]),
                                 reads=[pfn], writes=[dname])

                def tok_proj(col_off, t_in_grp):
                    i = kcnt[0] % 2
                    kcnt[0] += 1
                    pk, pkn = ps_k[i], "ps_k%d" % i
                    c0 = t_in_grp * 128
                    for k in range(8):
                        S.op("tensor", lambda e, k=k, pk=pk: e.matmul(
                            pk[:], hT[:, k, c0:c0 + 128], w_in_bf[:, k, col_off:col_off + 512],
                            start=(k == 0), stop=(k == 7)),
                            reads=["w_in_bf", "hT"], writes=[pkn])
                    return pk, pkn

                def do_group(tiles):
                    ntok = 128 * len(tiles)
                    mode = tiles[0]["mode"]
                    for ti, t in enumerate(tiles):
                        norm_tile(t["x"], t["col"], ti)
                    cut = cfg.get("cut", 99)
                    if cut <= 1:
                        return
                    if mode in ("own", "smp"):
                        feat_proj_h(qrT, "qrT", 0, ntok)
                        feat_proj_h(krT, "krT", 512, ntok)
                        feat_proj_h(qaT, "qaT", 2048, ntok)
                    if mode in ("win", "own", "smp"):
                        feat_proj_h(kaT, "kaT", 2560, ntok)
                    if mode in ("win", "own"):
                        sp0 = tiles[0]["span"] * 128
                        S.dma("sync", kT_scr[:, :, sp0:sp0 + ntok].rearrange("p q t -> q p t"), kaT[:, :, 0:ntok],
                              reads=["kaT"], writes=["kT_scr"])
                    if mode == "own":
                        o0 = tiles[0]["own"] * 128
                        S.dma("sync", qT_scr[:, :, o0:o0 + ntok].rearrange("p q t -> q p t"), qaT[:, :, 0:ntok],
                              reads=["qaT"], writes=["qT_scr"])
                    if mode == "smp":
                        s = tiles[0]["seq"]
                        S.dma("sync", qTs_scr[s], qaT[:, :, 0:128], reads=["qaT"], writes=["qTs_scr"])
                        S.dma("sync", kTs_scr[s], kaT[:, :, 0:128], reads=["kaT"], writes=["kTs_scr"])
                    if cut <= 2:
                        return
                    for ti, t in enumerate(tiles):
                        do_tile(ti, t, mode, cut)

                def do_tile(ti, t, mode, cut):
                    for _once in (0,):
                        c0 = ti * 128
                        tb = tabs[t["kind"]]
                        St, Sn = t["state"]
                        vcol = t["valid"]
                        pk, pkn = tok_proj(512, ti)
                        S.op("vector", lambda e, pk=pk, tb=tb: e.tensor_tensor(kdec[:], pk[:], tb["kd"][:], ALU.mult),
                             reads=[pkn, "tabs"], writes=["kdec"])
                        pk, pkn = tok_proj(1024, ti)
                        S.op("scalar", lambda e, pk=pk: e.copy(vr[:], pk[:]), reads=[pkn], writes=["vr"])
                        if mode in ("own", "smp"):
                            if not cfg.get("skip_sg"):
                                pk, pkn = tok_proj(1536, ti)
                                S.op("scalar", lambda e, pk=pk: e.activation(sg[:], pk[:], AF.Copy if cfg.get("nosilu") else AF.Silu), reads=[pkn], writes=["sg"])
                            if not cfg.get("skip_kaf"):
                                pk, pkn = tok_proj(2560, ti)
                                S.op("scalar", lambda e, pk=pk: e.copy(kaf[:], pk[:]), reads=[pkn], writes=["kaf"])
                            if mode == "own":
                                r0 = t["own"] * 128
                                if not cfg.get("nowk"):
                                    S.dma("sync", wk[r0:r0 + 128, :], kaf[:], reads=["kaf"], writes=["wk"])
                            else:
                                s = t["seq"]
                                S.dma("sync", sk[s * TSEQ:(s + 1) * TSEQ, :], kaf[0:TSEQ, :], reads=["kaf"], writes=["sk"])
                        if mode in ("win", "own", "smp"):
                            pk, pkn = tok_proj(3072, ti)
                            if mode != "win" and not cfg.get("skip_vaf"):
                                S.op("scalar", lambda e, pk=pk: e.copy(vaf[:], pk[:]), reads=[pkn], writes=["vaf"])
                                if mode == "own":
                                    r0 = t["own"] * 128
                                    if not cfg.get("nowk"):
                                        S.dma("sync", wv[r0:r0 + 128, :], vaf[:], reads=["vaf"], writes=["wv"])
                                else:
                                    s = t["seq"]
                                    S.dma("sync", sv[s * TSEQ:(s + 1) * TSEQ, :], vaf[0:TSEQ, :], reads=["vaf"], writes=["sv"])
                            vsrc = vcol if vcol is not None else ones_c[:, 0:1]
                            vin, vinn = (pk, pkn) if mode == "win" else (vaf, "vaf")
                            S.op("vector", lambda e, vin=vin, vsrc=vsrc: e.tensor_scalar(
                                vext[:, :, 0:64], vin[:].rearrange("p (h e) -> p h e", e=64), vsrc, None, ALU.mult),
                                reads=[vinn, "pval", "ones_c"], writes=["vext"])
                            S.op("vector", lambda e, vsrc=vsrc: e.tensor_copy(
                                vext[:, :, 64:65], vsrc.unsqueeze(1).to_broadcast([128, NH, 1])),
                                reads=["pval", "ones_c"], writes=["vext"])
                            if mode == "smp":
                                S.dma("sync", vs_scr[t["seq"]], vext[:].rearrange("p h e -> p (h e)"), reads=["vext"], writes=["vs_scr"])
                            else:
                                S.dma("sync", v_scr[t["span"]], vext[:].rearrange("p h e -> p (h e)"), reads=["vext"], writes=["v_scr"])
                        if cut <= 3:
                            continue
                        if mode in ("own", "smp"):
                            for h in range(NH):
                                p_, hf = h // 2, h % 2
                                pr = slice(0, 64) if cfg.get('pr0') else slice(64 * hf, 64 * hf + 64)
                                S.op("tensor", lambda e, h=h: e.matmul(
                                    ps_A[:, h, :], krT[:, h, c0:c0 + 128], qrT[:, h, c0:c0 + 128], start=True, stop=True),
                                    reads=["krT", "qrT"], writes=["ps_A"])
                            S.op("vector", lambda e, tb=tb: e.tensor_tensor(PT[:, 0:4, :], ps_A[:, 0:4, :], tb["dm"][:, 0:4, :], ALU.mult),
                                 reads=["ps_A", "tabs"], writes=["PT"])
                            S.op("vector", lambda e, tb=tb: e.tensor_tensor(PT[:, 4:8, :], ps_A[:, 4:8, :], tb["dm"][:, 4:8, :], ALU.mult),
                                 reads=["ps_A", "tabs"], writes=["PT"])
                            S.op("vector", lambda e, tb=tb: e.tensor_tensor(qdec[:], qrT[:, :, c0:c0 + 128], tb["qd"][:], ALU.mult),
                                 reads=["qrT", "tabs"], writes=["qdec"])
                            for h in range(NH):
                                p_, hf = h // 2, h % 2
                                pr = slice(0, 64) if cfg.get('pr0') else slice(64 * hf, 64 * hf + 64)
                                S.op("tensor", lambda e, h=h: e.matmul(
                                    ps_o[:, 64 * h:64 * h + 64], PT[:, h, :], vr[:, 64 * h:64 * h + 64], start=True, stop=False),
                                    reads=["PT", "vr"], writes=["ps_o"])
                                S.op("tensor", lambda e, h=h: e.matmul(
                                    ps_o[:, 64 * h:64 * h + 64], qdec[:, h, :], Sbf[:, h, :], start=False, stop=True),
                                    reads=["qdec", "Sbf"], writes=["ps_o"])
                        if cut <= 4:
                            continue
                        for h in range(NH):
                            S.op("tensor", lambda e, h=h: e.matmul(
                                ps_S[:, h, :], kdec[:, 64 * h:64 * h + 64], vr[:, 64 * h:64 * h + 64],
                                start=True, stop=True),
                                reads=["kdec", "vr"], writes=["ps_S"])
                        S.op("vector", lambda e, St=St, tb=tb: e.tensor_tensor(St[:], St[:], tb["gd"][:], ALU.mult),
                             reads=[Sn, "tabs"], writes=[Sn])
                        vsrc = vcol if vcol is not None else ones_c[:, 0:1]
                        S.op("vector", lambda e, St=St, vsrc=vsrc: e.scalar_tensor_tensor(
                            St[:], ps_S[:], vsrc[0:64, :], St[:], ALU.mult, ALU.add),
                            reads=["ps_S", Sn, "pval", "ones_c"], writes=[Sn])
                        S.op("vector", lambda e, St=St: e.tensor_copy(Sbf[:], St[:]), reads=[Sn], writes=["Sbf"])
                        if cut <= 5:
                            continue
                        if mode in ("own", "smp"):
                            o3 = lambda t_: t_[:].rearrange("p (h e) -> p h e", e=64)
                            S.op("scalar", lambda e: e.copy(osb[:], ps_o[:]), reads=["ps_o"], writes=["osb"])
                            S.op("vector", lambda e: e.tensor_reduce(st8[:], o3(osb), AX.X, ALU.add), reads=["osb"], writes=["st8"])
                            S.op("vector", lambda e: e.tensor_scalar(st8[:], st8[:], 1.0 / E, None, ALU.mult), reads=["st8"], writes=["st8"])
                            S.op("vector", lambda e: e.tensor_tensor(o3(cen), o3(osb), st8[:].unsqueeze(2).to_broadcast([128, NH, E]), ALU.subtract),
                                 reads=["osb", "st8"], writes=["cen"])
                            S.op("scalar", lambda e: e.activation(sq[:], cen[:], AF.Square), reads=["cen"], writes=["sq"])
                            S.op("vector", lambda e: e.tensor_reduce(st8b[:], o3(sq), AX.X, ALU.add), reads=["sq"], writes=["st8b"])
                            S.op("vector", lambda e: e.tensor_scalar(st8b[:], st8b[:], 1.0 / E, EPS, ALU.mult, ALU.add), reads=["st8b"], writes=["st8b"])
                            S.op("scalar", lambda e: e.activation(st8b[:], st8b[:], AF.Sqrt), reads=["st8b"], writes=["st8b"])
                            S.op("vector", lambda e: e.reciprocal(st8b[:], st8b[:]), reads=["st8b"], writes=["st8b"])
                            S.op("vector", lambda e: e.tensor_tensor(o3(cen), o3(cen), st8b[:].unsqueeze(2).to_broadcast([128, NH, E]), ALU.mult),
                                 reads=["cen", "st8b"], writes=["cen"])
                            S.op("vector", lambda e: e.tensor_tensor(sq[:], sg[:], gret[:], ALU.mult), reads=["sg", "gret"], writes=["sq"])
                            S.op("vector", lambda e: e.tensor_tensor(rety[:], cen[:], sq[:], ALU.mult), reads=["cen", "sq"], writes=["rety"])
                            if mode == "own":
                                S.dma("sync", y_scr[t["own"], :, 0:512], rety[:], reads=["rety"], writes=["y_scr"])
                            else:
                                s = t["seq"]
                                S.dma("sync", y_scr[OWN_T, s * TSEQ:(s + 1) * TSEQ, 0:512], rety[0:TSEQ, :], reads=["rety"], writes=["y_scr"])

                ps_S = ps(es, "ps_S", [64, NH, E])

                pre_skip = PRE_T - n_pre
                for g in range(pre_skip // 4, PRE_T // 4):
                    tl = []
                    for ti in range(4):
                        t = 4 * g + ti
                        md = "win" if t >= PRE_T - 16 else "pre"
                        tl.append(dict(x=xp[t * 128:(t + 1) * 128, :], col=0, kind=0, mode=md, valid=pval[:, t:t + 1],
                                       state=(Sp, "Sp"), span=t - (PRE_T - 16)))
                    do_group(tl)
                for g in range(cfg.get('n_own', OWN_T) // 4):
                    tl = []
                    for ti in range(4):
                        t = 4 * g + ti
                        tl.append(dict(x=xp[(PRE_T + t) * 128:(PRE_T + t + 1) * 128, :], col=0, kind=0, mode="own", valid=None,
                                       state=(Sp, "Sp"), span=16 + t, own=t))
                    do_group(tl)
                S.dma("sync", rp.rearrange("h e f -> e h f"), Sp[:], reads=["Sp"], writes=["rp"])
                n_smp = cfg.get("n_smp", NSEQ)
                for s in range(n_smp):
                    S.dma("sync", Ss[:], state_in[s].rearrange("h e f -> e h f"), writes=["Ss"])
                    S.op("vector", lambda e: e.tensor_copy(Sbf[:], Ss[:]), reads=["Ss"], writes=["Sbf"])
                    do_group([dict(x=xs_pad[s * 128:(s + 1) * 128, :], col=1 + s, kind=1, mode="smp", valid=None,
                                   state=(Ss, "Ss"), seq=s)])
                    S.dma("sync", rs[s].rearrange("h e f -> e h f"), Ss[:], reads=["Ss"], writes=["rs"])
                S.flush()
                if dbg and "B" not in stages:
                    o = dbg_out("y_scr", [NT, 128, D], BF16)
                    S.dma("sync", o[0:OWN_T, :, 0:512], y_scr[0:OWN_T, :, 0:512], reads=["y_scr"], writes=["dbg_y"])
                    S.dma("sync", o[OWN_T, 0:TSEQ * n_smp, 0:512], y_scr[OWN_T, 0:TSEQ * n_smp, 0:512], reads=["y_scr"], writes=["dbg_y"])
                    S.flush()


        if "B" in stages:
            with ExitStack() as es:
                kTh = [sb(es, "kTh0", [64, SPAN_T * 128], BF16), sb(es, "kTh1", [64, SPAN_T * 128], BF16)]
                qTh = [sb(es, "qTh0", [64, OWN_T * 128], BF16), sb(es, "qTh1", [64, OWN_T * 128], BF16)]
                v_all = sb(es, "v_all", [128, SPAN_T, NH * 65], BF16)
                Th = [sb(es, "Th0", [128, TOEP_W]), sb(es, "Th1", [128, TOEP_W])]
                s_sb = [sb(es, "s_sb0", [128, 512]), sb(es, "s_sb1", [128, 512])]
                PTb = [sb(es, "PTb0", [128, 512], BF16), sb(es, "PTb1", [128, 512], BF16)]
                rec = sb(es, "rec", [128, 4, 1])
                attb = sb(es, "attb", [128, 4, 64], BF16)
                ps_s = [ps(es, "ps_s0", [128, 512]), ps(es, "ps_s1", [128, 512])]
                ps_acc = [ps(es, "ps_acc0", [128, 4, 128]), ps(es, "ps_acc1", [128, 4, 128])]
                for a in range(SPAN_T):
                    S.dma("sync" if a % 2 == 0 else "scalar", v_all[:, a, :], v_scr[a], reads=["v_scr"], writes=["v_all"])
                it = 0
                n_heads_b = cfg.get("n_heads_b", NH)
                for h in range(n_heads_b):
                    hb = h % 2
                    S.dma("sync", kTh[hb][:], kT_scr[h], reads=["kT_scr"], writes=["kTh%d" % hb])
                    S.dma("scalar", qTh[hb][:], qT_scr[h], reads=["qT_scr"], writes=["qTh%d" % hb])
                    S.dma("sync", Th[hb][:], c_toep[h], writes=["Th%d" % hb])
                    for G in range(OWN_T // 4):
                        acc = ps_acc[G % 2]
                        accn = "ps_acc%d" % (G % 2)
                        for a in range(4 * G, 4 * G + 20):
                            i = it % 2
                            it += 1
                            cs = 384 + 128 * (16 + 4 * G - a)
                            S.op("tensor", lambda e, i=i, hb=hb, a=a, G=G: e.matmul(
                                ps_s[i][:], kTh[hb][:, a * 128:(a + 1) * 128], qTh[hb][:, G * 512:(G + 1) * 512],
                                start=True, stop=True),
                                reads=["kTh%d" % hb, "qTh%d" % hb], writes=["ps_s%d" % i])
                            S.op("vector", lambda e, i=i, hb=hb, cs=cs: e.scalar_tensor_tensor(
                                s_sb[i][:], ps_s[i][:], 0.125, Th[hb][:, cs:cs + 512], ALU.mult, ALU.add),
                                reads=["ps_s%d" % i, "Th%d" % hb], writes=["s_sb%d" % i])
                            S.op("scalar", lambda e, i=i: e.activation(PTb[i][:], s_sb[i][:], AF.Exp),
                                 reads=["s_sb%d" % i], writes=["PTb%d" % i])
                            for qi in range(4):
                                first, last = 4 * G + qi, 16 + 4 * G + qi
                                if a < first or a > last:
                                    continue
                                S.op("tensor", lambda e, i=i, qi=qi, a=a, h=h, acc=acc, first=first, last=last: e.matmul(
                                    acc[:, qi, 0:65], PTb[i][:, qi * 128:(qi + 1) * 128], v_all[:, a, h * 65:(h + 1) * 65],
                                    start=(a == first), stop=(a == last)),
                                    reads=["PTb%d" % i, "v_all"], writes=[accn])
                        S.op("vector", lambda e, acc=acc: e.reciprocal(rec[:], acc[:, :, 64:65]), reads=[accn], writes=["rec"])
                        S.op("vector", lambda e, acc=acc: e.tensor_tensor(
                            attb[:], acc[:, :, 0:64], rec[:].to_broadcast([128, 4, 64]), ALU.mult),
                            reads=[accn, "rec"], writes=["attb"])
                        S.dma("sync", y_scr[4 * G:4 * G + 4, :, 512 + 64 * h:512 + 64 * h + 64].rearrange("t p e -> p t e"),
                              attb[:], reads=["attb"], writes=["y_scr"])
                S.flush()
                if dbg and "C" not in stages:
                    o = dbg_out("y_att", [OWN_T, 128, 512], BF16)
                    S.dma("sync", o[:, :, 0:64 * n_heads_b], y_scr[0:OWN_T, :, 512:512 + 64 * n_heads_b], reads=["y_scr"], writes=["dbg_y"])
                    S.flush()


        if "B" in stages:
            with ExitStack() as es:
                sbias = sb(es, "sbias", [128, 17, NH * TSEQ])
                qTs = sb(es, "qTs", [64, NH, 128], BF16)
                kTs = sb(es, "kTs", [64, NH, 128], BF16)
                vs = sb(es, "vs", [128, NH * 65], BF16)
                ckb = [sb(es, "ckb0", [128, 4, 512]), sb(es, "ckb1", [128, 4, 512])]
                cvb = [sb(es, "cvb0", [128, 4, 512]), sb(es, "cvb1", [128, 4, 512])]
                kcT = [sb(es, "kcT0", [64, NH, 128], BF16), sb(es, "kcT1", [64, NH, 128], BF16)]
                vx = [sb(es, "vx0", [128, 4, NH, 65], BF16), sb(es, "vx1", [128, 4, NH, 65], BF16)]
                s4 = sb(es, "s4", [128, 4, NH * TSEQ])
                P4 = [sb(es, "P40", [128, 4, NH * TSEQ], BF16), sb(es, "P41", [128, 4, NH * TSEQ], BF16)]
                recs = sb(es, "recs", [TSEQ, NH, 1])
                atts = sb(es, "atts", [TSEQ, NH, 64], BF16)
                ps_kT = [ps(es, "ps_kT0", [64, 4, 128]), ps(es, "ps_kT1", [64, 4, 128])]
                ps_s4 = [ps(es, "ps_s40", [128, 4, NH * TSEQ]), ps(es, "ps_s41", [128, 4, NH * TSEQ])]
                ps_as = ps(es, "ps_as", [TSEQ, NH, 128])
                S.dma("sync", sbias[:], c_sbias.rearrange("p a h t -> p a (h t)"), writes=["sbias"])
                for i in range(2):
                    S.op("gpsimd", lambda e, i=i: e.memset(vx[i][:], 1.0), writes=["vx%d" % i])
                n_smp_b = cfg.get("n_smp", NSEQ)
                ci = 0
                kti = 0
                for s in range(n_smp_b):
                    S.dma("sync", qTs[:], qTs_scr[s], reads=["qTs_scr"], writes=["qTs"])
                    S.dma("sync", kTs[:], kTs_scr[s], reads=["kTs_scr"], writes=["kTs"])
                    S.dma("sync", vs[:], vs_scr[s], reads=["vs_scr"], writes=["vs"])
                    for ch in range(5):
                        b = ci % 2
                        ci += 1
                        ntile = 4 if ch < 4 else 1
                        if ch < 4:
                            S.dma("sync", ckb[b][:], ck[s, 512 * ch:512 * (ch + 1), :].rearrange("(a p) c -> p a c", p=128),
                                  writes=["ckb%d" % b])
                            S.dma("scalar", cvb[b][:], cv[s, 512 * ch:512 * (ch + 1), :].rearrange("(a p) c -> p a c", p=128),
                                  writes=["cvb%d" % b])
                            S.op("scalar", lambda e, b=b: e.copy(vx[b][:, :, :, 0:64], cvb[b][:].rearrange("p a (h e) -> p a h e", e=64)),
                                 reads=["cvb%d" % b], writes=["vx%d" % b])
                        pss = ps_s4[b]
                        pssn = "ps_s4%d" % b
                        for j in range(ntile):
                            if ch < 4:
                                kb = kti % 2
                                kti += 1
                                for hh in range(2):
                                    pk_ = ps_kT[hh]
                                    for h4 in range(4):
                                        h = 4 * hh + h4
                                        S.op("tensor", lambda e, b=b, j=j, h=h, h4=h4, pk_=pk_: e.transpose(
                                            pk_[:, h4, :], ckb[b][:, j, 64 * h:64 * h + 64], ident_f[:]),
                                            reads=["ckb%d" % b, "ident_f"], writes=["ps_kT%d" % hh])
                                    if hh == 0:
                                        S.op("vector", lambda e, kb=kb, pk_=pk_: e.tensor_copy(kcT[kb][:, 0:4, :], pk_[:]),
                                             reads=["ps_kT0"], writes=["kcT%d" % kb])
                                    else:
                                        S.op("scalar", lambda e, kb=kb, pk_=pk_: e.copy(kcT[kb][:, 4:8, :], pk_[:]),
                                             reads=["ps_kT1"], writes=["kcT%d" % kb])
                                ksrc, ksn = kcT[kb], "kcT%d" % kb
                            else:
                                ksrc, ksn = kTs, "kTs"
                            for h in range(NH):
                                S.op("tensor", lambda e, j=j, h=h, ksrc=ksrc, pss=pss: e.matmul(
                                    pss[:, j, h * TSEQ:(h + 1) * TSEQ], ksrc[:, h, :], qTs[:, h, 0:TSEQ], start=True, stop=True),
                                    reads=[ksn, "qTs"], writes=[pssn])
                        a0 = 4 * ch
                        S.op("vector", lambda e, pss=pss, a0=a0, ntile=ntile: e.scalar_tensor_tensor(
                            s4[:, 0:ntile, :], pss[:, 0:ntile, :], 0.125, sbias[:, a0:a0 + ntile, :], ALU.mult, ALU.add),
                            reads=[pssn, "sbias"], writes=["s4"])
                        S.op("scalar", lambda e, b=b, ntile=ntile: e.activation(P4[b][:, 0:ntile, :], s4[:, 0:ntile, :], AF.Exp),
                             reads=["s4"], writes=["P4%d" % b])
                        for j in range(ntile):
                            for h in range(NH):
                                if ch < 4:
                                    rhs = vx[b][:, j, h, :]
                                    rn = "vx%d" % b
                                else:
                                    rhs = vs[:, h * 65:(h + 1) * 65]
                                    rn = "vs"
                                S.op("tensor", lambda e, b=b, j=j, h=h, rhs=rhs, first=(ch == 0 and j == 0), last=(ch == 4): e.matmul(
                                    ps_as[:, h, 0:65], P4[b][:, j, h * TSEQ:(h + 1) * TSEQ], rhs, start=first, stop=last),
                                    reads=["P4%d" % b, rn], writes=["ps_as"])
                    S.op("vector", lambda e: e.reciprocal(recs[:], ps_as[:, :, 64:65]), reads=["ps_as"], writes=["recs"])
                    S.op("vector", lambda e: e.tensor_tensor(atts[:], ps_as[:, :, 0:64], recs[:].to_broadcast([TSEQ, NH, 64]), ALU.mult),
                         reads=["ps_as", "recs"], writes=["atts"])
                    S.dma("sync", y_scr[OWN_T, s * TSEQ:(s + 1) * TSEQ, 512:1024], atts[:].rearrange("p h e -> p (h e)"),
                          reads=["atts"], writes=["y_scr"])
                S.flush()
                if dbg and "C" not in stages:
                    o = dbg_out("y_atts", [128, 512], BF16)
                    S.dma("sync", o[0:TSEQ * n_smp_b, :], y_scr[OWN_T, 0:TSEQ * n_smp_b, 512:1024], reads=["y_scr"], writes=["dbg_y"])
                    S.flush()


        if "C" in stages:
            h2T_all = sb(es_all, "h2T_all", [128, 8, NT * 128], BF16)
            G_all = sb(es_all, "G_all", [128, NT, NEXP])
            y_acc = sb(es_all, "y_acc", [128, NT, D])
            with ExitStack() as es:
                w_out_bf = sb(es, "w_out_bf", [128, 8, D], BF16)
                GA = [sb(es, "GA0", [128, D]), sb(es, "GA1", [128, D])]
                wr_f = sb(es, "wr_f", [128, 8, NEXP])
                br = sb(es, "br", [1, NEXP])
                ones_r = sb(es, "ones_r", [1, 128])
                bd_sb = sb(es, "bd_sb", [NEXP, D])
                ybf = [sb(es, "ybf0", [128, D], BF16), sb(es, "ybf1", [128, D], BF16)]
                xc = [sb(es, "xc0", [128, D]), sb(es, "xc1", [128, D])]
                yT = sb(es, "yT", [128, 8, 128], BF16)
                junkc = sb(es, "junkc", [128, D], BF16)
                ssc = sb(es, "ssc", [128, 1])
                ssc2 = sb(es, "ssc2", [128, 2])
                rsc = sb(es, "rsc", [128, 1])
                t1 = sb(es, "t1", [128, D])
                x1 = sb(es, "x1", [128, D])
                xn2 = sb(es, "xn2", [128, D])
                tmp2 = sb(es, "tmp2", [128, 8, 128])
                h2f = sb(es, "h2f", [128, 8, 128])
                lg = sb(es, "lg", [128, NEXP])
                mx8 = sb(es, "mx8", [128, 8])
                nmx = sb(es, "nmx", [128, 1])
                msk = sb(es, "msk", [128, NEXP])
                ex = sb(es, "ex", [128, NEXP])
                s3 = sb(es, "s3", [128, 1])
                GT = sb(es, "GT", [NEXP, 128])
                ps_trc = ps(es, "ps_trc", [128, 8, 128], BF16)
                ps_mix = ps(es, "ps_mix", [128, D])
                ps_tr2 = ps(es, "ps_tr2", [128, 8, 128])
                ps_lg = ps(es, "ps_lg", [128, NEXP])
                ps_gt = ps(es, "ps_gt", [NEXP, 128])

                S.dma("gpsimd", w_out_bf[:, :, 0:512], w_out.rearrange("(k p) c -> p k c", p=128)[:, :, 0:512], writes=["w_out_bf"])
                S.dma("gpsimd", w_out_bf[:, :, 512:D], w_out.rearrange("(k p) c -> p k c", p=128)[:, :, 512:D], writes=["w_out_bf"])
                S.dma("sync", GA[0][:], gtab_scr[0], writes=["GA0"])
                S.dma("sync", GA[1][:], gtab_scr[1], writes=["GA1"])
                S.dma("sync", wr_f[:], w_router.rearrange("(k p) n -> p k n", p=128), writes=["wr_f"])
                S.dma("sync", br[:], b_router[:, :], writes=["br"])
                S.dma("sync", bd_sb[:], b_down[:, :], writes=["bd_sb"])
                S.op("gpsimd", lambda e: e.memset(ones_r[:], 1.0), writes=["ones_r"])
                n_tc = cfg.get("n_tc", NT)
                for tt in list(range(n_tc - 1)) + [NT - 1]:
                    smp = (tt == NT - 1)
                    i = tt % 2
                    yb, ybn = ybf[i], "ybf%d" % i
                    xb, xbn = xc[i], "xc%d" % i
                    ga, gan = (GA[1], "GA1") if smp else (GA[0], "GA0")
                    S.dma("sync", yb[:], y_scr[tt], reads=["y_scr"], writes=[ybn])
                    if smp:
                        S.dma("scalar", xb[:], xs[:, :], writes=[xbn])
                    else:
                        S.dma("scalar", xb[:], xp[(PRE_T + tt) * 128:(PRE_T + tt + 1) * 128, :], writes=[xbn])
                    for k in range(8):
                        S.op("tensor", lambda e, k=k, yb=yb: e.transpose(ps_trc[:, k, :], yb[:, k * 128:(k + 1) * 128], ident_b[:]),
                             reads=[ybn, "ident_b"], writes=["ps_trc"])
                    S.op("scalar", lambda e: e.copy(yT[:], ps_trc[:]), reads=["ps_trc"], writes=["yT"])
                    for half in range(2):
                        for k in range(8):
                            S.op("tensor", lambda e, k=k, half=half: e.matmul(
                                ps_mix[:, half * 512:(half + 1) * 512], yT[:, k, :], w_out_bf[:, k, half * 512:(half + 1) * 512],
                                start=(k == 0), stop=(k == 7)),
                                reads=["yT", "w_out_bf"], writes=["ps_mix"])
                    for half in range(2):
                        S.op("scalar", lambda e, half=half: e.activation(
                            junkc[:, half * 512:(half + 1) * 512], ps_mix[:, half * 512:(half + 1) * 512], AF.Square,
                            accum_out=ssc2[:, half:half + 1]),
                            reads=["ps_mix"], writes=["junkc", "ssc2"])
                    S.op("vector", lambda e: e.tensor_tensor(ssc[:], ssc2[:, 0:1], ssc2[:, 1:2], ALU.add), reads=["ssc2"], writes=["ssc"])
                    S.op("vector", lambda e: e.tensor_scalar(rsc[:], ssc[:], 1.0 / D, EPS, ALU.mult, ALU.add), reads=["ssc"], writes=["rsc"])
                    S.op("scalar", lambda e: e.activation(rsc[:], rsc[:], AF.Sqrt), reads=["rsc"], writes=["rsc"])
                    S.op("vector", lambda e: e.reciprocal(rsc[:], rsc[:]), reads=["rsc"], writes=["rsc"])
                    for half in range(2):
                        hs = slice(half * 512, (half + 1) * 512)
                        S.op("vector", lambda e, hs=hs, ga=ga: e.scalar_tensor_tensor(
                            t1[:, hs], ps_mix[:, hs], rsc[:, 0:1], ga[:, hs], ALU.mult, ALU.mult),
                            reads=["ps_mix", "rsc", gan], writes=["t1"])
                    S.op("vector", lambda e, xb=xb: e.tensor_tensor(x1[:], t1[:], xb[:], ALU.add), reads=["t1", xbn], writes=["x1"])
                    S.dma("sync", x1_scr[tt], x1[:], reads=["x1"], writes=["x1_scr"])
                    S.op("gpsimd", lambda e: e.memset(ssc[:], 0.0), reads=["rsc"], writes=["ssc"])
                    S.op("scalar", lambda e: e.activation(junkc[:], x1[:], AF.Square, accum_out=ssc[:]),
                         reads=["x1", "ssc"], writes=["junkc", "ssc"])
                    S.op("vector", lambda e: e.tensor_scalar(rsc[:], ssc[:], 1.0 / D, EPS, ALU.mult, ALU.add), reads=["ssc"], writes=["rsc"])
                    S.op("scalar", lambda e: e.activation(rsc[:], rsc[:], AF.Sqrt), reads=["rsc"], writes=["rsc"])
                    S.op("vector", lambda e: e.reciprocal(rsc[:], rsc[:]), reads=["rsc"], writes=["rsc"])
                    S.op("vector", lambda e: e.tensor_scalar(xn2[:], x1[:], rsc[:, 0:1], None, ALU.mult), reads=["x1", "rsc"], writes=["xn2"])
                    for k in range(8):
                        S.op("tensor", lambda e, k=k: e.transpose(ps_tr2[:, k, :], xn2[:, k * 128:(k + 1) * 128], ident_f[:]),
                             reads=["xn2", "ident_f"], writes=["ps_tr2"])
                    for hh in range(2):
                        ks = slice(4 * hh, 4 * hh + 4)
                        if smp:
                            a_b = A2[:, ks, 1:17].unsqueeze(3).to_broadcast([128, 4, NSEQ, TSEQ])
                            b_b = B2[:, ks, 1:17].unsqueeze(3).to_broadcast([128, 4, NSEQ, TSEQ])
                            v4 = lambda t_, ks=ks: t_[:, ks, :].rearrange("p k (s t) -> p k s t", t=TSEQ)
                        else:
                            a_b = A2[:, ks, 0:1].to_broadcast([128, 4, 128])
                            b_b = B2[:, ks, 0:1].to_broadcast([128, 4, 128])
                            v4 = lambda t_, ks=ks: t_[:, ks, :]
                        S.op("vector", lambda e, v4=v4, a_b=a_b: e.tensor_tensor(v4(tmp2), v4(ps_tr2), a_b, ALU.mult),
                             reads=["ps_tr2", "A2"], writes=["tmp2"])
                        S.op("vector", lambda e, v4=v4, b_b=b_b: e.tensor_tensor(v4(h2f), v4(tmp2), b_b, ALU.add),
                             reads=["tmp2", "B2"], writes=["h2f"])
                    S.op("scalar", lambda e, tt=tt: e.copy(h2T_all[:, :, tt * 128:(tt + 1) * 128], h2f[:]),
                         reads=["h2f"], writes=["h2T_all"])
                    for k in range(8):
                        S.op("tensor", lambda e, k=k: e.matmul(ps_lg[:], h2f[:, k, :], wr_f[:, k, :], start=(k == 0), stop=False),
                             reads=["h2f", "wr_f"], writes=["ps_lg"])
                    S.op("tensor", lambda e: e.matmul(ps_lg[:], ones_r[0:1, :], br[0:1, :], start=False, stop=True),
                         reads=["ones_r", "br"], writes=["ps_lg"])
                    S.op("vector", lambda e: e.tensor_copy(lg[:], ps_lg[:]), reads=["ps_lg"], writes=["lg"])
                    S.op("vector", lambda e: e.max(mx8[:], lg[:]), reads=["lg"], writes=["mx8"])
                    S.op("vector", lambda e: e.tensor_scalar(msk[:], lg[:], mx8[:, 3:4], None, ALU.is_ge), reads=["lg", "mx8"], writes=["msk"])
                    S.op("vector", lambda e: e.tensor_scalar(nmx[:], mx8[:, 0:1], -1.0, None, ALU.mult), reads=["mx8"], writes=["nmx"])
                    S.op("scalar", lambda e: e.activation(ex[:], lg[:], AF.Exp, bias=nmx[:, 0:1]), reads=["lg", "nmx"], writes=["ex"])
                    S.op("vector", lambda e: e.tensor_tensor(ex[:], ex[:], msk[:], ALU.mult), reads=["ex", "msk"], writes=["ex"])
                    S.op("vector", lambda e: e.tensor_reduce(s3[:], ex[:], AX.X, ALU.add), reads=["ex"], writes=["s3"])
                    S.op("vector", lambda e: e.reciprocal(s3[:], s3[:]), reads=["s3"], writes=["s3"])
                    S.op("vector", lambda e, tt=tt: e.tensor_scalar(G_all[:, tt, :], ex[:], s3[:, 0:1], None, ALU.mult),
                         reads=["ex", "s3"], writes=["G_all"])
                    S.op("tensor", lambda e, tt=tt: e.transpose(ps_gt[:], G_all[:, tt, :], ident_f[:]),
                         reads=["G_all", "ident_f"], writes=["ps_gt"])
                    S.op("vector", lambda e: e.tensor_copy(GT[:], ps_gt[:]), reads=["ps_gt"], writes=["GT"])
                    for half in range(2):
                        S.op("tensor", lambda e, half=half: e.matmul(
                            ps_mix[:, half * 512:(half + 1) * 512], GT[:, :], bd_sb[:, half * 512:(half + 1) * 512], start=True, stop=True),
                            reads=["GT", "bd_sb"], writes=["ps_mix"])
                    for half in range(2):
                        hs = slice(half * 512, (half + 1) * 512)
                        S.op("scalar", lambda e, hs=hs, tt=tt: e.copy(y_acc[:, tt, hs], ps_mix[:, hs]),
                             reads=["ps_mix"], writes=["y_acc"])
                if dbg and "D" not in stages:
                    o = dbg_out("G_all", [128, NT, NEXP])
                    S.dma("sync", o, G_all[:], reads=["G_all"], writes=["dbg_G"])
                    o = dbg_out("h2T", [128, 8, NT * 128], BF16)
                    S.dma("sync", o, h2T_all[:], reads=["h2T_all"], writes=["dbg_h"])
                    o = dbg_out("y_acc", [128, NT, D])
                    S.dma("sync", o, y_acc[:], reads=["y_acc"], writes=["dbg_ya"])
                S.flush()
                if dbg and "D" not in stages:
                    o = dbg_out("x1", [NT, 128, D])
                    for tt in list(range(n_tc - 1)) + [NT - 1]:
                        S.dma("sync", o[tt], x1_scr[tt], reads=["x1_scr"], writes=["dbg_x1"])
                    S.flush()


        if "D" in stages:
            bgu = sb(es_all, "bgu", [128, 8, 2, NEXP])
            with ExitStack() as es:
                bgu_raw = sb(es, "bgu_raw", [NEXP, 2 * D])
                ps_b = ps(es, "ps_b", [128, 512])
                S.dma("sync", bgu_raw[:], b_gu[:, :], writes=["bgu_raw"])
                braw = bgu_raw[:].rearrange("e (f j two) -> e f two j", f=8, j=128, two=2)
                for f in range(8):
                    for two in range(2):
                        S.op("tensor", lambda e, f=f, two=two: e.transpose(
                            ps_b[:, (2 * f + two) * NEXP:(2 * f + two + 1) * NEXP], braw[:, f, two, :], ident_f[0:NEXP, 0:NEXP]),
                            reads=["bgu_raw", "ident_f"], writes=["ps_b"])
                S.op("vector", lambda e: e.tensor_copy(bgu[:].rearrange("p f two e -> p (f two e)"), ps_b[:, 0:16 * NEXP]),
                     reads=["ps_b"], writes=["bgu"])
                S.op("vector", lambda e: e.tensor_scalar(bgu[:, :, 1, :], bgu[:, :, 1, :], 1.0 / 1.702, None, ALU.mult),
                     reads=["bgu"], writes=["bgu"])
                S.flush()
            with ExitStack() as es:
                actT = sb(es, "actT", [128, 8, NT * 128], BF16)
                stg = [sb(es, "stg0", [128, 8, 256]), sb(es, "stg1", [128, 8, 256])]
                Ugu = [sb(es, "Ugu0", [128, 8, 2, 128], BF16), sb(es, "Ugu1", [128, 8, 2, 128], BF16)]
                stw = [sb(es, "stw0", [128, D]), sb(es, "stw1", [128, D])]
                Wd = sb(es, "Wd", [128, 8, D], BF16)
                gc = [sb(es, "gc0", [128, 512]), sb(es, "gc1", [128, 512])]
                sig = [sb(es, "sig0", [128, 512]), sb(es, "sig1", [128, 512])]
                uu = [sb(es, "uu0", [128, 512]), sb(es, "uu1", [128, 512])]
                ps_g = [ps(es, "ps_g0", [128, 512]), ps(es, "ps_g1", [128, 512])]
                ps_u = [ps(es, "ps_u0", [128, 512]), ps(es, "ps_u1", [128, 512])]
                ps_d = [ps(es, "ps_d0", [128, D]), ps(es, "ps_d1", [128, D])]

                wgu_v = w_gu.rearrange("e (k p) c -> e p k c", p=128)
                wd_v = w_down.rearrange("e (f p) c -> e p f c", p=128)
                groups = [(0, 512), (512, 512), (1024, 512), (1536, 512), (2048, 128)]
                n_units = 8 * n_exp

                def load_gu(u):
                    e_, f = u // 8, u % 8
                    b = u % 2
                    S.dma("sync", stg[b][:], wgu_v[e_, :, :, 256 * f:256 * (f + 1)], writes=["stg%d" % b])

                def cast_gu(u):
                    b = u % 2
                    S.op("scalar", lambda e, b=b: e.copy(
                        Ugu[b][:], stg[b][:].rearrange("p k (j two) -> p k two j", two=2)),
                        reads=["stg%d" % b], writes=["Ugu%d" % b])

                def load_wd(e_, q):
                    b = q % 2
                    S.dma("sync", stw[b][:], wd_v[e_, :, q, :], writes=["stw%d" % b])

                def cast_wd(e_, q):
                    b = q % 2
                    S.op("scalar", lambda e, b=b, q=q: e.copy(Wd[:, q, :], stw[b][:]),
                         reads=["stw%d" % b], writes=["Wd"])

                load_gu(0)
                load_gu(1)
                cast_gu(0)
                cnt = 0
                dcnt = 0
                for e_ in range(n_exp):
                    for f in range(8):
                        u = 8 * e_ + f
                        b = u % 2
                        if u + 1 < n_units:
                            cast_gu(u + 1)
                        if f in (6, 7):
                            load_wd(e_, f - 6)
                        for (t0, n) in groups:
                            i = cnt % 2
                            cnt += 1
                            pg, pu = ps_g[i], ps_u[i]
                            for k in range(8):
                                S.op("tensor", lambda e, k=k, b=b, pg=pg, t0=t0, n=n: e.matmul(
                                    pg[:, 0:n], Ugu[b][:, k, 0, :], h2T_all[:, k, t0:t0 + n], start=(k == 0), stop=(k == 7)),
                                    reads=["Ugu%d" % b, "h2T_all"], writes=["ps_g%d" % i])
                            for k in range(8):
                                S.op("tensor", lambda e, k=k, b=b, pu=pu, t0=t0, n=n: e.matmul(
                                    pu[:, 0:n], Ugu[b][:, k, 1, :], h2T_all[:, k, t0:t0 + n], start=(k == 0), stop=(k == 7)),
                                    reads=["Ugu%d" % b, "h2T_all"], writes=["ps_u%d" % i])
                            S.op("scalar", lambda e, pu=pu, n=n, f=f, e_=e_, i=i: e.activation(
                                uu[i][:, 0:n], pu[:, 0:n], AF.Identity, bias=bgu[:, f, 1, e_:e_ + 1], scale=1.0 / 1.702),
                                reads=["ps_u%d" % i, "bgu"], writes=["uu%d" % i])
                            S.op("vector", lambda e, pg=pg, n=n, f=f, e_=e_, i=i: e.tensor_scalar(
                                gc[i][:, 0:n], pg[:, 0:n], bgu[:, f, 0, e_:e_ + 1], 7.0, ALU.add, ALU.min),
                                reads=["ps_g%d" % i, "bgu"], writes=["gc%d" % i])
                            S.op("scalar", lambda e, n=n, i=i: e.activation(sig[i][:, 0:n], gc[i][:, 0:n], AF.Silu, scale=1.702),
                                 reads=["gc%d" % i], writes=["sig%d" % i])
                            S.op("vector", lambda e, n=n, i=i: e.tensor_scalar(
                                uu[i][:, 0:n], uu[i][:, 0:n], 7.0 / 1.702, -7.0 / 1.702, ALU.min, ALU.max),
                                reads=["uu%d" % i], writes=["uu%d" % i])
                            S.op("vector", lambda e, n=n, f=f, t0=t0, i=i: e.scalar_tensor_tensor(
                                actT[:, f, t0:t0 + n], uu[i][:, 0:n], 1.0 / 1.702, sig[i][:, 0:n], ALU.add, ALU.mult),
                                reads=["sig%d" % i, "uu%d" % i], writes=["actT"])
                        if u + 2 < n_units:
                            load_gu(u + 2)
                    for q in range(8):
                        cast_wd(e_, q)
                        if q + 2 < 8:
                            load_wd(e_, q + 2)
                    for tt in range(NT):
                        i = dcnt % 2
                        dcnt += 1
                        pd = ps_d[i]
                        for half in range(2):
                            for f in range(8):
                                S.op("tensor", lambda e, f=f, half=half, tt=tt, pd=pd: e.matmul(
                                    pd[:, half * 512:(half + 1) * 512], actT[:, f, tt * 128:(tt + 1) * 128],
                                    Wd[:, f, half * 512:(half + 1) * 512], start=(f == 0), stop=(f == 7)),
                                    reads=["actT", "Wd"], writes=["ps_d%d" % i])
                        for half in range(2):
                            hs = slice(half * 512, (half + 1) * 512)
                            S.op("vector", lambda e, hs=hs, tt=tt, pd=pd, e_=e_: e.scalar_tensor_tensor(
                                y_acc[:, tt, hs], pd[:, hs], G_all[:, tt, e_:e_ + 1], y_acc[:, tt, hs], ALU.mult, ALU.add),
                                reads=["ps_d%d" % i, "G_all", "y_acc"], writes=["y_acc"])
                if dbg and "E" not in stages:
                    o = dbg_out("y_acc2", [128, NT, D])
                    S.dma("sync", o, y_acc[:], reads=["y_acc"], writes=["dbg_ya2"])
                S.flush()

        if "E" in stages:
            with ExitStack() as es:
                GF = [sb(es, "GF0", [128, D]), sb(es, "GF1", [128, D])]
                x1b = [sb(es, "x1b0", [128, D]), sb(es, "x1b1", [128, D])]
                junke = sb(es, "junke", [128, D], BF16)
                sse = sb(es, "sse", [128, 1])
                rse = sb(es, "rse", [128, 1])
                te = [sb(es, "te0", [128, D]), sb(es, "te1", [128, D])]
                S.dma("sync", GF[0][:], gtab_scr[2], writes=["GF0"])
                S.dma("sync", GF[1][:], gtab_scr[3], writes=["GF1"])
                for tt in range(NT):
                    smp = (tt == NT - 1)
                    i = tt % 2
                    gf, gfn = (GF[1], "GF1") if smp else (GF[0], "GF0")
                    S.dma("sync", x1b[i][:], x1_scr[tt], writes=["x1b%d" % i])
                    S.op("scalar", lambda e, tt=tt: e.activation(junke[:], y_acc[:, tt, :], AF.Square, accum_out=sse[:]),
                         reads=["y_acc"], writes=["junke", "sse"])
                    S.op("vector", lambda e: e.tensor_scalar(rse[:], sse[:], 1.0 / D, EPS, ALU.mult, ALU.add), reads=["sse"], writes=["rse"])
                    S.op("scalar", lambda e: e.activation(rse[:], rse[:], AF.Sqrt), reads=["rse"], writes=["rse"])
                    S.op("vector", lambda e: e.reciprocal(rse[:], rse[:]), reads=["rse"], writes=["rse"])
                    S.op("vector", lambda e, tt=tt, i=i, gf=gf: e.scalar_tensor_tensor(
                        te[i][:], y_acc[:, tt, :], rse[:, 0:1], gf[:], ALU.mult, ALU.mult),
                        reads=["y_acc", "rse", gfn], writes=["te%d" % i])
                    S.op("vector", lambda e, i=i: e.tensor_tensor(te[i][:], te[i][:], x1b[i][:], ALU.add),
                         reads=["te%d" % i, "x1b%d" % i], writes=["te%d" % i])
                    if smp:
                        S.dma("sync", ys[:, :], te[i][:], reads=["te%d" % i], writes=["ys"])
                    else:
                        S.dma("sync", yp[tt * 128:(tt + 1) * 128, :], te[i][:], reads=["te%d" % i], writes=["yp"])
                S.flush()

        if "E" not in stages:
            S.dma("sync", yp[:, :], xp[PRE_T * 128:(PRE_T + OWN_T) * 128, :], writes=["yp"])
            S.dma("sync", ys[:, :], xs[:, :], writes=["ys"])
            S.flush()
    return nc, dbg_outs


_CONST = {}


def _consts():
    if _CONST:
        return _CONST
    sel = np.zeros((2, 17, 128), np.float32)
    sel[0, 0, :] = 1.0
    for p in range(128):
        sel[1, 1 + p // TSEQ, p] = 1.0
    tp = _ret_tables(128)
    ts = _ret_tables(TSEQ)
    _CONST.update(
        c_ident=np.eye(128, dtype=np.float32),
        c_sel=sel,
        c_dm=np.stack([tp[0], ts[0]]),
        c_kd=np.stack([tp[1], ts[1]]),
        c_qd=np.stack([tp[2], ts[2]]),
        c_gd=np.stack([tp[3], ts[3]]),
        c_toep=_toeplitz_prompt(),
        c_sbias=_sample_bias(),
    )
    return _CONST


def prep_core_inputs(inp, c):
    b, qtr = c // 4, c % 4
    T0 = 2048 * qtr
    f = np.float32
    xpad = np.zeros(((PRE_T + OWN_T) * 128, D), f)
    lo = T0 - PRE_T * 128
    src_lo = max(lo, 0)
    xpad[src_lo - lo:] = inp["x_prompt"][b, src_lo:T0 + 2048]
    pvalid = np.zeros((128, PRE_T), f)
    for t in range(PRE_T):
        if lo + 128 * t >= 0:
            pvalid[:, t] = 1.0
    sl = slice(NSEQ * c, NSEQ * (c + 1))
    xs = inp["x_sample"][sl]
    xs_pad = np.zeros((NSEQ, 128, D), f)
    xs_pad[:, :TSEQ] = xs
    cvec = np.concatenate([inp["c_prompt"][b:b + 1], inp["c_sample"][sl]], axis=0)
    tr8 = lambda g: np.ascontiguousarray(g.reshape(8, 128).T)
    bc = lambda g, n: np.ascontiguousarray(np.broadcast_to(g.reshape(1, -1), (128, n)))
    m = dict(
        xp=xpad, pvalid=pvalid, xs_pad=xs_pad.reshape(NSEQ * 128, D), xs=np.ascontiguousarray(xs.reshape(128, D)),
        cvec=np.ascontiguousarray(cvec),
        state_in=np.ascontiguousarray(inp["state_ret"][0, sl]),
        ck=np.ascontiguousarray(inp["cache_win_k"][0, sl].reshape(NSEQ, 2048, 512)),
        cv=np.ascontiguousarray(inp["cache_win_v"][0, sl].reshape(NSEQ, 2048, 512)),
        w_ada=inp["w_ada"][0], b_ada=inp["b_ada"],
        g1T=tr8(inp["g_pre_mix"][0]), g3T=tr8(inp["g_pre_ffn"][0]),
        g2b=bc(inp["g_post_mix"][0], D), g4b=bc(inp["g_post_ffn"][0], D), gretb=bc(inp["g_ret"][0], 512),
        w_in=inp["w_in"][0], w_out=inp["w_out"][0], w_router=inp["w_router"][0], b_router=inp["b_router"],
        w_gu=inp["w_gate_up"][0], b_gu=inp["b_gate_up"][0], w_down=inp["w_down"][0], b_down=inp["b_down"][0],
    )
    m.update(_consts())
    return {k: np.ascontiguousarray(v, dtype=np.float32) for k, v in m.items()}


STAGES = "0ABCDE"


def kernel(**inputs):
    inp = {k: np.asarray(v) for k, v in inputs.items()}
    cfg = dict(stages=STAGES)
    nc, _ = build_nc(cfg)
    in_maps = []
    for c in range(NCORE):
        m = prep_core_inputs(inp, c)
        if "D" not in STAGES:
            m["w_gu"] = m["w_gu"][:1]
            m["w_down"] = m["w_down"][:1]
        if "B" not in STAGES:
            m["ck"] = m["ck"][:1]
            m["cv"] = m["cv"][:1]
        in_maps.append(m)
    res = run_bass_kernel_spmd(nc, in_maps, core_ids=list(range(NCORE)))
    r = res.results
    f = np.float32
    y_prompt = np.zeros((2, 8192, D), f)
    y_sample = np.zeros((128, TSEQ, D), f)
    ret_p = np.zeros((1, 2, NH, E, E), f)
    ret_s = np.zeros((1, 128, NH, E, E), f)
    wk_p = np.zeros((1, 2, 2048, NH, E), f)
    wv_p = np.zeros((1, 2, 2048, NH, E), f)
    k_s = np.zeros((1, 128, TSEQ, NH, E), f)
    v_s = np.zeros((1, 128, TSEQ, NH, E), f)
    for c in range(NCORE):
        b, qtr = c // 4, c % 4
        y_prompt[b, 2048 * qtr:2048 * (qtr + 1)] = r[c]["yp"]
        y_sample[NSEQ * c:NSEQ * (c + 1)] = r[c]["ys"].reshape(NSEQ, TSEQ, D)
        ret_s[0, NSEQ * c:NSEQ * (c + 1)] = r[c]["rs"]
        k_s[0, NSEQ * c:NSEQ * (c + 1)] = r[c]["sk"].reshape(NSEQ, TSEQ, NH, E)
        v_s[0, NSEQ * c:NSEQ * (c + 1)] = r[c]["sv"].reshape(NSEQ, TSEQ, NH, E)
        if qtr == 3:
            ret_p[0, b] = r[c]["rp"]
            wk_p[0, b] = r[c]["wk"].reshape(2048, NH, E)
            wv_p[0, b] = r[c]["wv"].reshape(2048, NH, E)
    return (y_prompt, y_sample, ret_p, ret_s, wk_p, wv_p, k_s, v_s)
```

```python
import math
from contextlib import ExitStack

import numpy as np
import concourse.bass as bass
import concourse.mybir as mybir
from concourse.bass_utils import run_bass_kernel_spmd

F32 = mybir.dt.float32
BF16 = mybir.dt.bfloat16
AF = mybir.ActivationFunctionType
ALU = mybir.AluOpType
AX = mybir.AxisListType

D = 1024
NH = 8
E = 64
DIN = 3584
NEXP = 32
EPS = 1e-6
NCORE = 8
OWN_T = 16
PRE_T = 48
SPAN_T = 32
NSEQ = 16
TSEQ = 8
NT = OWN_T + 1
TOEP_W = 384 + 128 * 16 + 512

ENGINES = ("tensor", "vector", "scalar", "gpsimd", "sync")
DMA_K = 6


class Sched:
    def __init__(self, nc, es):
        self.nc = nc
        self.q = {e: [] for e in ENGINES}
        self.cnt = {}
        self.seen = {}
        self.lastw = {}
        self.readers = {}
        self.sems = {}
        self.final = {}
        self.es = es
        self.n_ops = 0

    def _sem(self, name):
        if name not in self.sems:
            self.sems[name] = self.es.enter_context(self.nc.semaphore(name))
        return self.sems[name]

    def _new_token(self, eng, is_dma):
        if is_dma:
            st = "d_" + eng
            i = self.cnt.get(st, 0)
            self.cnt[st] = i + 1
            return ("%s%d" % (st, i % DMA_K), 16 * (i // DMA_K + 1)), i
        st = "c_" + eng
        i = self.cnt.get(st, 0) + 1
        self.cnt[st] = i
        return (st, i), i

    def _emit(self, eng, fn, reads, writes, is_dma):
        deps = {}

        def add(tok):
            if tok is None:
                return
            s, v = tok
            if deps.get(s, 0) < v:
                deps[s] = v

        for k in reads:
            add(self.lastw.get(k))
        for k in writes:
            add(self.lastw.get(k))
            for tok in self.readers.get(k, {}).values():
                add(tok)
        tok, idx = self._new_token(eng, is_dma)
        if is_dma and idx >= DMA_K:
            add((tok[0], tok[1] - 16))
        waits = []
        for s, v in deps.items():
            if eng == "tensor" and s == "c_tensor":
                continue
            if self.seen.get((eng, s), 0) >= v:
                continue
            self.seen[(eng, s)] = v
            waits.append((s, v))
        self.q[eng].append((waits, fn, tok, 16 if is_dma else 1))
        for k in reads:
            self.readers.setdefault(k, {})[tok[0]] = tok
        for k in writes:
            self.lastw[k] = tok
            self.readers[k] = {}
        if self.final.get(tok[0], 0) < tok[1]:
            self.final[tok[0]] = tok[1]
        self.n_ops += 1
        return tok

    def op(self, eng, fn, reads=(), writes=()):
        return self._emit(eng, fn, reads, writes, False)

    def dma(self, eng, out, in_, reads=(), writes=(), **kw):
        return self._emit(eng, lambda e: e.dma_start(out=out, in_=in_, **kw), reads, writes, True)

    def flush(self):
        nc = self.nc
        final = dict(self.final)
        for e in ENGINES:
            for waits, fn, tok, inc in self.q[e]:
                self._sem(tok[0])
        with nc.Block() as block:
            def run(engname):
                def body(e):
                    for waits, fn, tok, inc in self.q[engname]:
                        for s, v in waits:
                            e.wait_ge(self.sems[s], v)
                        ins = fn(e)
                        ins.then_inc(self.sems[tok[0]], inc)
                    for s, v in final.items():
                        if self.seen.get((engname, s), 0) < v:
                            e.wait_ge(self.sems[s], v)
                            self.seen[(engname, s)] = v
                return body
            block.tensor(run("tensor"))
            block.vector(run("vector"))
            block.scalar(run("scalar"))
            block.gpsimd(run("gpsimd"))
            block.sync(run("sync"))
        self.q = {e: [] for e in ENGINES}
        self.lastw = {}
        self.readers = {}


def _gammas():
    return 1.0 - 2.0 ** (-5.0 - np.arange(NH, dtype=np.float64))


def _ret_tables(L):
    g = _gammas()
    lg = np.log(g)
    j = np.arange(128)[:, None]
    i = np.arange(128)[None, :]
    dm = np.zeros((128, NH, 128), np.float64)
    ok = (i >= j) & (i < L) & (j < L)
    for h in range(NH):
        dm[:, h, :] = np.where(ok, np.exp((i - j) * lg[h]) / 8.0, 0.0)
    kd = np.zeros((128, NH, E), np.float64)
    for h in range(NH):
        col = np.where(np.arange(128) < L, np.exp((L - 1.0 - np.arange(128)) * lg[h]) / 8.0, 0.0)
        kd[:, h, :] = col[:, None]
    qd = np.zeros((64, NH, 128), np.float64)
    gd = np.zeros((64, NH, E), np.float64)
    for h in range(NH):
        qd[:, h, :] = np.exp((np.arange(128) + 1.0) * lg[h])[None, :]
        gd[:, h, :] = np.exp(L * lg[h])
    return (dm.astype(np.float32), kd.reshape(128, NH * E).astype(np.float32),
            qd.astype(np.float32), gd.astype(np.float32))


def _alibi_logw(dist):
    dist = np.asarray(dist, np.int64)
    cnt = ((dist >= 0) & (dist <= 128)).astype(np.float64)
    cnt += ((dist >= 0) & (dist <= 512) & (dist % 4 == 0))
    cnt += ((dist >= 0) & (dist <= 2048) & (dist % 16 == 0))
    return cnt


def _bias_fn(dist, h):
    slope = 2.0 ** (-8.0 * (h + 1.0) / NH)
    cnt = _alibi_logw(dist)
    with np.errstate(divide="ignore"):
        out = np.where(cnt > 0, -slope * np.maximum(dist, 0) + np.log(np.maximum(cnt, 1e-30)), -1e30)
    return out


def _toeplitz_prompt():
    p = np.arange(128)[:, None]
    c = np.arange(TOEP_W)[None, :]
    out = np.zeros((NH, 128, TOEP_W), np.float32)
    for h in range(NH):
        out[h] = _bias_fn(c - 384 - p, h).astype(np.float32)
    return out


def _sample_bias():
    out = np.zeros((128, 17, NH, TSEQ), np.float32)
    j = np.arange(128)[:, None]
    t = np.arange(TSEQ)[None, :]
    for a in range(16):
        for h in range(NH):
            out[:, a, h, :] = _bias_fn(2048 + t - (128 * a + j), h)
    for h in range(NH):
        b = _bias_fn(t - j, h)
        b = np.where(j < TSEQ, b, -1e30)
        out[:, 16, h, :] = b
    return out


def build_nc(cfg):
    nc = bass.Bass("TRN2", target_bir_lowering=False)
    dbg = cfg.get("debug", False)
    n_pre = cfg.get("n_pre", PRE_T)
    n_exp = cfg.get("n_exp", NEXP)
    stages = cfg.get("stages", "0ABCDE")

    def din(name, shape, dt=F32):
        return nc.dram_tensor(name, list(shape), dt, kind="ExternalInput").ap()

    def dout(name, shape, dt=F32):
        return nc.dram_tensor(name, list(shape), dt, kind="ExternalOutput").ap()

    def dscr(name, shape, dt):
        return nc.dram_tensor(name, list(shape), dt).ap()

    xp = din("xp", [(PRE_T + OWN_T) * 128, D])
    pvalid = din("pvalid", [128, PRE_T])
    xs_pad = din("xs_pad", [NSEQ * 128, D])
    xs = din("xs", [128, D])
    cvec = din("cvec", [17, D])
    state_in = din("state_in", [NSEQ, NH, E, E])
    bigb = "B" in stages
    ck = din("ck", [NSEQ if bigb else 1, 2048, NH * E])
    cv = din("cv", [NSEQ if bigb else 1, 2048, NH * E])
    w_ada = din("w_ada", [D, 6 * D])
    b_ada = din("b_ada", [1, 6 * D])
    g1T = din("g1T", [128, 8])
    g3T = din("g3T", [128, 8])
    g2b = din("g2b", [128, D])
    g4b = din("g4b", [128, D])
    gretb = din("gretb", [128, 512])
    w_in = din("w_in", [D, DIN])
    w_out = din("w_out", [D, D])
    w_router = din("w_router", [D, NEXP])
    b_router = din("b_router", [1, NEXP])
    big = "D" in stages
    w_gu = din("w_gu", [NEXP if big else 1, D, 2 * D])
    b_gu = din("b_gu", [NEXP, 2 * D])
    w_down = din("w_down", [NEXP if big else 1, D, D])
    b_down = din("b_down", [NEXP, D])
    c_ident = din("c_ident", [128, 128])
    c_sel = din("c_sel", [2, 17, 128])
    c_dm = din("c_dm", [2, 128, NH, 128])
    c_kd = din("c_kd", [2, 128, 512])
    c_qd = din("c_qd", [2, 64, NH, 128])
    c_gd = din("c_gd", [2, 64, NH, E])
    c_toep = din("c_toep", [NH, 128, TOEP_W])
    c_sbias = din("c_sbias", [128, 17, NH, TSEQ])

    yp = dout("yp", [OWN_T * 128, D])
    ys = dout("ys", [128, D])
    rp = dout("rp", [NH, E, E])
    rs = dout("rs", [NSEQ, NH, E, E])
    wk = dout("wk", [OWN_T * 128, 512])
    wv = dout("wv", [OWN_T * 128, 512])
    sk = dout("sk", [128, 512])
    sv = dout("sv", [128, 512])

    kT_scr = dscr("kT_scr", [NH, 64, SPAN_T * 128], BF16)
    qT_scr = dscr("qT_scr", [NH, 64, OWN_T * 128], BF16)
    v_scr = dscr("v_scr", [SPAN_T, 128, NH * 65], BF16)
    qTs_scr = dscr("qTs_scr", [NSEQ, 64, NH, 128], BF16)
    kTs_scr = dscr("kTs_scr", [NSEQ, 64, NH, 128], BF16)
    vs_scr = dscr("vs_scr", [NSEQ, 128, NH * 65], BF16)
    y_scr = dscr("y_scr", [NT, 128, D], BF16)
    x1_scr = dscr("x1_scr", [NT, 128, D], F32)
    gtab_scr = dscr("gtab_scr", [4, 128, D], F32)

    dbg_outs = {}

    def dbg_out(name, shape, dt=F32):
        if dbg:
            dbg_outs[name] = dout("dbg_" + name, shape, dt)
            return dbg_outs[name]
        return None

    with ExitStack() as es_all:
        S = Sched(nc, es_all)

        def sb(es, name, shape, dt=F32):
            return es.enter_context(nc.sbuf_tensor(name, list(shape), dt))

        def ps(es, name, shape, dt=F32):
            return es.enter_context(nc.psum_tensor(name, list(shape), dt))

        ident_f = sb(es_all, "ident_f", [128, 128])
        ident_b = sb(es_all, "ident_b", [128, 128], BF16)
        A1 = sb(es_all, "A1", [128, 8, 17])
        B1 = sb(es_all, "B1", [128, 8, 17])
        A2 = sb(es_all, "A2", [128, 8, 17])
        B2 = sb(es_all, "B2", [128, 8, 17])
        S.dma("sync", ident_f[:], c_ident[:, :], writes=["ident_f"])
        S.op("vector", lambda e: e.tensor_copy(ident_b[:], ident_f[:]), reads=["ident_f"], writes=["ident_b"])

        if "0" in stages:
            with ExitStack() as es:
                c17 = sb(es, "c17", [17, D])
                sc17 = sb(es, "sc17", [17, D])
                scT = sb(es, "scT", [128, 8, 17])
                brow = sb(es, "brow", [1, 6 * D])
                ones1 = sb(es, "ones1", [1, 32])
                g1 = sb(es, "g1", [128, 8])
                g3 = sb(es, "g3", [128, 8])
                gpost = [sb(es, "gpost0", [128, D]), sb(es, "gpost1", [128, D])]
                sel = sb(es, "sel", [17, 2, 128])
                blk = [sb(es, "wablk0", [128, 8, D]), sb(es, "wablk1", [128, 8, D])]
                gt_tok = sb(es, "gt_tok", [17, D])
                gtab = sb(es, "gtab", [128, D])
                ps_tr = ps(es, "ps_tr0", [128, 8, 17])
                ps_m = [ps(es, "ps_m0", [128, 8, 17]), ps(es, "ps_m1", [128, 8, 17])]
                ps_g = ps(es, "ps_g", [17, D])
                ps_t = ps(es, "ps_t", [128, D])

                S.dma("sync", c17[:], cvec[:, :], writes=["c17"])
                S.dma("sync", brow[:], b_ada[:, :], writes=["brow"])
                S.dma("sync", g1[:], g1T[:, :], writes=["g1"])
                S.dma("sync", g3[:], g3T[:, :], writes=["g3"])
                S.dma("sync", gpost[0][:], g2b[:, :], writes=["gpost0"])
                S.dma("sync", gpost[1][:], g4b[:, :], writes=["gpost1"])
                S.dma("sync", sel[:], c_sel.rearrange("a s p -> s a p"), writes=["sel"])
                S.op("gpsimd", lambda e: e.memset(ones1[:], 1.0), writes=["ones1"])
                S.op("scalar", lambda e: e.activation(sc17[:], c17[:], AF.Silu), reads=["c17"], writes=["sc17"])
                for k in range(8):
                    S.op("tensor", lambda e, k=k: e.transpose(ps_tr[:, k, :], sc17[0:17, k * 128:(k + 1) * 128],
                                                              ident_f[0:17, 0:17]),
                         reads=["sc17", "ident_f"], writes=["ps_tr0"])
                S.op("vector", lambda e: e.tensor_copy(scT[:], ps_tr[:]), reads=["ps_tr0"], writes=["scT"])

                wada_v = w_ada.rearrange("(k p) c -> p k c", p=128)
                for j in range(6):
                    bk = blk[j % 2]
                    bkn = "wablk%d" % (j % 2)
                    S.dma("sync", bk[:, 0:4, :], wada_v[:, 0:4, j * D:(j + 1) * D], writes=[bkn])
                    S.dma("scalar", bk[:, 4:8, :], wada_v[:, 4:8, j * D:(j + 1) * D], writes=[bkn + "b"])
                    if j in (0, 1, 3, 4):
                        pm = ps_m[j % 2]
                        pmn = "ps_m%d" % (j % 2)
                        for cc in range(8):
                            for k in range(8):
                                S.op("tensor", lambda e, cc=cc, k=k, pm=pm, bk=bk: e.matmul(
                                    pm[:, cc, :], bk[:, k, cc * 128:(cc + 1) * 128], scT[:, k, :],
                                    start=(k == 0), stop=False),
                                    reads=[bkn, bkn + "b", "scT"], writes=[pmn])
                            S.op("tensor", lambda e, cc=cc, pm=pm, j=j: e.matmul(
                                pm[:, cc, :], brow[0:1, j * D + cc * 128:j * D + (cc + 1) * 128], ones1[0:1, 0:17],
                                start=False, stop=True),
                                reads=["brow", "ones1"], writes=[pmn])
                        if j == 0:
                            S.op("vector", lambda e, pm=pm: e.tensor_copy(B1[:], pm[:]), reads=[pmn], writes=["B1"])
                        elif j == 3:
                            S.op("vector", lambda e, pm=pm: e.tensor_copy(B2[:], pm[:]), reads=[pmn], writes=["B2"])
                        else:
                            dst, gg, gn = (A1, g1, "g1") if j == 1 else (A2, g3, "g3")
                            dn = "A1" if j == 1 else "A2"
                            for cc in range(8):
                                S.op("vector", lambda e, cc=cc, pm=pm, dst=dst, gg=gg: e.tensor_scalar(
                                    dst[:, cc, :], pm[:, cc, :], 1.0, gg[:, cc:cc + 1], ALU.add, ALU.mult),
                                    reads=[pmn, gn], writes=[dn])
                    else:
                        gi = 0 if j == 2 else 1
                        for half in range(2):
                            for k in range(8):
                                S.op("tensor", lambda e, half=half, k=k, bk=bk: e.matmul(
                                    ps_g[0:17, half * 512:(half + 1) * 512], scT[:, k, :],
                                    bk[:, k, half * 512:(half + 1) * 512], start=(k == 0), stop=False),
                                    reads=[bkn, bkn + "b", "scT"], writes=["ps_g"])
                            S.op("tensor", lambda e, half=half, j=j: e.matmul(
                                ps_g[0:17, half * 512:(half + 1) * 512], ones1[0:1, 0:17],
                                brow[0:1, j * D + half * 512:j * D + (half + 1) * 512], start=False, stop=True),
                                reads=["brow", "ones1"], writes=["ps_g"])
                        S.op("vector", lambda e: e.tensor_copy(gt_tok[:], ps_g[:]), reads=["ps_g"], writes=["gt_tok"])
                        for which in range(2):
                            for half in range(2):
                                S.op("tensor", lambda e, which=which, half=half: e.matmul(
                                    ps_t[:, half * 512:(half + 1) * 512], sel[0:17, which, :],
                                    gt_tok[0:17, half * 512:(half + 1) * 512], start=True, stop=True),
                                    reads=["sel", "gt_tok"], writes=["ps_t"])
                            S.op("vector", lambda e, gi=gi: e.tensor_tensor(gtab[:], ps_t[:], gpost[gi][:], ALU.mult),
                                 reads=["ps_t", "gpost%d" % gi], writes=["gtab"])
                            S.dma("sync", gtab_scr[gi * 2 + which], gtab[:], reads=["gtab"], writes=["gtab_scr"])
                if dbg:
                    for nm, t in (("A1", A1), ("B1", B1), ("A2", A2), ("B2", B2)):
                        o = dbg_out(nm, [128, 8, 17])
                        S.dma("sync", o, t[:], reads=[nm], writes=["dbg_" + nm])
                    o = dbg_out("gtab", [4, 128, D])
                    with ExitStack() as es2:
                        pass
                S.flush()
                if dbg:
                    pass


        if "A" in stages:
            with ExitStack() as es:
                w_in_bf = sb(es, "w_in_bf", [128, 8, DIN], BF16)
                tabs = []
                for kind in range(2):
                    tabs.append(dict(
                        dm=sb(es, "dm%d" % kind, [128, NH, 128]), kd=sb(es, "kd%d" % kind, [128, 512]),
                        qd=sb(es, "qd%d" % kind, [64, NH, 128]), gd=sb(es, "gd%d" % kind, [64, NH, E])))
                gret = sb(es, "gret", [128, 512])
                pval = sb(es, "pval", [128, PRE_T])
                ones_c = sb(es, "ones_c", [128, 1])
                xt = [sb(es, "xt0", [128, D]), sb(es, "xt1", [128, D])]
                junk = sb(es, "junk", [128, D], BF16)
                ssum = sb(es, "ssum", [128, 1])
                rstd = sb(es, "rstd", [128, 1])
                xn = sb(es, "xn", [128, D], BF16)
                tmpm = sb(es, "tmpm", [128, 8, 128])
                hT = sb(es, "hT", [128, 8, 512], BF16)
                qrT = sb(es, "qrT", [64, NH, 512], BF16)
                krT = sb(es, "krT", [64, NH, 512], BF16)
                qaT = sb(es, "qaT", [64, NH, 512], BF16)
                kaT = sb(es, "kaT", [64, NH, 512], BF16)
                kdec = sb(es, "kdec", [128, 512], BF16)
                vr = sb(es, "vr", [128, 512], BF16)
                sg = sb(es, "sg", [128, 512])
                kaf = sb(es, "kaf", [128, 512])
                vaf = sb(es, "vaf", [128, 512])
                vext = sb(es, "vext", [128, NH, 65], BF16)
                PT = sb(es, "PT", [128, NH, 128], BF16)
                qdec = sb(es, "qdec", [64, NH, 128], BF16)
                Sp = sb(es, "Sp", [64, NH, E])
                Ss = sb(es, "Ss", [64, NH, E])
                Sbf = sb(es, "Sbf", [64, NH, E], BF16)
                osb = sb(es, "osb", [128, 512])
                cen = sb(es, "cen", [128, 512])
                sq = sb(es, "sq", [128, 512])
                st8 = sb(es, "st8", [128, 8])
                st8b = sb(es, "st8b", [128, 8])
                rety = sb(es, "rety", [128, 512], BF16)
                ps_tr = ps(es, "ps_trA", [128, 8, 128], BF16)
                ps_f = [ps(es, "ps_f0", [128, 512])]
                ps_k = [ps(es, "ps_k0", [128, 512]), ps(es, "ps_k1", [128, 512])]
                ps_A = ps(es, "ps_A", [128, NH, 128])
                ps_o = ps(es, "ps_o", [128, 512])

                w_in_v = w_in.rearrange("(k p) c -> p k c", p=128)
                for cb in range(7):
                    S.dma("gpsimd", w_in_bf[:, :, cb * 512:(cb + 1) * 512], w_in_v[:, :, cb * 512:(cb + 1) * 512],
                          writes=["w_in_bf"])
                for kind in range(2):
                    S.dma("sync", tabs[kind]["dm"][:], c_dm[kind], writes=["tabs"])
                    S.dma("sync", tabs[kind]["kd"][:], c_kd[kind], writes=["tabs"])
                    S.dma("sync", tabs[kind]["qd"][:], c_qd[kind], writes=["tabs"])
                    S.dma("sync", tabs[kind]["gd"][:], c_gd[kind], writes=["tabs"])
                S.dma("sync", gret[:], gretb[:, :], writes=["gret"])
                S.dma("sync", pval[:], pvalid[:, :], writes=["pval"])
                S.op("gpsimd", lambda e: e.memset(ones_c[:], 1.0), writes=["ones_c"])
                S.op("gpsimd", lambda e: e.memset(Sp[:], 0.0), writes=["Sp"])
                S.op("gpsimd", lambda e: e.memset(Sbf[:], 0.0), writes=["Sbf"])

                fcnt = [0]
                kcnt = [0]
                tcnt = [0]

                def norm_tile(x_src, col, t_in_grp):
                    i = tcnt[0] % 2
                    tcnt[0] += 1
                    xb, xbn = xt[i], "xt%d" % i
                    S.dma("sync", xb[:], x_src, writes=[xbn])
                    S.op("gpsimd", lambda e: e.memset(ssum[:], 0.0), writes=["ssum"])
                    S.op("scalar", lambda e: e.activation(junk[:], xb[:], AF.Square, accum_out=ssum[:]),
                         reads=[xbn], writes=["junk", "ssum"])
                    S.op("vector", lambda e: e.tensor_scalar(rstd[:], ssum[:], 1.0 / D, EPS, ALU.mult, ALU.add),
                         reads=["ssum"], writes=["rstd"])
                    S.op("scalar", lambda e: e.activation(rstd[:], rstd[:], AF.Sqrt), reads=["rstd"], writes=["rstd"])
                    S.op("vector", lambda e: e.reciprocal(rstd[:], rstd[:]), reads=["rstd"], writes=["rstd"])
                    S.op("vector", lambda e: e.tensor_scalar(xn[:], xb[:], rstd[:, 0:1], None, ALU.mult),
                         reads=[xbn, "rstd"], writes=["xn"])
                    for k in range(8):
                        S.op("tensor", lambda e, k=k: e.transpose(ps_tr[:, k, :], xn[:, k * 128:(k + 1) * 128], ident_b[:]),
                             reads=["xn", "ident_b"], writes=["ps_trA"])
                    S.op("vector", lambda e: e.tensor_tensor(
                        tmpm[:], ps_tr[:], A1[:, :, col:col + 1].to_broadcast([128, 8, 128]), ALU.mult),
                        reads=["ps_trA", "A1"], writes=["tmpm"])
                    c0 = t_in_grp * 128
                    S.op("vector", lambda e: e.tensor_tensor(
                        hT[:, :, c0:c0 + 128], tmpm[:], B1[:, :, col:col + 1].to_broadcast([128, 8, 128]), ALU.add),
                        reads=["tmpm", "B1"], writes=["hT"])

                def feat_proj(dst, dname, col_off, ntok):
                    for p in range(4):
                        pf, pfn = ps_f[0], "ps_f0"
                        for k in range(8):
                            S.op("tensor", lambda e, k=k, p=p, pf=pf: e.matmul(
                                pf[:, 0:ntok], w_in_bf[:, k, col_off + 128 * p:col_off + 128 * (p + 1)], hT[:, k, 0:ntok],
                                start=(k == 0), stop=(k == 7)),
                                reads=["w_in_bf", "hT"], writes=[pfn])
                        eng = "scalar" if (p % 2 == 0) else "vector"
                        if eng == "scalar":
                            S.op("scalar", lambda e, p=p, pf=pf: e.copy(dst[:, p, 0:ntok], pf[:, 0:ntok]),
                                 reads=[pfn], writes=[dname])
                        else:
                            S.op("vector", lambda e, p=p, pf=pf: e.tensor_copy(dst[:, p, 0:ntok], pf[:, 0:ntok]),
                                 reads=[pfn], writes=[dname])

                def feat_proj_h(dst, dname, col_off, ntok):
                    for h in range(NH):
                        pf, pfn = ps_f[0], "ps_f0"
                        for k in range(8):
                            S.op("tensor", lambda e, k=k, h=h, pf=pf: e.matmul(
                                pf[0:64, 0:ntok], w_in_bf[:, k, col_off + 64 * h:col_off + 64 * (h + 1)], hT[:, k, 0:ntok],
                                start=(k == 0), stop=(k == 7)),
                                reads=["w_in_bf", "hT"], writes=[pfn])
                        if h % 2 == 0:
                            S.op("scalar", lambda e, h=h, pf=pf: e.copy(dst[:, h, 0:ntok], pf[0:64, 0:ntok]),
                                 reads=[pfn], writes=[dname])
                        else:
                            S.op("vector", lambda e, h=h, pf=pf: e.tensor_copy(dst[:, h, 0:ntok], pf[0:64, 0:ntok]),
                                 reads=[pfn], writes=[dname])

                def tok_proj(col_off, t_in_grp):
                    i = kcnt[0] % 2
                    kcnt[0] += 1
                    pk, pkn = ps_k[i], "ps_k%d" % i
                    c0 = t_in_grp * 128
                    for k in range(8):
                        S.op("tensor", lambda e, k=k, pk=pk: e.matmul(
                            pk[:], hT[:, k, c0:c0 + 128], w_in_bf[:, k, col_off:col_off + 512],
                            start=(k == 0), stop=(k == 7)),
                            reads=["w_in_bf", "hT"], writes=[pkn])
                    return pk, pkn

                def do_group(tiles):
                    ntok = 128 * len(tiles)
                    mode = tiles[0]["mode"]
                    for ti, t in enumerate(tiles):
                        norm_tile(t["x"], t["col"], ti)
                    cut = cfg.get("cut", 99)
                    if cut <= 1:
                        return
                    if mode in ("own", "smp"):
                        feat_proj_h(qrT, "qrT", 0, ntok)
                        feat_proj_h(krT, "krT", 512, ntok)
                        feat_proj_h(qaT, "qaT", 2048, ntok)
                    if mode in ("win", "own", "smp"):
                        feat_proj_h(kaT, "kaT", 2560, ntok)
                    if mode in ("win", "own"):
                        sp0 = tiles[0]["span"] * 128
                        S.dma("sync", kT_scr[:, :, sp0:sp0 + ntok].rearrange("p q t -> q p t"), kaT[:, :, 0:ntok],
                              reads=["kaT"], writes=["kT_scr"])
                    if mode == "own":
                        o0 = tiles[0]["own"] * 128
                        S.dma("sync", qT_scr[:, :, o0:o0 + ntok].rearrange("p q t -> q p t"), qaT[:, :, 0:ntok],
                              reads=["qaT"], writes=["qT_scr"])
                    if mode == "smp":
                        s = tiles[0]["seq"]
                        S.dma("sync", qTs_scr[s], qaT[:, :, 0:128], reads=["qaT"], writes=["qTs_scr"])
                        S.dma("sync", kTs_scr[s], kaT[:, :, 0:128], reads=["kaT"], writes=["kTs_scr"])
                    if cut <= 2:
                        return
                    for ti, t in enumerate(tiles):
                        do_tile(ti, t, mode, cut)

                def do_tile(ti, t, mode, cut):
                    for _once in (0,):
                        c0 = ti * 128
                        tb = tabs[t["kind"]]
                        St, Sn = t["state"]
                        vcol = t["valid"]
                        pk, pkn = tok_proj(512, ti)
                        S.op("vector", lambda e, pk=pk, tb=tb: e.tensor_tensor(kdec[:], pk[:], tb["kd"][:], ALU.mult),
                             reads=[pkn, "tabs"], writes=["kdec"])
                        pk, pkn = tok_proj(1024, ti)
                        S.op("scalar", lambda e, pk=pk: e.copy(vr[:], pk[:]), reads=[pkn], writes=["vr"])
                        if mode in ("own", "smp"):
                            if not cfg.get("skip_sg"):
                                pk, pkn = tok_proj(1536, ti)
                                S.op("scalar", lambda e, pk=pk: e.activation(sg[:], pk[:], AF.Copy if cfg.get("nosilu") else AF.Silu), reads=[pkn], writes=["sg"])
                            if not cfg.get("skip_kaf"):
                                pk, pkn = tok_proj(2560, ti)
                                S.op("scalar", lambda e, pk=pk: e.copy(kaf[:], pk[:]), reads=[pkn], writes=["kaf"])
                            if mode == "own":
                                r0 = t["own"] * 128
                                if not cfg.get("nowk"):
                                    S.dma("sync", wk[r0:r0 + 128, :], kaf[:], reads=["kaf"], writes=["wk"])
                            else:
                                s = t["seq"]
                                S.dma("sync", sk[s * TSEQ:(s + 1) * TSEQ, :], kaf[0:TSEQ, :], reads=["kaf"], writes=["sk"])
                        if mode in ("win", "own", "smp"):
                            pk, pkn = tok_proj(3072, ti)
                            if mode != "win" and not cfg.get("skip_vaf"):
                                S.op("scalar", lambda e, pk=pk: e.copy(vaf[:], pk[:]), reads=[pkn], writes=["vaf"])
                                if mode == "own":
                                    r0 = t["own"] * 128
                                    if not cfg.get("nowk"):
                                        S.dma("sync", wv[r0:r0 + 128, :], vaf[:], reads=["vaf"], writes=["wv"])
                                else:
                                    s = t["seq"]
                                    S.dma("sync", sv[s * TSEQ:(s + 1) * TSEQ, :], vaf[0:TSEQ, :], reads=["vaf"], writes=["sv"])
                            vsrc = vcol if vcol is not None else ones_c[:, 0:1]
                            vin, vinn = (pk, pkn) if mode == "win" else (vaf, "vaf")
                            S.op("vector", lambda e, vin=vin, vsrc=vsrc: e.tensor_scalar(
                                vext[:, :, 0:64], vin[:].rearrange("p (h e) -> p h e", e=64), vsrc, None, ALU.mult),
                                reads=[vinn, "pval", "ones_c"], writes=["vext"])
                            S.op("vector", lambda e, vsrc=vsrc: e.tensor_copy(
                                vext[:, :, 64:65], vsrc.unsqueeze(1).to_broadcast([128, NH, 1])),
                                reads=["pval", "ones_c"], writes=["vext"])
                            if mode == "smp":
                                S.dma("sync", vs_scr[t["seq"]], vext[:].rearrange("p h e -> p (h e)"), reads=["vext"], writes=["vs_scr"])
                            else:
                                S.dma("sync", v_scr[t["span"]], vext[:].rearrange("p h e -> p (h e)"), reads=["vext"], writes=["v_scr"])
                        if cut <= 3:
                            continue
                        if mode in ("own", "smp"):
                            for h in range(NH):
                                p_, hf = h // 2, h % 2
                                pr = slice(0, 64) if cfg.get('pr0') else slice(64 * hf, 64 * hf + 64)
                                S.op("tensor", lambda e, h=h: e.matmul(
                                    ps_A[:, h, :], krT[:, h, c0:c0 + 128], qrT[:, h, c0:c0 + 128], start=True, stop=True),
                                    reads=["krT", "qrT"], writes=["ps_A"])
                            S.op("vector", lambda e, tb=tb: e.tensor_tensor(PT[:, 0:4, :], ps_A[:, 0:4, :], tb["dm"][:, 0:4, :], ALU.mult),
                                 reads=["ps_A", "tabs"], writes=["PT"])
                            S.op("vector", lambda e, tb=tb: e.tensor_tensor(PT[:, 4:8, :], ps_A[:, 4:8, :], tb["dm"][:, 4:8, :], ALU.mult),
                                 reads=["ps_A", "tabs"], writes=["PT"])
                            S.op("vector", lambda e, tb=tb: e.tensor_tensor(qdec[:], qrT[:, :, c0:c0 + 128], tb["qd"][:], ALU.mult),
                                 reads=["qrT", "tabs"], writes=["qdec"])
                            for h in range(NH):
                                p_, hf = h // 2, h % 2
                                pr = slice(0, 64) if cfg.get('pr0') else slice(64 * hf, 64 * hf + 64)
                                S.op("tensor", lambda e, h=h: e.matmul(
                                    ps_o[:, 64 * h:64 * h + 64], PT[:, h, :], vr[:, 64 * h:64 * h + 64], start=True, stop=False),
                                    reads=["PT", "vr"], writes=["ps_o"])
                                S.op("tensor", lambda e, h=h: e.matmul(
                                    ps_o[:, 64 * h:64 * h + 64], qdec[:, h, :], Sbf[:, h, :], start=False, stop=True),
                                    reads=["qdec", "Sbf"], writes=["ps_o"])
                        if cut <= 4:
                            continue
                        for h in range(NH):
                            S.op("tensor", lambda e, h=h: e.matmul(
                                ps_S[:, h, :], kdec[:, 64 * h:64 * h + 64], vr[:, 64 * h:64 * h + 64],
                                start=True, stop=True),
                                reads=["kdec", "vr"], writes=["ps_S"])
                        S.op("vector", lambda e, St=St, tb=tb: e.tensor_tensor(St[:], St[:], tb["gd"][:], ALU.mult),
                             reads=[Sn, "tabs"], writes=[Sn])
                        vsrc = vcol if vcol is not None else ones_c[:, 0:1]
                        S.op("vector", lambda e, St=St, vsrc=vsrc: e.scalar_tensor_tensor(
                            St[:], ps_S[:], vsrc[0:64, :], St[:], ALU.mult, ALU.add),
                            reads=["ps_S", Sn, "pval", "ones_c"], writes=[Sn])
                        S.op("vector", lambda e, St=St: e.tensor_copy(Sbf[:], St[:]), reads=[Sn], writes=["Sbf"])
                        if cut <= 5:
                            continue
                        if mode in ("own", "smp"):
                            o3 = lambda t_: t_[:].rearrange("p (h e) -> p h e", e=64)
                            S.op("scalar", lambda e: e.copy(osb[:], ps_o[:]), reads=["ps_o"], writes=["osb"])
                            S.op("vector", lambda e: e.tensor_reduce(st8[:], o3(osb), AX.X, ALU.add), reads=["osb"], writes=["st8"])
                            S.op("vector", lambda e: e.tensor_scalar(st8[:], st8[:], 1.0 / E, None, ALU.mult), reads=["st8"], writes=["st8"])
                            S.op("vector", lambda e: e.tensor_tensor(o3(cen), o3(osb), st8[:].unsqueeze(2).to_broadcast([128, NH, E]), ALU.subtract),
                                 reads=["osb", "st8"], writes=["cen"])
                            S.op("scalar", lambda e: e.activation(sq[:], cen[:], AF.Square), reads=["cen"], writes=["sq"])
                            S.op("vector", lambda e: e.tensor_reduce(st8b[:], o3(sq), AX.X, ALU.add), reads=["sq"], writes=["st8b"])
                            S.op("vector", lambda e: e.tensor_scalar(st8b[:], st8b[:], 1.0 / E, EPS, ALU.mult, ALU.add), reads=["st8b"], writes=["st8b"])
                            S.op("scalar", lambda e: e.activation(st8b[:], st8b[:], AF.Sqrt), reads=["st8b"], writes=["st8b"])
                            S.op("vector", lambda e: e.reciprocal(st8b[:], st8b[:]), reads=["st8b"], writes=["st8b"])
                            S.op("vector", lambda e: e.tensor_tensor(o3(cen), o3(cen), st8b[:].unsqueeze(2).to_broadcast([128, NH, E]), ALU.mult),
                                 reads=["cen", "st8b"], writes=["cen"])
                            S.op("vector", lambda e: e.tensor_tensor(sq[:], sg[:], gret[:], ALU.mult), reads=["sg", "gret"], writes=["sq"])
                            S.op("vector", lambda e: e.tensor_tensor(rety[:], cen[:], sq[:], ALU.mult), reads=["cen", "sq"], writes=["rety"])
                            if mode == "own":
                                S.dma("sync", y_scr[t["own"], :, 0:512], rety[:], reads=["rety"], writes=["y_scr"])
                            else:
                                s = t["seq"]
                                S.dma("sync", y_scr[OWN_T, s * TSEQ:(s + 1) * TSEQ, 0:512], rety[0:TSEQ, :], reads=["rety"], writes=["y_scr"])

                ps_S = ps(es, "ps_S", [64, NH, E])

                pre_skip = PRE_T - n_pre
                for g in range(pre_skip // 4, PRE_T // 4):
                    tl = []
                    for ti in range(4):
                        t = 4 * g + ti
                        md = "win" if t >= PRE_T - 16 else "pre"
                        tl.append(dict(x=xp[t * 128:(t + 1) * 128, :], col=0, kind=0, mode=md, valid=pval[:, t:t + 1],
                                       state=(Sp, "Sp"), span=t - (PRE_T - 16)))
                    do_group(tl)
                for g in range(cfg.get('n_own', OWN_T) // 4):
                    tl = []
                    for ti in range(4):
                        t = 4 * g + ti
                        tl.append(dict(x=xp[(PRE_T + t) * 128:(PRE_T + t + 1) * 128, :], col=0, kind=0, mode="own", valid=None,
                                       state=(Sp, "Sp"), span=16 + t, own=t))
                    do_group(tl)
                S.dma("sync", rp.rearrange("h e f -> e h f"), Sp[:], reads=["Sp"], writes=["rp"])
                n_smp = cfg.get("n_smp", NSEQ)
                for s in range(n_smp):
                    S.dma("sync", Ss[:], state_in[s].rearrange("h e f -> e h f"), writes=["Ss"])
                    S.op("vector", lambda e: e.tensor_copy(Sbf[:], Ss[:]), reads=["Ss"], writes=["Sbf"])
                    do_group([dict(x=xs_pad[s * 128:(s + 1) * 128, :], col=1 + s, kind=1, mode="smp", valid=None,
                                   state=(Ss, "Ss"), seq=s)])
                    S.dma("sync", rs[s].rearrange("h e f -> e h f"), Ss[:], reads=["Ss"], writes=["rs"])
                S.flush()
                if dbg and "B" not in stages:
                    o = dbg_out("y_scr", [NT, 128, D], BF16)
                    S.dma("sync", o[0:OWN_T, :, 0:512], y_scr[0:OWN_T, :, 0:512], reads=["y_scr"], writes=["dbg_y"])
                    S.dma("sync", o[OWN_T, 0:TSEQ * n_smp, 0:512], y_scr[OWN_T, 0:TSEQ * n_smp, 0:512], reads=["y_scr"], writes=["dbg_y"])
                    S.flush()


        if "B" in stages:
            with ExitStack() as es:
                kTh = [sb(es, "kTh0", [64, SPAN_T * 128], BF16), sb(es, "kTh1", [64, SPAN_T * 128], BF16)]
                qTh = [sb(es, "qTh0", [64, OWN_T * 128], BF16), sb(es, "qTh1", [64, OWN_T * 128], BF16)]
                v_all = sb(es, "v_all", [128, SPAN_T, NH * 65], BF16)
                Th = [sb(es, "Th0", [128, TOEP_W]), sb(es, "Th1", [128, TOEP_W])]
                NB_ = 4
                s_sb = [sb(es, "s_sb%d" % i, [128, 512]) for i in range(NB_)]
                PTb = [sb(es, "PTb%d" % i, [128, 512], BF16) for i in range(NB_)]
                rec = sb(es, "rec", [128, 4, 1])
                attb = sb(es, "attb", [128, 4, 64], BF16)
                ps_s = [ps(es, "ps_s%d" % i, [128, 512]) for i in range(NB_)]
                ps_acc = [ps(es, "ps_acc0", [128, 4, 128]), ps(es, "ps_acc1", [128, 4, 128])]
                recb = [sb(es, "recb0", [128, 4, 1]), sb(es, "recb1", [128, 4, 1])]
                attbb = [sb(es, "attbb0", [128, 4, 64], BF16), sb(es, "attbb1", [128, 4, 64], BF16)]
                for a in range(SPAN_T):
                    S.dma("sync" if a % 2 == 0 else "scalar", v_all[:, a, :], v_scr[a], reads=["v_scr"], writes=["v_all"])
                it = 0
                pend = []
                LA = 2

                def drain(keep):
                    while len(pend) > keep:
                        pend.pop(0)()
                n_heads_b = cfg.get("n_heads_b", NH)
                for h in range(n_heads_b):
                    hb = h % 2
                    S.dma("sync", kTh[hb][:], kT_scr[h], reads=["kT_scr"], writes=["kTh%d" % hb])
                    S.dma("scalar", qTh[hb][:], qT_scr[h], reads=["qT_scr"], writes=["qTh%d" % hb])
                    S.dma("sync", Th[hb][:], c_toep[h], writes=["Th%d" % hb])
                    for G in range(OWN_T // 4):
                        acc = ps_acc[G % 2]
                        accn = "ps_acc%d" % (G % 2)
                        for a in range(4 * G, 4 * G + 20):
                            i = it % NB_
                            it += 1
                            cs = 384 + 128 * (16 + 4 * G - a)
                            S.op("tensor", lambda e, i=i, hb=hb, a=a, G=G: e.matmul(
                                ps_s[i][:], kTh[hb][:, a * 128:(a + 1) * 128], qTh[hb][:, G * 512:(G + 1) * 512],
                                start=True, stop=True),
                                reads=["kTh%d" % hb, "qTh%d" % hb], writes=["ps_s%d" % i])
                            S.op("vector", lambda e, i=i, hb=hb, cs=cs: e.scalar_tensor_tensor(
                                s_sb[i][:], ps_s[i][:], 0.125, Th[hb][:, cs:cs + 512], ALU.mult, ALU.add),
                                reads=["ps_s%d" % i, "Th%d" % hb], writes=["s_sb%d" % i])
                            S.op("scalar", lambda e, i=i: e.activation(PTb[i][:], s_sb[i][:], AF.Exp),
                                 reads=["s_sb%d" % i], writes=["PTb%d" % i])
                            def pv(i=i, a=a, h=h, G=G, acc=acc, accn=accn):
                                for qi in range(4):
                                    first, last = 4 * G + qi, 16 + 4 * G + qi
                                    if a < first or a > last:
                                        continue
                                    S.op("tensor", lambda e, i=i, qi=qi, a=a, h=h, acc=acc, first=first, last=last: e.matmul(
                                        acc[:, qi, 0:65], PTb[i][:, qi * 128:(qi + 1) * 128], v_all[:, a, h * 65:(h + 1) * 65],
                                        start=(a == first), stop=(a == last)),
                                        reads=["PTb%d" % i, "v_all"], writes=[accn])
                            pend.append(pv)
                            drain(LA)
                        gb = G % 2
                        def epi(acc=acc, accn=accn, gb=gb, G=G, h=h):
                            S.op("vector", lambda e, acc=acc, gb=gb: e.reciprocal(recb[gb][:], acc[:, :, 64:65]), reads=[accn], writes=["recb%d" % gb])
                            S.op("vector", lambda e, acc=acc, gb=gb: e.tensor_tensor(
                                attbb[gb][:], acc[:, :, 0:64], recb[gb][:].to_broadcast([128, 4, 64]), ALU.mult),
                                reads=[accn, "recb%d" % gb], writes=["attbb%d" % gb])
                            S.dma("scalar", y_scr[4 * G:4 * G + 4, :, 512 + 64 * h:512 + 64 * h + 64].rearrange("t p e -> p t e"),
                                  attbb[gb][:], reads=["attbb%d" % gb], writes=["y_scr"])
                        pend.append(epi)
                drain(0)
                S.flush()
                if dbg and "C" not in stages:
                    o = dbg_out("y_att", [OWN_T, 128, 512], BF16)
                    S.dma("sync", o[:, :, 0:64 * n_heads_b], y_scr[0:OWN_T, :, 512:512 + 64 * n_heads_b], reads=["y_scr"], writes=["dbg_y"])
                    S.flush()


        if "B" in stages:
            with ExitStack() as es:
                sbias = sb(es, "sbias", [128, 17, NH * TSEQ])
                qTs = sb(es, "qTs", [64, NH, 128], BF16)
                kTs = sb(es, "kTs", [64, NH, 128], BF16)
                vs = sb(es, "vs", [128, NH * 65], BF16)
                ckb = [sb(es, "ckb0", [128, 4, 512]), sb(es, "ckb1", [128, 4, 512])]
                cvb = [sb(es, "cvb0", [128, 4, 512]), sb(es, "cvb1", [128, 4, 512])]
                kcT = [sb(es, "kcT0", [64, NH, 128], BF16), sb(es, "kcT1", [64, NH, 128], BF16)]
                vx = [sb(es, "vx0", [128, 4, NH, 65], BF16), sb(es, "vx1", [128, 4, NH, 65], BF16)]
                s4 = sb(es, "s4", [128, 4, NH * TSEQ])
                P4 = [sb(es, "P40", [128, 4, NH * TSEQ], BF16), sb(es, "P41", [128, 4, NH * TSEQ], BF16)]
                recs = sb(es, "recs", [TSEQ, NH, 1])
                atts = sb(es, "atts", [TSEQ, NH, 64], BF16)
                ps_kT = [ps(es, "ps_kT0", [64, 4, 128]), ps(es, "ps_kT1", [64, 4, 128])]
                ps_s4 = [ps(es, "ps_s40", [128, 4, NH * TSEQ]), ps(es, "ps_s41", [128, 4, NH * TSEQ])]
                ps_as = ps(es, "ps_as", [TSEQ, NH, 128])
                S.dma("sync", sbias[:], c_sbias.rearrange("p a h t -> p a (h t)"), writes=["sbias"])
                for i in range(2):
                    S.op("gpsimd", lambda e, i=i: e.memset(vx[i][:], 1.0), writes=["vx%d" % i])
                n_smp_b = 0 if cfg.get("skip_bs") else cfg.get("n_smp", NSEQ)
                ci = 0
                kti = 0
                pend_s = []

                def drain_s(keep):
                    while len(pend_s) > keep:
                        pend_s.pop(0)()
                for s in range(n_smp_b):
                    drain_s(0)
                    S.dma("sync", qTs[:], qTs_scr[s], reads=["qTs_scr"], writes=["qTs"])
                    S.dma("sync", kTs[:], kTs_scr[s], reads=["kTs_scr"], writes=["kTs"])
                    S.dma("sync", vs[:], vs_scr[s], reads=["vs_scr"], writes=["vs"])
                    for ch in range(5):
                        b = ci % 2
                        ci += 1
                        ntile = 4 if ch < 4 else 1
                        if ch < 4:
                            S.dma("sync", ckb[b][:], ck[s, 512 * ch:512 * (ch + 1), :].rearrange("(a p) c -> p a c", p=128),
                                  writes=["ckb%d" % b])
                            S.dma("scalar", cvb[b][:], cv[s, 512 * ch:512 * (ch + 1), :].rearrange("(a p) c -> p a c", p=128),
                                  writes=["cvb%d" % b])
                            S.op("scalar", lambda e, b=b: e.copy(vx[b][:, :, :, 0:64], cvb[b][:].rearrange("p a (h e) -> p a h e", e=64)),
                                 reads=["cvb%d" % b], writes=["vx%d" % b])
                        pss = ps_s4[b]
                        pssn = "ps_s4%d" % b
                        for j in range(ntile):
                            if ch < 4:
                                kb = kti % 2
                                kti += 1
                                for hh in range(2):
                                    pk_ = ps_kT[hh]
                                    for h4 in range(4):
                                        h = 4 * hh + h4
                                        S.op("tensor", lambda e, b=b, j=j, h=h, h4=h4, pk_=pk_: e.transpose(
                                            pk_[:, h4, :], ckb[b][:, j, 64 * h:64 * h + 64], ident_f[:]),
                                            reads=["ckb%d" % b, "ident_f"], writes=["ps_kT%d" % hh])
                                    if hh == 0:
                                        S.op("vector", lambda e, kb=kb, pk_=pk_: e.tensor_copy(kcT[kb][:, 0:4, :], pk_[:]),
                                             reads=["ps_kT0"], writes=["kcT%d" % kb])
                                    else:
                                        S.op("scalar", lambda e, kb=kb, pk_=pk_: e.copy(kcT[kb][:, 4:8, :], pk_[:]),
                                             reads=["ps_kT1"], writes=["kcT%d" % kb])
                                ksrc, ksn = kcT[kb], "kcT%d" % kb
                            else:
                                ksrc, ksn = kTs, "kTs"
                            for h in range(NH):
                                S.op("tensor", lambda e, j=j, h=h, ksrc=ksrc, pss=pss: e.matmul(
                                    pss[:, j, h * TSEQ:(h + 1) * TSEQ], ksrc[:, h, :], qTs[:, h, 0:TSEQ], start=True, stop=True),
                                    reads=[ksn, "qTs"], writes=[pssn])
                        a0 = 4 * ch
                        S.op("vector", lambda e, pss=pss, a0=a0, ntile=ntile: e.scalar_tensor_tensor(
                            s4[:, 0:ntile, :], pss[:, 0:ntile, :], 0.125, sbias[:, a0:a0 + ntile, :], ALU.mult, ALU.add),
                            reads=[pssn, "sbias"], writes=["s4"])
                        S.op("scalar", lambda e, b=b, ntile=ntile: e.activation(P4[b][:, 0:ntile, :], s4[:, 0:ntile, :], AF.Exp),
                             reads=["s4"], writes=["P4%d" % b])
                        def pv_s(b=b, ch=ch, ntile=ntile):
                            for j in range(ntile):
                                for h in range(NH):
                                    if ch < 4:
                                        rhs = vx[b][:, j, h, :]
                                        rn = "vx%d" % b
                                    else:
                                        rhs = vs[:, h * 65:(h + 1) * 65]
                                        rn = "vs"
                                    S.op("tensor", lambda e, b=b, j=j, h=h, rhs=rhs, first=(ch == 0 and j == 0), last=(ch == 4): e.matmul(
                                        ps_as[:, h, 0:65], P4[b][:, j, h * TSEQ:(h + 1) * TSEQ], rhs, start=first, stop=last),
                                        reads=["P4%d" % b, rn], writes=["ps_as"])
                        pend_s.append(pv_s)
                        drain_s(1)

                    def epi_s(s=s):
                        S.op("vector", lambda e: e.reciprocal(recs[:], ps_as[:, :, 64:65]), reads=["ps_as"], writes=["recs"])
                        S.op("vector", lambda e: e.tensor_tensor(atts[:], ps_as[:, :, 0:64], recs[:].to_broadcast([TSEQ, NH, 64]), ALU.mult),
                             reads=["ps_as", "recs"], writes=["atts"])
                        S.dma("sync", y_scr[OWN_T, s * TSEQ:(s + 1) * TSEQ, 512:1024], atts[:].rearrange("p h e -> p (h e)"),
                              reads=["atts"], writes=["y_scr"])
                    pend_s.append(epi_s)
                drain_s(0)
                S.flush()
                if dbg and "C" not in stages:
                    o = dbg_out("y_atts", [128, 512], BF16)
                    S.dma("sync", o[0:TSEQ * n_smp_b, :], y_scr[OWN_T, 0:TSEQ * n_smp_b, 512:1024], reads=["y_scr"], writes=["dbg_y"])
                    S.flush()


        if "C" in stages:
            h2T_all = sb(es_all, "h2T_all", [128, 8, NT * 128], BF16)
            G_all = sb(es_all, "G_all", [128, NT, NEXP])
            y_acc = sb(es_all, "y_acc", [128, NT, D])
            with ExitStack() as es:
                w_out_bf = sb(es, "w_out_bf", [128, 8, D], BF16)
                GA = [sb(es, "GA0", [128, D]), sb(es, "GA1", [128, D])]
                wr_f = sb(es, "wr_f", [128, 8, NEXP])
                br = sb(es, "br", [1, NEXP])
                ones_r = sb(es, "ones_r", [1, 128])
                bd_sb = sb(es, "bd_sb", [NEXP, D])
                ybf = [sb(es, "ybf0", [128, D], BF16), sb(es, "ybf1", [128, D], BF16)]
                xc = [sb(es, "xc0", [128, D]), sb(es, "xc1", [128, D])]
                yT = sb(es, "yT", [128, 8, 128], BF16)
                junkc = sb(es, "junkc", [128, D], BF16)
                ssc = sb(es, "ssc", [128, 1])
                ssc2 = sb(es, "ssc2", [128, 2])
                rsc = sb(es, "rsc", [128, 1])
                t1 = sb(es, "t1", [128, D])
                x1 = sb(es, "x1", [128, D])
                xn2 = sb(es, "xn2", [128, D])
                tmp2 = sb(es, "tmp2", [128, 8, 128])
                h2f = sb(es, "h2f", [128, 8, 128])
                lg = sb(es, "lg", [128, NEXP])
                mx8 = sb(es, "mx8", [128, 8])
                nmx = sb(es, "nmx", [128, 1])
                msk = sb(es, "msk", [128, NEXP])
                ex = sb(es, "ex", [128, NEXP])
                s3 = sb(es, "s3", [128, 1])
                GT = sb(es, "GT", [NEXP, 128])
                ps_trc = ps(es, "ps_trc", [128, 8, 128], BF16)
                ps_mix = ps(es, "ps_mix", [128, D])
                ps_tr2 = ps(es, "ps_tr2", [128, 8, 128])
                ps_lg = ps(es, "ps_lg", [128, NEXP])
                ps_gt = ps(es, "ps_gt", [NEXP, 128])

                S.dma("gpsimd", w_out_bf[:, :, 0:512], w_out.rearrange("(k p) c -> p k c", p=128)[:, :, 0:512], writes=["w_out_bf"])
                S.dma("gpsimd", w_out_bf[:, :, 512:D], w_out.rearrange("(k p) c -> p k c", p=128)[:, :, 512:D], writes=["w_out_bf"])
                S.dma("sync", GA[0][:], gtab_scr[0], writes=["GA0"])
                S.dma("sync", GA[1][:], gtab_scr[1], writes=["GA1"])
                S.dma("sync", wr_f[:], w_router.rearrange("(k p) n -> p k n", p=128), writes=["wr_f"])
                S.dma("sync", br[:], b_router[:, :], writes=["br"])
                S.dma("sync", bd_sb[:], b_down[:, :], writes=["bd_sb"])
                S.op("gpsimd", lambda e: e.memset(ones_r[:], 1.0), writes=["ones_r"])
                n_tc = cfg.get("n_tc", NT)
                for tt in list(range(n_tc - 1)) + [NT - 1]:
                    smp = (tt == NT - 1)
                    i = tt % 2
                    yb, ybn = ybf[i], "ybf%d" % i
                    xb, xbn = xc[i], "xc%d" % i
                    ga, gan = (GA[1], "GA1") if smp else (GA[0], "GA0")
                    S.dma("sync", yb[:], y_scr[tt], reads=["y_scr"], writes=[ybn])
                    if smp:
                        S.dma("scalar", xb[:], xs[:, :], writes=[xbn])
                    else:
                        S.dma("scalar", xb[:], xp[(PRE_T + tt) * 128:(PRE_T + tt + 1) * 128, :], writes=[xbn])
                    for k in range(8):
                        S.op("tensor", lambda e, k=k, yb=yb: e.transpose(ps_trc[:, k, :], yb[:, k * 128:(k + 1) * 128], ident_b[:]),
                             reads=[ybn, "ident_b"], writes=["ps_trc"])
                    S.op("scalar", lambda e: e.copy(yT[:], ps_trc[:]), reads=["ps_trc"], writes=["yT"])
                    for half in range(2):
                        for k in range(8):
                            S.op("tensor", lambda e, k=k, half=half: e.matmul(
                                ps_mix[:, half * 512:(half + 1) * 512], yT[:, k, :], w_out_bf[:, k, half * 512:(half + 1) * 512],
                                start=(k == 0), stop=(k == 7)),
                                reads=["yT", "w_out_bf"], writes=["ps_mix"])
                    for half in range(2):
                        S.op("scalar", lambda e, half=half: e.activation(
                            junkc[:, half * 512:(half + 1) * 512], ps_mix[:, half * 512:(half + 1) * 512], AF.Square,
                            accum_out=ssc2[:, half:half + 1]),
                            reads=["ps_mix"], writes=["junkc", "ssc2"])
                    S.op("vector", lambda e: e.tensor_tensor(ssc[:], ssc2[:, 0:1], ssc2[:, 1:2], ALU.add), reads=["ssc2"], writes=["ssc"])
                    S.op("vector", lambda e: e.tensor_scalar(rsc[:], ssc[:], 1.0 / D, EPS, ALU.mult, ALU.add), reads=["ssc"], writes=["rsc"])
                    S.op("scalar", lambda e: e.activation(rsc[:], rsc[:], AF.Sqrt), reads=["rsc"], writes=["rsc"])
                    S.op("vector", lambda e: e.reciprocal(rsc[:], rsc[:]), reads=["rsc"], writes=["rsc"])
                    for half in range(2):
                        hs = slice(half * 512, (half + 1) * 512)
                        S.op("vector", lambda e, hs=hs, ga=ga: e.scalar_tensor_tensor(
                            t1[:, hs], ps_mix[:, hs], rsc[:, 0:1], ga[:, hs], ALU.mult, ALU.mult),
                            reads=["ps_mix", "rsc", gan], writes=["t1"])
                    S.op("vector", lambda e, xb=xb: e.tensor_tensor(x1[:], t1[:], xb[:], ALU.add), reads=["t1", xbn], writes=["x1"])
                    S.dma("sync", x1_scr[tt], x1[:], reads=["x1"], writes=["x1_scr"])
                    S.op("gpsimd", lambda e: e.memset(ssc[:], 0.0), reads=["rsc"], writes=["ssc"])
                    S.op("scalar", lambda e: e.activation(junkc[:], x1[:], AF.Square, accum_out=ssc[:]),
                         reads=["x1", "ssc"], writes=["junkc", "ssc"])
                    S.op("vector", lambda e: e.tensor_scalar(rsc[:], ssc[:], 1.0 / D, EPS, ALU.mult, ALU.add), reads=["ssc"], writes=["rsc"])
                    S.op("scalar", lambda e: e.activation(rsc[:], rsc[:], AF.Sqrt), reads=["rsc"], writes=["rsc"])
                    S.op("vector", lambda e: e.reciprocal(rsc[:], rsc[:]), reads=["rsc"], writes=["rsc"])
                    S.op("vector", lambda e: e.tensor_scalar(xn2[:], x1[:], rsc[:, 0:1], None, ALU.mult), reads=["x1", "rsc"], writes=["xn2"])
                    for k in range(8):
                        S.op("tensor", lambda e, k=k: e.transpose(ps_tr2[:, k, :], xn2[:, k * 128:(k + 1) * 128], ident_f[:]),
                             reads=["xn2", "ident_f"], writes=["ps_tr2"])
                    for hh in range(2):
                        ks = slice(4 * hh, 4 * hh + 4)
                        if smp:
                            a_b = A2[:, ks, 1:17].unsqueeze(3).to_broadcast([128, 4, NSEQ, TSEQ])
                            b_b = B2[:, ks, 1:17].unsqueeze(3).to_broadcast([128, 4, NSEQ, TSEQ])
                            v4 = lambda t_, ks=ks: t_[:, ks, :].rearrange("p k (s t) -> p k s t", t=TSEQ)
                        else:
                            a_b = A2[:, ks, 0:1].to_broadcast([128, 4, 128])
                            b_b = B2[:, ks, 0:1].to_broadcast([128, 4, 128])
                            v4 = lambda t_, ks=ks: t_[:, ks, :]
                        S.op("vector", lambda e, v4=v4, a_b=a_b: e.tensor_tensor(v4(tmp2), v4(ps_tr2), a_b, ALU.mult),
                             reads=["ps_tr2", "A2"], writes=["tmp2"])
                        S.op("vector", lambda e, v4=v4, b_b=b_b: e.tensor_tensor(v4(h2f), v4(tmp2), b_b, ALU.add),
                             reads=["tmp2", "B2"], writes=["h2f"])
                    S.op("scalar", lambda e, tt=tt: e.copy(h2T_all[:, :, tt * 128:(tt + 1) * 128], h2f[:]),
                         reads=["h2f"], writes=["h2T_all"])
                    for k in range(8):
                        S.op("tensor", lambda e, k=k: e.matmul(ps_lg[:], h2f[:, k, :], wr_f[:, k, :], start=(k == 0), stop=False),
                             reads=["h2f", "wr_f"], writes=["ps_lg"])
                    S.op("tensor", lambda e: e.matmul(ps_lg[:], ones_r[0:1, :], br[0:1, :], start=False, stop=True),
                         reads=["ones_r", "br"], writes=["ps_lg"])
                    S.op("vector", lambda e: e.tensor_copy(lg[:], ps_lg[:]), reads=["ps_lg"], writes=["lg"])
                    S.op("vector", lambda e: e.max(mx8[:], lg[:]), reads=["lg"], writes=["mx8"])
                    S.op("vector", lambda e: e.tensor_scalar(msk[:], lg[:], mx8[:, 3:4], None, ALU.is_ge), reads=["lg", "mx8"], writes=["msk"])
                    S.op("vector", lambda e: e.tensor_scalar(nmx[:], mx8[:, 0:1], -1.0, None, ALU.mult), reads=["mx8"], writes=["nmx"])
                    S.op("scalar", lambda e: e.activation(ex[:], lg[:], AF.Exp, bias=nmx[:, 0:1]), reads=["lg", "nmx"], writes=["ex"])
                    S.op("vector", lambda e: e.tensor_tensor(ex[:], ex[:], msk[:], ALU.mult), reads=["ex", "msk"], writes=["ex"])
                    S.op("vector", lambda e: e.tensor_reduce(s3[:], ex[:], AX.X, ALU.add), reads=["ex"], writes=["s3"])
                    S.op("vector", lambda e: e.reciprocal(s3[:], s3[:]), reads=["s3"], writes=["s3"])
                    S.op("vector", lambda e, tt=tt: e.tensor_scalar(G_all[:, tt, :], ex[:], s3[:, 0:1], None, ALU.mult),
                         reads=["ex", "s3"], writes=["G_all"])
                    S.op("tensor", lambda e, tt=tt: e.transpose(ps_gt[:], G_all[:, tt, :], ident_f[:]),
                         reads=["G_all", "ident_f"], writes=["ps_gt"])
                    S.op("vector", lambda e: e.tensor_copy(GT[:], ps_gt[:]), reads=["ps_gt"], writes=["GT"])
                    for half in range(2):
                        S.op("tensor", lambda e, half=half: e.matmul(
                            ps_mix[:, half * 512:(half + 1) * 512], GT[:, :], bd_sb[:, half * 512:(half + 1) * 512], start=True, stop=True),
                            reads=["GT", "bd_sb"], writes=["ps_mix"])
                    for half in range(2):
                        hs = slice(half * 512, (half + 1) * 512)
                        S.op("scalar", lambda e, hs=hs, tt=tt: e.copy(y_acc[:, tt, hs], ps_mix[:, hs]),
                             reads=["ps_mix"], writes=["y_acc"])
                if dbg and "D" not in stages:
                    o = dbg_out("G_all", [128, NT, NEXP])
                    S.dma("sync", o, G_all[:], reads=["G_all"], writes=["dbg_G"])
                    o = dbg_out("h2T", [128, 8, NT * 128], BF16)
                    S.dma("sync", o, h2T_all[:], reads=["h2T_all"], writes=["dbg_h"])
                    o = dbg_out("y_acc", [128, NT, D])
                    S.dma("sync", o, y_acc[:], reads=["y_acc"], writes=["dbg_ya"])
                S.flush()
                if dbg and "D" not in stages:
                    o = dbg_out("x1", [NT, 128, D])
                    for tt in list(range(n_tc - 1)) + [NT - 1]:
                        S.dma("sync", o[tt], x1_scr[tt], reads=["x1_scr"], writes=["dbg_x1"])
                    S.flush()


        if "D" in stages:
            bgu = sb(es_all, "bgu", [128, 8, 2, NEXP])
            with ExitStack() as es:
                bgu_raw = sb(es, "bgu_raw", [NEXP, 2 * D])
                ps_b = ps(es, "ps_b", [128, 512])
                S.dma("sync", bgu_raw[:], b_gu[:, :], writes=["bgu_raw"])
                braw = bgu_raw[:].rearrange("e (f j two) -> e f two j", f=8, j=128, two=2)
                for f in range(8):
                    for two in range(2):
                        S.op("tensor", lambda e, f=f, two=two: e.transpose(
                            ps_b[:, (2 * f + two) * NEXP:(2 * f + two + 1) * NEXP], braw[:, f, two, :], ident_f[0:NEXP, 0:NEXP]),
                            reads=["bgu_raw", "ident_f"], writes=["ps_b"])
                S.op("vector", lambda e: e.tensor_copy(bgu[:].rearrange("p f two e -> p (f two e)"), ps_b[:, 0:16 * NEXP]),
                     reads=["ps_b"], writes=["bgu"])
                S.op("vector", lambda e: e.tensor_scalar(bgu[:, :, 1, :], bgu[:, :, 1, :], 1.0 / 1.702, None, ALU.mult),
                     reads=["bgu"], writes=["bgu"])
                S.flush()
            with ExitStack() as es:
                actT = sb(es, "actT", [128, 8, NT * 128], BF16)
                stg = [sb(es, "stg0", [128, 8, 256]), sb(es, "stg1", [128, 8, 256])]
                Ugu = [sb(es, "Ugu0", [128, 8, 2, 128], BF16), sb(es, "Ugu1", [128, 8, 2, 128], BF16)]
                stw = [sb(es, "stw0", [128, D]), sb(es, "stw1", [128, D])]
                Wd = sb(es, "Wd", [128, 8, D], BF16)
                gc = [sb(es, "gc0", [128, 512]), sb(es, "gc1", [128, 512])]
                sig = [sb(es, "sig0", [128, 512]), sb(es, "sig1", [128, 512])]
                uu = [sb(es, "uu0", [128, 512]), sb(es, "uu1", [128, 512])]
                ps_g = [ps(es, "ps_g0", [128, 512]), ps(es, "ps_g1", [128, 512])]
                ps_u = [ps(es, "ps_u0", [128, 512]), ps(es, "ps_u1", [128, 512])]
                ps_d = [ps(es, "ps_d0", [128, D]), ps(es, "ps_d1", [128, D])]

                wgu_v = w_gu.rearrange("e (k p) c -> e p k c", p=128)
                wd_v = w_down.rearrange("e (f p) c -> e p f c", p=128)
                groups = [(0, 512), (512, 512), (1024, 512), (1536, 512), (2048, 128)]
                n_units = 8 * n_exp

                def load_gu(u):
                    e_, f = u // 8, u % 8
                    b = u % 2
                    S.dma("sync", stg[b][:], wgu_v[e_, :, :, 256 * f:256 * (f + 1)], writes=["stg%d" % b])

                def cast_gu(u):
                    b = u % 2
                    S.op("scalar", lambda e, b=b: e.copy(
                        Ugu[b][:], stg[b][:].rearrange("p k (j two) -> p k two j", two=2)),
                        reads=["stg%d" % b], writes=["Ugu%d" % b])

                def load_wd(e_, q):
                    b = q % 2
                    S.dma("sync", stw[b][:], wd_v[e_, :, q, :], writes=["stw%d" % b])

                def cast_wd(e_, q):
                    b = q % 2
                    S.op("scalar", lambda e, b=b, q=q: e.copy(Wd[:, q, :], stw[b][:]),
                         reads=["stw%d" % b], writes=["Wd"])

                load_gu(0)
                load_gu(1)
                cast_gu(0)
                cnt = 0
                dcnt = 0
                for e_ in range(n_exp):
                    for f in range(8):
                        u = 8 * e_ + f
                        b = u % 2
                        if u + 1 < n_units:
                            cast_gu(u + 1)
                        if f in (6, 7):
                            load_wd(e_, f - 6)
                        for (t0, n) in groups:
                            i = cnt % 2
                            cnt += 1
                            pg, pu = ps_g[i], ps_u[i]
                            for k in range(8):
                                S.op("tensor", lambda e, k=k, b=b, pg=pg, t0=t0, n=n: e.matmul(
                                    pg[:, 0:n], Ugu[b][:, k, 0, :], h2T_all[:, k, t0:t0 + n], start=(k == 0), stop=(k == 7)),
                                    reads=["Ugu%d" % b, "h2T_all"], writes=["ps_g%d" % i])
                            for k in range(8):
                                S.op("tensor", lambda e, k=k, b=b, pu=pu, t0=t0, n=n: e.matmul(
                                    pu[:, 0:n], Ugu[b][:, k, 1, :], h2T_all[:, k, t0:t0 + n], start=(k == 0), stop=(k == 7)),
                                    reads=["Ugu%d" % b, "h2T_all"], writes=["ps_u%d" % i])
                            S.op("scalar", lambda e, pu=pu, n=n, f=f, e_=e_, i=i: e.activation(
                                uu[i][:, 0:n], pu[:, 0:n], AF.Identity, bias=bgu[:, f, 1, e_:e_ + 1], scale=1.0 / 1.702),
                                reads=["ps_u%d" % i, "bgu"], writes=["uu%d" % i])
                            S.op("vector", lambda e, pg=pg, n=n, f=f, e_=e_, i=i: e.tensor_scalar(
                                gc[i][:, 0:n], pg[:, 0:n], bgu[:, f, 0, e_:e_ + 1], 7.0, ALU.add, ALU.min),
                                reads=["ps_g%d" % i, "bgu"], writes=["gc%d" % i])
                            S.op("scalar", lambda e, n=n, i=i: e.activation(sig[i][:, 0:n], gc[i][:, 0:n], AF.Silu, scale=1.702),
                                 reads=["gc%d" % i], writes=["sig%d" % i])
                            S.op("vector", lambda e, n=n, i=i: e.tensor_scalar(
                                uu[i][:, 0:n], uu[i][:, 0:n], 7.0 / 1.702, -7.0 / 1.702, ALU.min, ALU.max),
                                reads=["uu%d" % i], writes=["uu%d" % i])
                            S.op("vector", lambda e, n=n, f=f, t0=t0, i=i: e.scalar_tensor_tensor(
                                actT[:, f, t0:t0 + n], uu[i][:, 0:n], 1.0 / 1.702, sig[i][:, 0:n], ALU.add, ALU.mult),
                                reads=["sig%d" % i, "uu%d" % i], writes=["actT"])
                        if u + 2 < n_units:
                            load_gu(u + 2)
                    for q in range(8):
                        cast_wd(e_, q)
                        if q + 2 < 8:
                            load_wd(e_, q + 2)
                    for tt in range(NT):
                        i = dcnt % 2
                        dcnt += 1
                        pd = ps_d[i]
                        for half in range(2):
                            for f in range(8):
                                S.op("tensor", lambda e, f=f, half=half, tt=tt, pd=pd: e.matmul(
                                    pd[:, half * 512:(half + 1) * 512], actT[:, f, tt * 128:(tt + 1) * 128],
                                    Wd[:, f, half * 512:(half + 1) * 512], start=(f == 0), stop=(f == 7)),
                                    reads=["actT", "Wd"], writes=["ps_d%d" % i])
                        for half in range(2):
                            hs = slice(half * 512, (half + 1) * 512)
                            S.op("vector", lambda e, hs=hs, tt=tt, pd=pd, e_=e_: e.scalar_tensor_tensor(
                                y_acc[:, tt, hs], pd[:, hs], G_all[:, tt, e_:e_ + 1], y_acc[:, tt, hs], ALU.mult, ALU.add),
                                reads=["ps_d%d" % i, "G_all", "y_acc"], writes=["y_acc"])
                if dbg and "E" not in stages:
                    o = dbg_out("y_acc2", [128, NT, D])
                    S.dma("sync", o, y_acc[:], reads=["y_acc"], writes=["dbg_ya2"])
                S.flush()

        if "E" in stages:
            with ExitStack() as es:
                GF = [sb(es, "GF0", [128, D]), sb(es, "GF1", [128, D])]
                x1b = [sb(es, "x1b0", [128, D]), sb(es, "x1b1", [128, D])]
                junke = sb(es, "junke", [128, D], BF16)
                sse = sb(es, "sse", [128, 1])
                rse = sb(es, "rse", [128, 1])
                te = [sb(es, "te0", [128, D]), sb(es, "te1", [128, D])]
                S.dma("sync", GF[0][:], gtab_scr[2], writes=["GF0"])
                S.dma("sync", GF[1][:], gtab_scr[3], writes=["GF1"])
                for tt in range(NT):
                    smp = (tt == NT - 1)
                    i = tt % 2
                    gf, gfn = (GF[1], "GF1") if smp else (GF[0], "GF0")
                    S.dma("sync", x1b[i][:], x1_scr[tt], writes=["x1b%d" % i])
                    S.op("scalar", lambda e, tt=tt: e.activation(junke[:], y_acc[:, tt, :], AF.Square, accum_out=sse[:]),
                         reads=["y_acc"], writes=["junke", "sse"])
                    S.op("vector", lambda e: e.tensor_scalar(rse[:], sse[:], 1.0 / D, EPS, ALU.mult, ALU.add), reads=["sse"], writes=["rse"])
                    S.op("scalar", lambda e: e.activation(rse[:], rse[:], AF.Sqrt), reads=["rse"], writes=["rse"])
                    S.op("vector", lambda e: e.reciprocal(rse[:], rse[:]), reads=["rse"], writes=["rse"])
                    S.op("vector", lambda e, tt=tt, i=i, gf=gf: e.scalar_tensor_tensor(
                        te[i][:], y_acc[:, tt, :], rse[:, 0:1], gf[:], ALU.mult, ALU.mult),
                        reads=["y_acc", "rse", gfn], writes=["te%d" % i])
                    S.op("vector", lambda e, i=i: e.tensor_tensor(te[i][:], te[i][:], x1b[i][:], ALU.add),
                         reads=["te%d" % i, "x1b%d" % i], writes=["te%d" % i])
                    if smp:
                        S.dma("sync", ys[:, :], te[i][:], reads=["te%d" % i], writes=["ys"])
                    else:
                        S.dma("sync", yp[tt * 128:(tt + 1) * 128, :], te[i][:], reads=["te%d" % i], writes=["yp"])
                S.flush()

        if "E" not in stages:
            S.dma("sync", yp[:, :], xp[PRE_T * 128:(PRE_T + OWN_T) * 128, :], writes=["yp"])
            S.dma("sync", ys[:, :], xs[:, :], writes=["ys"])
            S.flush()
    return nc, dbg_outs


_CONST = {}


def _consts():
    if _CONST:
        return _CONST
    sel = np.zeros((2, 17, 128), np.float32)
    sel[0, 0, :] = 1.0
    for p in range(128):
        sel[1, 1 + p // TSEQ, p] = 1.0
    tp = _ret_tables(128)
    ts = _ret_tables(TSEQ)
    _CONST.update(
        c_ident=np.eye(128, dtype=np.float32),
        c_sel=sel,
        c_dm=np.stack([tp[0], ts[0]]),
        c_kd=np.stack([tp[1], ts[1]]),
        c_qd=np.stack([tp[2], ts[2]]),
        c_gd=np.stack([tp[3], ts[3]]),
        c_toep=_toeplitz_prompt(),
        c_sbias=_sample_bias(),
    )
    return _CONST


def prep_core_inputs(inp, c):
    b, qtr = c // 4, c % 4
    T0 = 2048 * qtr
    f = np.float32
    xpad = np.zeros(((PRE_T + OWN_T) * 128, D), f)
    lo = T0 - PRE_T * 128
    src_lo = max(lo, 0)
    xpad[src_lo - lo:] = inp["x_prompt"][b, src_lo:T0 + 2048]
    pvalid = np.zeros((128, PRE_T), f)
    for t in range(PRE_T):
        if lo + 128 * t >= 0:
            pvalid[:, t] = 1.0
    sl = slice(NSEQ * c, NSEQ * (c + 1))
    xs = inp["x_sample"][sl]
    xs_pad = np.zeros((NSEQ, 128, D), f)
    xs_pad[:, :TSEQ] = xs
    cvec = np.concatenate([inp["c_prompt"][b:b + 1], inp["c_sample"][sl]], axis=0)
    tr8 = lambda g: np.ascontiguousarray(g.reshape(8, 128).T)
    bc = lambda g, n: np.ascontiguousarray(np.broadcast_to(g.reshape(1, -1), (128, n)))
    m = dict(
        xp=xpad, pvalid=pvalid, xs_pad=xs_pad.reshape(NSEQ * 128, D), xs=np.ascontiguousarray(xs.reshape(128, D)),
        cvec=np.ascontiguousarray(cvec),
        state_in=np.ascontiguousarray(inp["state_ret"][0, sl]),
        ck=np.ascontiguousarray(inp["cache_win_k"][0, sl].reshape(NSEQ, 2048, 512)),
        cv=np.ascontiguousarray(inp["cache_win_v"][0, sl].reshape(NSEQ, 2048, 512)),
        w_ada=inp["w_ada"][0], b_ada=inp["b_ada"],
        g1T=tr8(inp["g_pre_mix"][0]), g3T=tr8(inp["g_pre_ffn"][0]),
        g2b=bc(inp["g_post_mix"][0], D), g4b=bc(inp["g_post_ffn"][0], D), gretb=bc(inp["g_ret"][0], 512),
        w_in=inp["w_in"][0], w_out=inp["w_out"][0], w_router=inp["w_router"][0], b_router=inp["b_router"],
        w_gu=inp["w_gate_up"][0], b_gu=inp["b_gate_up"][0], w_down=inp["w_down"][0], b_down=inp["b_down"][0],
    )
    m.update(_consts())
    return {k: np.ascontiguousarray(v, dtype=np.float32) for k, v in m.items()}


STAGES = "0ABCDE"


def kernel(**inputs):
    inp = {k: np.asarray(v) for k, v in inputs.items()}
    cfg = dict(stages=STAGES)
    nc, _ = build_nc(cfg)
    in_maps = []
    for c in range(NCORE):
        m = prep_core_inputs(inp, c)
        if "D" not in STAGES:
            m["w_gu"] = m["w_gu"][:1]
            m["w_down"] = m["w_down"][:1]
        if "B" not in STAGES:
            m["ck"] = m["ck"][:1]
            m["cv"] = m["cv"][:1]
        in_maps.append(m)
    res = run_bass_kernel_spmd(nc, in_maps, core_ids=list(range(NCORE)))
    r = res.results
    f = np.float32
    y_prompt = np.zeros((2, 8192, D), f)
    y_sample = np.zeros((128, TSEQ, D), f)
    ret_p = np.zeros((1, 2, NH, E, E), f)
    ret_s = np.zeros((1, 128, NH, E, E), f)
    wk_p = np.zeros((1, 2, 2048, NH, E), f)
    wv_p = np.zeros((1, 2, 2048, NH, E), f)
    k_s = np.zeros((1, 128, TSEQ, NH, E), f)
    v_s = np.zeros((1, 128, TSEQ, NH, E), f)
    for c in range(NCORE):
        b, qtr = c // 4, c % 4
        y_prompt[b, 2048 * qtr:2048 * (qtr + 1)] = r[c]["yp"]
        y_sample[NSEQ * c:NSEQ * (c + 1)] = r[c]["ys"].reshape(NSEQ, TSEQ, D)
        ret_s[0, NSEQ * c:NSEQ * (c + 1)] = r[c]["rs"]
        k_s[0, NSEQ * c:NSEQ * (c + 1)] = r[c]["sk"].reshape(NSEQ, TSEQ, NH, E)
        v_s[0, NSEQ * c:NSEQ * (c + 1)] = r[c]["sv"].reshape(NSEQ, TSEQ, NH, E)
        if qtr == 3:
            ret_p[0, b] = r[c]["rp"]
            wk_p[0, b] = r[c]["wk"].reshape(2048, NH, E)
            wv_p[0, b] = r[c]["wv"].reshape(2048, NH, E)
    return (y_prompt, y_sample, ret_p, ret_s, wk_p, wv_p, k_s, v_s)
```

```python
import math
from contextlib import ExitStack

import numpy as np
import concourse.bass as bass
import concourse.mybir as mybir
from concourse.bass_utils import run_bass_kernel_spmd

F32 = mybir.dt.float32
BF16 = mybir.dt.bfloat16
AF = mybir.ActivationFunctionType
ALU = mybir.AluOpType
AX = mybir.AxisListType

D = 1024
NH = 8
E = 64
DIN = 3584
NEXP = 32
EPS = 1e-6
NCORE = 8
OWN_T = 16
PRE_T = 48
SPAN_T = 32
NSEQ = 16
TSEQ = 8
NT = OWN_T + 1
TOEP_W = 384 + 128 * 16 + 512

ENGINES = ("tensor", "vector", "scalar", "gpsimd", "sync")
DMA_K = 6


class Sched:
    def __init__(self, nc, es):
        self.nc = nc
        self.q = {e: [] for e in ENGINES}
        self.cnt = {}
        self.seen = {}
        self.lastw = {}
        self.readers = {}
        self.sems = {}
        self.final = {}
        self.es = es
        self.n_ops = 0

    def _sem(self, name):
        if name not in self.sems:
            self.sems[name] = self.es.enter_context(self.nc.semaphore(name))
        return self.sems[name]

    def _new_token(self, eng, is_dma):
        if is_dma:
            st = "d_" + eng
            i = self.cnt.get(st, 0)
            self.cnt[st] = i + 1
            return ("%s%d" % (st, i % DMA_K), 16 * (i // DMA_K + 1)), i
        st = "c_" + eng
        i = self.cnt.get(st, 0) + 1
        self.cnt[st] = i
        return (st, i), i

    def _emit(self, eng, fn, reads, writes, is_dma):
        deps = {}

        def add(tok):
            if tok is None:
                return
            s, v = tok
            if deps.get(s, 0) < v:
                deps[s] = v

        for k in reads:
            add(self.lastw.get(k))
        for k in writes:
            add(self.lastw.get(k))
            for tok in self.readers.get(k, {}).values():
                add(tok)
        tok, idx = self._new_token(eng, is_dma)
        if is_dma and idx >= DMA_K:
            add((tok[0], tok[1] - 16))
        waits = []
        for s, v in deps.items():
            if eng == "tensor" and s == "c_tensor":
                continue
            if self.seen.get((eng, s), 0) >= v:
                continue
            self.seen[(eng, s)] = v
            waits.append((s, v))
        self.q[eng].append((waits, fn, tok, 16 if is_dma else 1))
        for k in reads:
            self.readers.setdefault(k, {})[tok[0]] = tok
        for k in writes:
            self.lastw[k] = tok
            self.readers[k] = {}
        if self.final.get(tok[0], 0) < tok[1]:
            self.final[tok[0]] = tok[1]
        self.n_ops += 1
        return tok

    def op(self, eng, fn, reads=(), writes=()):
        return self._emit(eng, fn, reads, writes, False)

    def dma(self, eng, out, in_, reads=(), writes=(), **kw):
        return self._emit(eng, lambda e: e.dma_start(out=out, in_=in_, **kw), reads, writes, True)

    def flush(self):
        nc = self.nc
        final = dict(self.final)
        for e in ENGINES:
            for waits, fn, tok, inc in self.q[e]:
                self._sem(tok[0])
        with nc.Block() as block:
            def run(engname):
                def body(e):
                    for waits, fn, tok, inc in self.q[engname]:
                        for s, v in waits:
                            e.wait_ge(self.sems[s], v)
                        ins = fn(e)
                        ins.then_inc(self.sems[tok[0]], inc)
                    for s, v in final.items():
                        if self.seen.get((engname, s), 0) < v:
                            e.wait_ge(self.sems[s], v)
                            self.seen[(engname, s)] = v
                return body
            block.tensor(run("tensor"))
            block.vector(run("vector"))
            block.scalar(run("scalar"))
            block.gpsimd(run("gpsimd"))
            block.sync(run("sync"))
        self.q = {e: [] for e in ENGINES}
        self.lastw = {}
        self.readers = {}


def _gammas():
    return 1.0 - 2.0 ** (-5.0 - np.arange(NH, dtype=np.float64))


def _ret_tables(L):
    g = _gammas()
    lg = np.log(g)
    j = np.arange(128)[:, None]
    i = np.arange(128)[None, :]
    dm = np.zeros((128, NH, 128), np.float64)
    ok = (i >= j) & (i < L) & (j < L)
    for h in range(NH):
        dm[:, h, :] = np.where(ok, np.exp((i - j) * lg[h]) / 8.0, 0.0)
    kd = np.zeros((128, NH, E), np.float64)
    for h in range(NH):
        col = np.where(np.arange(128) < L, np.exp((L - 1.0 - np.arange(128)) * lg[h]) / 8.0, 0.0)
        kd[:, h, :] = col[:, None]
    qd = np.zeros((64, NH, 128), np.float64)
    gd = np.zeros((64, NH, E), np.float64)
    for h in range(NH):
        qd[:, h, :] = np.exp((np.arange(128) + 1.0) * lg[h])[None, :]
        gd[:, h, :] = np.exp(L * lg[h])
    return (dm.astype(np.float32), kd.reshape(128, NH * E).astype(np.float32),
            qd.astype(np.float32), gd.astype(np.float32))


def _alibi_logw(dist):
    dist = np.asarray(dist, np.int64)
    cnt = ((dist >= 0) & (dist <= 128)).astype(np.float64)
    cnt += ((dist >= 0) & (dist <= 512) & (dist % 4 == 0))
    cnt += ((dist >= 0) & (dist <= 2048) & (dist % 16 == 0))
    return cnt


def _bias_fn(dist, h):
    slope = 2.0 ** (-8.0 * (h + 1.0) / NH)
    cnt = _alibi_logw(dist)
    with np.errstate(divide="ignore"):
        out = np.where(cnt > 0, -slope * np.maximum(dist, 0) + np.log(np.maximum(cnt, 1e-30)), -1e30)
    return out


def _toeplitz_prompt():
    p = np.arange(128)[:, None]
    c = np.arange(TOEP_W)[None, :]
    out = np.zeros((NH, 128, TOEP_W), np.float32)
    for h in range(NH):
        out[h] = _bias_fn(c - 384 - p, h).astype(np.float32)
    return out


def _sample_bias():
    out = np.zeros((128, 17, NH, TSEQ), np.float32)
    j = np.arange(128)[:, None]
    t = np.arange(TSEQ)[None, :]
    for a in range(16):
        for h in range(NH):
            out[:, a, h, :] = _bias_fn(2048 + t - (128 * a + j), h)
    for h in range(NH):
        b = _bias_fn(t - j, h)
        b = np.where(j < TSEQ, b, -1e30)
        out[:, 16, h, :] = b
    return out


def build_nc(cfg):
    nc = bass.Bass("TRN2", target_bir_lowering=False)
    dbg = cfg.get("debug", False)
    n_pre = cfg.get("n_pre", PRE_T)
    n_exp = cfg.get("n_exp", NEXP)
    stages = cfg.get("stages", "0ABCDE")

    def din(name, shape, dt=F32):
        return nc.dram_tensor(name, list(shape), dt, kind="ExternalInput").ap()

    def dout(name, shape, dt=F32):
        return nc.dram_tensor(name, list(shape), dt, kind="ExternalOutput").ap()

    def dscr(name, shape, dt):
        return nc.dram_tensor(name, list(shape), dt).ap()

    xp = din("xp", [(PRE_T + OWN_T) * 128, D])
    pvalid = din("pvalid", [128, PRE_T])
    xs_pad = din("xs_pad", [NSEQ * 128, D])
    xs = din("xs", [128, D])
    cvec = din("cvec", [17, D])
    state_in = din("state_in", [NSEQ, NH, E, E])
    bigb = "B" in stages
    ck = din("ck", [NSEQ if bigb else 1, 2048, NH * E])
    cv = din("cv", [NSEQ if bigb else 1, 2048, NH * E])
    w_ada = din("w_ada", [D, 6 * D])
    b_ada = din("b_ada", [1, 6 * D])
    g1T = din("g1T", [128, 8])
    g3T = din("g3T", [128, 8])
    g2b = din("g2b", [128, D])
    g4b = din("g4b", [128, D])
    gretb = din("gretb", [128, 512])
    w_in = din("w_in", [D, DIN])
    w_out = din("w_out", [D, D])
    w_router = din("w_router", [D, NEXP])
    b_router = din("b_router", [1, NEXP])
    big = "D" in stages
    w_gu = din("w_gu", [NEXP if big else 1, D, 2 * D])
    b_gu = din("b_gu", [NEXP, 2 * D])
    w_down = din("w_down", [NEXP if big else 1, D, D])
    b_down = din("b_down", [NEXP, D])
    c_ident = din("c_ident", [128, 128])
    c_sel = din("c_sel", [2, 17, 128])
    c_dm = din("c_dm", [2, 128, NH, 128])
    c_kd = din("c_kd", [2, 128, 512])
    c_qd = din("c_qd", [2, 64, NH, 128])
    c_gd = din("c_gd", [2, 64, NH, E])
    c_toep = din("c_toep", [NH, 128, TOEP_W])
    c_sbias = din("c_sbias", [128, 17, NH, TSEQ])

    yp = dout("yp", [OWN_T * 128, D])
    ys = dout("ys", [128, D])
    rp = dout("rp", [NH, E, E])
    rs = dout("rs", [NSEQ, NH, E, E])
    wk = dout("wk", [OWN_T * 128, 512])
    wv = dout("wv", [OWN_T * 128, 512])
    sk = dout("sk", [128, 512])
    sv = dout("sv", [128, 512])

    kT_scr = dscr("kT_scr", [NH, 64, SPAN_T * 128], BF16)
    qT_scr = dscr("qT_scr", [NH, 64, OWN_T * 128], BF16)
    v_scr = dscr("v_scr", [SPAN_T, 128, NH * 65], BF16)
    qTs_scr = dscr("qTs_scr", [NSEQ, 64, NH, 128], BF16)
    kTs_scr = dscr("kTs_scr", [NSEQ, 64, NH, 128], BF16)
    vs_scr = dscr("vs_scr", [NSEQ, 128, NH * 65], BF16)
    y_scr = dscr("y_scr", [NT, 128, D], BF16)
    x1_scr = dscr("x1_scr", [NT, 128, D], F32)
    gtab_scr = dscr("gtab_scr", [4, 128, D], F32)

    dbg_outs = {}

    def dbg_out(name, shape, dt=F32):
        if dbg:
            dbg_outs[name] = dout("dbg_" + name, shape, dt)
            return dbg_outs[name]
        return None

    with ExitStack() as es_all:
        S = Sched(nc, es_all)

        def sb(es, name, shape, dt=F32):
            return es.enter_context(nc.sbuf_tensor(name, list(shape), dt))

        def ps(es, name, shape, dt=F32):
            return es.enter_context(nc.psum_tensor(name, list(shape), dt))

        ident_f = sb(es_all, "ident_f", [128, 128])
        ident_b = sb(es_all, "ident_b", [128, 128], BF16)
        A1 = sb(es_all, "A1", [128, 8, 17])
        B1 = sb(es_all, "B1", [128, 8, 17])
        A2 = sb(es_all, "A2", [128, 8, 17])
        B2 = sb(es_all, "B2", [128, 8, 17])
        S.dma("sync", ident_f[:], c_ident[:, :], writes=["ident_f"])
        S.op("vector", lambda e: e.tensor_copy(ident_b[:], ident_f[:]), reads=["ident_f"], writes=["ident_b"])

        if "0" in stages:
            with ExitStack() as es:
                c17 = sb(es, "c17", [17, D])
                sc17 = sb(es, "sc17", [17, D])
                scT = sb(es, "scT", [128, 8, 17])
                brow = sb(es, "brow", [1, 6 * D])
                ones1 = sb(es, "ones1", [1, 32])
                g1 = sb(es, "g1", [128, 8])
                g3 = sb(es, "g3", [128, 8])
                gpost = [sb(es, "gpost0", [128, D]), sb(es, "gpost1", [128, D])]
                sel = sb(es, "sel", [17, 2, 128])
                blk = [sb(es, "wablk0", [128, 8, D]), sb(es, "wablk1", [128, 8, D])]
                gt_tok = sb(es, "gt_tok", [17, D])
                gtab = sb(es, "gtab", [128, D])
                ps_tr = ps(es, "ps_tr0", [128, 8, 17])
                ps_m = [ps(es, "ps_m0", [128, 8, 17]), ps(es, "ps_m1", [128, 8, 17])]
                ps_g = ps(es, "ps_g", [17, D])
                ps_t = ps(es, "ps_t", [128, D])

                S.dma("sync", c17[:], cvec[:, :], writes=["c17"])
                S.dma("sync", brow[:], b_ada[:, :], writes=["brow"])
                S.dma("sync", g1[:], g1T[:, :], writes=["g1"])
                S.dma("sync", g3[:], g3T[:, :], writes=["g3"])
                S.dma("sync", gpost[0][:], g2b[:, :], writes=["gpost0"])
                S.dma("sync", gpost[1][:], g4b[:, :], writes=["gpost1"])
                S.dma("sync", sel[:], c_sel.rearrange("a s p -> s a p"), writes=["sel"])
                S.op("gpsimd", lambda e: e.memset(ones1[:], 1.0), writes=["ones1"])
                S.op("scalar", lambda e: e.activation(sc17[:], c17[:], AF.Silu), reads=["c17"], writes=["sc17"])
                for k in range(8):
                    S.op("tensor", lambda e, k=k: e.transpose(ps_tr[:, k, :], sc17[0:17, k * 128:(k + 1) * 128],
                                                              ident_f[0:17, 0:17]),
                         reads=["sc17", "ident_f"], writes=["ps_tr0"])
                S.op("vector", lambda e: e.tensor_copy(scT[:], ps_tr[:]), reads=["ps_tr0"], writes=["scT"])

                wada_v = w_ada.rearrange("(k p) c -> p k c", p=128)
                for j in range(6):
                    bk = blk[j % 2]
                    bkn = "wablk%d" % (j % 2)
                    S.dma("sync", bk[:, 0:4, :], wada_v[:, 0:4, j * D:(j + 1) * D], writes=[bkn])
                    S.dma("scalar", bk[:, 4:8, :], wada_v[:, 4:8, j * D:(j + 1) * D], writes=[bkn + "b"])
                    if j in (0, 1, 3, 4):
                        pm = ps_m[j % 2]
                        pmn = "ps_m%d" % (j % 2)
                        for cc in range(8):
                            for k in range(8):
                                S.op("tensor", lambda e, cc=cc, k=k, pm=pm, bk=bk: e.matmul(
                                    pm[:, cc, :], bk[:, k, cc * 128:(cc + 1) * 128], scT[:, k, :],
                                    start=(k == 0), stop=False),
                                    reads=[bkn, bkn + "b", "scT"], writes=[pmn])
                            S.op("tensor", lambda e, cc=cc, pm=pm, j=j: e.matmul(
                                pm[:, cc, :], brow[0:1, j * D + cc * 128:j * D + (cc + 1) * 128], ones1[0:1, 0:17],
                                start=False, stop=True),
                                reads=["brow", "ones1"], writes=[pmn])
                        if j == 0:
                            S.op("vector", lambda e, pm=pm: e.tensor_copy(B1[:], pm[:]), reads=[pmn], writes=["B1"])
                        elif j == 3:
                            S.op("vector", lambda e, pm=pm: e.tensor_copy(B2[:], pm[:]), reads=[pmn], writes=["B2"])
                        else:
                            dst, gg, gn = (A1, g1, "g1") if j == 1 else (A2, g3, "g3")
                            dn = "A1" if j == 1 else "A2"
                            for cc in range(8):
                                S.op("vector", lambda e, cc=cc, pm=pm, dst=dst, gg=gg: e.tensor_scalar(
                                    dst[:, cc, :], pm[:, cc, :], 1.0, gg[:, cc:cc + 1], ALU.add, ALU.mult),
                                    reads=[pmn, gn], writes=[dn])
                    else:
                        gi = 0 if j == 2 else 1
                        for half in range(2):
                            for k in range(8):
                                S.op("tensor", lambda e, half=half, k=k, bk=bk: e.matmul(
                                    ps_g[0:17, half * 512:(half + 1) * 512], scT[:, k, :],
                                    bk[:, k, half * 512:(half + 1) * 512], start=(k == 0), stop=False),
                                    reads=[bkn, bkn + "b", "scT"], writes=["ps_g"])
                            S.op("tensor", lambda e, half=half, j=j: e.matmul(
                                ps_g[0:17, half * 512:(half + 1) * 512], ones1[0:1, 0:17],
                                brow[0:1, j * D + half * 512:j * D + (half + 1) * 512], start=False, stop=True),
                                reads=["brow", "ones1"], writes=["ps_g"])
                        S.op("vector", lambda e: e.tensor_copy(gt_tok[:], ps_g[:]), reads=["ps_g"], writes=["gt_tok"])
                        for which in range(2):
                            for half in range(2):
                                S.op("tensor", lambda e, which=which, half=half: e.matmul(
                                    ps_t[:, half * 512:(half + 1) * 512], sel[0:17, which, :],
                                    gt_tok[0:17, half * 512:(half + 1) * 512], start=True, stop=True),
                                    reads=["sel", "gt_tok"], writes=["ps_t"])
                            S.op("vector", lambda e, gi=gi: e.tensor_tensor(gtab[:], ps_t[:], gpost[gi][:], ALU.mult),
                                 reads=["ps_t", "gpost%d" % gi], writes=["gtab"])
                            S.dma("sync", gtab_scr[gi * 2 + which], gtab[:], reads=["gtab"], writes=["gtab_scr"])
                if dbg:
                    for nm, t in (("A1", A1), ("B1", B1), ("A2", A2), ("B2", B2)):
                        o = dbg_out(nm, [128, 8, 17])
                        S.dma("sync", o, t[:], reads=[nm], writes=["dbg_" + nm])
                    o = dbg_out("gtab", [4, 128, D])
                    with ExitStack() as es2:
                        pass
                S.flush()
                if dbg:
                    pass


        if "A" in stages:
            with ExitStack() as es:
                w_in_bf = sb(es, "w_in_bf", [128, 8, DIN], BF16)
                tabs = []
                for kind in range(2):
                    tabs.append(dict(
                        dm=sb(es, "dm%d" % kind, [128, NH, 128]), kd=sb(es, "kd%d" % kind, [128, 512]),
                        qd=sb(es, "qd%d" % kind, [64, NH, 128]), gd=sb(es, "gd%d" % kind, [64, NH, E])))
                gret = sb(es, "gret", [128, 512])
                pval = sb(es, "pval", [128, PRE_T])
                ones_c = sb(es, "ones_c", [128, 1])
                xt = [sb(es, "xt0", [128, D]), sb(es, "xt1", [128, D])]
                junk = sb(es, "junk", [128, D], BF16)
                ssum = sb(es, "ssum", [128, 1])
                rstd = sb(es, "rstd", [128, 1])
                xn = sb(es, "xn", [128, D], BF16)
                tmpm = sb(es, "tmpm", [128, 8, 128])
                hT2 = [sb(es, "hT0", [128, 8, 512], BF16), sb(es, "hT1", [128, 8, 512], BF16)]
                gpar = [0]
                qrT = sb(es, "qrT", [64, NH, 512], BF16)
                krT = sb(es, "krT", [64, NH, 512], BF16)
                qaT = sb(es, "qaT", [64, NH, 512], BF16)
                kaT = sb(es, "kaT", [64, NH, 512], BF16)
                kdec = sb(es, "kdec", [128, 512], BF16)
                vr = sb(es, "vr", [128, 512], BF16)
                sg = sb(es, "sg", [128, 512])
                kaf = sb(es, "kaf", [128, 512])
                vaf = sb(es, "vaf", [128, 512])
                vext = sb(es, "vext", [128, NH, 65], BF16)
                PT = sb(es, "PT", [128, NH, 128], BF16)
                qdec = sb(es, "qdec", [64, NH, 128], BF16)
                Sp = sb(es, "Sp", [64, NH, E])
                Ss = sb(es, "Ss", [64, NH, E])
                Sbf = sb(es, "Sbf", [64, NH, E], BF16)
                osb = sb(es, "osb", [128, 512])
                cen = sb(es, "cen", [128, 512])
                sq = sb(es, "sq", [128, 512])
                st8 = sb(es, "st8", [128, 8])
                st8b = sb(es, "st8b", [128, 8])
                rety = sb(es, "rety", [128, 512], BF16)
                ps_tr = ps(es, "ps_trA", [128, 8, 128], BF16)
                ps_f = [ps(es, "ps_f0", [128, 512])]
                ps_k = [ps(es, "ps_k0", [128, 512]), ps(es, "ps_k1", [128, 512])]
                ps_A = ps(es, "ps_A", [128, NH, 128])
                ps_o = ps(es, "ps_o", [128, 512])

                w_in_v = w_in.rearrange("(k p) c -> p k c", p=128)
                for cb in range(7):
                    S.dma("gpsimd", w_in_bf[:, :, cb * 512:(cb + 1) * 512], w_in_v[:, :, cb * 512:(cb + 1) * 512],
                          writes=["w_in_bf"])
                for kind in range(2):
                    S.dma("sync", tabs[kind]["dm"][:], c_dm[kind], writes=["tabs"])
                    S.dma("sync", tabs[kind]["kd"][:], c_kd[kind], writes=["tabs"])
                    S.dma("sync", tabs[kind]["qd"][:], c_qd[kind], writes=["tabs"])
                    S.dma("sync", tabs[kind]["gd"][:], c_gd[kind], writes=["tabs"])
                S.dma("sync", gret[:], gretb[:, :], writes=["gret"])
                S.dma("sync", pval[:], pvalid[:, :], writes=["pval"])
                S.op("gpsimd", lambda e: e.memset(ones_c[:], 1.0), writes=["ones_c"])
                S.op("gpsimd", lambda e: e.memset(Sp[:], 0.0), writes=["Sp"])
                S.op("gpsimd", lambda e: e.memset(Sbf[:], 0.0), writes=["Sbf"])

                fcnt = [0]
                kcnt = [0]
                tcnt = [0]

                def norm_tile(x_src, col, t_in_grp, gp):
                    hT, hTn = hT2[gp], "hT%d" % gp
                    i = tcnt[0] % 2
                    tcnt[0] += 1
                    xb, xbn = xt[i], "xt%d" % i
                    S.dma("sync", xb[:], x_src, writes=[xbn])
                    S.op("gpsimd", lambda e: e.memset(ssum[:], 0.0), writes=["ssum"])
                    S.op("scalar", lambda e: e.activation(junk[:], xb[:], AF.Square, accum_out=ssum[:]),
                         reads=[xbn], writes=["junk", "ssum"])
                    S.op("vector", lambda e: e.tensor_scalar(rstd[:], ssum[:], 1.0 / D, EPS, ALU.mult, ALU.add),
                         reads=["ssum"], writes=["rstd"])
                    S.op("scalar", lambda e: e.activation(rstd[:], rstd[:], AF.Sqrt), reads=["rstd"], writes=["rstd"])
                    S.op("vector", lambda e: e.reciprocal(rstd[:], rstd[:]), reads=["rstd"], writes=["rstd"])
                    S.op("vector", lambda e: e.tensor_scalar(xn[:], xb[:], rstd[:, 0:1], None, ALU.mult),
                         reads=[xbn, "rstd"], writes=["xn"])
                    for k in range(8):
                        S.op("tensor", lambda e, k=k: e.transpose(ps_tr[:, k, :], xn[:, k * 128:(k + 1) * 128], ident_b[:]),
                             reads=["xn", "ident_b"], writes=["ps_trA"])
                    S.op("vector", lambda e: e.tensor_tensor(
                        tmpm[:], ps_tr[:], A1[:, :, col:col + 1].to_broadcast([128, 8, 128]), ALU.mult),
                        reads=["ps_trA", "A1"], writes=["tmpm"])
                    c0 = t_in_grp * 128
                    S.op("vector", lambda e: e.tensor_tensor(
                        hT[:, :, c0:c0 + 128], tmpm[:], B1[:, :, col:col + 1].to_broadcast([128, 8, 128]), ALU.add),
                        reads=["tmpm", "B1"], writes=[hTn])

                def feat_proj(dst, dname, col_off, ntok):
                    for p in range(4):
                        pf, pfn = ps_f[0], "ps_f0"
                        for k in range(8):
                            S.op("tensor", lambda e, k=k, p=p, pf=pf: e.matmul(
                                pf[:, 0:ntok], w_in_bf[:, k, col_off + 128 * p:col_off + 128 * (p + 1)], hT[:, k, 0:ntok],
                                start=(k == 0), stop=(k == 7)),
                                reads=["w_in_bf", "hT"], writes=[pfn])
                        eng = "scalar" if (p % 2 == 0) else "vector"
                        if eng == "scalar":
                            S.op("scalar", lambda e, p=p, pf=pf: e.copy(dst[:, p, 0:ntok], pf[:, 0:ntok]),
                                 reads=[pfn], writes=[dname])
                        else:
                            S.op("vector", lambda e, p=p, pf=pf: e.tensor_copy(dst[:, p, 0:ntok], pf[:, 0:ntok]),
                                 reads=[pfn], writes=[dname])

                pproj = [(ps_f[0], "ps_f0"), (ps_k[0], "ps_k0"), (ps_k[1], "ps_k1")]

                def next_pp():
                    i = kcnt[0] % 3
                    kcnt[0] += 1
                    return pproj[i]

                def feat_proj_h(dst, dname, col_off, ntok):
                    hT, hTn = hT2[gpar[0]], "hT%d" % gpar[0]
                    for h in range(NH):
                        pf, pfn = next_pp()
                        for k in range(8):
                            S.op("tensor", lambda e, k=k, h=h, pf=pf, hT=hT: e.matmul(
                                pf[0:64, 0:ntok], w_in_bf[:, k, col_off + 64 * h:col_off + 64 * (h + 1)], hT[:, k, 0:ntok],
                                start=(k == 0), stop=(k == 7)),
                                reads=["w_in_bf", hTn], writes=[pfn])
                        if h % 2 == 0:
                            S.op("scalar", lambda e, h=h, pf=pf: e.copy(dst[:, h, 0:ntok], pf[0:64, 0:ntok]),
                                 reads=[pfn], writes=[dname])
                        else:
                            S.op("vector", lambda e, h=h, pf=pf: e.tensor_copy(dst[:, h, 0:ntok], pf[0:64, 0:ntok]),
                                 reads=[pfn], writes=[dname])

                def tok_proj(col_off, t_in_grp):
                    pk, pkn = next_pp()
                    hT, hTn = hT2[gpar[0]], "hT%d" % gpar[0]
                    c0 = t_in_grp * 128
                    for k in range(8):
                        S.op("tensor", lambda e, k=k, pk=pk, hT=hT: e.matmul(
                            pk[:], hT[:, k, c0:c0 + 128], w_in_bf[:, k, col_off:col_off + 512],
                            start=(k == 0), stop=(k == 7)),
                            reads=["w_in_bf", hTn], writes=[pkn])
                    return pk, pkn

                def do_group(tiles):
                    ntok = 128 * len(tiles)
                    mode = tiles[0]["mode"]
                    cut = cfg.get("cut", 99)
                    if cut <= 1:
                        return
                    if mode in ("own", "smp"):
                        feat_proj_h(qrT, "qrT", 0, ntok)
                        feat_proj_h(krT, "krT", 512, ntok)
                        feat_proj_h(qaT, "qaT", 2048, ntok)
                    if mode in ("win", "own", "smp"):
                        feat_proj_h(kaT, "kaT", 2560, ntok)
                    if mode in ("win", "own"):
                        sp0 = tiles[0]["span"] * 128
                        S.dma("sync", kT_scr[:, :, sp0:sp0 + ntok].rearrange("p q t -> q p t"), kaT[:, :, 0:ntok],
                              reads=["kaT"], writes=["kT_scr"])
                    if mode == "own":
                        o0 = tiles[0]["own"] * 128
                        S.dma("sync", qT_scr[:, :, o0:o0 + ntok].rearrange("p q t -> q p t"), qaT[:, :, 0:ntok],
                              reads=["qaT"], writes=["qT_scr"])
                    if mode == "smp":
                        s = tiles[0]["seq"]
                        S.dma("sync", qTs_scr[s], qaT[:, :, 0:128], reads=["qaT"], writes=["qTs_scr"])
                        S.dma("sync", kTs_scr[s], kaT[:, :, 0:128], reads=["kaT"], writes=["kTs_scr"])
                    if cut <= 2:
                        return
                    for ti, t in enumerate(tiles):
                        do_tile(ti, t, mode, cut)

                def do_tile(ti, t, mode, cut):
                    for _once in (0,):
                        c0 = ti * 128
                        tb = tabs[t["kind"]]
                        St, Sn = t["state"]
                        vcol = t["valid"]
                        pk, pkn = tok_proj(512, ti)
                        S.op("vector", lambda e, pk=pk, tb=tb: e.tensor_tensor(kdec[:], pk[:], tb["kd"][:], ALU.mult),
                             reads=[pkn, "tabs"], writes=["kdec"])
                        pk, pkn = tok_proj(1024, ti)
                        S.op("scalar", lambda e, pk=pk: e.copy(vr[:], pk[:]), reads=[pkn], writes=["vr"])
                        if mode in ("own", "smp"):
                            if not cfg.get("skip_sg"):
                                pk, pkn = tok_proj(1536, ti)
                                S.op("scalar", lambda e, pk=pk: e.activation(sg[:], pk[:], AF.Copy if cfg.get("nosilu") else AF.Silu), reads=[pkn], writes=["sg"])
                            if not cfg.get("skip_kaf"):
                                pk, pkn = tok_proj(2560, ti)
                                S.op("scalar", lambda e, pk=pk: e.copy(kaf[:], pk[:]), reads=[pkn], writes=["kaf"])
                            if mode == "own":
                                r0 = t["own"] * 128
                                if not cfg.get("nowk"):
                                    S.dma("sync", wk[r0:r0 + 128, :], kaf[:], reads=["kaf"], writes=["wk"])
                            else:
                                s = t["seq"]
                                S.dma("sync", sk[s * TSEQ:(s + 1) * TSEQ, :], kaf[0:TSEQ, :], reads=["kaf"], writes=["sk"])
                        if mode in ("win", "own", "smp"):
                            pk, pkn = tok_proj(3072, ti)
                            if mode != "win" and not cfg.get("skip_vaf"):
                                S.op("scalar", lambda e, pk=pk: e.copy(vaf[:], pk[:]), reads=[pkn], writes=["vaf"])
                                if mode == "own":
                                    r0 = t["own"] * 128
                                    if not cfg.get("nowk"):
                                        S.dma("sync", wv[r0:r0 + 128, :], vaf[:], reads=["vaf"], writes=["wv"])
                                else:
                                    s = t["seq"]
                                    S.dma("sync", sv[s * TSEQ:(s + 1) * TSEQ, :], vaf[0:TSEQ, :], reads=["vaf"], writes=["sv"])
                            vsrc = vcol if vcol is not None else ones_c[:, 0:1]
                            vin, vinn = (pk, pkn) if mode == "win" else (vaf, "vaf")
                            S.op("vector", lambda e, vin=vin, vsrc=vsrc: e.tensor_scalar(
                                vext[:, :, 0:64], vin[:].rearrange("p (h e) -> p h e", e=64), vsrc, None, ALU.mult),
                                reads=[vinn, "pval", "ones_c"], writes=["vext"])
                            S.op("vector", lambda e, vsrc=vsrc: e.tensor_copy(
                                vext[:, :, 64:65], vsrc.unsqueeze(1).to_broadcast([128, NH, 1])),
                                reads=["pval", "ones_c"], writes=["vext"])
                            if mode == "smp":
                                S.dma("sync", vs_scr[t["seq"]], vext[:].rearrange("p h e -> p (h e)"), reads=["vext"], writes=["vs_scr"])
                            else:
                                S.dma("sync", v_scr[t["span"]], vext[:].rearrange("p h e -> p (h e)"), reads=["vext"], writes=["v_scr"])
                        if cut <= 3:
                            continue
                        if mode in ("own", "smp"):
                            for h in range(NH):
                                p_, hf = h // 2, h % 2
                                pr = slice(0, 64) if cfg.get('pr0') else slice(64 * hf, 64 * hf + 64)
                                S.op("tensor", lambda e, h=h: e.matmul(
                                    ps_A[:, h, :], krT[:, h, c0:c0 + 128], qrT[:, h, c0:c0 + 128], start=True, stop=True),
                                    reads=["krT", "qrT"], writes=["ps_A"])
                            S.op("vector", lambda e, tb=tb: e.tensor_tensor(PT[:, 0:4, :], ps_A[:, 0:4, :], tb["dm"][:, 0:4, :], ALU.mult),
                                 reads=["ps_A", "tabs"], writes=["PT"])
                            S.op("vector", lambda e, tb=tb: e.tensor_tensor(PT[:, 4:8, :], ps_A[:, 4:8, :], tb["dm"][:, 4:8, :], ALU.mult),
                                 reads=["ps_A", "tabs"], writes=["PT"])
                            S.op("vector", lambda e, tb=tb: e.tensor_tensor(qdec[:], qrT[:, :, c0:c0 + 128], tb["qd"][:], ALU.mult),
                                 reads=["qrT", "tabs"], writes=["qdec"])
                            for h in range(NH):
                                p_, hf = h // 2, h % 2
                                pr = slice(0, 64) if cfg.get('pr0') else slice(64 * hf, 64 * hf + 64)
                                S.op("tensor", lambda e, h=h: e.matmul(
                                    ps_o[:, 64 * h:64 * h + 64], PT[:, h, :], vr[:, 64 * h:64 * h + 64], start=True, stop=False),
                                    reads=["PT", "vr"], writes=["ps_o"])
                                S.op("tensor", lambda e, h=h: e.matmul(
                                    ps_o[:, 64 * h:64 * h + 64], qdec[:, h, :], Sbf[:, h, :], start=False, stop=True),
                                    reads=["qdec", "Sbf"], writes=["ps_o"])
                        if cut <= 4:
                            continue
                        for h in range(NH):
                            S.op("tensor", lambda e, h=h: e.matmul(
                                ps_S[:, h, :], kdec[:, 64 * h:64 * h + 64], vr[:, 64 * h:64 * h + 64],
                                start=True, stop=True),
                                reads=["kdec", "vr"], writes=["ps_S"])
                        S.op("vector", lambda e, St=St, tb=tb: e.tensor_tensor(St[:], St[:], tb["gd"][:], ALU.mult),
                             reads=[Sn, "tabs"], writes=[Sn])
                        vsrc = vcol if vcol is not None else ones_c[:, 0:1]
                        S.op("vector", lambda e, St=St, vsrc=vsrc: e.scalar_tensor_tensor(
                            St[:], ps_S[:], vsrc[0:64, :], St[:], ALU.mult, ALU.add),
                            reads=["ps_S", Sn, "pval", "ones_c"], writes=[Sn])
                        S.op("vector", lambda e, St=St: e.tensor_copy(Sbf[:], St[:]), reads=[Sn], writes=["Sbf"])
                        if cut <= 5:
                            continue
                        if mode in ("own", "smp"):
                            o3 = lambda t_: t_[:].rearrange("p (h e) -> p h e", e=64)
                            S.op("scalar", lambda e: e.copy(osb[:], ps_o[:]), reads=["ps_o"], writes=["osb"])
                            S.op("vector", lambda e: e.tensor_reduce(st8[:], o3(osb), AX.X, ALU.add), reads=["osb"], writes=["st8"])
                            S.op("vector", lambda e: e.tensor_scalar(st8[:], st8[:], 1.0 / E, None, ALU.mult), reads=["st8"], writes=["st8"])
                            S.op("vector", lambda e: e.tensor_tensor(o3(cen), o3(osb), st8[:].unsqueeze(2).to_broadcast([128, NH, E]), ALU.subtract),
                                 reads=["osb", "st8"], writes=["cen"])
                            S.op("scalar", lambda e: e.activation(sq[:], cen[:], AF.Square), reads=["cen"], writes=["sq"])
                            S.op("vector", lambda e: e.tensor_reduce(st8b[:], o3(sq), AX.X, ALU.add), reads=["sq"], writes=["st8b"])
                            S.op("vector", lambda e: e.tensor_scalar(st8b[:], st8b[:], 1.0 / E, EPS, ALU.mult, ALU.add), reads=["st8b"], writes=["st8b"])
                            S.op("scalar", lambda e: e.activation(st8b[:], st8b[:], AF.Sqrt), reads=["st8b"], writes=["st8b"])
                            S.op("vector", lambda e: e.reciprocal(st8b[:], st8b[:]), reads=["st8b"], writes=["st8b"])
                            S.op("vector", lambda e: e.tensor_tensor(o3(cen), o3(cen), st8b[:].unsqueeze(2).to_broadcast([128, NH, E]), ALU.mult),
                                 reads=["cen", "st8b"], writes=["cen"])
                            S.op("vector", lambda e: e.tensor_tensor(sq[:], sg[:], gret[:], ALU.mult), reads=["sg", "gret"], writes=["sq"])
                            S.op("vector", lambda e: e.tensor_tensor(rety[:], cen[:], sq[:], ALU.mult), reads=["cen", "sq"], writes=["rety"])
                            if mode == "own":
                                S.dma("sync", y_scr[t["own"], :, 0:512], rety[:], reads=["rety"], writes=["y_scr"])
                            else:
                                s = t["seq"]
                                S.dma("sync", y_scr[OWN_T, s * TSEQ:(s + 1) * TSEQ, 0:512], rety[0:TSEQ, :], reads=["rety"], writes=["y_scr"])

                ps_S = ps(es, "ps_S", [64, NH, E])

                pre_skip = PRE_T - n_pre
                glist = []
                for g in range(pre_skip // 4, PRE_T // 4):
                    tl = []
                    for ti in range(4):
                        t = 4 * g + ti
                        md = "win" if t >= PRE_T - 16 else "pre"
                        tl.append(dict(x=xp[t * 128:(t + 1) * 128, :], col=0, kind=0, mode=md, valid=pval[:, t:t + 1],
                                       state=(Sp, "Sp"), span=t - (PRE_T - 16)))
                    glist.append((tl, None, None))
                n_owng = cfg.get('n_own', OWN_T) // 4
                for g in range(n_owng):
                    tl = []
                    for ti in range(4):
                        t = 4 * g + ti
                        tl.append(dict(x=xp[(PRE_T + t) * 128:(PRE_T + t + 1) * 128, :], col=0, kind=0, mode="own", valid=None,
                                       state=(Sp, "Sp"), span=16 + t, own=t))
                    glist.append((tl, None, None))
                n_smp = cfg.get("n_smp", NSEQ)

                def rp_out():
                    S.dma("sync", rp.rearrange("h e f -> e h f"), Sp[:], reads=["Sp"], writes=["rp"])
                if glist:
                    glist[-1] = (glist[-1][0], None, rp_out)
                else:
                    rp_out()
                for s in range(n_smp):
                    def pro(s=s):
                        S.dma("sync", Ss[:], state_in[s].rearrange("h e f -> e h f"), writes=["Ss"])
                        S.op("vector", lambda e: e.tensor_copy(Sbf[:], Ss[:]), reads=["Ss"], writes=["Sbf"])

                    def epi(s=s):
                        S.dma("sync", rs[s].rearrange("h e f -> e h f"), Ss[:], reads=["Ss"], writes=["rs"])
                    glist.append(([dict(x=xs_pad[s * 128:(s + 1) * 128, :], col=1 + s, kind=1, mode="smp", valid=None,
                                        state=(Ss, "Ss"), seq=s)], pro, epi))

                def norm_group(gi):
                    for ti, t in enumerate(glist[gi][0]):
                        norm_tile(t["x"], t["col"], ti, gi % 2)
                if glist:
                    norm_group(0)
                for gi, (tl, pro, epi) in enumerate(glist):
                    if gi + 1 < len(glist):
                        norm_group(gi + 1)
                    gpar[0] = gi % 2
                    if pro is not None:
                        pro()
                    do_group(tl)
                    if epi is not None:
                        epi()
                S.flush()
                if dbg and "B" not in stages:
                    o = dbg_out("y_scr", [NT, 128, D], BF16)
                    S.dma("sync", o[0:OWN_T, :, 0:512], y_scr[0:OWN_T, :, 0:512], reads=["y_scr"], writes=["dbg_y"])
                    S.dma("sync", o[OWN_T, 0:TSEQ * n_smp, 0:512], y_scr[OWN_T, 0:TSEQ * n_smp, 0:512], reads=["y_scr"], writes=["dbg_y"])
                    S.flush()


        if "B" in stages:
            with ExitStack() as es:
                kTh = [sb(es, "kTh0", [64, SPAN_T * 128], BF16), sb(es, "kTh1", [64, SPAN_T * 128], BF16)]
                qTh = [sb(es, "qTh0", [64, OWN_T * 128], BF16), sb(es, "qTh1", [64, OWN_T * 128], BF16)]
                v_all = sb(es, "v_all", [128, SPAN_T, NH * 65], BF16)
                Th = [sb(es, "Th0", [128, TOEP_W]), sb(es, "Th1", [128, TOEP_W])]
                NB_ = 4
                s_sb = [sb(es, "s_sb%d" % i, [128, 512]) for i in range(NB_)]
                PTb = [sb(es, "PTb%d" % i, [128, 512], BF16) for i in range(NB_)]
                rec = sb(es, "rec", [128, 4, 1])
                attb = sb(es, "attb", [128, 4, 64], BF16)
                ps_s = [ps(es, "ps_s%d" % i, [128, 512]) for i in range(NB_)]
                ps_acc = [ps(es, "ps_acc0", [128, 4, 128]), ps(es, "ps_acc1", [128, 4, 128])]
                recb = [sb(es, "recb0", [128, 4, 1]), sb(es, "recb1", [128, 4, 1])]
                attbb = [sb(es, "attbb0", [128, 4, 64], BF16), sb(es, "attbb1", [128, 4, 64], BF16)]
                for a in range(SPAN_T):
                    S.dma("sync" if a % 2 == 0 else "scalar", v_all[:, a, :], v_scr[a], reads=["v_scr"], writes=["v_all"])
                it = 0
                pend = []
                LA = 2

                def drain(keep):
                    while len(pend) > keep:
                        pend.pop(0)()
                n_heads_b = cfg.get("n_heads_b", NH)
                for h in range(n_heads_b):
                    hb = h % 2
                    S.dma("sync", kTh[hb][:], kT_scr[h], reads=["kT_scr"], writes=["kTh%d" % hb])
                    S.dma("scalar", qTh[hb][:], qT_scr[h], reads=["qT_scr"], writes=["qTh%d" % hb])
                    S.dma("sync", Th[hb][:], c_toep[h], writes=["Th%d" % hb])
                    for G in range(OWN_T // 4):
                        acc = ps_acc[G % 2]
                        accn = "ps_acc%d" % (G % 2)
                        for a in range(4 * G, 4 * G + 20):
                            i = it % NB_
                            it += 1
                            cs = 384 + 128 * (16 + 4 * G - a)
                            S.op("tensor", lambda e, i=i, hb=hb, a=a, G=G: e.matmul(
                                ps_s[i][:], kTh[hb][:, a * 128:(a + 1) * 128], qTh[hb][:, G * 512:(G + 1) * 512],
                                start=True, stop=True),
                                reads=["kTh%d" % hb, "qTh%d" % hb], writes=["ps_s%d" % i])
                            S.op("vector", lambda e, i=i, hb=hb, cs=cs: e.scalar_tensor_tensor(
                                s_sb[i][:], ps_s[i][:], 0.125, Th[hb][:, cs:cs + 512], ALU.mult, ALU.add),
                                reads=["ps_s%d" % i, "Th%d" % hb], writes=["s_sb%d" % i])
                            S.op("scalar", lambda e, i=i: e.activation(PTb[i][:], s_sb[i][:], AF.Exp),
                                 reads=["s_sb%d" % i], writes=["PTb%d" % i])
                            def pv(i=i, a=a, h=h, G=G, acc=acc, accn=accn):
                                for qi in range(4):
                                    first, last = 4 * G + qi, 16 + 4 * G + qi
                                    if a < first or a > last:
                                        continue
                                    S.op("tensor", lambda e, i=i, qi=qi, a=a, h=h, acc=acc, first=first, last=last: e.matmul(
                                        acc[:, qi, 0:65], PTb[i][:, qi * 128:(qi + 1) * 128], v_all[:, a, h * 65:(h + 1) * 65],
                                        start=(a == first), stop=(a == last)),
                                        reads=["PTb%d" % i, "v_all"], writes=[accn])
                            pend.append(pv)
                            drain(LA)
                        gb = G % 2
                        def epi(acc=acc, accn=accn, gb=gb, G=G, h=h):
                            S.op("vector", lambda e, acc=acc, gb=gb: e.reciprocal(recb[gb][:], acc[:, :, 64:65]), reads=[accn], writes=["recb%d" % gb])
                            S.op("vector", lambda e, acc=acc, gb=gb: e.tensor_tensor(
                                attbb[gb][:], acc[:, :, 0:64], recb[gb][:].to_broadcast([128, 4, 64]), ALU.mult),
                                reads=[accn, "recb%d" % gb], writes=["attbb%d" % gb])
                            S.dma("scalar", y_scr[4 * G:4 * G + 4, :, 512 + 64 * h:512 + 64 * h + 64].rearrange("t p e -> p t e"),
                                  attbb[gb][:], reads=["attbb%d" % gb], writes=["y_scr"])
                        pend.append(epi)
                drain(0)
                S.flush()
                if dbg and "C" not in stages:
                    o = dbg_out("y_att", [OWN_T, 128, 512], BF16)
                    S.dma("sync", o[:, :, 0:64 * n_heads_b], y_scr[0:OWN_T, :, 512:512 + 64 * n_heads_b], reads=["y_scr"], writes=["dbg_y"])
                    S.flush()


        if "B" in stages:
            with ExitStack() as es:
                sbias = sb(es, "sbias", [128, 17, NH * TSEQ])
                qTs = sb(es, "qTs", [64, NH, 128], BF16)
                kTs = sb(es, "kTs", [64, NH, 128], BF16)
                vs = sb(es, "vs", [128, NH * 65], BF16)
                ckb = [sb(es, "ckb0", [128, 4, 512]), sb(es, "ckb1", [128, 4, 512])]
                cvb = [sb(es, "cvb0", [128, 4, 512]), sb(es, "cvb1", [128, 4, 512])]
                kcT = [sb(es, "kcT0", [64, NH, 128], BF16), sb(es, "kcT1", [64, NH, 128], BF16)]
                vx = [sb(es, "vx0", [128, 4, NH, 65], BF16), sb(es, "vx1", [128, 4, NH, 65], BF16)]
                s4 = sb(es, "s4", [128, 4, NH * TSEQ])
                P4 = [sb(es, "P40", [128, 4, NH * TSEQ], BF16), sb(es, "P41", [128, 4, NH * TSEQ], BF16)]
                recs = sb(es, "recs", [TSEQ, NH, 1])
                atts = sb(es, "atts", [TSEQ, NH, 64], BF16)
                ps_kT = [ps(es, "ps_kT0", [64, 4, 128]), ps(es, "ps_kT1", [64, 4, 128])]
                ps_s4 = [ps(es, "ps_s40", [128, 4, NH * TSEQ]), ps(es, "ps_s41", [128, 4, NH * TSEQ])]
                ps_as = ps(es, "ps_as", [TSEQ, NH, 128])
                S.dma("sync", sbias[:], c_sbias.rearrange("p a h t -> p a (h t)"), writes=["sbias"])
                for i in range(2):
                    S.op("gpsimd", lambda e, i=i: e.memset(vx[i][:], 1.0), writes=["vx%d" % i])
                n_smp_b = 0 if cfg.get("skip_bs") else cfg.get("n_smp", NSEQ)
                ci = 0
                kti = 0
                pend_s = []

                def drain_s(keep):
                    while len(pend_s) > keep:
                        pend_s.pop(0)()
                for s in range(n_smp_b):
                    drain_s(0)
                    S.dma("sync", qTs[:], qTs_scr[s], reads=["qTs_scr"], writes=["qTs"])
                    S.dma("sync", kTs[:], kTs_scr[s], reads=["kTs_scr"], writes=["kTs"])
                    S.dma("sync", vs[:], vs_scr[s], reads=["vs_scr"], writes=["vs"])
                    for ch in range(5):
                        b = ci % 2
                        ci += 1
                        ntile = 4 if ch < 4 else 1
                        if ch < 4:
                            S.dma("sync", ckb[b][:], ck[s, 512 * ch:512 * (ch + 1), :].rearrange("(a p) c -> p a c", p=128),
                                  writes=["ckb%d" % b])
                            S.dma("scalar", cvb[b][:], cv[s, 512 * ch:512 * (ch + 1), :].rearrange("(a p) c -> p a c", p=128),
                                  writes=["cvb%d" % b])
                            S.op("scalar", lambda e, b=b: e.copy(vx[b][:, :, :, 0:64], cvb[b][:].rearrange("p a (h e) -> p a h e", e=64)),
                                 reads=["cvb%d" % b], writes=["vx%d" % b])
                        pss = ps_s4[b]
                        pssn = "ps_s4%d" % b
                        for j in range(ntile):
                            if ch < 4:
                                kb = kti % 2
                                kti += 1
                                for hh in range(2):
                                    pk_ = ps_kT[hh]
                                    for h4 in range(4):
                                        h = 4 * hh + h4
                                        S.op("tensor", lambda e, b=b, j=j, h=h, h4=h4, pk_=pk_: e.transpose(
                                            pk_[:, h4, :], ckb[b][:, j, 64 * h:64 * h + 64], ident_f[:]),
                                            reads=["ckb%d" % b, "ident_f"], writes=["ps_kT%d" % hh])
                                    if hh == 0:
                                        S.op("vector", lambda e, kb=kb, pk_=pk_: e.tensor_copy(kcT[kb][:, 0:4, :], pk_[:]),
                                             reads=["ps_kT0"], writes=["kcT%d" % kb])
                                    else:
                                        S.op("scalar", lambda e, kb=kb, pk_=pk_: e.copy(kcT[kb][:, 4:8, :], pk_[:]),
                                             reads=["ps_kT1"], writes=["kcT%d" % kb])
                                ksrc, ksn = kcT[kb], "kcT%d" % kb
                            else:
                                ksrc, ksn = kTs, "kTs"
                            for h in range(NH):
                                S.op("tensor", lambda e, j=j, h=h, ksrc=ksrc, pss=pss: e.matmul(
                                    pss[:, j, h * TSEQ:(h + 1) * TSEQ], ksrc[:, h, :], qTs[:, h, 0:TSEQ], start=True, stop=True),
                                    reads=[ksn, "qTs"], writes=[pssn])
                        a0 = 4 * ch
                        S.op("vector", lambda e, pss=pss, a0=a0, ntile=ntile: e.scalar_tensor_tensor(
                            s4[:, 0:ntile, :], pss[:, 0:ntile, :], 0.125, sbias[:, a0:a0 + ntile, :], ALU.mult, ALU.add),
                            reads=[pssn, "sbias"], writes=["s4"])
                        S.op("scalar", lambda e, b=b, ntile=ntile: e.activation(P4[b][:, 0:ntile, :], s4[:, 0:ntile, :], AF.Exp),
                             reads=["s4"], writes=["P4%d" % b])
                        def pv_s(b=b, ch=ch, ntile=ntile):
                            for j in range(ntile):
                                for h in range(NH):
                                    if ch < 4:
                                        rhs = vx[b][:, j, h, :]
                                        rn = "vx%d" % b
                                    else:
                                        rhs = vs[:, h * 65:(h + 1) * 65]
                                        rn = "vs"
                                    S.op("tensor", lambda e, b=b, j=j, h=h, rhs=rhs, first=(ch == 0 and j == 0), last=(ch == 4): e.matmul(
                                        ps_as[:, h, 0:65], P4[b][:, j, h * TSEQ:(h + 1) * TSEQ], rhs, start=first, stop=last),
                                        reads=["P4%d" % b, rn], writes=["ps_as"])
                        pend_s.append(pv_s)
                        drain_s(1)

                    def epi_s(s=s):
                        S.op("vector", lambda e: e.reciprocal(recs[:], ps_as[:, :, 64:65]), reads=["ps_as"], writes=["recs"])
                        S.op("vector", lambda e: e.tensor_tensor(atts[:], ps_as[:, :, 0:64], recs[:].to_broadcast([TSEQ, NH, 64]), ALU.mult),
                             reads=["ps_as", "recs"], writes=["atts"])
                        S.dma("sync", y_scr[OWN_T, s * TSEQ:(s + 1) * TSEQ, 512:1024], atts[:].rearrange("p h e -> p (h e)"),
                              reads=["atts"], writes=["y_scr"])
                    pend_s.append(epi_s)
                drain_s(0)
                S.flush()
                if dbg and "C" not in stages:
                    o = dbg_out("y_atts", [128, 512], BF16)
                    S.dma("sync", o[0:TSEQ * n_smp_b, :], y_scr[OWN_T, 0:TSEQ * n_smp_b, 512:1024], reads=["y_scr"], writes=["dbg_y"])
                    S.flush()


        if "C" in stages:
            h2T_all = sb(es_all, "h2T_all", [128, 8, NT * 128], BF16)
            G_all = sb(es_all, "G_all", [128, NT, NEXP])
            y_acc = sb(es_all, "y_acc", [128, NT, D])
            with ExitStack() as es:
                w_out_bf = sb(es, "w_out_bf", [128, 8, D], BF16)
                GA = [sb(es, "GA0", [128, D]), sb(es, "GA1", [128, D])]
                wr_f = sb(es, "wr_f", [128, 8, NEXP])
                br = sb(es, "br", [1, NEXP])
                ones_r = sb(es, "ones_r", [1, 128])
                bd_sb = sb(es, "bd_sb", [NEXP, D])
                ybf = [sb(es, "ybf0", [128, D], BF16), sb(es, "ybf1", [128, D], BF16)]
                xc = [sb(es, "xc0", [128, D]), sb(es, "xc1", [128, D])]
                yT = sb(es, "yT", [128, 8, 128], BF16)
                junkc = sb(es, "junkc", [128, D], BF16)
                ssc = sb(es, "ssc", [128, 1])
                ssc2 = sb(es, "ssc2", [128, 2])
                rsc = sb(es, "rsc", [128, 1])
                t1 = sb(es, "t1", [128, D])
                x1 = sb(es, "x1", [128, D])
                xn2 = sb(es, "xn2", [128, D])
                tmp2 = sb(es, "tmp2", [128, 8, 128])
                h2f = sb(es, "h2f", [128, 8, 128])
                lg = sb(es, "lg", [128, NEXP])
                mx8 = sb(es, "mx8", [128, 8])
                nmx = sb(es, "nmx", [128, 1])
                msk = sb(es, "msk", [128, NEXP])
                ex = sb(es, "ex", [128, NEXP])
                s3 = sb(es, "s3", [128, 1])
                GT = sb(es, "GT", [NEXP, 128])
                ps_trc = ps(es, "ps_trc", [128, 8, 128], BF16)
                ps_mix = ps(es, "ps_mix", [128, D])
                ps_tr2 = ps(es, "ps_tr2", [128, 8, 128])
                ps_lg = ps(es, "ps_lg", [128, NEXP])
                ps_gt = ps(es, "ps_gt", [NEXP, 128])

                S.dma("gpsimd", w_out_bf[:, :, 0:512], w_out.rearrange("(k p) c -> p k c", p=128)[:, :, 0:512], writes=["w_out_bf"])
                S.dma("gpsimd", w_out_bf[:, :, 512:D], w_out.rearrange("(k p) c -> p k c", p=128)[:, :, 512:D], writes=["w_out_bf"])
                S.dma("sync", GA[0][:], gtab_scr[0], writes=["GA0"])
                S.dma("sync", GA[1][:], gtab_scr[1], writes=["GA1"])
                S.dma("sync", wr_f[:], w_router.rearrange("(k p) n -> p k n", p=128), writes=["wr_f"])
                S.dma("sync", br[:], b_router[:, :], writes=["br"])
                S.dma("sync", bd_sb[:], b_down[:, :], writes=["bd_sb"])
                S.op("gpsimd", lambda e: e.memset(ones_r[:], 1.0), writes=["ones_r"])
                n_tc = cfg.get("n_tc", NT)
                for tt in list(range(n_tc - 1)) + [NT - 1]:
                    smp = (tt == NT - 1)
                    i = tt % 2
                    yb, ybn = ybf[i], "ybf%d" % i
                    xb, xbn = xc[i], "xc%d" % i
                    ga, gan = (GA[1], "GA1") if smp else (GA[0], "GA0")
                    S.dma("sync", yb[:], y_scr[tt], reads=["y_scr"], writes=[ybn])
                    if smp:
                        S.dma("scalar", xb[:], xs[:, :], writes=[xbn])
                    else:
                        S.dma("scalar", xb[:], xp[(PRE_T + tt) * 128:(PRE_T + tt + 1) * 128, :], writes=[xbn])
                    for k in range(8):
                        S.op("tensor", lambda e, k=k, yb=yb: e.transpose(ps_trc[:, k, :], yb[:, k * 128:(k + 1) * 128], ident_b[:]),
                             reads=[ybn, "ident_b"], writes=["ps_trc"])
                    S.op("scalar", lambda e: e.copy(yT[:], ps_trc[:]), reads=["ps_trc"], writes=["yT"])
                    for half in range(2):
                        for k in range(8):
                            S.op("tensor", lambda e, k=k, half=half: e.matmul(
                                ps_mix[:, half * 512:(half + 1) * 512], yT[:, k, :], w_out_bf[:, k, half * 512:(half + 1) * 512],
                                start=(k == 0), stop=(k == 7)),
                                reads=["yT", "w_out_bf"], writes=["ps_mix"])
                    for half in range(2):
                        S.op("scalar", lambda e, half=half: e.activation(
                            junkc[:, half * 512:(half + 1) * 512], ps_mix[:, half * 512:(half + 1) * 512], AF.Square,
                            accum_out=ssc2[:, half:half + 1]),
                            reads=["ps_mix"], writes=["junkc", "ssc2"])
                    S.op("vector", lambda e: e.tensor_tensor(ssc[:], ssc2[:, 0:1], ssc2[:, 1:2], ALU.add), reads=["ssc2"], writes=["ssc"])
                    S.op("vector", lambda e: e.tensor_scalar(rsc[:], ssc[:], 1.0 / D, EPS, ALU.mult, ALU.add), reads=["ssc"], writes=["rsc"])
                    S.op("scalar", lambda e: e.activation(rsc[:], rsc[:], AF.Sqrt), reads=["rsc"], writes=["rsc"])
                    S.op("vector", lambda e: e.reciprocal(rsc[:], rsc[:]), reads=["rsc"], writes=["rsc"])
                    for half in range(2):
                        hs = slice(half * 512, (half + 1) * 512)
                        S.op("vector", lambda e, hs=hs, ga=ga: e.scalar_tensor_tensor(
                            t1[:, hs], ps_mix[:, hs], rsc[:, 0:1], ga[:, hs], ALU.mult, ALU.mult),
                            reads=["ps_mix", "rsc", gan], writes=["t1"])
                    S.op("vector", lambda e, xb=xb: e.tensor_tensor(x1[:], t1[:], xb[:], ALU.add), reads=["t1", xbn], writes=["x1"])
                    S.dma("sync", x1_scr[tt], x1[:], reads=["x1"], writes=["x1_scr"])
                    S.op("gpsimd", lambda e: e.memset(ssc[:], 0.0), reads=["rsc"], writes=["ssc"])
                    S.op("scalar", lambda e: e.activation(junkc[:], x1[:], AF.Square, accum_out=ssc[:]),
                         reads=["x1", "ssc"], writes=["junkc", "ssc"])
                    S.op("vector", lambda e: e.tensor_scalar(rsc[:], ssc[:], 1.0 / D, EPS, ALU.mult, ALU.add), reads=["ssc"], writes=["rsc"])
                    S.op("scalar", lambda e: e.activation(rsc[:], rsc[:], AF.Sqrt), reads=["rsc"], writes=["rsc"])
                    S.op("vector", lambda e: e.reciprocal(rsc[:], rsc[:]), reads=["rsc"], writes=["rsc"])
                    S.op("vector", lambda e: e.tensor_scalar(xn2[:], x1[:], rsc[:, 0:1], None, ALU.mult), reads=["x1", "rsc"], writes=["xn2"])
                    for k in range(8):
                        S.op("tensor", lambda e, k=k: e.transpose(ps_tr2[:, k, :], xn2[:, k * 128:(k + 1) * 128], ident_f[:]),
                             reads=["xn2", "ident_f"], writes=["ps_tr2"])
                    for hh in range(2):
                        ks = slice(4 * hh, 4 * hh + 4)
                        if smp:
                            a_b = A2[:, ks, 1:17].unsqueeze(3).to_broadcast([128, 4, NSEQ, TSEQ])
                            b_b = B2[:, ks, 1:17].unsqueeze(3).to_broadcast([128, 4, NSEQ, TSEQ])
                            v4 = lambda t_, ks=ks: t_[:, ks, :].rearrange("p k (s t) -> p k s t", t=TSEQ)
                        else:
                            a_b = A2[:, ks, 0:1].to_broadcast([128, 4, 128])
                            b_b = B2[:, ks, 0:1].to_broadcast([128, 4, 128])
                            v4 = lambda t_, ks=ks: t_[:, ks, :]
                        S.op("vector", lambda e, v4=v4, a_b=a_b: e.tensor_tensor(v4(tmp2), v4(ps_tr2), a_b, ALU.mult),
                             reads=["ps_tr2", "A2"], writes=["tmp2"])
                        S.op("vector", lambda e, v4=v4, b_b=b_b: e.tensor_tensor(v4(h2f), v4(tmp2), b_b, ALU.add),
                             reads=["tmp2", "B2"], writes=["h2f"])
                    S.op("scalar", lambda e, tt=tt: e.copy(h2T_all[:, :, tt * 128:(tt + 1) * 128], h2f[:]),
                         reads=["h2f"], writes=["h2T_all"])
                    for k in range(8):
                        S.op("tensor", lambda e, k=k: e.matmul(ps_lg[:], h2f[:, k, :], wr_f[:, k, :], start=(k == 0), stop=False),
                             reads=["h2f", "wr_f"], writes=["ps_lg"])
                    S.op("tensor", lambda e: e.matmul(ps_lg[:], ones_r[0:1, :], br[0:1, :], start=False, stop=True),
                         reads=["ones_r", "br"], writes=["ps_lg"])
                    S.op("vector", lambda e: e.tensor_copy(lg[:], ps_lg[:]), reads=["ps_lg"], writes=["lg"])
                    S.op("vector", lambda e: e.max(mx8[:], lg[:]), reads=["lg"], writes=["mx8"])
                    S.op("vector", lambda e: e.tensor_scalar(msk[:], lg[:], mx8[:, 3:4], None, ALU.is_ge), reads=["lg", "mx8"], writes=["msk"])
                    S.op("vector", lambda e: e.tensor_scalar(nmx[:], mx8[:, 0:1], -1.0, None, ALU.mult), reads=["mx8"], writes=["nmx"])
                    S.op("scalar", lambda e: e.activation(ex[:], lg[:], AF.Exp, bias=nmx[:, 0:1]), reads=["lg", "nmx"], writes=["ex"])
                    S.op("vector", lambda e: e.tensor_tensor(ex[:], ex[:], msk[:], ALU.mult), reads=["ex", "msk"], writes=["ex"])
                    S.op("vector", lambda e: e.tensor_reduce(s3[:], ex[:], AX.X, ALU.add), reads=["ex"], writes=["s3"])
                    S.op("vector", lambda e: e.reciprocal(s3[:], s3[:]), reads=["s3"], writes=["s3"])
                    S.op("vector", lambda e, tt=tt: e.tensor_scalar(G_all[:, tt, :], ex[:], s3[:, 0:1], None, ALU.mult),
                         reads=["ex", "s3"], writes=["G_all"])
                    S.op("tensor", lambda e, tt=tt: e.transpose(ps_gt[:], G_all[:, tt, :], ident_f[:]),
                         reads=["G_all", "ident_f"], writes=["ps_gt"])
                    S.op("vector", lambda e: e.tensor_copy(GT[:], ps_gt[:]), reads=["ps_gt"], writes=["GT"])
                    for half in range(2):
                        S.op("tensor", lambda e, half=half: e.matmul(
                            ps_mix[:, half * 512:(half + 1) * 512], GT[:, :], bd_sb[:, half * 512:(half + 1) * 512], start=True, stop=True),
                            reads=["GT", "bd_sb"], writes=["ps_mix"])
                    for half in range(2):
                        hs = slice(half * 512, (half + 1) * 512)
                        S.op("scalar", lambda e, hs=hs, tt=tt: e.copy(y_acc[:, tt, hs], ps_mix[:, hs]),
                             reads=["ps_mix"], writes=["y_acc"])
                if dbg and "D" not in stages:
                    o = dbg_out("G_all", [128, NT, NEXP])
                    S.dma("sync", o, G_all[:], reads=["G_all"], writes=["dbg_G"])
                    o = dbg_out("h2T", [128, 8, NT * 128], BF16)
                    S.dma("sync", o, h2T_all[:], reads=["h2T_all"], writes=["dbg_h"])
                    o = dbg_out("y_acc", [128, NT, D])
                    S.dma("sync", o, y_acc[:], reads=["y_acc"], writes=["dbg_ya"])
                S.flush()
                if dbg and "D" not in stages:
                    o = dbg_out("x1", [NT, 128, D])
                    for tt in list(range(n_tc - 1)) + [NT - 1]:
                        S.dma("sync", o[tt], x1_scr[tt], reads=["x1_scr"], writes=["dbg_x1"])
                    S.flush()


        if "D" in stages:
            bgu = sb(es_all, "bgu", [128, 8, 2, NEXP])
            with ExitStack() as es:
                bgu_raw = sb(es, "bgu_raw", [NEXP, 2 * D])
                ps_b = ps(es, "ps_b", [128, 512])
                S.dma("sync", bgu_raw[:], b_gu[:, :], writes=["bgu_raw"])
                braw = bgu_raw[:].rearrange("e (f j two) -> e f two j", f=8, j=128, two=2)
                for f in range(8):
                    for two in range(2):
                        S.op("tensor", lambda e, f=f, two=two: e.transpose(
                            ps_b[:, (2 * f + two) * NEXP:(2 * f + two + 1) * NEXP], braw[:, f, two, :], ident_f[0:NEXP, 0:NEXP]),
                            reads=["bgu_raw", "ident_f"], writes=["ps_b"])
                S.op("vector", lambda e: e.tensor_copy(bgu[:].rearrange("p f two e -> p (f two e)"), ps_b[:, 0:16 * NEXP]),
                     reads=["ps_b"], writes=["bgu"])
                S.op("vector", lambda e: e.tensor_scalar(bgu[:, :, 1, :], bgu[:, :, 1, :], 1.0 / 1.702, None, ALU.mult),
                     reads=["bgu"], writes=["bgu"])
                S.flush()
            with ExitStack() as es:
                actT = sb(es, "actT", [128, 8, NT * 128], BF16)
                stg = [sb(es, "stg0", [128, 8, 256]), sb(es, "stg1", [128, 8, 256])]
                Ugu = [sb(es, "Ugu0", [128, 8, 2, 128], BF16), sb(es, "Ugu1", [128, 8, 2, 128], BF16)]
                stw = [sb(es, "stw0", [128, D]), sb(es, "stw1", [128, D])]
                Wd = sb(es, "Wd", [128, 8, D], BF16)
                gc = [sb(es, "gc0", [128, 512]), sb(es, "gc1", [128, 512])]
                sig = [sb(es, "sig0", [128, 512]), sb(es, "sig1", [128, 512])]
                uu = [sb(es, "uu0", [128, 512]), sb(es, "uu1", [128, 512])]
                ps_g = [ps(es, "ps_g0", [128, 512]), ps(es, "ps_g1", [128, 512])]
                ps_u = [ps(es, "ps_u0", [128, 512]), ps(es, "ps_u1", [128, 512])]
                ps_d = [ps(es, "ps_d0", [128, D]), ps(es, "ps_d1", [128, D])]

                wgu_v = w_gu.rearrange("e (k p) c -> e p k c", p=128)
                wd_v = w_down.rearrange("e (f p) c -> e p f c", p=128)
                groups = [(0, 512), (512, 512), (1024, 512), (1536, 512), (2048, 128)]
                n_units = 8 * n_exp

                def load_gu(u):
                    e_, f = u // 8, u % 8
                    b = u % 2
                    S.dma("sync", stg[b][:], wgu_v[e_, :, :, 256 * f:256 * (f + 1)], writes=["stg%d" % b])

                def cast_gu(u):
                    b = u % 2
                    S.op("scalar", lambda e, b=b: e.copy(
                        Ugu[b][:], stg[b][:].rearrange("p k (j two) -> p k two j", two=2)),
                        reads=["stg%d" % b], writes=["Ugu%d" % b])

                def load_wd(e_, q):
                    b = q % 2
                    S.dma("sync", stw[b][:], wd_v[e_, :, q, :], writes=["stw%d" % b])

                def cast_wd(e_, q):
                    b = q % 2
                    S.op("scalar", lambda e, b=b, q=q: e.copy(Wd[:, q, :], stw[b][:]),
                         reads=["stw%d" % b], writes=["Wd"])

                load_gu(0)
                load_gu(1)
                cast_gu(0)
                cnt = 0
                dcnt = 0
                for e_ in range(n_exp):
                    for f in range(8):
                        u = 8 * e_ + f
                        b = u % 2
                        if u + 1 < n_units:
                            cast_gu(u + 1)
                        if f in (6, 7):
                            load_wd(e_, f - 6)
                        for (t0, n) in groups:
                            i = cnt % 2
                            cnt += 1
                            pg, pu = ps_g[i], ps_u[i]
                            for k in range(8):
                                S.op("tensor", lambda e, k=k, b=b, pg=pg, t0=t0, n=n: e.matmul(
                                    pg[:, 0:n], Ugu[b][:, k, 0, :], h2T_all[:, k, t0:t0 + n], start=(k == 0), stop=(k == 7)),
                                    reads=["Ugu%d" % b, "h2T_all"], writes=["ps_g%d" % i])
                            for k in range(8):
                                S.op("tensor", lambda e, k=k, b=b, pu=pu, t0=t0, n=n: e.matmul(
                                    pu[:, 0:n], Ugu[b][:, k, 1, :], h2T_all[:, k, t0:t0 + n], start=(k == 0), stop=(k == 7)),
                                    reads=["Ugu%d" % b, "h2T_all"], writes=["ps_u%d" % i])
                            S.op("scalar", lambda e, pu=pu, n=n, f=f, e_=e_, i=i: e.activation(
                                uu[i][:, 0:n], pu[:, 0:n], AF.Identity, bias=bgu[:, f, 1, e_:e_ + 1], scale=1.0 / 1.702),
                                reads=["ps_u%d" % i, "bgu"], writes=["uu%d" % i])
                            S.op("vector", lambda e, pg=pg, n=n, f=f, e_=e_, i=i: e.tensor_scalar(
                                gc[i][:, 0:n], pg[:, 0:n], bgu[:, f, 0, e_:e_ + 1], 7.0, ALU.add, ALU.min),
                                reads=["ps_g%d" % i, "bgu"], writes=["gc%d" % i])
                            S.op("scalar", lambda e, n=n, i=i: e.activation(sig[i][:, 0:n], gc[i][:, 0:n], AF.Silu, scale=1.702),
                                 reads=["gc%d" % i], writes=["sig%d" % i])
                            S.op("vector", lambda e, n=n, i=i: e.tensor_scalar(
                                uu[i][:, 0:n], uu[i][:, 0:n], 7.0 / 1.702, -7.0 / 1.702, ALU.min, ALU.max),
                                reads=["uu%d" % i], writes=["uu%d" % i])
                            S.op("vector", lambda e, n=n, f=f, t0=t0, i=i: e.scalar_tensor_tensor(
                                actT[:, f, t0:t0 + n], uu[i][:, 0:n], 1.0 / 1.702, sig[i][:, 0:n], ALU.add, ALU.mult),
                                reads=["sig%d" % i, "uu%d" % i], writes=["actT"])
                        if u + 2 < n_units:
                            load_gu(u + 2)
                    for q in range(8):
                        cast_wd(e_, q)
                        if q + 2 < 8:
                            load_wd(e_, q + 2)
                    for tt in range(NT):
                        i = dcnt % 2
                        dcnt += 1
                        pd = ps_d[i]
                        for half in range(2):
                            for f in range(8):
                                S.op("tensor", lambda e, f=f, half=half, tt=tt, pd=pd: e.matmul(
                                    pd[:, half * 512:(half + 1) * 512], actT[:, f, tt * 128:(tt + 1) * 128],
                                    Wd[:, f, half * 512:(half + 1) * 512], start=(f == 0), stop=(f == 7)),
                                    reads=["actT", "Wd"], writes=["ps_d%d" % i])
                        for half in range(2):
                            hs = slice(half * 512, (half + 1) * 512)
                            S.op("vector", lambda e, hs=hs, tt=tt, pd=pd, e_=e_: e.scalar_tensor_tensor(
                                y_acc[:, tt, hs], pd[:, hs], G_all[:, tt, e_:e_ + 1], y_acc[:, tt, hs], ALU.mult, ALU.add),
                                reads=["ps_d%d" % i, "G_all", "y_acc"], writes=["y_acc"])
                if dbg and "E" not in stages:
                    o = dbg_out("y_acc2", [128, NT, D])
                    S.dma("sync", o, y_acc[:], reads=["y_acc"], writes=["dbg_ya2"])
                S.flush()

        if "E" in stages:
            with ExitStack() as es:
                GF = [sb(es, "GF0", [128, D]), sb(es, "GF1", [128, D])]
                x1b = [sb(es, "x1b0", [128, D]), sb(es, "x1b1", [128, D])]
                junke = sb(es, "junke", [128, D], BF16)
                sse = sb(es, "sse", [128, 1])
                rse = sb(es, "rse", [128, 1])
                te = [sb(es, "te0", [128, D]), sb(es, "te1", [128, D])]
                S.dma("sync", GF[0][:], gtab_scr[2], writes=["GF0"])
                S.dma("sync", GF[1][:], gtab_scr[3], writes=["GF1"])
                for tt in range(NT):
                    smp = (tt == NT - 1)
                    i = tt % 2
                    gf, gfn = (GF[1], "GF1") if smp else (GF[0], "GF0")
                    S.dma("sync", x1b[i][:], x1_scr[tt], writes=["x1b%d" % i])
                    S.op("scalar", lambda e, tt=tt: e.activation(junke[:], y_acc[:, tt, :], AF.Square, accum_out=sse[:]),
                         reads=["y_acc"], writes=["junke", "sse"])
                    S.op("vector", lambda e: e.tensor_scalar(rse[:], sse[:], 1.0 / D, EPS, ALU.mult, ALU.add), reads=["sse"], writes=["rse"])
                    S.op("scalar", lambda e: e.activation(rse[:], rse[:], AF.Sqrt), reads=["rse"], writes=["rse"])
                    S.op("vector", lambda e: e.reciprocal(rse[:], rse[:]), reads=["rse"], writes=["rse"])
                    S.op("vector", lambda e, tt=tt, i=i, gf=gf: e.scalar_tensor_tensor(
                        te[i][:], y_acc[:, tt, :], rse[:, 0:1], gf[:], ALU.mult, ALU.mult),
                        reads=["y_acc", "rse", gfn], writes=["te%d" % i])
                    S.op("vector", lambda e, i=i: e.tensor_tensor(te[i][:], te[i][:], x1b[i][:], ALU.add),
                         reads=["te%d" % i, "x1b%d" % i], writes=["te%d" % i])
                    if smp:
                        S.dma("sync", ys[:, :], te[i][:], reads=["te%d" % i], writes=["ys"])
                    else:
                        S.dma("sync", yp[tt * 128:(tt + 1) * 128, :], te[i][:], reads=["te%d" % i], writes=["yp"])
                S.flush()

        if "E" not in stages:
            S.dma("sync", yp[:, :], xp[PRE_T * 128:(PRE_T + OWN_T) * 128, :], writes=["yp"])
            S.dma("sync", ys[:, :], xs[:, :], writes=["ys"])
            S.flush()
    return nc, dbg_outs


_CONST = {}


def _consts():
    if _CONST:
        return _CONST
    sel = np.zeros((2, 17, 128), np.float32)
    sel[0, 0, :] = 1.0
    for p in range(128):
        sel[1, 1 + p // TSEQ, p] = 1.0
    tp = _ret_tables(128)
    ts = _ret_tables(TSEQ)
    _CONST.update(
        c_ident=np.eye(128, dtype=np.float32),
        c_sel=sel,
        c_dm=np.stack([tp[0], ts[0]]),
        c_kd=np.stack([tp[1], ts[1]]),
        c_qd=np.stack([tp[2], ts[2]]),
        c_gd=np.stack([tp[3], ts[3]]),
        c_toep=_toeplitz_prompt(),
        c_sbias=_sample_bias(),
    )
    return _CONST


def prep_core_inputs(inp, c):
    b, qtr = c // 4, c % 4
    T0 = 2048 * qtr
    f = np.float32
    xpad = np.zeros(((PRE_T + OWN_T) * 128, D), f)
    lo = T0 - PRE_T * 128
    src_lo = max(lo, 0)
    xpad[src_lo - lo:] = inp["x_prompt"][b, src_lo:T0 + 2048]
    pvalid = np.zeros((128, PRE_T), f)
    for t in range(PRE_T):
        if lo + 128 * t >= 0:
            pvalid[:, t] = 1.0
    sl = slice(NSEQ * c, NSEQ * (c + 1))
    xs = inp["x_sample"][sl]
    xs_pad = np.zeros((NSEQ, 128, D), f)
    xs_pad[:, :TSEQ] = xs
    cvec = np.concatenate([inp["c_prompt"][b:b + 1], inp["c_sample"][sl]], axis=0)
    tr8 = lambda g: np.ascontiguousarray(g.reshape(8, 128).T)
    bc = lambda g, n: np.ascontiguousarray(np.broadcast_to(g.reshape(1, -1), (128, n)))
    m = dict(
        xp=xpad, pvalid=pvalid, xs_pad=xs_pad.reshape(NSEQ * 128, D), xs=np.ascontiguousarray(xs.reshape(128, D)),
        cvec=np.ascontiguousarray(cvec),
        state_in=np.ascontiguousarray(inp["state_ret"][0, sl]),
        ck=np.ascontiguousarray(inp["cache_win_k"][0, sl].reshape(NSEQ, 2048, 512)),
        cv=np.ascontiguousarray(inp["cache_win_v"][0, sl].reshape(NSEQ, 2048, 512)),
        w_ada=inp["w_ada"][0], b_ada=inp["b_ada"],
        g1T=tr8(inp["g_pre_mix"][0]), g3T=tr8(inp["g_pre_ffn"][0]),
        g2b=bc(inp["g_post_mix"][0], D), g4b=bc(inp["g_post_ffn"][0], D), gretb=bc(inp["g_ret"][0], 512),
        w_in=inp["w_in"][0], w_out=inp["w_out"][0], w_router=inp["w_router"][0], b_router=inp["b_router"],
        w_gu=inp["w_gate_up"][0], b_gu=inp["b_gate_up"][0], w_down=inp["w_down"][0], b_down=inp["b_down"][0],
    )
    m.update(_consts())
    return {k: np.ascontiguousarray(v, dtype=np.float32) for k, v in m.items()}


STAGES = "0ABCDE"


def kernel(**inputs):
    inp = {k: np.asarray(v) for k, v in inputs.items()}
    cfg = dict(stages=STAGES)
    nc, _ = build_nc(cfg)
    in_maps = []
    for c in range(NCORE):
        m = prep_core_inputs(inp, c)
        if "D" not in STAGES:
            m["w_gu"] = m["w_gu"][:1]
            m["w_down"] = m["w_down"][:1]
        if "B" not in STAGES:
            m["ck"] = m["ck"][:1]
            m["cv"] = m["cv"][:1]
        in_maps.append(m)
    res = run_bass_kernel_spmd(nc, in_maps, core_ids=list(range(NCORE)))
    r = res.results
    f = np.float32
    y_prompt = np.zeros((2, 8192, D), f)
    y_sample = np.zeros((128, TSEQ, D), f)
    ret_p = np.zeros((1, 2, NH, E, E), f)
    ret_s = np.zeros((1, 128, NH, E, E), f)
    wk_p = np.zeros((1, 2, 2048, NH, E), f)
    wv_p = np.zeros((1, 2, 2048, NH, E), f)
    k_s = np.zeros((1, 128, TSEQ, NH, E), f)
    v_s = np.zeros((1, 128, TSEQ, NH, E), f)
    for c in range(NCORE):
        b, qtr = c // 4, c % 4
        y_prompt[b, 2048 * qtr:2048 * (qtr + 1)] = r[c]["yp"]
        y_sample[NSEQ * c:NSEQ * (c + 1)] = r[c]["ys"].reshape(NSEQ, TSEQ, D)
        ret_s[0, NSEQ * c:NSEQ * (c + 1)] = r[c]["rs"]
        k_s[0, NSEQ * c:NSEQ * (c + 1)] = r[c]["sk"].reshape(NSEQ, TSEQ, NH, E)
        v_s[0, NSEQ * c:NSEQ * (c + 1)] = r[c]["sv"].reshape(NSEQ, TSEQ, NH, E)
        if qtr == 3:
            ret_p[0, b] = r[c]["rp"]
            wk_p[0, b] = r[c]["wk"].reshape(2048, NH, E)
            wv_p[0, b] = r[c]["wv"].reshape(2048, NH, E)
    return (y_prompt, y_sample, ret_p, ret_s, wk_p, wv_p, k_s, v_s)
```

```python
import math
from contextlib import ExitStack

import numpy as np
import concourse.bass as bass
import concourse.mybir as mybir
from concourse.bass_utils import run_bass_kernel_spmd

F32 = mybir.dt.float32
BF16 = mybir.dt.bfloat16
AF = mybir.ActivationFunctionType
ALU = mybir.AluOpType
AX = mybir.AxisListType

D = 1024
NH = 8
E = 64
DIN = 3584
NEXP = 32
EPS = 1e-6
NCORE = 8
OWN_T = 16
PRE_T = 48
SPAN_T = 32
NSEQ = 16
TSEQ = 8
NT = OWN_T + 1
TOEP_W = 384 + 128 * 16 + 512

ENGINES = ("tensor", "vector", "scalar", "gpsimd", "sync")
DMA_K = 6


class Sched:
    def __init__(self, nc, es):
        self.nc = nc
        self.q = {e: [] for e in ENGINES}
        self.cnt = {}
        self.seen = {}
        self.lastw = {}
        self.readers = {}
        self.sems = {}
        self.final = {}
        self.es = es
        self.n_ops = 0

    def _sem(self, name):
        if name not in self.sems:
            self.sems[name] = self.es.enter_context(self.nc.semaphore(name))
        return self.sems[name]

    def _new_token(self, eng, is_dma):
        if is_dma:
            st = "d_" + eng
            i = self.cnt.get(st, 0)
            self.cnt[st] = i + 1
            return ("%s%d" % (st, i % DMA_K), 16 * (i // DMA_K + 1)), i
        st = "c_" + eng
        i = self.cnt.get(st, 0) + 1
        self.cnt[st] = i
        return (st, i), i

    def _emit(self, eng, fn, reads, writes, is_dma):
        deps = {}

        def add(tok):
            if tok is None:
                return
            s, v = tok
            if deps.get(s, 0) < v:
                deps[s] = v

        for k in reads:
            add(self.lastw.get(k))
        for k in writes:
            add(self.lastw.get(k))
            for tok in self.readers.get(k, {}).values():
                add(tok)
        tok, idx = self._new_token(eng, is_dma)
        if is_dma and idx >= DMA_K:
            add((tok[0], tok[1] - 16))
        waits = []
        for s, v in deps.items():
            if eng == "tensor" and s == "c_tensor":
                continue
            if self.seen.get((eng, s), 0) >= v:
                continue
            self.seen[(eng, s)] = v
            waits.append((s, v))
        self.q[eng].append((waits, fn, tok, 16 if is_dma else 1))
        for k in reads:
            self.readers.setdefault(k, {})[tok[0]] = tok
        for k in writes:
            self.lastw[k] = tok
            self.readers[k] = {}
        if self.final.get(tok[0], 0) < tok[1]:
            self.final[tok[0]] = tok[1]
        self.n_ops += 1
        return tok

    def op(self, eng, fn, reads=(), writes=()):
        return self._emit(eng, fn, reads, writes, False)

    def dma(self, eng, out, in_, reads=(), writes=(), **kw):
        return self._emit(eng, lambda e: e.dma_start(out=out, in_=in_, **kw), reads, writes, True)

    def flush(self):
        nc = self.nc
        final = dict(self.final)
        for e in ENGINES:
            for waits, fn, tok, inc in self.q[e]:
                self._sem(tok[0])
        with nc.Block() as block:
            def run(engname):
                def body(e):
                    for waits, fn, tok, inc in self.q[engname]:
                        for s, v in waits:
                            e.wait_ge(self.sems[s], v)
                        ins = fn(e)
                        ins.then_inc(self.sems[tok[0]], inc)
                    for s, v in final.items():
                        if self.seen.get((engname, s), 0) < v:
                            e.wait_ge(self.sems[s], v)
                            self.seen[(engname, s)] = v
                return body
            block.tensor(run("tensor"))
            block.vector(run("vector"))
            block.scalar(run("scalar"))
            block.gpsimd(run("gpsimd"))
            block.sync(run("sync"))
        self.q = {e: [] for e in ENGINES}
        self.lastw = {}
        self.readers = {}


def _gammas():
    return 1.0 - 2.0 ** (-5.0 - np.arange(NH, dtype=np.float64))


def _ret_tables(L):
    g = _gammas()
    lg = np.log(g)
    j = np.arange(128)[:, None]
    i = np.arange(128)[None, :]
    dm = np.zeros((128, NH, 128), np.float64)
    ok = (i >= j) & (i < L) & (j < L)
    for h in range(NH):
        dm[:, h, :] = np.where(ok, np.exp((i - j) * lg[h]) / 8.0, 0.0)
    kd = np.zeros((128, NH, E), np.float64)
    for h in range(NH):
        col = np.where(np.arange(128) < L, np.exp((L - 1.0 - np.arange(128)) * lg[h]) / 8.0, 0.0)
        kd[:, h, :] = col[:, None]
    qd = np.zeros((64, NH, 128), np.float64)
    gd = np.zeros((64, NH, E), np.float64)
    for h in range(NH):
        qd[:, h, :] = np.exp((np.arange(128) + 1.0) * lg[h])[None, :]
        gd[:, h, :] = np.exp(L * lg[h])
    return (dm.astype(np.float32), kd.reshape(128, NH * E).astype(np.float32),
            qd.astype(np.float32), gd.astype(np.float32))


def _alibi_logw(dist):
    dist = np.asarray(dist, np.int64)
    cnt = ((dist >= 0) & (dist <= 128)).astype(np.float64)
    cnt += ((dist >= 0) & (dist <= 512) & (dist % 4 == 0))
    cnt += ((dist >= 0) & (dist <= 2048) & (dist % 16 == 0))
    return cnt


def _bias_fn(dist, h):
    slope = 2.0 ** (-8.0 * (h + 1.0) / NH)
    cnt = _alibi_logw(dist)
    with np.errstate(divide="ignore"):
        out = np.where(cnt > 0, -slope * np.maximum(dist, 0) + np.log(np.maximum(cnt, 1e-30)), -1e30)
    return out


def _toeplitz_prompt():
    p = np.arange(128)[:, None]
    c = np.arange(TOEP_W)[None, :]
    out = np.zeros((NH, 128, TOEP_W), np.float32)
    for h in range(NH):
        out[h] = _bias_fn(c - 384 - p, h).astype(np.float32)
    return out


def _sample_bias():
    out = np.zeros((128, 17, NH, TSEQ), np.float32)
    j = np.arange(128)[:, None]
    t = np.arange(TSEQ)[None, :]
    for a in range(16):
        for h in range(NH):
            out[:, a, h, :] = _bias_fn(2048 + t - (128 * a + j), h)
    for h in range(NH):
        b = _bias_fn(t - j, h)
        b = np.where(j < TSEQ, b, -1e30)
        out[:, 16, h, :] = b
    return out


def build_nc(cfg):
    nc = bass.Bass("TRN2", target_bir_lowering=False)
    dbg = cfg.get("debug", False)
    n_pre = cfg.get("n_pre", PRE_T)
    n_exp = cfg.get("n_exp", NEXP)
    stages = cfg.get("stages", "0ABCDE")

    def din(name, shape, dt=F32):
        return nc.dram_tensor(name, list(shape), dt, kind="ExternalInput").ap()

    def dout(name, shape, dt=F32):
        return nc.dram_tensor(name, list(shape), dt, kind="ExternalOutput").ap()

    def dscr(name, shape, dt):
        return nc.dram_tensor(name, list(shape), dt).ap()

    xp = din("xp", [(PRE_T + OWN_T) * 128, D])
    pvalid = din("pvalid", [128, PRE_T])
    xs_pad = din("xs_pad", [NSEQ * 128, D])
    xs = din("xs", [128, D])
    cvec = din("cvec", [17, D])
    state_in = din("state_in", [NSEQ, NH, E, E])
    bigb = "B" in stages
    ck = din("ck", [NSEQ if bigb else 1, 2048, NH * E])
    cv = din("cv", [NSEQ if bigb else 1, 2048, NH * E])
    w_ada = din("w_ada", [D, 6 * D])
    b_ada = din("b_ada", [1, 6 * D])
    g1T = din("g1T", [128, 8])
    g3T = din("g3T", [128, 8])
    g2b = din("g2b", [128, D])
    g4b = din("g4b", [128, D])
    gretb = din("gretb", [128, 512])
    w_in = din("w_in", [D, DIN])
    w_out = din("w_out", [D, D])
    w_router = din("w_router", [D, NEXP])
    b_router = din("b_router", [1, NEXP])
    big = "D" in stages
    w_gu = din("w_gu", [NEXP if big else 1, D, 2 * D])
    b_gu = din("b_gu", [NEXP, 2 * D])
    w_down = din("w_down", [NEXP if big else 1, D, D])
    b_down = din("b_down", [NEXP, D])
    c_ident = din("c_ident", [128, 128])
    c_sel = din("c_sel", [2, 17, 128])
    c_dm = din("c_dm", [2, 128, NH, 128])
    c_kd = din("c_kd", [2, 128, 512])
    c_qd = din("c_qd", [2, 64, NH, 128])
    c_gd = din("c_gd", [2, 64, NH, E])
    c_toep = din("c_toep", [NH, 128, TOEP_W])
    c_sbias = din("c_sbias", [128, 17, NH, TSEQ])

    yp = dout("yp", [OWN_T * 128, D])
    ys = dout("ys", [128, D])
    rp = dout("rp", [NH, E, E])
    rs = dout("rs", [NSEQ, NH, E, E])
    wk = dout("wk", [OWN_T * 128, 512])
    wv = dout("wv", [OWN_T * 128, 512])
    sk = dout("sk", [128, 512])
    sv = dout("sv", [128, 512])

    kT_scr = dscr("kT_scr", [NH, 64, SPAN_T * 128], BF16)
    qT_scr = dscr("qT_scr", [NH, 64, OWN_T * 128], BF16)
    v_scr = dscr("v_scr", [SPAN_T, 128, NH * 65], BF16)
    qTs_scr = dscr("qTs_scr", [NSEQ, 64, NH, 128], BF16)
    kTs_scr = dscr("kTs_scr", [NSEQ, 64, NH, 128], BF16)
    vs_scr = dscr("vs_scr", [NSEQ, 128, NH * 65], BF16)
    y_scr = dscr("y_scr", [NT, 128, D], BF16)
    x1_scr = dscr("x1_scr", [NT, 128, D], F32)
    gtab_scr = dscr("gtab_scr", [4, 128, D], F32)

    dbg_outs = {}

    def dbg_out(name, shape, dt=F32):
        if dbg:
            dbg_outs[name] = dout("dbg_" + name, shape, dt)
            return dbg_outs[name]
        return None

    with ExitStack() as es_all:
        S = Sched(nc, es_all)

        def sb(es, name, shape, dt=F32):
            return es.enter_context(nc.sbuf_tensor(name, list(shape), dt))

        def ps(es, name, shape, dt=F32):
            return es.enter_context(nc.psum_tensor(name, list(shape), dt))

        ident_f = sb(es_all, "ident_f", [128, 128])
        ident_b = sb(es_all, "ident_b", [128, 128], BF16)
        A1 = sb(es_all, "A1", [128, 8, 17])
        B1 = sb(es_all, "B1", [128, 8, 17])
        A2 = sb(es_all, "A2", [128, 8, 17])
        B2 = sb(es_all, "B2", [128, 8, 17])
        S.dma("sync", ident_f[:], c_ident[:, :], writes=["ident_f"])
        S.op("vector", lambda e: e.tensor_copy(ident_b[:], ident_f[:]), reads=["ident_f"], writes=["ident_b"])

        if "0" in stages:
            with ExitStack() as es:
                c17 = sb(es, "c17", [17, D])
                sc17 = sb(es, "sc17", [17, D])
                scT = sb(es, "scT", [128, 8, 17])
                brow = sb(es, "brow", [1, 6 * D])
                ones1 = sb(es, "ones1", [1, 32])
                g1 = sb(es, "g1", [128, 8])
                g3 = sb(es, "g3", [128, 8])
                gpost = [sb(es, "gpost0", [128, D]), sb(es, "gpost1", [128, D])]
                sel = sb(es, "sel", [17, 2, 128])
                blk = [sb(es, "wablk0", [128, 8, D]), sb(es, "wablk1", [128, 8, D])]
                gt_tok = sb(es, "gt_tok", [17, D])
                gtab = sb(es, "gtab", [128, D])
                ps_tr = ps(es, "ps_tr0", [128, 8, 17])
                ps_m = [ps(es, "ps_m0", [128, 8, 17]), ps(es, "ps_m1", [128, 8, 17])]
                ps_g = ps(es, "ps_g", [17, D])
                ps_t = ps(es, "ps_t", [128, D])

                S.dma("sync", c17[:], cvec[:, :], writes=["c17"])
                S.dma("sync", brow[:], b_ada[:, :], writes=["brow"])
                S.dma("sync", g1[:], g1T[:, :], writes=["g1"])
                S.dma("sync", g3[:], g3T[:, :], writes=["g3"])
                S.dma("sync", gpost[0][:], g2b[:, :], writes=["gpost0"])
                S.dma("sync", gpost[1][:], g4b[:, :], writes=["gpost1"])
                S.dma("sync", sel[:], c_sel.rearrange("a s p -> s a p"), writes=["sel"])
                S.op("gpsimd", lambda e: e.memset(ones1[:], 1.0), writes=["ones1"])
                S.op("scalar", lambda e: e.activation(sc17[:], c17[:], AF.Silu), reads=["c17"], writes=["sc17"])
                for k in range(8):
                    S.op("tensor", lambda e, k=k: e.transpose(ps_tr[:, k, :], sc17[0:17, k * 128:(k + 1) * 128],
                                                              ident_f[0:17, 0:17]),
                         reads=["sc17", "ident_f"], writes=["ps_tr0"])
                S.op("vector", lambda e: e.tensor_copy(scT[:], ps_tr[:]), reads=["ps_tr0"], writes=["scT"])

                wada_v = w_ada.rearrange("(k p) c -> p k c", p=128)
                for j in range(6):
                    bk = blk[j % 2]
                    bkn = "wablk%d" % (j % 2)
                    S.dma("sync", bk[:, 0:4, :], wada_v[:, 0:4, j * D:(j + 1) * D], writes=[bkn])
                    S.dma("scalar", bk[:, 4:8, :], wada_v[:, 4:8, j * D:(j + 1) * D], writes=[bkn + "b"])
                    if j in (0, 1, 3, 4):
                        pm = ps_m[j % 2]
                        pmn = "ps_m%d" % (j % 2)
                        for cc in range(8):
                            for k in range(8):
                                S.op("tensor", lambda e, cc=cc, k=k, pm=pm, bk=bk: e.matmul(
                                    pm[:, cc, :], bk[:, k, cc * 128:(cc + 1) * 128], scT[:, k, :],
                                    start=(k == 0), stop=False),
                                    reads=[bkn, bkn + "b", "scT"], writes=[pmn])
                            S.op("tensor", lambda e, cc=cc, pm=pm, j=j: e.matmul(
                                pm[:, cc, :], brow[0:1, j * D + cc * 128:j * D + (cc + 1) * 128], ones1[0:1, 0:17],
                                start=False, stop=True),
                                reads=["brow", "ones1"], writes=[pmn])
                        if j == 0:
                            S.op("vector", lambda e, pm=pm: e.tensor_copy(B1[:], pm[:]), reads=[pmn], writes=["B1"])
                        elif j == 3:
                            S.op("vector", lambda e, pm=pm: e.tensor_copy(B2[:], pm[:]), reads=[pmn], writes=["B2"])
                        else:
                            dst, gg, gn = (A1, g1, "g1") if j == 1 else (A2, g3, "g3")
                            dn = "A1" if j == 1 else "A2"
                            for cc in range(8):
                                S.op("vector", lambda e, cc=cc, pm=pm, dst=dst, gg=gg: e.tensor_scalar(
                                    dst[:, cc, :], pm[:, cc, :], 1.0, gg[:, cc:cc + 1], ALU.add, ALU.mult),
                                    reads=[pmn, gn], writes=[dn])
                    else:
                        gi = 0 if j == 2 else 1
                        for half in range(2):
                            for k in range(8):
                                S.op("tensor", lambda e, half=half, k=k, bk=bk: e.matmul(
                                    ps_g[0:17, half * 512:(half + 1) * 512], scT[:, k, :],
                                    bk[:, k, half * 512:(half + 1) * 512], start=(k == 0), stop=False),
                                    reads=[bkn, bkn + "b", "scT"], writes=["ps_g"])
                            S.op("tensor", lambda e, half=half, j=j: e.matmul(
                                ps_g[0:17, half * 512:(half + 1) * 512], ones1[0:1, 0:17],
                                brow[0:1, j * D + half * 512:j * D + (half + 1) * 512], start=False, stop=True),
                                reads=["brow", "ones1"], writes=["ps_g"])
                        S.op("vector", lambda e: e.tensor_copy(gt_tok[:], ps_g[:]), reads=["ps_g"], writes=["gt_tok"])
                        for which in range(2):
                            for half in range(2):
                                S.op("tensor", lambda e, which=which, half=half: e.matmul(
                                    ps_t[:, half * 512:(half + 1) * 512], sel[0:17, which, :],
                                    gt_tok[0:17, half * 512:(half + 1) * 512], start=True, stop=True),
                                    reads=["sel", "gt_tok"], writes=["ps_t"])
                            S.op("vector", lambda e, gi=gi: e.tensor_tensor(gtab[:], ps_t[:], gpost[gi][:], ALU.mult),
                                 reads=["ps_t", "gpost%d" % gi], writes=["gtab"])
                            S.dma("sync", gtab_scr[gi * 2 + which], gtab[:], reads=["gtab"], writes=["gtab_scr"])
                if dbg:
                    for nm, t in (("A1", A1), ("B1", B1), ("A2", A2), ("B2", B2)):
                        o = dbg_out(nm, [128, 8, 17])
                        S.dma("sync", o, t[:], reads=[nm], writes=["dbg_" + nm])
                    o = dbg_out("gtab", [4, 128, D])
                    with ExitStack() as es2:
                        pass
                S.flush()
                if dbg:
                    pass


        if "A" in stages:
            with ExitStack() as es:
                w_in_bf = sb(es, "w_in_bf", [128, 8, DIN], BF16)
                tabs = []
                for kind in range(2):
                    tabs.append(dict(
                        dm=sb(es, "dm%d" % kind, [128, NH, 128]), kd=sb(es, "kd%d" % kind, [128, 512]),
                        qd=sb(es, "qd%d" % kind, [64, NH, 128]), gd=sb(es, "gd%d" % kind, [64, NH, E])))
                gret = sb(es, "gret", [128, 512])
                pval = sb(es, "pval", [128, PRE_T])
                ones_c = sb(es, "ones_c", [128, 1])
                xt = [sb(es, "xt0", [128, D]), sb(es, "xt1", [128, D])]
                junk = sb(es, "junk", [128, D], BF16)
                ssum = sb(es, "ssum", [128, 1])
                rstd = sb(es, "rstd", [128, 1])
                xn = sb(es, "xn", [128, D], BF16)
                tmpm = sb(es, "tmpm", [128, 8, 128])
                hT2 = [sb(es, "hT0", [128, 8, 512], BF16), sb(es, "hT1", [128, 8, 512], BF16)]
                gpar = [0]
                qrT = sb(es, "qrT", [64, NH, 512], BF16)
                krT = sb(es, "krT", [64, NH, 512], BF16)
                qaT = sb(es, "qaT", [64, NH, 512], BF16)
                kaT = sb(es, "kaT", [64, NH, 512], BF16)
                kdec = sb(es, "kdec", [128, 512], BF16)
                vr = sb(es, "vr", [128, 512], BF16)
                sg = sb(es, "sg", [128, 512])
                kaf = sb(es, "kaf", [128, 512])
                vaf = sb(es, "vaf", [128, 512])
                vext = sb(es, "vext", [128, NH, 65], BF16)
                PT = sb(es, "PT", [128, NH, 128], BF16)
                qdec = sb(es, "qdec", [64, NH, 128], BF16)
                Sp = sb(es, "Sp", [64, NH, E])
                Ss = sb(es, "Ss", [64, NH, E])
                Sbf = sb(es, "Sbf", [64, NH, E], BF16)
                osb = sb(es, "osb", [128, 512])
                cen = sb(es, "cen", [128, 512])
                sq = sb(es, "sq", [128, 512])
                st8 = sb(es, "st8", [128, 8])
                st8b = sb(es, "st8b", [128, 8])
                rety = sb(es, "rety", [128, 512], BF16)
                ps_tr = ps(es, "ps_trA", [128, 8, 128], BF16)
                ps_f = [ps(es, "ps_f0", [128, 512])]
                ps_k = [ps(es, "ps_k0", [128, 512]), ps(es, "ps_k1", [128, 512])]
                ps_A = ps(es, "ps_A", [128, NH, 128])
                ps_o = ps(es, "ps_o", [128, 512])

                w_in_v = w_in.rearrange("(k p) c -> p k c", p=128)
                for cb in range(7):
                    S.dma("gpsimd", w_in_bf[:, :, cb * 512:(cb + 1) * 512], w_in_v[:, :, cb * 512:(cb + 1) * 512],
                          writes=["w_in_bf"])
                for kind in range(2):
                    S.dma("sync", tabs[kind]["dm"][:], c_dm[kind], writes=["tabs"])
                    S.dma("sync", tabs[kind]["kd"][:], c_kd[kind], writes=["tabs"])
                    S.dma("sync", tabs[kind]["qd"][:], c_qd[kind], writes=["tabs"])
                    S.dma("sync", tabs[kind]["gd"][:], c_gd[kind], writes=["tabs"])
                S.dma("sync", gret[:], gretb[:, :], writes=["gret"])
                S.dma("sync", pval[:], pvalid[:, :], writes=["pval"])
                S.op("gpsimd", lambda e: e.memset(ones_c[:], 1.0), writes=["ones_c"])
                S.op("gpsimd", lambda e: e.memset(Sp[:], 0.0), writes=["Sp"])
                S.op("gpsimd", lambda e: e.memset(Sbf[:], 0.0), writes=["Sbf"])

                fcnt = [0]
                kcnt = [0]
                tcnt = [0]

                def norm_tile(x_src, col, t_in_grp, gp):
                    hT, hTn = hT2[gp], "hT%d" % gp
                    i = tcnt[0] % 2
                    tcnt[0] += 1
                    xb, xbn = xt[i], "xt%d" % i
                    S.dma("sync", xb[:], x_src, writes=[xbn])
                    S.op("gpsimd", lambda e: e.memset(ssum[:], 0.0), writes=["ssum"])
                    S.op("scalar", lambda e: e.activation(junk[:], xb[:], AF.Square, accum_out=ssum[:]),
                         reads=[xbn], writes=["junk", "ssum"])
                    S.op("vector", lambda e: e.tensor_scalar(rstd[:], ssum[:], 1.0 / D, EPS, ALU.mult, ALU.add),
                         reads=["ssum"], writes=["rstd"])
                    S.op("scalar", lambda e: e.activation(rstd[:], rstd[:], AF.Sqrt), reads=["rstd"], writes=["rstd"])
                    S.op("vector", lambda e: e.reciprocal(rstd[:], rstd[:]), reads=["rstd"], writes=["rstd"])
                    S.op("vector", lambda e: e.tensor_scalar(xn[:], xb[:], rstd[:, 0:1], None, ALU.mult),
                         reads=[xbn, "rstd"], writes=["xn"])
                    for k in range(8):
                        S.op("tensor", lambda e, k=k: e.transpose(ps_tr[:, k, :], xn[:, k * 128:(k + 1) * 128], ident_b[:]),
                             reads=["xn", "ident_b"], writes=["ps_trA"])
                    S.op("vector", lambda e: e.tensor_tensor(
                        tmpm[:], ps_tr[:], A1[:, :, col:col + 1].to_broadcast([128, 8, 128]), ALU.mult),
                        reads=["ps_trA", "A1"], writes=["tmpm"])
                    c0 = t_in_grp * 128
                    S.op("vector", lambda e: e.tensor_tensor(
                        hT[:, :, c0:c0 + 128], tmpm[:], B1[:, :, col:col + 1].to_broadcast([128, 8, 128]), ALU.add),
                        reads=["tmpm", "B1"], writes=[hTn])

                def feat_proj(dst, dname, col_off, ntok):
                    for p in range(4):
                        pf, pfn = ps_f[0], "ps_f0"
                        for k in range(8):
                            S.op("tensor", lambda e, k=k, p=p, pf=pf: e.matmul(
                                pf[:, 0:ntok], w_in_bf[:, k, col_off + 128 * p:col_off + 128 * (p + 1)], hT[:, k, 0:ntok],
                                start=(k == 0), stop=(k == 7)),
                                reads=["w_in_bf", "hT"], writes=[pfn])
                        eng = "scalar" if (p % 2 == 0) else "vector"
                        if eng == "scalar":
                            S.op("scalar", lambda e, p=p, pf=pf: e.copy(dst[:, p, 0:ntok], pf[:, 0:ntok]),
                                 reads=[pfn], writes=[dname])
                        else:
                            S.op("vector", lambda e, p=p, pf=pf: e.tensor_copy(dst[:, p, 0:ntok], pf[:, 0:ntok]),
                                 reads=[pfn], writes=[dname])

                pproj = [(ps_f[0], "ps_f0"), (ps_k[0], "ps_k0"), (ps_k[1], "ps_k1")]

                def next_pp():
                    i = kcnt[0] % 3
                    kcnt[0] += 1
                    return pproj[i]

                def feat_proj_h(dst, dname, col_off, ntok):
                    hT, hTn = hT2[gpar[0]], "hT%d" % gpar[0]
                    for h in range(NH):
                        pf, pfn = next_pp()
                        for k in range(8):
                            S.op("tensor", lambda e, k=k, h=h, pf=pf, hT=hT: e.matmul(
                                pf[0:64, 0:ntok], w_in_bf[:, k, col_off + 64 * h:col_off + 64 * (h + 1)], hT[:, k, 0:ntok],
                                start=(k == 0), stop=(k == 7)),
                                reads=["w_in_bf", hTn], writes=[pfn])
                        if h % 2 == 0:
                            S.op("scalar", lambda e, h=h, pf=pf: e.copy(dst[:, h, 0:ntok], pf[0:64, 0:ntok]),
                                 reads=[pfn], writes=[dname])
                        else:
                            S.op("vector", lambda e, h=h, pf=pf: e.tensor_copy(dst[:, h, 0:ntok], pf[0:64, 0:ntok]),
                                 reads=[pfn], writes=[dname])

                def tok_proj(col_off, t_in_grp):
                    pk, pkn = next_pp()
                    hT, hTn = hT2[gpar[0]], "hT%d" % gpar[0]
                    c0 = t_in_grp * 128
                    for k in range(8):
                        S.op("tensor", lambda e, k=k, pk=pk, hT=hT: e.matmul(
                            pk[:], hT[:, k, c0:c0 + 128], w_in_bf[:, k, col_off:col_off + 512],
                            start=(k == 0), stop=(k == 7)),
                            reads=["w_in_bf", hTn], writes=[pkn])
                    return pk, pkn

                def do_group(tiles):
                    ntok = 128 * len(tiles)
                    mode = tiles[0]["mode"]
                    cut = cfg.get("cut", 99)
                    if cut <= 1:
                        return
                    if mode in ("own", "smp"):
                        feat_proj_h(qrT, "qrT", 0, ntok)
                        feat_proj_h(krT, "krT", 512, ntok)
                        feat_proj_h(qaT, "qaT", 2048, ntok)
                    if mode in ("win", "own", "smp"):
                        feat_proj_h(kaT, "kaT", 2560, ntok)
                    if mode in ("win", "own"):
                        sp0 = tiles[0]["span"] * 128
                        S.dma("sync", kT_scr[:, :, sp0:sp0 + ntok].rearrange("p q t -> q p t"), kaT[:, :, 0:ntok],
                              reads=["kaT"], writes=["kT_scr"])
                    if mode == "own":
                        o0 = tiles[0]["own"] * 128
                        S.dma("sync", qT_scr[:, :, o0:o0 + ntok].rearrange("p q t -> q p t"), qaT[:, :, 0:ntok],
                              reads=["qaT"], writes=["qT_scr"])
                    if mode == "smp":
                        s = tiles[0]["seq"]
                        S.dma("sync", qTs_scr[s], qaT[:, :, 0:128], reads=["qaT"], writes=["qTs_scr"])
                        S.dma("sync", kTs_scr[s], kaT[:, :, 0:128], reads=["kaT"], writes=["kTs_scr"])
                    if cut <= 2:
                        return
                    for ti, t in enumerate(tiles):
                        do_tile(ti, t, mode, cut)

                def do_tile(ti, t, mode, cut):
                    for _once in (0,):
                        c0 = ti * 128
                        tb = tabs[t["kind"]]
                        St, Sn = t["state"]
                        vcol = t["valid"]
                        pk, pkn = tok_proj(512, ti)
                        S.op("vector", lambda e, pk=pk, tb=tb: e.tensor_tensor(kdec[:], pk[:], tb["kd"][:], ALU.mult),
                             reads=[pkn, "tabs"], writes=["kdec"])
                        pk, pkn = tok_proj(1024, ti)
                        S.op("scalar", lambda e, pk=pk: e.copy(vr[:], pk[:]), reads=[pkn], writes=["vr"])
                        if mode in ("own", "smp"):
                            if not cfg.get("skip_sg"):
                                pk, pkn = tok_proj(1536, ti)
                                S.op("scalar", lambda e, pk=pk: e.activation(sg[:], pk[:], AF.Copy if cfg.get("nosilu") else AF.Silu), reads=[pkn], writes=["sg"])
                            if not cfg.get("skip_kaf"):
                                pk, pkn = tok_proj(2560, ti)
                                S.op("scalar", lambda e, pk=pk: e.copy(kaf[:], pk[:]), reads=[pkn], writes=["kaf"])
                            if mode == "own":
                                r0 = t["own"] * 128
                                if not cfg.get("nowk"):
                                    S.dma("sync", wk[r0:r0 + 128, :], kaf[:], reads=["kaf"], writes=["wk"])
                            else:
                                s = t["seq"]
                                S.dma("sync", sk[s * TSEQ:(s + 1) * TSEQ, :], kaf[0:TSEQ, :], reads=["kaf"], writes=["sk"])
                        if mode in ("win", "own", "smp"):
                            pk, pkn = tok_proj(3072, ti)
                            if mode != "win" and not cfg.get("skip_vaf"):
                                S.op("scalar", lambda e, pk=pk: e.copy(vaf[:], pk[:]), reads=[pkn], writes=["vaf"])
                                if mode == "own":
                                    r0 = t["own"] * 128
                                    if not cfg.get("nowk"):
                                        S.dma("sync", wv[r0:r0 + 128, :], vaf[:], reads=["vaf"], writes=["wv"])
                                else:
                                    s = t["seq"]
                                    S.dma("sync", sv[s * TSEQ:(s + 1) * TSEQ, :], vaf[0:TSEQ, :], reads=["vaf"], writes=["sv"])
                            vsrc = vcol if vcol is not None else ones_c[:, 0:1]
                            vin, vinn = (pk, pkn) if mode == "win" else (vaf, "vaf")
                            S.op("vector", lambda e, vin=vin, vsrc=vsrc: e.tensor_scalar(
                                vext[:, :, 0:64], vin[:].rearrange("p (h e) -> p h e", e=64), vsrc, None, ALU.mult),
                                reads=[vinn, "pval", "ones_c"], writes=["vext"])
                            S.op("vector", lambda e, vsrc=vsrc: e.tensor_copy(
                                vext[:, :, 64:65], vsrc.unsqueeze(1).to_broadcast([128, NH, 1])),
                                reads=["pval", "ones_c"], writes=["vext"])
                            if mode == "smp":
                                S.dma("sync", vs_scr[t["seq"]], vext[:].rearrange("p h e -> p (h e)"), reads=["vext"], writes=["vs_scr"])
                            else:
                                S.dma("sync", v_scr[t["span"]], vext[:].rearrange("p h e -> p (h e)"), reads=["vext"], writes=["v_scr"])
                        if cut <= 3:
                            continue
                        if mode in ("own", "smp"):
                            for h in range(NH):
                                p_, hf = h // 2, h % 2
                                pr = slice(0, 64) if cfg.get('pr0') else slice(64 * hf, 64 * hf + 64)
                                S.op("tensor", lambda e, h=h: e.matmul(
                                    ps_A[:, h, :], krT[:, h, c0:c0 + 128], qrT[:, h, c0:c0 + 128], start=True, stop=True),
                                    reads=["krT", "qrT"], writes=["ps_A"])
                            S.op("vector", lambda e, tb=tb: e.tensor_tensor(PT[:, 0:4, :], ps_A[:, 0:4, :], tb["dm"][:, 0:4, :], ALU.mult),
                                 reads=["ps_A", "tabs"], writes=["PT"])
                            S.op("vector", lambda e, tb=tb: e.tensor_tensor(PT[:, 4:8, :], ps_A[:, 4:8, :], tb["dm"][:, 4:8, :], ALU.mult),
                                 reads=["ps_A", "tabs"], writes=["PT"])
                            S.op("vector", lambda e, tb=tb: e.tensor_tensor(qdec[:], qrT[:, :, c0:c0 + 128], tb["qd"][:], ALU.mult),
                                 reads=["qrT", "tabs"], writes=["qdec"])
                            for h in range(NH):
                                p_, hf = h // 2, h % 2
                                pr = slice(0, 64) if cfg.get('pr0') else slice(64 * hf, 64 * hf + 64)
                                S.op("tensor", lambda e, h=h: e.matmul(
                                    ps_o[:, 64 * h:64 * h + 64], PT[:, h, :], vr[:, 64 * h:64 * h + 64], start=True, stop=False),
                                    reads=["PT", "vr"], writes=["ps_o"])
                                S.op("tensor", lambda e, h=h: e.matmul(
                                    ps_o[:, 64 * h:64 * h + 64], qdec[:, h, :], Sbf[:, h, :], start=False, stop=True),
                                    reads=["qdec", "Sbf"], writes=["ps_o"])
                        if cut <= 4:
                            continue
                        for h in range(NH):
                            S.op("tensor", lambda e, h=h: e.matmul(
                                ps_S[:, h, :], kdec[:, 64 * h:64 * h + 64], vr[:, 64 * h:64 * h + 64],
                                start=True, stop=True),
                                reads=["kdec", "vr"], writes=["ps_S"])
                        S.op("vector", lambda e, St=St, tb=tb: e.tensor_tensor(St[:], St[:], tb["gd"][:], ALU.mult),
                             reads=[Sn, "tabs"], writes=[Sn])
                        vsrc = vcol if vcol is not None else ones_c[:, 0:1]
                        S.op("vector", lambda e, St=St, vsrc=vsrc: e.scalar_tensor_tensor(
                            St[:], ps_S[:], vsrc[0:64, :], St[:], ALU.mult, ALU.add),
                            reads=["ps_S", Sn, "pval", "ones_c"], writes=[Sn])
                        S.op("vector", lambda e, St=St: e.tensor_copy(Sbf[:], St[:]), reads=[Sn], writes=["Sbf"])
                        if cut <= 5:
                            continue
                        if mode in ("own", "smp"):
                            o3 = lambda t_: t_[:].rearrange("p (h e) -> p h e", e=64)
                            S.op("scalar", lambda e: e.copy(osb[:], ps_o[:]), reads=["ps_o"], writes=["osb"])
                            S.op("vector", lambda e: e.tensor_reduce(st8[:], o3(osb), AX.X, ALU.add), reads=["osb"], writes=["st8"])
                            S.op("vector", lambda e: e.tensor_scalar(st8[:], st8[:], 1.0 / E, None, ALU.mult), reads=["st8"], writes=["st8"])
                            S.op("vector", lambda e: e.tensor_tensor(o3(cen), o3(osb), st8[:].unsqueeze(2).to_broadcast([128, NH, E]), ALU.subtract),
                                 reads=["osb", "st8"], writes=["cen"])
                            S.op("scalar", lambda e: e.activation(sq[:], cen[:], AF.Square), reads=["cen"], writes=["sq"])
                            S.op("vector", lambda e: e.tensor_reduce(st8b[:], o3(sq), AX.X, ALU.add), reads=["sq"], writes=["st8b"])
                            S.op("vector", lambda e: e.tensor_scalar(st8b[:], st8b[:], 1.0 / E, EPS, ALU.mult, ALU.add), reads=["st8b"], writes=["st8b"])
                            S.op("scalar", lambda e: e.activation(st8b[:], st8b[:], AF.Sqrt), reads=["st8b"], writes=["st8b"])
                            S.op("vector", lambda e: e.reciprocal(st8b[:], st8b[:]), reads=["st8b"], writes=["st8b"])
                            S.op("vector", lambda e: e.tensor_tensor(o3(cen), o3(cen), st8b[:].unsqueeze(2).to_broadcast([128, NH, E]), ALU.mult),
                                 reads=["cen", "st8b"], writes=["cen"])
                            S.op("vector", lambda e: e.tensor_tensor(sq[:], sg[:], gret[:], ALU.mult), reads=["sg", "gret"], writes=["sq"])
                            S.op("vector", lambda e: e.tensor_tensor(rety[:], cen[:], sq[:], ALU.mult), reads=["cen", "sq"], writes=["rety"])
                            if mode == "own":
                                S.dma("sync", y_scr[t["own"], :, 0:512], rety[:], reads=["rety"], writes=["y_scr"])
                            else:
                                s = t["seq"]
                                S.dma("sync", y_scr[OWN_T, s * TSEQ:(s + 1) * TSEQ, 0:512], rety[0:TSEQ, :], reads=["rety"], writes=["y_scr"])

                ps_S = ps(es, "ps_S", [64, NH, E])

                pre_skip = PRE_T - n_pre
                glist = []
                for g in range(pre_skip // 4, PRE_T // 4):
                    tl = []
                    for ti in range(4):
                        t = 4 * g + ti
                        md = "win" if t >= PRE_T - 16 else "pre"
                        tl.append(dict(x=xp[t * 128:(t + 1) * 128, :], col=0, kind=0, mode=md, valid=pval[:, t:t + 1],
                                       state=(Sp, "Sp"), span=t - (PRE_T - 16)))
                    glist.append((tl, None, None))
                n_owng = cfg.get('n_own', OWN_T) // 4
                for g in range(n_owng):
                    tl = []
                    for ti in range(4):
                        t = 4 * g + ti
                        tl.append(dict(x=xp[(PRE_T + t) * 128:(PRE_T + t + 1) * 128, :], col=0, kind=0, mode="own", valid=None,
                                       state=(Sp, "Sp"), span=16 + t, own=t))
                    glist.append((tl, None, None))
                n_smp = cfg.get("n_smp", NSEQ)

                def rp_out():
                    S.dma("sync", rp.rearrange("h e f -> e h f"), Sp[:], reads=["Sp"], writes=["rp"])
                if glist:
                    glist[-1] = (glist[-1][0], None, rp_out)
                else:
                    rp_out()
                for s in range(n_smp):
                    def pro(s=s):
                        S.dma("sync", Ss[:], state_in[s].rearrange("h e f -> e h f"), writes=["Ss"])
                        S.op("vector", lambda e: e.tensor_copy(Sbf[:], Ss[:]), reads=["Ss"], writes=["Sbf"])

                    def epi(s=s):
                        S.dma("sync", rs[s].rearrange("h e f -> e h f"), Ss[:], reads=["Ss"], writes=["rs"])
                    glist.append(([dict(x=xs_pad[s * 128:(s + 1) * 128, :], col=1 + s, kind=1, mode="smp", valid=None,
                                        state=(Ss, "Ss"), seq=s)], pro, epi))

                def norm_group(gi):
                    for ti, t in enumerate(glist[gi][0]):
                        norm_tile(t["x"], t["col"], ti, gi % 2)
                if glist:
                    norm_group(0)
                for gi, (tl, pro, epi) in enumerate(glist):
                    if gi + 1 < len(glist):
                        norm_group(gi + 1)
                    gpar[0] = gi % 2
                    if pro is not None:
                        pro()
                    do_group(tl)
                    if epi is not None:
                        epi()
                S.flush()
                if dbg and "B" not in stages:
                    o = dbg_out("y_scr", [NT, 128, D], BF16)
                    S.dma("sync", o[0:OWN_T, :, 0:512], y_scr[0:OWN_T, :, 0:512], reads=["y_scr"], writes=["dbg_y"])
                    S.dma("sync", o[OWN_T, 0:TSEQ * n_smp, 0:512], y_scr[OWN_T, 0:TSEQ * n_smp, 0:512], reads=["y_scr"], writes=["dbg_y"])
                    S.flush()


        if "B" in stages:
            with ExitStack() as es:
                kTh = [sb(es, "kTh0", [64, SPAN_T * 128], BF16), sb(es, "kTh1", [64, SPAN_T * 128], BF16)]
                qTh = [sb(es, "qTh0", [64, OWN_T * 128], BF16), sb(es, "qTh1", [64, OWN_T * 128], BF16)]
                v_all = sb(es, "v_all", [128, SPAN_T, NH * 65], BF16)
                Th = [sb(es, "Th0", [128, TOEP_W]), sb(es, "Th1", [128, TOEP_W])]
                NB_ = 4
                s_sb = [sb(es, "s_sb%d" % i, [128, 512]) for i in range(NB_)]
                PTb = [sb(es, "PTb%d" % i, [128, 512], BF16) for i in range(NB_)]
                rec = sb(es, "rec", [128, 4, 1])
                attb = sb(es, "attb", [128, 4, 64], BF16)
                ps_s = [ps(es, "ps_s%d" % i, [128, 512]) for i in range(NB_)]
                ps_acc = [ps(es, "ps_acc0", [128, 4, 128]), ps(es, "ps_acc1", [128, 4, 128])]
                recb = [sb(es, "recb0", [128, 4, 1]), sb(es, "recb1", [128, 4, 1])]
                attbb = [sb(es, "attbb0", [128, 4, 64], BF16), sb(es, "attbb1", [128, 4, 64], BF16)]
                for a in range(SPAN_T):
                    S.dma("sync" if a % 2 == 0 else "scalar", v_all[:, a, :], v_scr[a], reads=["v_scr"], writes=["v_all"])
                it = 0
                pend = []
                LA = 2

                def drain(keep):
                    while len(pend) > keep:
                        pend.pop(0)()
                n_heads_b = cfg.get("n_heads_b", NH)
                for h in range(n_heads_b):
                    hb = h % 2
                    S.dma("sync", kTh[hb][:], kT_scr[h], reads=["kT_scr"], writes=["kTh%d" % hb])
                    S.dma("scalar", qTh[hb][:], qT_scr[h], reads=["qT_scr"], writes=["qTh%d" % hb])
                    S.dma("sync", Th[hb][:], c_toep[h], writes=["Th%d" % hb])
                    for G in range(OWN_T // 4):
                        acc = ps_acc[G % 2]
                        accn = "ps_acc%d" % (G % 2)
                        for a in range(4 * G, 4 * G + 20):
                            i = it % NB_
                            it += 1
                            cs = 384 + 128 * (16 + 4 * G - a)
                            S.op("tensor", lambda e, i=i, hb=hb, a=a, G=G: e.matmul(
                                ps_s[i][:], kTh[hb][:, a * 128:(a + 1) * 128], qTh[hb][:, G * 512:(G + 1) * 512],
                                start=True, stop=True),
                                reads=["kTh%d" % hb, "qTh%d" % hb], writes=["ps_s%d" % i])
                            S.op("vector", lambda e, i=i, hb=hb, cs=cs: e.scalar_tensor_tensor(
                                s_sb[i][:], ps_s[i][:], 0.125, Th[hb][:, cs:cs + 512], ALU.mult, ALU.add),
                                reads=["ps_s%d" % i, "Th%d" % hb], writes=["s_sb%d" % i])
                            S.op("scalar", lambda e, i=i: e.activation(PTb[i][:], s_sb[i][:], AF.Exp),
                                 reads=["s_sb%d" % i], writes=["PTb%d" % i])
                            def pv(i=i, a=a, h=h, G=G, acc=acc, accn=accn):
                                for qi in range(4):
                                    first, last = 4 * G + qi, 16 + 4 * G + qi
                                    if a < first or a > last:
                                        continue
                                    S.op("tensor", lambda e, i=i, qi=qi, a=a, h=h, acc=acc, first=first, last=last: e.matmul(
                                        acc[:, qi, 0:65], PTb[i][:, qi * 128:(qi + 1) * 128], v_all[:, a, h * 65:(h + 1) * 65],
                                        start=(a == first), stop=(a == last)),
                                        reads=["PTb%d" % i, "v_all"], writes=[accn])
                            pend.append(pv)
                            drain(LA)
                        gb = G % 2
                        def epi(acc=acc, accn=accn, gb=gb, G=G, h=h):
                            S.op("vector", lambda e, acc=acc, gb=gb: e.reciprocal(recb[gb][:], acc[:, :, 64:65]), reads=[accn], writes=["recb%d" % gb])
                            S.op("vector", lambda e, acc=acc, gb=gb: e.tensor_tensor(
                                attbb[gb][:], acc[:, :, 0:64], recb[gb][:].to_broadcast([128, 4, 64]), ALU.mult),
                                reads=[accn, "recb%d" % gb], writes=["attbb%d" % gb])
                            S.dma("scalar", y_scr[4 * G:4 * G + 4, :, 512 + 64 * h:512 + 64 * h + 64].rearrange("t p e -> p t e"),
                                  attbb[gb][:], reads=["attbb%d" % gb], writes=["y_scr"])
                        pend.append(epi)
                drain(0)
                S.flush()
                if dbg and "C" not in stages:
                    o = dbg_out("y_att", [OWN_T, 128, 512], BF16)
                    S.dma("sync", o[:, :, 0:64 * n_heads_b], y_scr[0:OWN_T, :, 512:512 + 64 * n_heads_b], reads=["y_scr"], writes=["dbg_y"])
                    S.flush()


        if "B" in stages:
            with ExitStack() as es:
                sbias = sb(es, "sbias", [128, 17, NH * TSEQ])
                qTs = sb(es, "qTs", [64, NH, 128], BF16)
                kTs = sb(es, "kTs", [64, NH, 128], BF16)
                vs = sb(es, "vs", [128, NH * 65], BF16)
                ckb = [sb(es, "ckb0", [128, 4, 512]), sb(es, "ckb1", [128, 4, 512])]
                cvb = [sb(es, "cvb0", [128, 4, 512]), sb(es, "cvb1", [128, 4, 512])]
                kcT = [sb(es, "kcT0", [64, NH, 128], BF16), sb(es, "kcT1", [64, NH, 128], BF16)]
                vx = [sb(es, "vx0", [128, 4, NH, 65], BF16), sb(es, "vx1", [128, 4, NH, 65], BF16)]
                s4 = sb(es, "s4", [128, 4, NH * TSEQ])
                P4 = [sb(es, "P40", [128, 4, NH * TSEQ], BF16), sb(es, "P41", [128, 4, NH * TSEQ], BF16)]
                recs = sb(es, "recs", [TSEQ, NH, 1])
                atts = sb(es, "atts", [TSEQ, NH, 64], BF16)
                ps_kT = [ps(es, "ps_kT0", [64, 4, 128], BF16), ps(es, "ps_kT1", [64, 4, 128], BF16)]
                ckbf = [sb(es, "ckbf0", [128, 4, 512], BF16), sb(es, "ckbf1", [128, 4, 512], BF16)]
                ps_s4 = [ps(es, "ps_s40", [128, 4, NH * TSEQ]), ps(es, "ps_s41", [128, 4, NH * TSEQ])]
                ps_as = ps(es, "ps_as", [TSEQ, NH, 128])
                S.dma("sync", sbias[:], c_sbias.rearrange("p a h t -> p a (h t)"), writes=["sbias"])
                for i in range(2):
                    S.op("gpsimd", lambda e, i=i: e.memset(vx[i][:], 1.0), writes=["vx%d" % i])
                n_smp_b = 0 if cfg.get("skip_bs") else cfg.get("n_smp", NSEQ)
                ci = 0
                kti = 0
                pend_s = []

                def drain_s(keep):
                    while len(pend_s) > keep:
                        pend_s.pop(0)()
                for s in range(n_smp_b):
                    drain_s(0)
                    S.dma("sync", qTs[:], qTs_scr[s], reads=["qTs_scr"], writes=["qTs"])
                    S.dma("sync", kTs[:], kTs_scr[s], reads=["kTs_scr"], writes=["kTs"])
                    S.dma("sync", vs[:], vs_scr[s], reads=["vs_scr"], writes=["vs"])
                    for ch in range(5):
                        b = ci % 2
                        ci += 1
                        ntile = 4 if ch < 4 else 1
                        if ch < 4:
                            S.dma("sync", ckb[b][:], ck[s, 512 * ch:512 * (ch + 1), :].rearrange("(a p) c -> p a c", p=128),
                                  writes=["ckb%d" % b])
                            S.dma("scalar", cvb[b][:], cv[s, 512 * ch:512 * (ch + 1), :].rearrange("(a p) c -> p a c", p=128),
                                  writes=["cvb%d" % b])
                            S.op("scalar", lambda e, b=b: e.copy(vx[b][:, :, :, 0:64], cvb[b][:].rearrange("p a (h e) -> p a h e", e=64)),
                                 reads=["cvb%d" % b], writes=["vx%d" % b])
                            S.op("vector", lambda e, b=b: e.tensor_copy(ckbf[b][:], ckb[b][:]),
                                 reads=["ckb%d" % b], writes=["ckbf%d" % b])
                        pss = ps_s4[b]
                        pssn = "ps_s4%d" % b
                        for j in range(ntile):
                            if ch < 4:
                                kb = kti % 2
                                kti += 1
                                for hh in range(2):
                                    pk_ = ps_kT[hh]
                                    for h4 in range(4):
                                        h = 4 * hh + h4
                                        S.op("tensor", lambda e, b=b, j=j, h=h, h4=h4, pk_=pk_: e.transpose(
                                            pk_[:, h4, :], ckbf[b][:, j, 64 * h:64 * h + 64], ident_b[:]),
                                            reads=["ckbf%d" % b, "ident_b"], writes=["ps_kT%d" % hh])
                                    if hh == 0:
                                        S.op("vector", lambda e, kb=kb, pk_=pk_: e.tensor_copy(kcT[kb][:, 0:4, :], pk_[:]),
                                             reads=["ps_kT0"], writes=["kcT%d" % kb])
                                    else:
                                        S.op("scalar", lambda e, kb=kb, pk_=pk_: e.copy(kcT[kb][:, 4:8, :], pk_[:]),
                                             reads=["ps_kT1"], writes=["kcT%d" % kb])
                                ksrc, ksn = kcT[kb], "kcT%d" % kb
                            else:
                                ksrc, ksn = kTs, "kTs"
                            for h in range(NH):
                                S.op("tensor", lambda e, j=j, h=h, ksrc=ksrc, pss=pss: e.matmul(
                                    pss[:, j, h * TSEQ:(h + 1) * TSEQ], ksrc[:, h, :], qTs[:, h, 0:TSEQ], start=True, stop=True),
                                    reads=[ksn, "qTs"], writes=[pssn])
                        a0 = 4 * ch
                        S.op("vector", lambda e, pss=pss, a0=a0, ntile=ntile: e.scalar_tensor_tensor(
                            s4[:, 0:ntile, :], pss[:, 0:ntile, :], 0.125, sbias[:, a0:a0 + ntile, :], ALU.mult, ALU.add),
                            reads=[pssn, "sbias"], writes=["s4"])
                        S.op("scalar", lambda e, b=b, ntile=ntile: e.activation(P4[b][:, 0:ntile, :], s4[:, 0:ntile, :], AF.Exp),
                             reads=["s4"], writes=["P4%d" % b])
                        def pv_s(b=b, ch=ch, ntile=ntile):
                            for j in range(ntile):
                                for h in range(NH):
                                    if ch < 4:
                                        rhs = vx[b][:, j, h, :]
                                        rn = "vx%d" % b
                                    else:
                                        rhs = vs[:, h * 65:(h + 1) * 65]
                                        rn = "vs"
                                    S.op("tensor", lambda e, b=b, j=j, h=h, rhs=rhs, first=(ch == 0 and j == 0), last=(ch == 4): e.matmul(
                                        ps_as[:, h, 0:65], P4[b][:, j, h * TSEQ:(h + 1) * TSEQ], rhs, start=first, stop=last),
                                        reads=["P4%d" % b, rn], writes=["ps_as"])
                        pend_s.append(pv_s)
                        drain_s(1)

                    def epi_s(s=s):
                        S.op("vector", lambda e: e.reciprocal(recs[:], ps_as[:, :, 64:65]), reads=["ps_as"], writes=["recs"])
                        S.op("vector", lambda e: e.tensor_tensor(atts[:], ps_as[:, :, 0:64], recs[:].to_broadcast([TSEQ, NH, 64]), ALU.mult),
                             reads=["ps_as", "recs"], writes=["atts"])
                        S.dma("sync", y_scr[OWN_T, s * TSEQ:(s + 1) * TSEQ, 512:1024], atts[:].rearrange("p h e -> p (h e)"),
                              reads=["atts"], writes=["y_scr"])
                    pend_s.append(epi_s)
                drain_s(0)
                S.flush()
                if dbg and "C" not in stages:
                    o = dbg_out("y_atts", [128, 512], BF16)
                    S.dma("sync", o[0:TSEQ * n_smp_b, :], y_scr[OWN_T, 0:TSEQ * n_smp_b, 512:1024], reads=["y_scr"], writes=["dbg_y"])
                    S.flush()


        if "C" in stages:
            h2T_all = sb(es_all, "h2T_all", [128, 8, NT * 128], BF16)
            G_all = sb(es_all, "G_all", [128, NT, NEXP])
            y_acc = sb(es_all, "y_acc", [128, NT, D])
            with ExitStack() as es:
                w_out_bf = sb(es, "w_out_bf", [128, 8, D], BF16)
                GA = [sb(es, "GA0", [128, D]), sb(es, "GA1", [128, D])]
                wr_f = sb(es, "wr_f", [128, 8, NEXP])
                br = sb(es, "br", [1, NEXP])
                ones_r = sb(es, "ones_r", [1, 128])
                bd_sb = sb(es, "bd_sb", [NEXP, D])
                ybf = [sb(es, "ybf0", [128, D], BF16), sb(es, "ybf1", [128, D], BF16)]
                xc = [sb(es, "xc0", [128, D]), sb(es, "xc1", [128, D])]
                yT = sb(es, "yT", [128, 8, 128], BF16)
                junkc = sb(es, "junkc", [128, D], BF16)
                ssc = sb(es, "ssc", [128, 1])
                ssc2 = sb(es, "ssc2", [128, 2])
                rsc = sb(es, "rsc", [128, 1])
                t1 = sb(es, "t1", [128, D])
                x1 = sb(es, "x1", [128, D])
                xn2 = sb(es, "xn2", [128, D])
                tmp2 = sb(es, "tmp2", [128, 8, 128])
                h2f = sb(es, "h2f", [128, 8, 128])
                lg = sb(es, "lg", [128, NEXP])
                mx8 = sb(es, "mx8", [128, 8])
                nmx = sb(es, "nmx", [128, 1])
                msk = sb(es, "msk", [128, NEXP])
                ex = sb(es, "ex", [128, NEXP])
                s3 = sb(es, "s3", [128, 1])
                GT = sb(es, "GT", [NEXP, 128])
                ps_trc = ps(es, "ps_trc", [128, 8, 128], BF16)
                ps_mix = ps(es, "ps_mix", [128, D])
                ps_tr2 = ps(es, "ps_tr2", [128, 8, 128])
                ps_lg = ps(es, "ps_lg", [128, NEXP])
                ps_gt = ps(es, "ps_gt", [NEXP, 128])

                S.dma("gpsimd", w_out_bf[:, :, 0:512], w_out.rearrange("(k p) c -> p k c", p=128)[:, :, 0:512], writes=["w_out_bf"])
                S.dma("gpsimd", w_out_bf[:, :, 512:D], w_out.rearrange("(k p) c -> p k c", p=128)[:, :, 512:D], writes=["w_out_bf"])
                S.dma("sync", GA[0][:], gtab_scr[0], writes=["GA0"])
                S.dma("sync", GA[1][:], gtab_scr[1], writes=["GA1"])
                S.dma("sync", wr_f[:], w_router.rearrange("(k p) n -> p k n", p=128), writes=["wr_f"])
                S.dma("sync", br[:], b_router[:, :], writes=["br"])
                S.dma("sync", bd_sb[:], b_down[:, :], writes=["bd_sb"])
                S.op("gpsimd", lambda e: e.memset(ones_r[:], 1.0), writes=["ones_r"])
                n_tc = cfg.get("n_tc", NT)
                for tt in list(range(n_tc - 1)) + [NT - 1]:
                    smp = (tt == NT - 1)
                    i = tt % 2
                    yb, ybn = ybf[i], "ybf%d" % i
                    xb, xbn = xc[i], "xc%d" % i
                    ga, gan = (GA[1], "GA1") if smp else (GA[0], "GA0")
                    S.dma("sync", yb[:], y_scr[tt], reads=["y_scr"], writes=[ybn])
                    if smp:
                        S.dma("scalar", xb[:], xs[:, :], writes=[xbn])
                    else:
                        S.dma("scalar", xb[:], xp[(PRE_T + tt) * 128:(PRE_T + tt + 1) * 128, :], writes=[xbn])
                    for k in range(8):
                        S.op("tensor", lambda e, k=k, yb=yb: e.transpose(ps_trc[:, k, :], yb[:, k * 128:(k + 1) * 128], ident_b[:]),
                             reads=[ybn, "ident_b"], writes=["ps_trc"])
                    S.op("scalar", lambda e: e.copy(yT[:], ps_trc[:]), reads=["ps_trc"], writes=["yT"])
                    for half in range(2):
                        for k in range(8):
                            S.op("tensor", lambda e, k=k, half=half: e.matmul(
                                ps_mix[:, half * 512:(half + 1) * 512], yT[:, k, :], w_out_bf[:, k, half * 512:(half + 1) * 512],
                                start=(k == 0), stop=(k == 7)),
                                reads=["yT", "w_out_bf"], writes=["ps_mix"])
                    for half in range(2):
                        S.op("scalar", lambda e, half=half: e.activation(
                            junkc[:, half * 512:(half + 1) * 512], ps_mix[:, half * 512:(half + 1) * 512], AF.Square,
                            accum_out=ssc2[:, half:half + 1]),
                            reads=["ps_mix"], writes=["junkc", "ssc2"])
                    S.op("vector", lambda e: e.tensor_tensor(ssc[:], ssc2[:, 0:1], ssc2[:, 1:2], ALU.add), reads=["ssc2"], writes=["ssc"])
                    S.op("vector", lambda e: e.tensor_scalar(rsc[:], ssc[:], 1.0 / D, EPS, ALU.mult, ALU.add), reads=["ssc"], writes=["rsc"])
                    S.op("scalar", lambda e: e.activation(rsc[:], rsc[:], AF.Sqrt), reads=["rsc"], writes=["rsc"])
                    S.op("vector", lambda e: e.reciprocal(rsc[:], rsc[:]), reads=["rsc"], writes=["rsc"])
                    for half in range(2):
                        hs = slice(half * 512, (half + 1) * 512)
                        S.op("vector", lambda e, hs=hs, ga=ga: e.scalar_tensor_tensor(
                            t1[:, hs], ps_mix[:, hs], rsc[:, 0:1], ga[:, hs], ALU.mult, ALU.mult),
                            reads=["ps_mix", "rsc", gan], writes=["t1"])
                    S.op("vector", lambda e, xb=xb: e.tensor_tensor(x1[:], t1[:], xb[:], ALU.add), reads=["t1", xbn], writes=["x1"])
                    S.dma("sync", x1_scr[tt], x1[:], reads=["x1"], writes=["x1_scr"])
                    S.op("gpsimd", lambda e: e.memset(ssc[:], 0.0), reads=["rsc"], writes=["ssc"])
                    S.op("scalar", lambda e: e.activation(junkc[:], x1[:], AF.Square, accum_out=ssc[:]),
                         reads=["x1", "ssc"], writes=["junkc", "ssc"])
                    S.op("vector", lambda e: e.tensor_scalar(rsc[:], ssc[:], 1.0 / D, EPS, ALU.mult, ALU.add), reads=["ssc"], writes=["rsc"])
                    S.op("scalar", lambda e: e.activation(rsc[:], rsc[:], AF.Sqrt), reads=["rsc"], writes=["rsc"])
                    S.op("vector", lambda e: e.reciprocal(rsc[:], rsc[:]), reads=["rsc"], writes=["rsc"])
                    S.op("vector", lambda e: e.tensor_scalar(xn2[:], x1[:], rsc[:, 0:1], None, ALU.mult), reads=["x1", "rsc"], writes=["xn2"])
                    for k in range(8):
                        S.op("tensor", lambda e, k=k: e.transpose(ps_tr2[:, k, :], xn2[:, k * 128:(k + 1) * 128], ident_f[:]),
                             reads=["xn2", "ident_f"], writes=["ps_tr2"])
                    for hh in range(2):
                        ks = slice(4 * hh, 4 * hh + 4)
                        if smp:
                            a_b = A2[:, ks, 1:17].unsqueeze(3).to_broadcast([128, 4, NSEQ, TSEQ])
                            b_b = B2[:, ks, 1:17].unsqueeze(3).to_broadcast([128, 4, NSEQ, TSEQ])
                            v4 = lambda t_, ks=ks: t_[:, ks, :].rearrange("p k (s t) -> p k s t", t=TSEQ)
                        else:
                            a_b = A2[:, ks, 0:1].to_broadcast([128, 4, 128])
                            b_b = B2[:, ks, 0:1].to_broadcast([128, 4, 128])
                            v4 = lambda t_, ks=ks: t_[:, ks, :]
                        S.op("vector", lambda e, v4=v4, a_b=a_b: e.tensor_tensor(v4(tmp2), v4(ps_tr2), a_b, ALU.mult),
                             reads=["ps_tr2", "A2"], writes=["tmp2"])
                        S.op("vector", lambda e, v4=v4, b_b=b_b: e.tensor_tensor(v4(h2f), v4(tmp2), b_b, ALU.add),
                             reads=["tmp2", "B2"], writes=["h2f"])
                    S.op("scalar", lambda e, tt=tt: e.copy(h2T_all[:, :, tt * 128:(tt + 1) * 128], h2f[:]),
                         reads=["h2f"], writes=["h2T_all"])
                    for k in range(8):
                        S.op("tensor", lambda e, k=k: e.matmul(ps_lg[:], h2f[:, k, :], wr_f[:, k, :], start=(k == 0), stop=False),
                             reads=["h2f", "wr_f"], writes=["ps_lg"])
                    S.op("tensor", lambda e: e.matmul(ps_lg[:], ones_r[0:1, :], br[0:1, :], start=False, stop=True),
                         reads=["ones_r", "br"], writes=["ps_lg"])
                    S.op("vector", lambda e: e.tensor_copy(lg[:], ps_lg[:]), reads=["ps_lg"], writes=["lg"])
                    S.op("vector", lambda e: e.max(mx8[:], lg[:]), reads=["lg"], writes=["mx8"])
                    S.op("vector", lambda e: e.tensor_scalar(msk[:], lg[:], mx8[:, 3:4], None, ALU.is_ge), reads=["lg", "mx8"], writes=["msk"])
                    S.op("vector", lambda e: e.tensor_scalar(nmx[:], mx8[:, 0:1], -1.0, None, ALU.mult), reads=["mx8"], writes=["nmx"])
                    S.op("scalar", lambda e: e.activation(ex[:], lg[:], AF.Exp, bias=nmx[:, 0:1]), reads=["lg", "nmx"], writes=["ex"])
                    S.op("vector", lambda e: e.tensor_tensor(ex[:], ex[:], msk[:], ALU.mult), reads=["ex", "msk"], writes=["ex"])
                    S.op("vector", lambda e: e.tensor_reduce(s3[:], ex[:], AX.X, ALU.add), reads=["ex"], writes=["s3"])
                    S.op("vector", lambda e: e.reciprocal(s3[:], s3[:]), reads=["s3"], writes=["s3"])
                    S.op("vector", lambda e, tt=tt: e.tensor_scalar(G_all[:, tt, :], ex[:], s3[:, 0:1], None, ALU.mult),
                         reads=["ex", "s3"], writes=["G_all"])
                    S.op("tensor", lambda e, tt=tt: e.transpose(ps_gt[:], G_all[:, tt, :], ident_f[:]),
                         reads=["G_all", "ident_f"], writes=["ps_gt"])
                    S.op("vector", lambda e: e.tensor_copy(GT[:], ps_gt[:]), reads=["ps_gt"], writes=["GT"])
                    for half in range(2):
                        S.op("tensor", lambda e, half=half: e.matmul(
                            ps_mix[:, half * 512:(half + 1) * 512], GT[:, :], bd_sb[:, half * 512:(half + 1) * 512], start=True, stop=True),
                            reads=["GT", "bd_sb"], writes=["ps_mix"])
                    for half in range(2):
                        hs = slice(half * 512, (half + 1) * 512)
                        S.op("scalar", lambda e, hs=hs, tt=tt: e.copy(y_acc[:, tt, hs], ps_mix[:, hs]),
                             reads=["ps_mix"], writes=["y_acc"])
                if dbg and "D" not in stages:
                    o = dbg_out("G_all", [128, NT, NEXP])
                    S.dma("sync", o, G_all[:], reads=["G_all"], writes=["dbg_G"])
                    o = dbg_out("h2T", [128, 8, NT * 128], BF16)
                    S.dma("sync", o, h2T_all[:], reads=["h2T_all"], writes=["dbg_h"])
                    o = dbg_out("y_acc", [128, NT, D])
                    S.dma("sync", o, y_acc[:], reads=["y_acc"], writes=["dbg_ya"])
                S.flush()
                if dbg and "D" not in stages:
                    o = dbg_out("x1", [NT, 128, D])
                    for tt in list(range(n_tc - 1)) + [NT - 1]:
                        S.dma("sync", o[tt], x1_scr[tt], reads=["x1_scr"], writes=["dbg_x1"])
                    S.flush()


        if "D" in stages:
            bgu = sb(es_all, "bgu", [128, 8, 2, NEXP])
            with ExitStack() as es:
                bgu_raw = sb(es, "bgu_raw", [NEXP, 2 * D])
                ps_b = ps(es, "ps_b", [128, 512])
                S.dma("sync", bgu_raw[:], b_gu[:, :], writes=["bgu_raw"])
                braw = bgu_raw[:].rearrange("e (f j two) -> e f two j", f=8, j=128, two=2)
                for f in range(8):
                    for two in range(2):
                        S.op("tensor", lambda e, f=f, two=two: e.transpose(
                            ps_b[:, (2 * f + two) * NEXP:(2 * f + two + 1) * NEXP], braw[:, f, two, :], ident_f[0:NEXP, 0:NEXP]),
                            reads=["bgu_raw", "ident_f"], writes=["ps_b"])
                S.op("vector", lambda e: e.tensor_copy(bgu[:].rearrange("p f two e -> p (f two e)"), ps_b[:, 0:16 * NEXP]),
                     reads=["ps_b"], writes=["bgu"])
                S.op("vector", lambda e: e.tensor_scalar(bgu[:, :, 1, :], bgu[:, :, 1, :], 1.0 / 1.702, None, ALU.mult),
                     reads=["bgu"], writes=["bgu"])
                S.flush()
            with ExitStack() as es:
                actT = sb(es, "actT", [128, 8, NT * 128], BF16)
                stg = [sb(es, "stg0", [128, 8, 256]), sb(es, "stg1", [128, 8, 256])]
                Ugu = [sb(es, "Ugu0", [128, 8, 2, 128], BF16), sb(es, "Ugu1", [128, 8, 2, 128], BF16)]
                stw = [sb(es, "stw0", [128, D]), sb(es, "stw1", [128, D])]
                Wd = sb(es, "Wd", [128, 8, D], BF16)
                gc = [sb(es, "gc0", [128, 512]), sb(es, "gc1", [128, 512])]
                sig = [sb(es, "sig0", [128, 512]), sb(es, "sig1", [128, 512])]
                uu = [sb(es, "uu0", [128, 512]), sb(es, "uu1", [128, 512])]
                ps_g = [ps(es, "ps_g0", [128, 512]), ps(es, "ps_g1", [128, 512])]
                ps_u = [ps(es, "ps_u0", [128, 512]), ps(es, "ps_u1", [128, 512])]
                ps_d = [ps(es, "ps_d0", [128, D]), ps(es, "ps_d1", [128, D])]

                wgu_v = w_gu.rearrange("e (k p) c -> e p k c", p=128)
                wd_v = w_down.rearrange("e (f p) c -> e p f c", p=128)
                groups = [(0, 512), (512, 512), (1024, 512), (1536, 512), (2048, 128)]
                n_units = 8 * n_exp

                def load_gu(u):
                    e_, f = u // 8, u % 8
                    b = u % 2
                    S.dma("sync", stg[b][:], wgu_v[e_, :, :, 256 * f:256 * (f + 1)], writes=["stg%d" % b])

                def cast_gu(u):
                    b = u % 2
                    S.op("scalar", lambda e, b=b: e.copy(
                        Ugu[b][:], stg[b][:].rearrange("p k (j two) -> p k two j", two=2)),
                        reads=["stg%d" % b], writes=["Ugu%d" % b])

                def load_wd(e_, q):
                    b = q % 2
                    S.dma("sync", stw[b][:], wd_v[e_, :, q, :], writes=["stw%d" % b])

                def cast_wd(e_, q):
                    b = q % 2
                    S.op("scalar", lambda e, b=b, q=q: e.copy(Wd[:, q, :], stw[b][:]),
                         reads=["stw%d" % b], writes=["Wd"])

                load_gu(0)
                load_gu(1)
                cast_gu(0)
                cnt = 0
                dcnt = 0
                for e_ in range(n_exp):
                    for f in range(8):
                        u = 8 * e_ + f
                        b = u % 2
                        if u + 1 < n_units:
                            cast_gu(u + 1)
                        if f in (6, 7):
                            load_wd(e_, f - 6)
                        for (t0, n) in groups:
                            i = cnt % 2
                            cnt += 1
                            pg, pu = ps_g[i], ps_u[i]
                            for k in range(8):
                                S.op("tensor", lambda e, k=k, b=b, pg=pg, t0=t0, n=n: e.matmul(
                                    pg[:, 0:n], Ugu[b][:, k, 0, :], h2T_all[:, k, t0:t0 + n], start=(k == 0), stop=(k == 7)),
                                    reads=["Ugu%d" % b, "h2T_all"], writes=["ps_g%d" % i])
                            for k in range(8):
                                S.op("tensor", lambda e, k=k, b=b, pu=pu, t0=t0, n=n: e.matmul(
                                    pu[:, 0:n], Ugu[b][:, k, 1, :], h2T_all[:, k, t0:t0 + n], start=(k == 0), stop=(k == 7)),
                                    reads=["Ugu%d" % b, "h2T_all"], writes=["ps_u%d" % i])
                            S.op("scalar", lambda e, pu=pu, n=n, f=f, e_=e_, i=i: e.activation(
                                uu[i][:, 0:n], pu[:, 0:n], AF.Identity, bias=bgu[:, f, 1, e_:e_ + 1], scale=1.0 / 1.702),
                                reads=["ps_u%d" % i, "bgu"], writes=["uu%d" % i])
                            S.op("vector", lambda e, pg=pg, n=n, f=f, e_=e_, i=i: e.tensor_scalar(
                                gc[i][:, 0:n], pg[:, 0:n], bgu[:, f, 0, e_:e_ + 1], 7.0, ALU.add, ALU.min),
                                reads=["ps_g%d" % i, "bgu"], writes=["gc%d" % i])
                            S.op("scalar", lambda e, n=n, i=i: e.activation(sig[i][:, 0:n], gc[i][:, 0:n], AF.Silu, scale=1.702),
                                 reads=["gc%d" % i], writes=["sig%d" % i])
                            S.op("vector", lambda e, n=n, i=i: e.tensor_scalar(
                                uu[i][:, 0:n], uu[i][:, 0:n], 7.0 / 1.702, -7.0 / 1.702, ALU.min, ALU.max),
                                reads=["uu%d" % i], writes=["uu%d" % i])
                            S.op("vector", lambda e, n=n, f=f, t0=t0, i=i: e.scalar_tensor_tensor(
                                actT[:, f, t0:t0 + n], uu[i][:, 0:n], 1.0 / 1.702, sig[i][:, 0:n], ALU.add, ALU.mult),
                                reads=["sig%d" % i, "uu%d" % i], writes=["actT"])
                        if u + 2 < n_units:
                            load_gu(u + 2)
                    for q in range(8):
                        cast_wd(e_, q)
                        if q + 2 < 8:
                            load_wd(e_, q + 2)
                    for tt in range(NT):
                        i = dcnt % 2
                        dcnt += 1
                        pd = ps_d[i]
                        for half in range(2):
                            for f in range(8):
                                S.op("tensor", lambda e, f=f, half=half, tt=tt, pd=pd: e.matmul(
                                    pd[:, half * 512:(half + 1) * 512], actT[:, f, tt * 128:(tt + 1) * 128],
                                    Wd[:, f, half * 512:(half + 1) * 512], start=(f == 0), stop=(f == 7)),
                                    reads=["actT", "Wd"], writes=["ps_d%d" % i])
                        for half in range(2):
                            hs = slice(half * 512, (half + 1) * 512)
                            S.op("vector", lambda e, hs=hs, tt=tt, pd=pd, e_=e_: e.scalar_tensor_tensor(
                                y_acc[:, tt, hs], pd[:, hs], G_all[:, tt, e_:e_ + 1], y_acc[:, tt, hs], ALU.mult, ALU.add),
                                reads=["ps_d%d" % i, "G_all", "y_acc"], writes=["y_acc"])
                if dbg and "E" not in stages:
                    o = dbg_out("y_acc2", [128, NT, D])
                    S.dma("sync", o, y_acc[:], reads=["y_acc"], writes=["dbg_ya2"])
                S.flush()

        if "E" in stages:
            with ExitStack() as es:
                GF = [sb(es, "GF0", [128, D]), sb(es, "GF1", [128, D])]
                x1b = [sb(es, "x1b0", [128, D]), sb(es, "x1b1", [128, D])]
                junke = sb(es, "junke", [128, D], BF16)
                sse = sb(es, "sse", [128, 1])
                rse = sb(es, "rse", [128, 1])
                te = [sb(es, "te0", [128, D]), sb(es, "te1", [128, D])]
                S.dma("sync", GF[0][:], gtab_scr[2], writes=["GF0"])
                S.dma("sync", GF[1][:], gtab_scr[3], writes=["GF1"])
                for tt in range(NT):
                    smp = (tt == NT - 1)
                    i = tt % 2
                    gf, gfn = (GF[1], "GF1") if smp else (GF[0], "GF0")
                    S.dma("sync", x1b[i][:], x1_scr[tt], writes=["x1b%d" % i])
                    S.op("scalar", lambda e, tt=tt: e.activation(junke[:], y_acc[:, tt, :], AF.Square, accum_out=sse[:]),
                         reads=["y_acc"], writes=["junke", "sse"])
                    S.op("vector", lambda e: e.tensor_scalar(rse[:], sse[:], 1.0 / D, EPS, ALU.mult, ALU.add), reads=["sse"], writes=["rse"])
                    S.op("scalar", lambda e: e.activation(rse[:], rse[:], AF.Sqrt), reads=["rse"], writes=["rse"])
                    S.op("vector", lambda e: e.reciprocal(rse[:], rse[:]), reads=["rse"], writes=["rse"])
                    S.op("vector", lambda e, tt=tt, i=i, gf=gf: e.scalar_tensor_tensor(
                        te[i][:], y_acc[:, tt, :], rse[:, 0:1], gf[:], ALU.mult, ALU.mult),
                        reads=["y_acc", "rse", gfn], writes=["te%d" % i])
                    S.op("vector", lambda e, i=i: e.tensor_tensor(te[i][:], te[i][:], x1b[i][:], ALU.add),
                         reads=["te%d" % i, "x1b%d" % i], writes=["te%d" % i])
                    if smp:
                        S.dma("sync", ys[:, :], te[i][:], reads=["te%d" % i], writes=["ys"])
                    else:
                        S.dma("sync", yp[tt * 128:(tt + 1) * 128, :], te[i][:], reads=["te%d" % i], writes=["yp"])
                S.flush()

        if "E" not in stages:
            S.dma("sync", yp[:, :], xp[PRE_T * 128:(PRE_T + OWN_T) * 128, :], writes=["yp"])
            S.dma("sync", ys[:, :], xs[:, :], writes=["ys"])
            S.flush()
    return nc, dbg_outs


_CONST = {}


def _consts():
    if _CONST:
        return _CONST
    sel = np.zeros((2, 17, 128), np.float32)
    sel[0, 0, :] = 1.0
    for p in range(128):
        sel[1, 1 + p // TSEQ, p] = 1.0
    tp = _ret_tables(128)
    ts = _ret_tables(TSEQ)
    _CONST.update(
        c_ident=np.eye(128, dtype=np.float32),
        c_sel=sel,
        c_dm=np.stack([tp[0], ts[0]]),
        c_kd=np.stack([tp[1], ts[1]]),
        c_qd=np.stack([tp[2], ts[2]]),
        c_gd=np.stack([tp[3], ts[3]]),
        c_toep=_toeplitz_prompt(),
        c_sbias=_sample_bias(),
    )
    return _CONST


def prep_core_inputs(inp, c):
    b, qtr = c // 4, c % 4
    T0 = 2048 * qtr
    f = np.float32
    xpad = np.zeros(((PRE_T + OWN_T) * 128, D), f)
    lo = T0 - PRE_T * 128
    src_lo = max(lo, 0)
    xpad[src_lo - lo:] = inp["x_prompt"][b, src_lo:T0 + 2048]
    pvalid = np.zeros((128, PRE_T), f)
    for t in range(PRE_T):
        if lo + 128 * t >= 0:
            pvalid[:, t] = 1.0
    sl = slice(NSEQ * c, NSEQ * (c + 1))
    xs = inp["x_sample"][sl]
    xs_pad = np.zeros((NSEQ, 128, D), f)
    xs_pad[:, :TSEQ] = xs
    cvec = np.concatenate([inp["c_prompt"][b:b + 1], inp["c_sample"][sl]], axis=0)
    tr8 = lambda g: np.ascontiguousarray(g.reshape(8, 128).T)
    bc = lambda g, n: np.ascontiguousarray(np.broadcast_to(g.reshape(1, -1), (128, n)))
    m = dict(
        xp=xpad, pvalid=pvalid, xs_pad=xs_pad.reshape(NSEQ * 128, D), xs=np.ascontiguousarray(xs.reshape(128, D)),
        cvec=np.ascontiguousarray(cvec),
        state_in=np.ascontiguousarray(inp["state_ret"][0, sl]),
        ck=np.ascontiguousarray(inp["cache_win_k"][0, sl].reshape(NSEQ, 2048, 512)),
        cv=np.ascontiguousarray(inp["cache_win_v"][0, sl].reshape(NSEQ, 2048, 512)),
        w_ada=inp["w_ada"][0], b_ada=inp["b_ada"],
        g1T=tr8(inp["g_pre_mix"][0]), g3T=tr8(inp["g_pre_ffn"][0]),
        g2b=bc(inp["g_post_mix"][0], D), g4b=bc(inp["g_post_ffn"][0], D), gretb=bc(inp["g_ret"][0], 512),
        w_in=inp["w_in"][0], w_out=inp["w_out"][0], w_router=inp["w_router"][0], b_router=inp["b_router"],
        w_gu=inp["w_gate_up"][0], b_gu=inp["b_gate_up"][0], w_down=inp["w_down"][0], b_down=inp["b_down"][0],
    )
    m.update(_consts())
    return {k: np.ascontiguousarray(v, dtype=np.float32) for k, v in m.items()}


STAGES = "0ABCDE"


def kernel(**inputs):
    inp = {k: np.asarray(v) for k, v in inputs.items()}
    cfg = dict(stages=STAGES)
    nc, _ = build_nc(cfg)
    in_maps = []
    for c in range(NCORE):
        m = prep_core_inputs(inp, c)
        if "D" not in STAGES:
            m["w_gu"] = m["w_gu"][:1]
            m["w_down"] = m["w_down"][:1]
        if "B" not in STAGES:
            m["ck"] = m["ck"][:1]
            m["cv"] = m["cv"][:1]
        in_maps.append(m)
    res = run_bass_kernel_spmd(nc, in_maps, core_ids=list(range(NCORE)))
    r = res.results
    f = np.float32
    y_prompt = np.zeros((2, 8192, D), f)
    y_sample = np.zeros((128, TSEQ, D), f)
    ret_p = np.zeros((1, 2, NH, E, E), f)
    ret_s = np.zeros((1, 128, NH, E, E), f)
    wk_p = np.zeros((1, 2, 2048, NH, E), f)
    wv_p = np.zeros((1, 2, 2048, NH, E), f)
    k_s = np.zeros((1, 128, TSEQ, NH, E), f)
    v_s = np.zeros((1, 128, TSEQ, NH, E), f)
    for c in range(NCORE):
        b, qtr = c // 4, c % 4
        y_prompt[b, 2048 * qtr:2048 * (qtr + 1)] = r[c]["yp"]
        y_sample[NSEQ * c:NSEQ * (c + 1)] = r[c]["ys"].reshape(NSEQ, TSEQ, D)
        ret_s[0, NSEQ * c:NSEQ * (c + 1)] = r[c]["rs"]
        k_s[0, NSEQ * c:NSEQ * (c + 1)] = r[c]["sk"].reshape(NSEQ, TSEQ, NH, E)
        v_s[0, NSEQ * c:NSEQ * (c + 1)] = r[c]["sv"].reshape(NSEQ, TSEQ, NH, E)
        if qtr == 3:
            ret_p[0, b] = r[c]["rp"]
            wk_p[0, b] = r[c]["wk"].reshape(2048, NH, E)
            wv_p[0, b] = r[c]["wv"].reshape(2048, NH, E)
    return (y_prompt, y_sample, ret_p, ret_s, wk_p, wv_p, k_s, v_s)
```
